# Optimizing a Trainium2 kernel written in Bass

```python
import math
import jax
import jax.numpy as jnp
from jax import lax
import numpy as np


D_MODEL = 1024
BATCH = 1
SEQ = 16384
DEPTH = 4

N_MIXERS = 2
S5_GROUP = 16
S5_GROUPS = D_MODEL // S5_GROUP
S5_STATE = 64
N_HEADS = 16
HEAD_DIM = D_MODEL // N_HEADS
IDX_HEADS = 8
IDX_DIM = 64
TOPK_MAX = 256
Q_BLOCK = 128
ROPE_THETA = 10000.0
D_FF = ((8 * D_MODEL // 3 + 255) // 256) * 256
N_S5 = (DEPTH + 1) // 2
N_DSA = DEPTH // 2
DSA_PROJ = 3 * D_MODEL + IDX_HEADS * IDX_DIM + IDX_DIM + IDX_HEADS
EPS = 1e-6

kernel_name = 'hybrid_s5_dsa_swiglu_trunk'


def rmsnorm(x, gain):
    xf = x.astype(jnp.float32)
    y = xf * lax.rsqrt(jnp.mean(xf * xf, axis=-1, keepdims=True) + EPS)
    if gain is not None:
        y = y * gain.astype(jnp.float32)
    return y.astype(x.dtype)


def rope_tables(length):
    inv_freq = ROPE_THETA ** (-jnp.arange(0, HEAD_DIM, 2, dtype=jnp.float32) / HEAD_DIM)
    ang = jnp.arange(length, dtype=jnp.float32)[:, None] * inv_freq[None, :]
    return jnp.cos(ang), jnp.sin(ang)


def apply_rope(t, cos, sin):
    tf = t.astype(jnp.float32)
    t1, t2 = jnp.split(tf, 2, axis=-1)
    c = cos[None, :, None, :]
    s = sin[None, :, None, :]
    out = jnp.concatenate([t1 * c - t2 * s, t2 * c + t1 * s], axis=-1)
    return out.astype(t.dtype)


def _ssm_combine(e1, e2):
    a1, b1 = e1
    a2, b2 = e2
    return a1 * a2, a2 * b1 + b2


def s5_mixer(u, lam_re, lam_im, log_dt, b_re, b_im, c_re, c_im, d_skip, w_glu):
    bsz, length, _ = u.shape
    f32 = jnp.float32
    lam = lax.complex(lam_re.astype(f32), lam_im.astype(f32))
    dt = jnp.exp(log_dt.astype(f32))[:, None]
    a_bar = jnp.exp(lam * dt)
    b_bar = ((a_bar - 1.0) / lam)[..., None] * lax.complex(b_re.astype(f32), b_im.astype(f32))
    ug = u.astype(f32).reshape(bsz, length, S5_GROUPS, S5_GROUP).astype(jnp.complex64)
    bu = jnp.einsum('blgh,gph->blgp', ug, b_bar)
    a_seq = jnp.broadcast_to(a_bar, bu.shape)
    _, states = lax.associative_scan(_ssm_combine, (a_seq, bu), axis=1)
    c = lax.complex(c_re.astype(f32), c_im.astype(f32))
    y = jnp.einsum('blgp,ghp->blgh', states, c).real.reshape(bsz, length, D_MODEL)
    y = y + d_skip.astype(f32) * u.astype(f32)
    y = jax.nn.gelu(y).astype(u.dtype)
    val, gate = jnp.split(y @ w_glu, 2, axis=-1)
    return val * jax.nn.sigmoid(gate)


def dsa_mixer(h, w_in, q_gain, k_gain, w_o, cos, sin, topk):
    bsz, length, _ = h.shape
    f32 = jnp.float32
    proj = h @ w_in
    cuts = [D_MODEL, 2 * D_MODEL, 3 * D_MODEL, 3 * D_MODEL + IDX_HEADS * IDX_DIM,
            3 * D_MODEL + IDX_HEADS * IDX_DIM + IDX_DIM]
    q, k, v, qi, ki, wi = jnp.split(proj, cuts, axis=-1)
    q = q.reshape(bsz, length, N_HEADS, HEAD_DIM)
    k = k.reshape(bsz, length, N_HEADS, HEAD_DIM)
    v = v.reshape(bsz, length, N_HEADS, HEAD_DIM)
    qi = qi.reshape(bsz, length, IDX_HEADS, IDX_DIM)
    q = apply_rope(rmsnorm(q, q_gain), cos, sin)
    k = apply_rope(rmsnorm(k, k_gain), cos, sin)
    qi = apply_rope(qi, cos, sin)
    ki = apply_rope(rmsnorm(ki, None)[:, :, None, :], cos, sin)[:, :, 0, :]
    wi = wi.astype(f32) * (IDX_HEADS ** -0.5)
    nb = length // Q_BLOCK
    kpos = jnp.arange(length, dtype=jnp.int32)
    qpos_blocks = kpos.reshape(nb, Q_BLOCK)
    att_scale = HEAD_DIM ** -0.5
    idx_scale = IDX_DIM ** -0.5

    def to_blocks(t):
        return t.reshape(bsz, nb, Q_BLOCK, *t.shape[2:]).swapaxes(0, 1)

    def attend_block(args):
        qb, qib, wb, qpos = args
        s_idx = jnp.einsum('bqhd,bkd->bqhk', qib, ki).astype(f32) * idx_scale
        score = jnp.einsum('bqhk,bqh->bqk', jax.nn.relu(s_idx), wb)
        causal = kpos[None, None, :] <= qpos[None, :, None]
        score = jnp.where(causal, score, -jnp.inf)
        _, sel = lax.top_k(score, topk)
        kg = jax.vmap(lambda kb, ib: kb[ib])(k, sel)
        vg = jax.vmap(lambda vb, ib: vb[ib])(v, sel)
        logits = jnp.einsum('bqhd,bqkhd->bhqk', qb, kg).astype(f32) * att_scale
        valid = (sel <= qpos[None, :, None])[:, None, :, :]
        logits = jnp.where(valid, logits, -jnp.inf)
        p = jax.nn.softmax(logits, axis=-1).astype(vg.dtype)
        return jnp.einsum('bhqk,bqkhd->bqhd', p, vg)

    out = lax.map(attend_block, (to_blocks(q), to_blocks(qi), to_blocks(wi), qpos_blocks))
    out = out.swapaxes(0, 1).reshape(bsz, length, D_MODEL)
    return out @ w_o


def swiglu(h, w_gate_up, w_down):
    gate, up = jnp.split(h @ w_gate_up, 2, axis=-1)
    return (jax.nn.silu(gate) * up) @ w_down


def setup_inputs(seed: int = 0) -> dict:
    key = jax.random.key(seed)
    ks = jax.random.split(key, 20)
    f32 = jnp.float32
    nrm = lambda k, shape, s: jax.random.normal(k, shape, f32) * s
    x = jax.random.normal(ks[0], (BATCH, SEQ, D_MODEL), f32)
    n_idx = jnp.arange(S5_STATE, dtype=f32)
    s5_lambda_re = -0.5 + nrm(ks[1], (N_S5, S5_GROUPS, S5_STATE), 0.01)
    s5_lambda_im = math.pi * n_idx[None, None, :] + nrm(ks[2], (N_S5, S5_GROUPS, S5_STATE), 0.01)
    s5_log_dt = jax.random.uniform(ks[3], (N_S5, S5_GROUPS), f32, math.log(1e-3), math.log(1e-1))
    s5_b_re = nrm(ks[4], (N_S5, S5_GROUPS, S5_STATE, S5_GROUP), (2 * S5_GROUP) ** -0.5)
    s5_b_im = nrm(ks[5], (N_S5, S5_GROUPS, S5_STATE, S5_GROUP), (2 * S5_GROUP) ** -0.5)
    s5_c_re = nrm(ks[6], (N_S5, S5_GROUPS, S5_GROUP, S5_STATE), (2 * S5_STATE) ** -0.5)
    s5_c_im = nrm(ks[7], (N_S5, S5_GROUPS, S5_GROUP, S5_STATE), (2 * S5_STATE) ** -0.5)
    s5_d = nrm(ks[8], (N_S5, D_MODEL), 1.0)
    s5_w_glu = nrm(ks[9], (N_S5, D_MODEL, 2 * D_MODEL), D_MODEL ** -0.5)
    dsa_w_in = nrm(ks[10], (N_DSA, D_MODEL, DSA_PROJ), D_MODEL ** -0.5)
    dsa_q_norm = 1.0 + nrm(ks[11], (N_DSA, HEAD_DIM), 0.02)
    dsa_k_norm = 1.0 + nrm(ks[12], (N_DSA, HEAD_DIM), 0.02)
    dsa_w_o = nrm(ks[13], (N_DSA, D_MODEL, D_MODEL), D_MODEL ** -0.5)
    ffn_w_gate_up = nrm(ks[14], (DEPTH, D_MODEL, 2 * D_FF), D_MODEL ** -0.5)
    ffn_w_down = nrm(ks[15], (DEPTH, D_FF, D_MODEL), D_FF ** -0.5)
    norm_mix = 1.0 + nrm(ks[16], (DEPTH, D_MODEL), 0.02)
    norm_ffn = 1.0 + nrm(ks[17], (DEPTH, D_MODEL), 0.02)
    return {'x': x, 's5_lambda_re': s5_lambda_re, 's5_lambda_im': s5_lambda_im, 's5_log_dt': s5_log_dt,
            's5_b_re': s5_b_re, 's5_b_im': s5_b_im, 's5_c_re': s5_c_re, 's5_c_im': s5_c_im,
            's5_d': s5_d, 's5_w_glu': s5_w_glu, 'dsa_w_in': dsa_w_in, 'dsa_q_norm': dsa_q_norm,
            'dsa_k_norm': dsa_k_norm, 'dsa_w_o': dsa_w_o, 'ffn_w_gate_up': ffn_w_gate_up,
            'ffn_w_down': ffn_w_down, 'norm_mix': norm_mix, 'norm_ffn': norm_ffn}


def reference(x, s5_lambda_re, s5_lambda_im, s5_log_dt, s5_b_re, s5_b_im, s5_c_re, s5_c_im,
              s5_d, s5_w_glu, dsa_w_in, dsa_q_norm, dsa_k_norm, dsa_w_o, ffn_w_gate_up,
              ffn_w_down, norm_mix, norm_ffn):
    length = x.shape[1]
    topk = min(TOPK_MAX, length // 4)
    cos, sin = rope_tables(length)
    for i in range(DEPTH):
        h = rmsnorm(x, norm_mix[i])
        j = i // N_MIXERS
        if i % N_MIXERS == 0:
            mix = s5_mixer(h, s5_lambda_re[j], s5_lambda_im[j], s5_log_dt[j], s5_b_re[j], s5_b_im[j],
                           s5_c_re[j], s5_c_im[j], s5_d[j], s5_w_glu[j])
        else:
            mix = dsa_mixer(h, dsa_w_in[j], dsa_q_norm[j], dsa_k_norm[j], dsa_w_o[j], cos, sin, topk)
        x = x + mix.astype(x.dtype)
        x = x + swiglu(rmsnorm(x, norm_ffn[i]), ffn_w_gate_up[i], ffn_w_down[i]).astype(x.dtype)
    return x
```

```python
import math
from contextlib import ExitStack

import numpy as np
import concourse.bass as bass
import concourse.mybir as mybir
from concourse.bass_utils import run_bass_kernel_spmd

F32 = mybir.dt.float32
BF16 = mybir.dt.bfloat16
ALU = mybir.AluOpType
AF = mybir.ActivationFunctionType
AX = mybir.AxisListType

NCORES = 8
D = 1024
KD = D // 128
SEQ = 16384
NT = SEQ // NCORES
TT = 512
NTT = NT // TT
DFF = 2816
KF = DFF // 128
EPS = 1e-6

ENGS = ("pe", "act", "dve", "pool", "sp")
SAME_ENGINE_SYNC = ("act", "dve", "pool")


class Buf:
    __slots__ = ("name", "w", "r", "dsem", "dcount")

    def __init__(self, name):
        self.name = name
        self.w = None
        self.r = {}
        self.dsem = None
        self.dcount = 0


class Prog:
    def __init__(self, nc, es):
        self.nc = nc
        self.es = es
        self.ops = {e: [] for e in ENGS}
        self.dma_bufs = []
        self.final_tokens = []
        self.bar = []
        self.bar_epoch = 0
        self.eng_epoch = {e: 0 for e in ENGS}

    def sb(self, name, shape, dt, es=None):
        return (es or self.es).enter_context(self.nc.sbuf_tensor(name, list(shape), dt))

    def ps(self, name, shape, dt=F32, es=None):
        return (es or self.es).enter_context(self.nc.psum_tensor(name, list(shape), dt))

    def _deps(self, reads, writes):
        need = []
        for b in reads:
            if b.w is not None:
                need.append(b.w)
        for b in writes:
            if b.w is not None:
                need.append(b.w)
            need.extend(b.r.values())
        return need

    def barrier(self):
        toks = []
        for e in ENGS:
            for idx in range(len(self.ops[e]) - 1, -1, -1):
                if self.ops[e][idx]["dma"] is None:
                    toks.append(("e", e, idx))
                    break
        for b in self.dma_bufs:
            toks.append(("d", b, b.dcount))
        self.bar = toks
        self.bar_epoch += 1

    def _bar_need(self, eng):
        if self.eng_epoch[eng] < self.bar_epoch:
            self.eng_epoch[eng] = self.bar_epoch
            return list(self.bar)
        return []

    def op(self, eng, fn, reads=(), writes=()):
        need = self._deps(reads, writes) + self._bar_need(eng)
        idx = len(self.ops[eng])
        tok = ("e", eng, idx)
        self.ops[eng].append({"need": need, "fn": fn, "dma": None})
        for b in reads:
            b.r[eng] = tok
        for b in writes:
            b.w = tok
            b.r = {}
        return tok

    def dma(self, eng, fn, sembuf, reads=(), writes=()):
        need = self._deps(reads, writes) + self._bar_need(eng)
        if sembuf.dsem is None:
            sembuf.dsem = self.es.enter_context(self.nc.semaphore("d_" + sembuf.name))
            self.dma_bufs.append(sembuf)
        sembuf.dcount += 16
        tok = ("d", sembuf, sembuf.dcount)
        self.ops[eng].append({"need": need, "fn": fn, "dma": sembuf})
        for b in reads:
            b.r[("d", id(sembuf))] = tok
        for b in writes:
            b.w = tok
            b.r = {}
        return tok

    def finish(self, tokens):
        self.final_tokens.extend(tokens)

    def emit(self):
        nc = self.nc
        needed = {e: set() for e in ENGS}
        for e in ENGS:
            for i, o in enumerate(self.ops[e]):
                for t in o["need"]:
                    if t[0] == "e":
                        if t[1] == e and e not in SAME_ENGINE_SYNC:
                            continue
                        needed[t[1]].add(t[2])
        for t in self.final_tokens:
            if t[0] == "e":
                needed[t[1]].add(t[2])
        rank = {}
        for e in ENGS:
            rank[e] = {i: n + 1 for n, i in enumerate(sorted(needed[e]))}
        sems = {e: self.es.enter_context(nc.semaphore("s_" + e)) for e in ENGS}
        final_tokens = self.final_tokens

        def run(e, engine):
            known = {}
            def wait(tok):
                if tok[0] == "e":
                    if tok[1] == e and e not in SAME_ENGINE_SYNC:
                        return
                    key, sem, val = tok[1], sems[tok[1]], rank[tok[1]][tok[2]]
                else:
                    key, sem, val = id(tok[1]), tok[1].dsem, tok[2]
                if known.get(key, 0) >= val:
                    return
                known[key] = val
                engine.wait_ge(sem, val)
            for i, o in enumerate(self.ops[e]):
                for t in o["need"]:
                    wait(t)
                ins = o["fn"](engine)
                if o["dma"] is not None:
                    ins.then_inc(o["dma"].dsem, 16)
                elif i in rank[e]:
                    ins.then_inc(sems[e], 1)
            if e == "sp":
                for t in final_tokens:
                    wait(t)

        with nc.Block() as block:
            @block.tensor
            def _(eng):
                run("pe", eng)

            @block.scalar
            def _(eng):
                run("act", eng)

            @block.vector
            def _(eng):
                run("dve", eng)

            @block.gpsimd
            def _(eng):
                run("pool", eng)

            @block.sync
            def _(eng):
                run("sp", eng)


class Ctx:
    def __init__(self, P):
        self.P = P
        nc = P.nc
        self.ones = P.sb("c_ones", [128, 128], BF16)
        self.b_ones = Buf("ones")
        P.op("pool", lambda g: g.memset(self.ones[:], 1.0), writes=[self.b_ones])
        self.eps_col = P.sb("c_eps", [128, 1], F32)
        P.op("pool", lambda g: g.memset(self.eps_col[:], EPS), writes=[self.b_ones])
        self.psum = [P.ps(f"ps{i}", [128, 512], F32) for i in range(8)]
        self.b_ps = [Buf(f"ps{i}") for i in range(8)]
        self.ps_rr = 0

    def next_ps(self):
        i = self.ps_rr
        self.ps_rr = (self.ps_rr + 1) % 8
        return self.psum[i], self.b_ps[i]


def emit_rmsnorm_T(P, C, xT, b_x, gain, b_gain, gcol0, hT, b_h, tts, es, tag):
    sq = [P.sb(f"{tag}_sq{i}", [128, TT], BF16, es) for i in range(2)]
    b_sq = [Buf(f"{tag}_sq{i}") for i in range(2)]
    rstd = [P.sb(f"{tag}_rstd{i}", [128, TT], F32, es) for i in range(2)]
    b_rstd = [Buf(f"{tag}_rstd{i}") for i in range(2)]
    n = 0
    for j, tt in enumerate(tts):
        ts = slice(tt * TT, (tt + 1) * TT)
        ps, b_ps = C.next_ps()
        for k in range(KD):
            s, bs = sq[n % 2], b_sq[n % 2]
            n += 1
            P.op("act", lambda e, s=s, k=k, ts=ts: e.activation(out=s[:], in_=xT[:, k, ts], func=AF.Square),
                 reads=[b_x[k][tt]], writes=[bs])
            P.op("pe", lambda e, ps=ps, s=s, k=k: e.matmul(ps[:], lhsT=C.ones[:], rhs=s[:],
                                                             start=(k == 0), stop=(k == KD - 1)),
                 reads=[bs, C.b_ones], writes=[b_ps])
        r, br = rstd[j % 2], b_rstd[j % 2]
        P.op("act", lambda e, r=r, ps=ps: e.activation(out=r[:], in_=ps[:], func=AF.Sqrt, scale=1.0 / D, bias=C.eps_col[:]),
             reads=[b_ps, C.b_ones], writes=[br])
        P.op("dve", lambda e, r=r: e.reciprocal(out=r[:], in_=r[:]),
             reads=[br], writes=[br])
        js = slice(j * TT, (j + 1) * TT)
        for k in range(KD):
            P.op("dve", lambda e, k=k, ts=ts, js=js, r=r: e.scalar_tensor_tensor(
                out=hT[:, k, js], in0=xT[:, k, ts], scalar=gain[:, gcol0 + k:gcol0 + k + 1], in1=r[:],
                op0=ALU.mult, op1=ALU.mult),
                reads=[b_x[k][tt], br, b_gain], writes=[b_h[j]])


def emit_ffn(P, C, xT, b_x, wgu, wd, gain, b_gain, gcol0, tag="ffn"):
    nc = P.nc
    with ExitStack() as es:
        HT = 2 * TT
        hT = P.sb(f"{tag}_hT", [128, KD, HT], BF16, es)
        aT = P.sb(f"{tag}_aT", [128, KF, HT], BF16, es)
        wg_f = [P.sb(f"{tag}_wgf{i}", [128, KD, 256], F32, es) for i in range(2)]
        wg_b = [P.sb(f"{tag}_wgb{i}", [128, KD, 256], BF16, es) for i in range(2)]
        wd_f = [P.sb(f"{tag}_wdf{i}", [128, KF, 128], F32, es) for i in range(2)]
        wd_b = [P.sb(f"{tag}_wdb{i}", [128, KF, 128], BF16, es) for i in range(2)]
        sg = [P.sb(f"{tag}_sg{i}", [128, TT], F32, es) for i in range(2)]
        b_hT = [Buf(f"{tag}_hT{j}") for j in range(2)]
        b_aT = [[Buf(f"{tag}_aT{n}_{j}") for j in range(2)] for n in range(KF)]
        b_wgf = [Buf(f"{tag}_wgf{i}") for i in range(2)]
        b_wgb = [Buf(f"{tag}_wgb{i}") for i in range(2)]
        b_wdf = [Buf(f"{tag}_wdf{i}") for i in range(2)]
        b_wdb = [Buf(f"{tag}_wdb{i}") for i in range(2)]
        b_sg = [Buf(f"{tag}_sg{i}") for i in range(2)]
        wgu_v = wgu.rearrange("(k p) n -> p k n", p=128)
        wd_v = wd.rearrange("(k p) n -> p k n", p=128)
        nsg = 0
        nw = 0
        nwd = 0
        for half in range(NT // HT):
            tts = [half * 2, half * 2 + 1]
            emit_rmsnorm_T(P, C, xT, b_x, gain, b_gain, gcol0, hT, b_hT, tts, es, f"{tag}n{half}")
            for n in range(KF):
                s = nw % 2
                nw += 1
                P.dma("sp", lambda e, s=s, n=n: e.dma_start(out=wg_f[s][:, :, 0:128],
                                                            in_=wgu_v[:, :, n * 128:(n + 1) * 128]),
                      b_wgf[s], writes=[b_wgf[s]])
                P.dma("sp", lambda e, s=s, n=n: e.dma_start(out=wg_f[s][:, :, 128:256],
                                                            in_=wgu_v[:, :, DFF + n * 128:DFF + (n + 1) * 128]),
                      b_wgf[s], writes=[])
                b_wgf[s].w = ("d", b_wgf[s], b_wgf[s].dcount)
                P.op("pool", lambda e, s=s: e.tensor_copy(out=wg_b[s][:], in_=wg_f[s][:]),
                     reads=[b_wgf[s]], writes=[b_wgb[s]])
                for j in range(2):
                    js = slice(j * TT, (j + 1) * TT)
                    pg, b_pg = C.next_ps()
                    pu, b_pu = C.next_ps()
                    for k in range(KD):
                        P.op("pe", lambda e, pg=pg, s=s, k=k, js=js: e.matmul(
                            pg[:], lhsT=wg_b[s][:, k, 0:128], rhs=hT[:, k, js], start=(k == 0), stop=(k == KD - 1)),
                            reads=[b_wgb[s], b_hT[j]], writes=[b_pg])
                    for k in range(KD):
                        P.op("pe", lambda e, pu=pu, s=s, k=k, js=js: e.matmul(
                            pu[:], lhsT=wg_b[s][:, k, 128:256], rhs=hT[:, k, js], start=(k == 0), stop=(k == KD - 1)),
                            reads=[b_wgb[s], b_hT[j]], writes=[b_pu])
                    q = nsg % 2
                    nsg += 1
                    P.op("act", lambda e, q=q, pg=pg: e.activation(out=sg[q][:], in_=pg[:], func=AF.Silu),
                         reads=[b_pg], writes=[b_sg[q]])
                    P.op("dve", lambda e, q=q, pu=pu, n=n, js=js: e.tensor_tensor(
                        out=aT[:, n, js], in0=pu[:], in1=sg[q][:], op=ALU.mult),
                        reads=[b_pu, b_sg[q]], writes=[b_aT[n][j]])
            for m in range(KD):
                s = nwd % 2
                nwd += 1
                P.dma("sp", lambda e, s=s, m=m: e.dma_start(out=wd_f[s][:], in_=wd_v[:, :, m * 128:(m + 1) * 128]),
                      b_wdf[s], writes=[b_wdf[s]])
                P.op("pool", lambda e, s=s: e.tensor_copy(out=wd_b[s][:], in_=wd_f[s][:]),
                     reads=[b_wdf[s]], writes=[b_wdb[s]])
                for j in range(2):
                    tt = tts[j]
                    js = slice(j * TT, (j + 1) * TT)
                    ts = slice(tt * TT, (tt + 1) * TT)
                    po, b_po = C.next_ps()
                    for n in range(KF):
                        P.op("pe", lambda e, po=po, s=s, n=n, js=js: e.matmul(
                            po[:], lhsT=wd_b[s][:, n, :], rhs=aT[:, n, js], start=(n == 0), stop=(n == KF - 1)),
                            reads=[b_wdb[s], b_aT[n][j]], writes=[b_po])
                    P.op("dve", lambda e, po=po, m=m, ts=ts: e.tensor_tensor(
                        out=xT[:, m, ts], in0=po[:], in1=xT[:, m, ts], op=ALU.add),
                        reads=[b_po, b_x[m][tt]], writes=[b_x[m][tt]])
    P.barrier()


def load_xT(P, xT, b_x, x_dram, eng="sp"):
    xv = x_dram.rearrange("(k p) t -> p k t", p=128)
    for k in range(KD):
        for tt in range(NTT):
            ts = slice(tt * TT, (tt + 1) * TT)
            P.dma(eng, lambda e, k=k, ts=ts: e.dma_start(out=xT[:, k, ts], in_=xv[:, k, ts]),
                  b_x[k][tt], writes=[b_x[k][tt]])


def store_xT(P, xT, b_x, y_dram, eng="sp"):
    yv = y_dram.rearrange("(k p) t -> p k t", p=128)
    toks = []
    for k in range(KD):
        for tt in range(NTT):
            ts = slice(tt * TT, (tt + 1) * TT)
            toks.append(P.dma(eng, lambda e, k=k, ts=ts: e.dma_start(out=yv[:, k, ts], in_=xT[:, k, ts]),
                              b_x[k][tt], reads=[b_x[k][tt]]))
    P.finish(toks)


def build_ffn_prog():
    nc = bass.Bass("TRN2", target_bir_lowering=False)
    x = nc.dram_tensor("xT", [D, NT], F32, kind="ExternalInput").ap()
    wgu = nc.dram_tensor("wgu", [D, 2 * DFF], F32, kind="ExternalInput").ap()
    wd = nc.dram_tensor("wd", [DFF, D], F32, kind="ExternalInput").ap()
    gain = nc.dram_tensor("gain", [128, KD], F32, kind="ExternalInput").ap()
    y = nc.dram_tensor("yT", [D, NT], F32, kind="ExternalOutput").ap()
    with ExitStack() as es:
        P = Prog(nc, es)
        C = Ctx(P)
        xT = P.sb("xT_sb", [128, KD, NT], F32)
        b_x = [[Buf(f"x{k}_{t}") for t in range(NTT)] for k in range(KD)]
        g_sb = P.sb("gain_sb", [128, KD], F32)
        b_g = Buf("gain")
        P.dma("sp", lambda e: e.dma_start(out=g_sb[:], in_=gain[:]), b_g, writes=[b_g])
        load_xT(P, xT, b_x, x)
        emit_ffn(P, C, xT, b_x, wgu, wd, g_sb, b_g, 0)
        store_xT(P, xT, b_x, y)
        P.emit()
    return nc


def col_layout(v):
    return np.ascontiguousarray(np.asarray(v, np.float32).reshape(-1, 128).T)


def to_core_T(x2d, c):
    blocks = x2d.reshape(SEQ // 128, 128, -1)[c::NCORES]
    return np.ascontiguousarray(blocks.reshape(NT, -1).T)


def from_core_T(parts):
    dd = parts[0].shape[0]
    out = np.empty((SEQ // 128, 128, dd), np.float32)
    for c, p in enumerate(parts):
        out[c::NCORES] = p.T.reshape(NT // 128, 128, dd)
    return out.reshape(SEQ, dd)


def build_prenorm_prog():
    nc = bass.Bass("TRN2", target_bir_lowering=False)
    x = nc.dram_tensor("xT", [D, NT], F32, kind="ExternalInput").ap()
    gain = nc.dram_tensor("gain", [128, KD], F32, kind="ExternalInput").ap()
    y = nc.dram_tensor("hT", [D, NT], F32, kind="ExternalOutput").ap()
    with ExitStack() as es:
        P = Prog(nc, es)
        C = Ctx(P)
        xT = P.sb("xT_sb", [128, KD, NT], F32)
        b_x = [[Buf(f"x{k}_{t}") for t in range(NTT)] for k in range(KD)]
        g_sb = P.sb("gain_sb", [128, KD], F32)
        b_g = Buf("gain")
        P.dma("sp", lambda e: e.dma_start(out=g_sb[:], in_=gain[:]), b_g, writes=[b_g])
        load_xT(P, xT, b_x, x)
        hT = P.sb("hT_sb", [128, KD, NT], F32)
        b_h = [Buf(f"h{t}") for t in range(NTT)]
        emit_rmsnorm_T(P, C, xT, b_x, g_sb, b_g, 0, hT, b_h, list(range(NTT)), es, "pn")
        yv = y.rearrange("(k p) t -> p k t", p=128)
        toks = []
        for tt in range(NTT):
            ts = slice(tt * TT, (tt + 1) * TT)
            toks.append(P.dma("sp", lambda e, ts=ts: e.dma_start(out=yv[:, :, ts], in_=hT[:, :, ts]),
                              b_h[tt], reads=[b_h[tt]]))
        P.finish(toks)
        P.emit()
    return nc


S5T = 512
TWO_PI = 2.0 * math.pi

PI_LO = 3.1415925
CW1 = 6.28125
CW2 = TWO_PI - 6.28125


def emit_range_reduce(P, dst, src, ti, tf, reads, writes):
    rw = list(reads) + list(writes)
    P.op("dve", lambda e: e.tensor_scalar(out=tf, in0=src, scalar1=1.0 / TWO_PI, scalar2=None, op0=ALU.mult),
         reads=rw, writes=writes)
    P.op("dve", lambda e: e.tensor_copy(out=ti, in_=tf), reads=rw, writes=writes)
    P.op("dve", lambda e: e.tensor_copy(out=tf, in_=ti), reads=rw, writes=writes)
    P.op("dve", lambda e: e.scalar_tensor_tensor(out=dst, in0=tf, scalar=-CW1, in1=src, op0=ALU.mult, op1=ALU.add),
         reads=rw, writes=writes)
    P.op("dve", lambda e: e.scalar_tensor_tensor(out=dst, in0=tf, scalar=-CW2, in1=dst, op0=ALU.mult, op1=ALU.add),
         reads=rw, writes=writes)
    P.op("dve", lambda e: e.tensor_scalar(out=tf, in0=dst, scalar1=math.pi, scalar2=-TWO_PI, op0=ALU.is_gt, op1=ALU.mult),
         reads=rw, writes=writes)
    P.op("dve", lambda e: e.tensor_tensor(out=dst, in0=dst, in1=tf, op=ALU.add), reads=rw, writes=writes)
    P.op("dve", lambda e: e.tensor_scalar(out=tf, in0=dst, scalar1=-math.pi, scalar2=TWO_PI, op0=ALU.is_lt, op1=ALU.mult),
         reads=rw, writes=writes)
    P.op("dve", lambda e: e.tensor_tensor(out=dst, in0=dst, in1=tf, op=ALU.add), reads=rw, writes=writes)
    P.op("dve", lambda e: e.tensor_scalar(out=dst, in0=dst, scalar1=PI_LO, scalar2=-PI_LO, op0=ALU.min, op1=ALU.max),
         reads=rw, writes=writes)


def build_s5_prog(debug=0):
    nc = bass.Bass("TRN2", target_bir_lowering=False)
    u_d = nc.dram_tensor("uT", [128, SEQ], F32, kind="ExternalInput").ap()
    par_d = nc.dram_tensor("par", [128, 16], F32, kind="ExternalInput").ap()
    bt_d = nc.dram_tensor("bt", [128, 2, 512], F32, kind="ExternalInput").ap()
    ct_d = nc.dram_tensor("ct", [128, 2, 4, 128], F32, kind="ExternalInput").ap()
    tau_d = nc.dram_tensor("tau", [128, S5T], F32, kind="ExternalInput").ap()
    y_d = nc.dram_tensor("gyT", [128, SEQ], F32, kind="ExternalOutput").ap()
    NCH = SEQ // S5T
    with ExitStack() as es:
        P = Prog(nc, es)
        C = Ctx(P)
        par = P.sb("par_sb", [128, 16], F32)
        bt = P.sb("bt_sb", [128, 2, 512], F32)
        ct = P.sb("ct_sb", [128, 2, 4, 128], F32)
        tau = P.sb("tau_sb", [128, S5T], F32)
        b_par, b_bt, b_ct, b_tau = Buf("par"), Buf("bt"), Buf("ct"), Buf("tau")
        P.dma("sp", lambda e: e.dma_start(out=par[:], in_=par_d[:]), b_par, writes=[b_par])
        P.dma("sp", lambda e: e.dma_start(out=bt[:], in_=bt_d[:]), b_bt, writes=[b_bt])
        P.dma("sp", lambda e: e.dma_start(out=ct[:], in_=ct_d[:]), b_ct, writes=[b_ct])
        P.dma("sp", lambda e: e.dma_start(out=tau[:], in_=tau_d[:]), b_tau, writes=[b_tau])
        sm = P.sb("s5_small", [128, 24, 4], F32)
        b_sm = Buf("s5_small")
        halfpi = P.sb("halfpi", [128, 1], F32)
        P.op("pool", lambda e: e.memset(halfpi[:], math.pi / 2), writes=[b_sm])
        lam_re, lam_im, logdt, dcol = par[:, 0:4], par[:, 4:8], par[:, 8:12], par[:, 12:13]
        (DT, LR, TH, R, THR, ABS, ARE, AIM, NR, NUM_RE, NUM_IM, DEN, KRE, KIM, T1, T2, PHT, CT_, ST_, NST_) = range(20)
        col = lambda i: sm[:, i, :]

        def V(fn, reads=(b_par, b_sm)):
            P.op("dve", fn, reads=list(reads), writes=[b_sm])

        def A(fn):
            P.op("act", fn, reads=[b_par, b_sm], writes=[b_sm])

        def sincos(src, s_out, c_out, shape_ap_abs):
            A(lambda e: e.activation(out=s_out, in_=src, func=AF.Sin))
            A(lambda e: e.activation(out=shape_ap_abs, in_=src, func=AF.Abs))
            A(lambda e: e.activation(out=c_out, in_=shape_ap_abs, func=AF.Sin, scale=-1.0, bias=halfpi[:]))

        def reduce_phase(out, src_fn_desc):
            pass

        A(lambda e: e.activation(out=col(DT), in_=logdt, func=AF.Exp))
        V(lambda e: e.tensor_tensor(out=col(LR), in0=lam_re, in1=col(DT), op=ALU.mult))
        V(lambda e: e.tensor_tensor(out=col(TH), in0=lam_im, in1=col(DT), op=ALU.mult))
        A(lambda e: e.activation(out=col(R), in_=col(LR), func=AF.Exp))
        smi = P.sb("s5_smi", [128, 4], mybir.dt.int32)
        emit_range_reduce(P, col(THR), col(TH), smi[:], col(T1), [b_par], [b_sm])
        sincos(col(THR), col(AIM), col(ARE), col(ABS))
        V(lambda e: e.tensor_tensor(out=col(ARE), in0=col(ARE), in1=col(R), op=ALU.mult))
        V(lambda e: e.tensor_tensor(out=col(AIM), in0=col(AIM), in1=col(R), op=ALU.mult))
        V(lambda e: e.tensor_scalar(out=col(NR), in0=col(ARE), scalar1=-1.0, scalar2=None, op0=ALU.add))
        V(lambda e: e.tensor_tensor(out=col(T1), in0=col(NR), in1=lam_re, op=ALU.mult))
        V(lambda e: e.tensor_tensor(out=col(T2), in0=col(AIM), in1=lam_im, op=ALU.mult))
        V(lambda e: e.tensor_tensor(out=col(NUM_RE), in0=col(T1), in1=col(T2), op=ALU.add))
        V(lambda e: e.tensor_tensor(out=col(T1), in0=col(AIM), in1=lam_re, op=ALU.mult))
        V(lambda e: e.tensor_tensor(out=col(T2), in0=col(NR), in1=lam_im, op=ALU.mult))
        V(lambda e: e.tensor_tensor(out=col(NUM_IM), in0=col(T1), in1=col(T2), op=ALU.subtract))
        V(lambda e: e.tensor_tensor(out=col(T1), in0=lam_re, in1=lam_re, op=ALU.mult))
        V(lambda e: e.tensor_tensor(out=col(T2), in0=lam_im, in1=lam_im, op=ALU.mult))
        V(lambda e: e.tensor_tensor(out=col(DEN), in0=col(T1), in1=col(T2), op=ALU.add))
        V(lambda e: e.reciprocal(out=col(DEN), in_=col(DEN)))
        V(lambda e: e.tensor_tensor(out=col(KRE), in0=col(NUM_RE), in1=col(DEN), op=ALU.mult))
        V(lambda e: e.tensor_tensor(out=col(KIM), in0=col(NUM_IM), in1=col(DEN), op=ALU.mult))
        V(lambda e: e.tensor_scalar(out=col(T2), in0=col(THR), scalar1=float(S5T), scalar2=None, op0=ALU.mult))
        emit_range_reduce(P, col(PHT), col(T2), smi[:], col(T1), [b_par], [b_sm])
        sincos(col(PHT), col(ST_), col(CT_), col(ABS))
        V(lambda e: e.tensor_scalar(out=col(NST_), in0=col(ST_), scalar1=-1.0, scalar2=None, op0=ALU.mult))
        L = P.sb("s5_L", [128, 3, 4, 128], BF16)
        b_L = Buf("s5_L")
        ctmp = P.sb("s5_ctmp", [128, 2, 128], F32)
        b_ctmp = Buf("s5_ctmp")
        for k in range(4):
            kre, kim = sm[:, KRE, k:k + 1], sm[:, KIM, k:k + 1]
            P.op("dve", lambda e, k=k, kim=kim: e.tensor_scalar(out=ctmp[:, 0, :], in0=ct[:, 1, k, :], scalar1=kim,
                                                                 scalar2=-1.0, op0=ALU.mult, op1=ALU.mult),
                 reads=[b_ct, b_sm], writes=[b_ctmp])
            P.op("dve", lambda e, k=k, kre=kre: e.scalar_tensor_tensor(out=ctmp[:, 0, :], in0=ct[:, 0, k, :], scalar=kre,
                                                                        in1=ctmp[:, 0, :], op0=ALU.mult, op1=ALU.add),
                 reads=[b_ct, b_sm, b_ctmp], writes=[b_ctmp])
            P.op("dve", lambda e, k=k, kre=kre: e.tensor_scalar(out=ctmp[:, 1, :], in0=ct[:, 1, k, :], scalar1=kre,
                                                                 scalar2=None, op0=ALU.mult),
                 reads=[b_ct, b_sm], writes=[b_ctmp])
            P.op("dve", lambda e, k=k, kim=kim: e.scalar_tensor_tensor(out=ctmp[:, 1, :], in0=ct[:, 0, k, :], scalar=kim,
                                                                        in1=ctmp[:, 1, :], op0=ALU.mult, op1=ALU.add),
                 reads=[b_ct, b_sm, b_ctmp], writes=[b_ctmp])
            P.op("dve", lambda e, k=k: e.tensor_copy(out=L[:, 0, k, :], in_=ctmp[:, 0, :]), reads=[b_ctmp], writes=[b_L])
            P.op("dve", lambda e, k=k: e.tensor_scalar(out=L[:, 1, k, :], in0=ctmp[:, 0, :], scalar1=-1.0, scalar2=None,
                                                       op0=ALU.mult), reads=[b_ctmp], writes=[b_L])
            P.op("dve", lambda e, k=k: e.tensor_scalar(out=L[:, 2, k, :], in0=ctmp[:, 1, :], scalar1=-1.0, scalar2=None,
                                                       op0=ALU.mult), reads=[b_ctmp], writes=[b_L])
        btb = P.sb("s5_btb", [128, 2, 512], BF16)
        b_btb = Buf("s5_btb")
        P.op("dve", lambda e: e.tensor_copy(out=btb[:], in_=bt[:]), reads=[b_bt], writes=[b_btb])
        cosT = P.sb("s5_cos", [128, 4, S5T], F32)
        sinT = P.sb("s5_sin", [128, 4, S5T], F32)
        rT = P.sb("s5_rT", [128, 4, S5T], F32)
        b_tab = Buf("s5_tab")
        ph = P.sb("s5_ph", [128, S5T], F32)
        pha = P.sb("s5_pha", [128, S5T], F32)
        phx = P.sb("s5_phx", [128, S5T], F32)
        phi = P.sb("s5_phi", [128, S5T], mybir.dt.int32)
        b_ph = Buf("s5_ph")
        for k in range(4):
            P.op("dve", lambda e, k=k: e.tensor_scalar(out=phx[:], in0=tau[:], scalar1=sm[:, THR, k:k + 1],
                                                       scalar2=None, op0=ALU.mult),
                 reads=[b_tau, b_sm, b_tab], writes=[b_ph])
            emit_range_reduce(P, ph[:], phx[:], phi[:], pha[:], [b_sm], [b_ph])
            P.op("act", lambda e, k=k: e.activation(out=sinT[:, k, :], in_=ph[:], func=AF.Sin),
                 reads=[b_ph], writes=[b_tab])
            P.op("act", lambda e: e.activation(out=pha[:], in_=ph[:], func=AF.Abs),
                 reads=[b_ph], writes=[b_ph])
            P.op("act", lambda e, k=k: e.activation(out=cosT[:, k, :], in_=pha[:], func=AF.Sin, scale=-1.0, bias=halfpi[:]),
                 reads=[b_ph, b_sm], writes=[b_tab])
            P.op("dve", lambda e, k=k: e.tensor_scalar(out=rT[:, k, :], in0=tau[:], scalar1=0.0, scalar2=sm[:, R, k:k + 1],
                                                       op0=ALU.mult, op1=ALU.add),
                 reads=[b_tau, b_sm], writes=[b_tab])
        if debug == 2:
            tk = [P.dma("sp", lambda e: e.dma_start(out=y_d[:, 0:96], in_=sm[:].rearrange("p a b -> p (a b)")), b_sm, reads=[b_sm]),
                  P.dma("sp", lambda e: e.dma_start(out=y_d[:, 1024:3072], in_=cosT[:].rearrange("p a b -> p (a b)")), b_tab, reads=[b_tab]),
                  P.dma("sp", lambda e: e.dma_start(out=y_d[:, 3072:5120], in_=sinT[:].rearrange("p a b -> p (a b)")), b_tab, reads=[b_tab]),
                  P.dma("sp", lambda e: e.dma_start(out=y_d[:, 5120:7168], in_=rT[:].rearrange("p a b -> p (a b)")), b_tab, reads=[b_tab])]
            P.finish(tk)
            P.emit()
            return nc
        NB = 2
        uf = [P.sb(f"s5_uf{i}", [128, S5T], F32) for i in range(NB)]
        ub = [P.sb(f"s5_ub{i}", [128, S5T], BF16) for i in range(NB)]
        b_uf = [Buf(f"s5_uf{i}") for i in range(NB)]
        b_ub = [Buf(f"s5_ub{i}") for i in range(NB)]
        sA = [P.sb(f"s5_sA{i}", [128, S5T], F32) for i in range(2)]
        sB = [P.sb(f"s5_sB{i}", [128, S5T], F32) for i in range(2)]
        b_sA = [Buf(f"s5_sA{i}") for i in range(2)]
        b_sB = [Buf(f"s5_sB{i}") for i in range(2)]
        t_ = [[P.sb(f"s5_t{j}_{i}", [128, S5T], F32) for i in range(2)] for j in range(4)]
        b_t = [[Buf(f"s5_t{j}_{i}") for i in range(2)] for j in range(4)]
        bre = [P.sb(f"s5_bre{i}", [128, S5T], F32) for i in range(2)]
        bim = [P.sb(f"s5_bim{i}", [128, S5T], F32) for i in range(2)]
        b_bre = [Buf(f"s5_bre{i}") for i in range(2)]
        b_bim = [Buf(f"s5_bim{i}") for i in range(2)]
        zre = [P.sb(f"s5_zre{i}", [128, S5T], F32) for i in range(2)]
        zim = [P.sb(f"s5_zim{i}", [128, S5T], F32) for i in range(2)]
        b_zre = [Buf(f"s5_zre{i}") for i in range(2)]
        b_zim = [Buf(f"s5_zim{i}") for i in range(2)]
        pp = [[P.sb(f"s5_p{j}_{i}", [128, S5T], BF16) for i in range(2)] for j in range(4)]
        b_pp = [[Buf(f"s5_p{j}_{i}") for i in range(2)] for j in range(4)]
        init = P.sb("s5_init", [128, 2, 4], F32)
        itmp = P.sb("s5_itmp", [128, 2, 4], F32)
        b_init = [Buf(f"s5_init{k}") for k in range(4)]
        P.op("dve", lambda e: e.memset(init[:], 0.0), writes=b_init)
        ysb = [P.sb(f"s5_y{i}", [128, S5T], F32) for i in range(2)]
        g1 = [P.sb(f"s5_g1{i}", [128, S5T], F32) for i in range(2)]
        g2 = [P.sb(f"s5_g2{i}", [128, S5T], F32) for i in range(2)]
        b_y = [Buf(f"s5_y{i}") for i in range(2)]
        b_g1 = [Buf(f"s5_g1{i}") for i in range(2)]
        b_g2 = [Buf(f"s5_g2{i}") for i in range(2)]
        toks = []
        it = 0
        for c in range(NCH):
            cs = slice(c * S5T, (c + 1) * S5T)
            ui = c % NB
            P.dma("sp", lambda e, ui=ui, cs=cs: e.dma_start(out=uf[ui][:], in_=u_d[:, cs]), b_uf[ui], writes=[b_uf[ui]])
            P.op("act", lambda e, ui=ui: e.activation(out=ub[ui][:], in_=uf[ui][:], func=AF.Copy),
                 reads=[b_uf[ui]], writes=[b_ub[ui]])
            yps, b_yps = C.psum[6 + c % 2], C.b_ps[6 + c % 2]
            for k in range(4):
                s = it % 2
                pa, b_pa = C.psum[(2 * it) % 6], C.b_ps[(2 * it) % 6]
                pb, b_pb = C.psum[(2 * it + 1) % 6], C.b_ps[(2 * it + 1) % 6]
                it += 1
                ks = slice(k * 128, (k + 1) * 128)
                P.op("pe", lambda e, pa=pa, ks=ks, ui=ui: e.matmul(pa[:], lhsT=btb[:, 0, ks], rhs=ub[ui][:], start=True, stop=True),
                     reads=[b_btb, b_ub[ui]], writes=[b_pa])
                P.op("pe", lambda e, pb=pb, ks=ks, ui=ui: e.matmul(pb[:], lhsT=btb[:, 1, ks], rhs=ub[ui][:], start=True, stop=True),
                     reads=[b_btb, b_ub[ui]], writes=[b_pb])
                P.op("act", lambda e, s=s, pa=pa: e.activation(out=sA[s][:], in_=pa[:], func=AF.Copy), reads=[b_pa], writes=[b_sA[s]])
                P.op("act", lambda e, s=s, pb=pb: e.activation(out=sB[s][:], in_=pb[:], func=AF.Copy), reads=[b_pb], writes=[b_sB[s]])
                P.op("pool", lambda e, s=s, k=k: e.tensor_tensor(out=t_[0][s][:], in0=sA[s][:], in1=cosT[:, k, :], op=ALU.mult),
                     reads=[b_sA[s], b_tab], writes=[b_t[0][s]])
                P.op("dve", lambda e, s=s, k=k: e.tensor_tensor(out=t_[1][s][:], in0=sB[s][:], in1=sinT[:, k, :], op=ALU.mult),
                     reads=[b_sB[s], b_tab], writes=[b_t[1][s]])
                P.op("pool", lambda e, s=s, k=k: e.tensor_tensor(out=t_[2][s][:], in0=sB[s][:], in1=cosT[:, k, :], op=ALU.mult),
                     reads=[b_sB[s], b_tab], writes=[b_t[2][s]])
                P.op("dve", lambda e, s=s, k=k: e.tensor_tensor(out=t_[3][s][:], in0=sA[s][:], in1=sinT[:, k, :], op=ALU.mult),
                     reads=[b_sA[s], b_tab], writes=[b_t[3][s]])
                P.op("pool", lambda e, s=s: e.tensor_tensor(out=bre[s][:], in0=t_[0][s][:], in1=t_[1][s][:], op=ALU.add),
                     reads=[b_t[0][s], b_t[1][s]], writes=[b_bre[s]])
                P.op("pool", lambda e, s=s: e.tensor_tensor(out=bim[s][:], in0=t_[2][s][:], in1=t_[3][s][:], op=ALU.subtract),
                     reads=[b_t[2][s], b_t[3][s]], writes=[b_bim[s]])
                P.op("dve", lambda e, s=s, k=k: e.tensor_tensor_scan(out=zre[s][:], data0=rT[:, k, :], data1=bre[s][:],
                                                                      initial=init[:, 0, k:k + 1], op0=ALU.mult, op1=ALU.add),
                     reads=[b_tab, b_bre[s], b_init[k]], writes=[b_zre[s]])
                P.op("dve", lambda e, s=s, k=k: e.tensor_tensor_scan(out=zim[s][:], data0=rT[:, k, :], data1=bim[s][:],
                                                                      initial=init[:, 1, k:k + 1], op0=ALU.mult, op1=ALU.add),
                     reads=[b_tab, b_bim[s], b_init[k]], writes=[b_zim[s]])
                zlr, zli = zre[s][:, S5T - 1:S5T], zim[s][:, S5T - 1:S5T]
                cT_, sT_, nsT_ = sm[:, CT_, k:k + 1], sm[:, ST_, k:k + 1], sm[:, NST_, k:k + 1]
                P.op("dve", lambda e, k=k, zlr=zlr, cT_=cT_: e.tensor_tensor(out=itmp[:, 0, k:k + 1], in0=zlr, in1=cT_, op=ALU.mult),
                     reads=[b_zre[s], b_sm], writes=[b_init[k]])
                P.op("dve", lambda e, k=k, zlr=zlr, sT_=sT_: e.tensor_tensor(out=itmp[:, 1, k:k + 1], in0=zlr, in1=sT_, op=ALU.mult),
                     reads=[b_zre[s], b_sm], writes=[b_init[k]])
                P.op("dve", lambda e, k=k, zli=zli, nsT_=nsT_: e.scalar_tensor_tensor(
                    out=init[:, 0, k:k + 1], in0=zli, scalar=nsT_, in1=itmp[:, 0, k:k + 1], op0=ALU.mult, op1=ALU.add),
                    reads=[b_zim[s], b_sm], writes=[b_init[k]])
                P.op("dve", lambda e, k=k, zli=zli, cT_=cT_: e.scalar_tensor_tensor(
                    out=init[:, 1, k:k + 1], in0=zli, scalar=cT_, in1=itmp[:, 1, k:k + 1], op0=ALU.mult, op1=ALU.add),
                    reads=[b_zim[s], b_sm], writes=[b_init[k]])
                P.op("pool", lambda e, s=s, k=k: e.tensor_tensor(out=pp[0][s][:], in0=zre[s][:], in1=cosT[:, k, :], op=ALU.mult),
                     reads=[b_zre[s], b_tab], writes=[b_pp[0][s]])
                P.op("pool", lambda e, s=s, k=k: e.tensor_tensor(out=pp[1][s][:], in0=zim[s][:], in1=sinT[:, k, :], op=ALU.mult),
                     reads=[b_zim[s], b_tab], writes=[b_pp[1][s]])
                P.op("dve", lambda e, s=s, k=k: e.tensor_tensor(out=pp[2][s][:], in0=zre[s][:], in1=sinT[:, k, :], op=ALU.mult),
                     reads=[b_zre[s], b_tab], writes=[b_pp[2][s]])
                P.op("pool", lambda e, s=s, k=k: e.tensor_tensor(out=pp[3][s][:], in0=zim[s][:], in1=cosT[:, k, :], op=ALU.mult),
                     reads=[b_zim[s], b_tab], writes=[b_pp[3][s]])
                for j, li in enumerate((0, 1, 2, 2)):
                    P.op("pe", lambda e, yps=yps, li=li, k=k, j=j, s=s: e.matmul(
                        yps[:], lhsT=L[:, li, k, :], rhs=pp[j][s][:], start=(k == 0 and j == 0), stop=(k == 3 and j == 3)),
                        reads=[b_L, b_pp[j][s]], writes=[b_yps])
            q = c % 2
            P.op("dve", lambda e, q=q, ui=ui, yps=yps: e.scalar_tensor_tensor(
                out=ysb[q][:], in0=uf[ui][:], scalar=dcol, in1=yps[:], op0=ALU.mult, op1=ALU.add),
                reads=[b_uf[ui], b_par, b_yps], writes=[b_y[q]])
            P.op("pool", lambda e, q=q: e.tensor_tensor(out=g1[q][:], in0=ysb[q][:], in1=ysb[q][:], op=ALU.mult),
                 reads=[b_y[q]], writes=[b_g1[q]])
            P.op("pool", lambda e, q=q: e.tensor_scalar(out=g1[q][:], in0=g1[q][:], scalar1=0.044715, scalar2=1.0,
                                                        op0=ALU.mult, op1=ALU.add),
                 reads=[b_g1[q]], writes=[b_g1[q]])
            P.op("pool", lambda e, q=q: e.tensor_tensor(out=g1[q][:], in0=g1[q][:], in1=ysb[q][:], op=ALU.mult),
                 reads=[b_g1[q], b_y[q]], writes=[b_g1[q]])
            P.op("act", lambda e, q=q: e.activation(out=g2[q][:], in_=g1[q][:], func=AF.Sigmoid, scale=1.5957691216057308),
                 reads=[b_g1[q]], writes=[b_g2[q]])
            P.op("pool", lambda e, q=q: e.tensor_tensor(out=g2[q][:], in0=g2[q][:], in1=ysb[q][:], op=ALU.mult),
                 reads=[b_g2[q], b_y[q]], writes=[b_g2[q]])
            if debug == 1:
                toks.append(P.dma("sp", lambda e, q=q, cs=cs: e.dma_start(out=y_d[:, cs], in_=ysb[q][:]), b_g2[q], reads=[b_g2[q], b_y[q]]))
                continue
            toks.append(P.dma("sp", lambda e, q=q, cs=cs: e.dma_start(out=y_d[:, cs], in_=g2[q][:]), b_g2[q], reads=[b_g2[q]]))
        P.finish(toks[-2:])
        P.emit()
    return nc


def s5_host_layout(lam_re, lam_im, log_dt, b_re, b_im, c_re, c_im, d, core):
    g0 = core * 8
    par = np.zeros((128, 16), np.float32)
    bt = np.zeros((128, 2, 512), np.float32)
    ct = np.zeros((128, 2, 4, 128), np.float32)
    for gl in range(8):
        g = g0 + gl
        k, p0 = gl // 2, (gl % 2) * 64
        par[p0:p0 + 64, 0 + k] = lam_re[g]
        par[p0:p0 + 64, 4 + k] = lam_im[g]
        par[p0:p0 + 64, 8 + k] = log_dt[g]
        bt[16 * gl:16 * gl + 16, 0, 64 * gl:64 * gl + 64] = b_re[g].T
        bt[16 * gl:16 * gl + 16, 1, 64 * gl:64 * gl + 64] = b_im[g].T
        ct[p0:p0 + 64, 0, k, 16 * gl:16 * gl + 16] = c_re[g].T
        ct[p0:p0 + 64, 1, k, 16 * gl:16 * gl + 16] = c_im[g].T
    par[:, 12] = d[core * 128:(core + 1) * 128]
    return par, bt, ct


def emit_glu(P, C, xT, b_x, gy_dram, wglu):
    with ExitStack() as es:
        gyb = P.sb("glu_gyb", [128, KD, NT], BF16, es)
        b_gyb = [Buf(f"glu_gyb{t}") for t in range(NTT)]
        st = [P.sb(f"glu_st{i}", [128, TT], F32, es) for i in range(2)]
        b_st = [Buf(f"glu_st{i}") for i in range(2)]
        gv = gy_dram.rearrange("(k p) t -> p k t", p=128)
        n = 0
        for tt in range(NTT):
            ts = slice(tt * TT, (tt + 1) * TT)
            for k in range(KD):
                s = n % 2
                n += 1
                P.dma("sp", lambda e, s=s, k=k, ts=ts: e.dma_start(out=st[s][:], in_=gv[:, k, ts]), b_st[s], writes=[b_st[s]])
                P.op("pool", lambda e, s=s, k=k, ts=ts: e.tensor_copy(out=gyb[:, k, ts], in_=st[s][:]),
                     reads=[b_st[s]], writes=[b_gyb[tt]])
        w_f = [P.sb(f"glu_wf{i}", [128, KD, 256], F32, es) for i in range(2)]
        w_b = [P.sb(f"glu_wb{i}", [128, KD, 256], BF16, es) for i in range(2)]
        b_wf = [Buf(f"glu_wf{i}") for i in range(2)]
        b_wb = [Buf(f"glu_wb{i}") for i in range(2)]
        sg = [P.sb(f"glu_sg{i}", [128, TT], F32, es) for i in range(2)]
        b_sg = [Buf(f"glu_sg{i}") for i in range(2)]
        wv = wglu.rearrange("(k p) n -> p k n", p=128)
        q = 0
        for m in range(KD):
            s = m % 2
            P.dma("sp", lambda e, s=s, m=m: e.dma_start(out=w_f[s][:, :, 0:128], in_=wv[:, :, m * 128:(m + 1) * 128]),
                  b_wf[s], writes=[b_wf[s]])
            P.dma("sp", lambda e, s=s, m=m: e.dma_start(out=w_f[s][:, :, 128:256], in_=wv[:, :, D + m * 128:D + (m + 1) * 128]),
                  b_wf[s], writes=[])
            b_wf[s].w = ("d", b_wf[s], b_wf[s].dcount)
            P.op("pool", lambda e, s=s: e.tensor_copy(out=w_b[s][:], in_=w_f[s][:]), reads=[b_wf[s]], writes=[b_wb[s]])
            for tt in range(NTT):
                ts = slice(tt * TT, (tt + 1) * TT)
                pv, b_pv = C.next_ps()
                pg, b_pg = C.next_ps()
                for k in range(KD):
                    P.op("pe", lambda e, pv=pv, s=s, k=k, ts=ts: e.matmul(pv[:], lhsT=w_b[s][:, k, 0:128], rhs=gyb[:, k, ts],
                                                                         start=(k == 0), stop=(k == KD - 1)),
                         reads=[b_wb[s], b_gyb[tt]], writes=[b_pv])
                for k in range(KD):
                    P.op("pe", lambda e, pg=pg, s=s, k=k, ts=ts: e.matmul(pg[:], lhsT=w_b[s][:, k, 128:256], rhs=gyb[:, k, ts],
                                                                         start=(k == 0), stop=(k == KD - 1)),
                         reads=[b_wb[s], b_gyb[tt]], writes=[b_pg])
                qq = q % 2
                q += 1
                P.op("act", lambda e, qq=qq, pg=pg: e.activation(out=sg[qq][:], in_=pg[:], func=AF.Sigmoid),
                     reads=[b_pg], writes=[b_sg[qq]])
                P.op("dve", lambda e, qq=qq, pv=pv: e.tensor_tensor(out=sg[qq][:], in0=pv[:], in1=sg[qq][:], op=ALU.mult),
                     reads=[b_pv, b_sg[qq]], writes=[b_sg[qq]])
                P.op("pool", lambda e, qq=qq, m=m, ts=ts: e.tensor_tensor(out=xT[:, m, ts], in0=xT[:, m, ts], in1=sg[qq][:], op=ALU.add),
                     reads=[b_sg[qq], b_x[m][tt]], writes=[b_x[m][tt]])
    P.barrier()


def build_glu_ffn_prog():
    nc = bass.Bass("TRN2", target_bir_lowering=False)
    x = nc.dram_tensor("xT", [D, NT], F32, kind="ExternalInput").ap()
    gy = nc.dram_tensor("gyT", [D, NT], F32, kind="ExternalInput").ap()
    wglu = nc.dram_tensor("wglu", [D, 2 * D], F32, kind="ExternalInput").ap()
    wgu = nc.dram_tensor("wgu", [D, 2 * DFF], F32, kind="ExternalInput").ap()
    wd = nc.dram_tensor("wd", [DFF, D], F32, kind="ExternalInput").ap()
    gain = nc.dram_tensor("gain", [128, KD], F32, kind="ExternalInput").ap()
    y = nc.dram_tensor("yT", [D, NT], F32, kind="ExternalOutput").ap()
    with ExitStack() as es:
        P = Prog(nc, es)
        C = Ctx(P)
        xT = P.sb("xT_sb", [128, KD, NT], F32)
        b_x = [[Buf(f"x{k}_{t}") for t in range(NTT)] for k in range(KD)]
        g_sb = P.sb("gain_sb", [128, KD], F32)
        b_g = Buf("gain")
        P.dma("sp", lambda e: e.dma_start(out=g_sb[:], in_=gain[:]), b_g, writes=[b_g])
        load_xT(P, xT, b_x, x)
        emit_glu(P, C, xT, b_x, gy, wglu)
        emit_ffn(P, C, xT, b_x, wgu, wd, g_sb, b_g, 0)
        store_xT(P, xT, b_x, y)
        P.emit()
    return nc


_PROGS = {}


def _prog(name, builder):
    if name not in _PROGS:
        _PROGS[name] = builder()
    return _PROGS[name]


def _run(name, builder, in_maps):
    import time
    t0 = time.time()
    nc = _prog(name, builder)
    t1 = time.time()
    res = run_bass_kernel_spmd(nc, in_maps, core_ids=list(range(NCORES))).results
    nb = sum(v.nbytes for m in in_maps for v in m.values())
    print(f"[launch {name}] build {t1 - t0:.1f}s run {time.time() - t1:.1f}s in_bytes {nb / 1e6:.0f}MB", flush=True)
    return res


def run_s5_layer(xT_parts, j, i, inp):
    gm = col_layout(inp["norm_mix"][i])
    res = _run("prenorm", build_prenorm_prog, [{"xT": xT_parts[c], "gain": gm} for c in range(NCORES)])
    h_full = from_core_T([r["hT"] for r in res])
    tau = np.tile(np.arange(S5T, dtype=np.float32)[None], (128, 1))
    maps = []
    for c in range(NCORES):
        par, bt, ct = s5_host_layout(inp["s5_lambda_re"][j], inp["s5_lambda_im"][j], inp["s5_log_dt"][j],
                                     inp["s5_b_re"][j], inp["s5_b_im"][j], inp["s5_c_re"][j], inp["s5_c_im"][j],
                                     inp["s5_d"][j], c)
        maps.append({"uT": np.ascontiguousarray(h_full[:, c * 128:(c + 1) * 128].T), "par": par, "bt": bt, "ct": ct, "tau": tau})
    res = _run("s5", build_s5_prog, maps)
    gy_full = np.concatenate([r["gyT"] for r in res], axis=0).T
    gf = col_layout(inp["norm_ffn"][i])
    maps = [{"xT": xT_parts[c], "gyT": to_core_T(gy_full, c), "wglu": inp["s5_w_glu"][j],
             "wgu": inp["ffn_w_gate_up"][i], "wd": inp["ffn_w_down"][i], "gain": gf} for c in range(NCORES)]
    res = _run("glu_ffn", build_glu_ffn_prog, maps)
    return [r["yT"] for r in res]


NH = 16
HD = 64
NIH = 8
PROJ = 3 * D + NIH * HD + HD + NIH


def build_dsa_proj_prog():
    nc = bass.Bass("TRN2", target_bir_lowering=False)
    x = nc.dram_tensor("xT", [D, NT], F32, kind="ExternalInput").ap()
    w_in = nc.dram_tensor("w_in", [D, PROJ], F32, kind="ExternalInput").ap()
    gain = nc.dram_tensor("gain", [128, KD], F32, kind="ExternalInput").ap()
    qk_g = nc.dram_tensor("qk_gain", [128, 2], F32, kind="ExternalInput").ap()
    cs_d = nc.dram_tensor("cossin", [128, 2, NT], F32, kind="ExternalInput").ap()
    cm_d = nc.dram_tensor("cmat", [128, 2, 128], F32, kind="ExternalInput").ap()
    qT_d = nc.dram_tensor("qT", [D, NT], BF16, kind="ExternalOutput").ap()
    kT_d = nc.dram_tensor("kT", [D, NT], BF16, kind="ExternalOutput").ap()
    v_d = nc.dram_tensor("v", [NT, D], BF16, kind="ExternalOutput").ap()
    qiT_d = nc.dram_tensor("qiT", [NIH * HD, NT], BF16, kind="ExternalOutput").ap()
    kiT_d = nc.dram_tensor("kiT", [HD, NT], BF16, kind="ExternalOutput").ap()
    w_d = nc.dram_tensor("w", [NT, NIH], F32, kind="ExternalOutput").ap()
    with ExitStack() as es:
        P = Prog(nc, es)
        C = Ctx(P)
        xT = P.sb("xT_sb", [128, KD, NT], F32)
        b_x = [[Buf(f"x{k}_{t}") for t in range(NTT)] for k in range(KD)]
        g_sb = P.sb("gain_sb", [128, KD], F32)
        qkg = P.sb("qkg_sb", [128, 2], F32)
        cs = P.sb("cs_sb", [128, 2, NT], F32)
        cm = P.sb("cm_sb", [128, 2, 128], F32)
        b_g, b_qkg, b_cs, b_cm = Buf("gain"), Buf("qkg"), Buf("cs"), Buf("cm")
        P.dma("sp", lambda e: e.dma_start(out=g_sb[:], in_=gain[:]), b_g, writes=[b_g])
        P.dma("sp", lambda e: e.dma_start(out=qkg[:], in_=qk_g[:]), b_qkg, writes=[b_qkg])
        P.dma("sp", lambda e: e.dma_start(out=cm[:], in_=cm_d[:]), b_cm, writes=[b_cm])
        P.dma("sp", lambda e: e.dma_start(out=cs[:, 0, :], in_=cs_d[:, 0, :]), b_cs, writes=[b_cs])
        P.dma("sp", lambda e: e.dma_start(out=cs[:, 1, :], in_=cs_d[:, 1, :]), b_cs, writes=[])
        b_cs.w = ("d", b_cs, b_cs.dcount)
        load_xT(P, xT, b_x, x)
        bones = P.sb("bones_bf", [128, 128], BF16)
        b_bones = Buf("bones")
        P.op("dve", lambda e: e.tensor_copy(out=bones[:], in_=cm[:, 0, :]), reads=[b_cm], writes=[b_bones])
        P.op("dve", lambda e: e.tensor_scalar(out=qkg[:, 0:1], in0=qkg[:, 0:1], scalar1=HD ** -0.5, scalar2=None, op0=ALU.mult),
             reads=[b_qkg], writes=[b_qkg])
        hT = P.sb("hT_sb", [128, KD, NT], BF16)
        b_h = [Buf(f"h{t}") for t in range(NTT)]
        emit_rmsnorm_T(P, C, xT, b_x, g_sb, b_g, 0, hT, b_h, list(range(NTT)), es, "pn")
        wv_ = w_in.rearrange("(k p) n -> p k n", p=128)
        w_f = [P.sb(f"pj_wf{i}", [128, KD, 128], F32) for i in range(2)]
        w_b = [P.sb(f"pj_wb{i}", [128, KD, 128], BF16) for i in range(2)]
        b_wf = [Buf(f"pj_wf{i}") for i in range(2)]
        b_wb = [Buf(f"pj_wb{i}") for i in range(2)]
        sq = [P.sb(f"pj_sq{i}", [128, TT], BF16) for i in range(2)]
        rs = [P.sb(f"pj_rs{i}", [128, TT], F32) for i in range(2)]
        tf = [P.sb(f"pj_t{i}", [128, TT], F32) for i in range(2)]
        o1 = [P.sb(f"pj_o1{i}", [128, TT], F32) for i in range(2)]
        o2 = [P.sb(f"pj_o2{i}", [128, TT], F32) for i in range(2)]
        ob = [P.sb(f"pj_ob{i}", [128, TT], BF16) for i in range(2)]
        b_sq = [Buf(f"pj_sq{i}") for i in range(2)]
        b_rs = [Buf(f"pj_rs{i}") for i in range(2)]
        b_tf = [Buf(f"pj_t{i}") for i in range(2)]
        b_o1 = [Buf(f"pj_o1{i}") for i in range(2)]
        b_o2 = [Buf(f"pj_o2{i}") for i in range(2)]
        b_ob = [Buf(f"pj_ob{i}") for i in range(2)]
        toks = []
        tiles = []
        for m in range(8):
            tiles.append((m * 128, 128, "norm", qT_d, m * 128, 0))
        for m in range(8):
            tiles.append((D + m * 128, 128, "norm", kT_d, m * 128, 1))
        for m in range(4):
            tiles.append((3 * D + m * 128, 128, "plain", qiT_d, m * 128, None))
        tiles.append((3 * D + NIH * HD, 64, "normnog", kiT_d, 0, None))
        it = 0
        for ti, (c0, M, kind, od, r0, gc) in enumerate(tiles):
            s = ti % 2
            P.dma("sp", lambda e, s=s, c0=c0, M=M: e.dma_start(out=w_f[s][:, :, 0:M], in_=wv_[:, :, c0:c0 + M]),
                  b_wf[s], writes=[b_wf[s]])
            P.op("pool", lambda e, s=s, M=M: e.tensor_copy(out=w_b[s][:, :, 0:M], in_=w_f[s][:, :, 0:M]),
                 reads=[b_wf[s]], writes=[b_wb[s]])
            for tt in range(NTT):
                ts = slice(tt * TT, (tt + 1) * TT)
                u = it % 2
                it += 1
                ps, b_ps = C.next_ps()
                for k in range(KD):
                    P.op("pe", lambda e, ps=ps, s=s, k=k, ts=ts, M=M: e.matmul(ps[0:M, :], lhsT=w_b[s][:, k, 0:M], rhs=hT[:, k, ts],
                                                                              start=(k == 0), stop=(k == KD - 1)),
                         reads=[b_wb[s], b_h[tt]], writes=[b_ps])
                if kind == "plain":
                    P.op("act", lambda e, u=u, ps=ps, M=M: e.activation(out=tf[u][0:M, :], in_=ps[0:M, :], func=AF.Copy),
                         reads=[b_ps], writes=[b_tf[u]])
                else:
                    P.op("act", lambda e, u=u, ps=ps, M=M: e.activation(out=sq[u][0:M, :], in_=ps[0:M, :], func=AF.Square),
                         reads=[b_ps], writes=[b_sq[u]])
                    p2, b_p2 = C.next_ps()
                    P.op("pe", lambda e, p2=p2, u=u, M=M: e.matmul(p2[0:M, :], lhsT=bones[0:M, 0:M], rhs=sq[u][0:M, :], start=True, stop=True),
                         reads=[b_bones, b_sq[u]], writes=[b_p2])
                    P.op("act", lambda e, u=u, p2=p2, M=M: e.activation(out=rs[u][0:M, :], in_=p2[0:M, :], func=AF.Sqrt, scale=1.0 / HD,
                                                                   bias=C.eps_col[0:M, :]),
                         reads=[b_p2, C.b_ones], writes=[b_rs[u]])
                    P.op("dve", lambda e, u=u, M=M: e.reciprocal(out=rs[u][0:M, :], in_=rs[u][0:M, :]), reads=[b_rs[u]], writes=[b_rs[u]])
                    if kind == "norm":
                        P.op("dve", lambda e, u=u, ps=ps, gc=gc, M=M: e.scalar_tensor_tensor(
                            out=tf[u][0:M, :], in0=ps[0:M, :], scalar=qkg[0:M, gc:gc + 1], in1=rs[u][0:M, :], op0=ALU.mult, op1=ALU.mult),
                            reads=[b_ps, b_qkg, b_rs[u]], writes=[b_tf[u]])
                    else:
                        P.op("dve", lambda e, u=u, ps=ps, M=M: e.tensor_tensor(out=tf[u][0:M, :], in0=ps[0:M, :], in1=rs[u][0:M, :], op=ALU.mult),
                             reads=[b_ps, b_rs[u]], writes=[b_tf[u]])
                p3, b_p3 = C.next_ps()
                P.op("pe", lambda e, p3=p3, u=u, M=M: e.matmul(p3[0:M, :], lhsT=cm[0:M, 1, 0:M], rhs=tf[u][0:M, :], start=True, stop=True),
                     reads=[b_cm, b_tf[u]], writes=[b_p3])
                P.op("pool", lambda e, u=u, ts=ts, M=M: e.tensor_tensor(out=o1[u][0:M, :], in0=tf[u][0:M, :], in1=cs[0:M, 0, ts], op=ALU.mult),
                     reads=[b_tf[u], b_cs], writes=[b_o1[u]])
                P.op("dve", lambda e, u=u, ts=ts, p3=p3, M=M: e.tensor_tensor(out=o2[u][0:M, :], in0=p3[0:M, :], in1=cs[0:M, 1, ts], op=ALU.mult),
                     reads=[b_p3, b_cs], writes=[b_o2[u]])
                P.op("pool", lambda e, u=u, M=M: e.tensor_tensor(out=ob[u][0:M, :], in0=o1[u][0:M, :], in1=o2[u][0:M, :], op=ALU.add),
                     reads=[b_o1[u], b_o2[u]], writes=[b_ob[u]])
                toks.append(P.dma("sp", lambda e, u=u, od=od, r0=r0, ts=ts, M=M: e.dma_start(out=od[r0:r0 + M, ts], in_=ob[u][0:M, :]),
                                  b_ob[u], reads=[b_ob[u]]))
        wvf = [P.sb(f"pj_vf{i}", [128, KD, 512], F32) for i in range(1)]
        wvb = [P.sb(f"pj_vb{i}", [128, KD, 512], BF16) for i in range(2)]
        b_wvf = [Buf("pj_vf0")]
        b_wvb = [Buf(f"pj_vb{i}") for i in range(2)]
        vo = [P.sb(f"pj_vo{i}", [128, 512], BF16) for i in range(2)]
        b_vo = [Buf(f"pj_vo{i}") for i in range(2)]
        for hf in range(2):
            P.dma("sp", lambda e, hf=hf: e.dma_start(out=wvf[0][:], in_=wv_[:, :, 2 * D + hf * 512:2 * D + (hf + 1) * 512]),
                  b_wvf[0], writes=[b_wvf[0]])
            P.op("pool", lambda e, hf=hf: e.tensor_copy(out=wvb[hf][:], in_=wvf[0][:]), reads=[b_wvf[0]], writes=[b_wvb[hf]])
        ww_f = P.sb("pj_wwf", [128, KD, NIH], F32)
        ww_b = P.sb("pj_wwb", [128, KD, NIH], BF16)
        b_wwf, b_wwb = Buf("pj_wwf"), Buf("pj_wwb")
        P.dma("sp", lambda e: e.dma_start(out=ww_f[:], in_=wv_[:, :, PROJ - NIH:PROJ]), b_wwf, writes=[b_wwf])
        P.op("pool", lambda e: e.tensor_copy(out=ww_b[:], in_=ww_f[:]), reads=[b_wwf], writes=[b_wwb])
        wo_sb = P.sb("pj_wo", [128, NT // 128, NIH], F32)
        b_wo = Buf("pj_wo")
        n = 0
        for blk in range(NT // 128):
            tt = blk // 4
            bs = slice(blk * 128, (blk + 1) * 128)
            for hf in range(2):
                u = n % 2
                n += 1
                ps, b_ps = C.next_ps()
                for k in range(KD):
                    P.op("pe", lambda e, ps=ps, k=k, bs=bs, hf=hf: e.matmul(ps[:], lhsT=hT[:, k, bs], rhs=wvb[hf][:, k, :],
                                                                           start=(k == 0), stop=(k == KD - 1)),
                         reads=[b_wvb[hf], b_h[tt]], writes=[b_ps])
                P.op("act", lambda e, u=u, ps=ps: e.activation(out=vo[u][:], in_=ps[:], func=AF.Copy), reads=[b_ps], writes=[b_vo[u]])
                toks.append(P.dma("sp", lambda e, u=u, bs=bs, hf=hf: e.dma_start(out=v_d[bs, hf * 512:(hf + 1) * 512], in_=vo[u][:]),
                                  b_vo[u], reads=[b_vo[u]]))
            ps, b_ps = C.next_ps()
            for k in range(KD):
                P.op("pe", lambda e, ps=ps, k=k, bs=bs: e.matmul(ps[:, 0:NIH], lhsT=hT[:, k, bs], rhs=ww_b[:, k, :],
                                                                 start=(k == 0), stop=(k == KD - 1)),
                     reads=[b_wwb, b_h[tt]], writes=[b_ps])
            P.op("act", lambda e, ps=ps, blk=blk: e.activation(out=wo_sb[:, blk, :], in_=ps[:, 0:NIH], func=AF.Copy,
                                                               scale=(NIH ** -0.5) * (HD ** -0.5)),
                 reads=[b_ps], writes=[b_wo])
        toks.append(P.dma("sp", lambda e: e.dma_start(out=w_d.rearrange("(b p) h -> p b h", p=128), in_=wo_sb[:]), b_wo, reads=[b_wo]))
        P.finish(toks)
        P.emit()
    return nc


def rope_consts(core):
    blocks = np.arange(SEQ // 128)[core::NCORES]
    pos = (blocks[:, None] * 128 + np.arange(128)[None, :]).reshape(-1).astype(np.float32)
    inv_freq = (10000.0 ** (-np.arange(0, HD, 2, dtype=np.float32) / HD)).astype(np.float32)
    ang = pos[None, :] * inv_freq[:, None]
    cos, sin = np.cos(ang).astype(np.float32), np.sin(ang).astype(np.float32)
    cs = np.empty((128, 2, NT), np.float32)
    for p in range(128):
        cs[p, 0] = cos[p % 32]
        cs[p, 1] = sin[p % 32]
    return cs


def const_mats():
    cm = np.zeros((128, 2, 128), np.float32)
    for p in range(128):
        for m in range(128):
            if p // 64 == m // 64:
                cm[p, 0, m] = 1.0
    for m in range(128):
        if (m % 64) < 32:
            cm[m + 32, 1, m] = -1.0
        else:
            cm[m - 32, 1, m] = 1.0
    return cm


TOPK = 256
NEG_SEL = -1.0e30
NEG_MASK = -2.0e30


def build_dsa_attn_prog(nblk=NT // 128):
    nc = bass.Bass("TRN2", target_bir_lowering=False)
    x_d = nc.dram_tensor("xT", [D, NT], F32, kind="ExternalInput").ap()
    qT_d = nc.dram_tensor("qT", [D, NT], BF16, kind="ExternalInput").ap()
    qiT_d = nc.dram_tensor("qiT", [NIH * HD, NT], BF16, kind="ExternalInput").ap()
    w_d = nc.dram_tensor("wq", [128, NT // 128, NIH], F32, kind="ExternalInput").ap()
    kT_d = nc.dram_tensor("kTf", [D, SEQ], BF16, kind="ExternalInput").ap()
    v_d = nc.dram_tensor("vf", [SEQ, D], BF16, kind="ExternalInput").ap()
    kiT_d = nc.dram_tensor("kiTf", [HD, SEQ], BF16, kind="ExternalInput").ap()
    pen_d = nc.dram_tensor("pen", [128, 1024], F32, kind="ExternalInput").ap()
    id_d = nc.dram_tensor("ident", [128, 128], F32, kind="ExternalInput").ap()
    wo_d = nc.dram_tensor("w_o", [D, D], F32, kind="ExternalInput").ap()
    y_d = nc.dram_tensor("yT", [D, NT], F32, kind="ExternalOutput").ap()
    qT_v = qT_d.rearrange("(h p) t -> p h t", p=64)
    qiT_v = qiT_d.rearrange("(h p) t -> p h t", p=64)
    kT_v = kT_d.rearrange("(h p) t -> p h t", p=64)
    x_v = x_d.rearrange("(k p) t -> p k t", p=128)
    y_v = y_d.rearrange("(k p) t -> p k t", p=128)
    wo_v = wo_d.rearrange("(h p) n -> p h n", p=64)
    with ExitStack() as es:
        P = Prog(nc, es)
        ones = P.sb("c_ones", [128, 64], BF16)
        b_c = Buf("consts")
        P.op("pool", lambda e: e.memset(ones[:], 1.0), writes=[b_c])
        pen = P.sb("pen_sb", [128, 1024], F32)
        penz = P.sb("penz_sb", [128, 1024], BF16)
        idf = P.sb("id_f", [128, 128], F32)
        idb = P.sb("id_b", [128, 128], BF16)
        wq = P.sb("wq_sb", [128, NT // 128, NIH], F32)
        b_pen, b_id, b_wq = Buf("pen"), Buf("ident"), Buf("wq")
        P.dma("sp", lambda e: e.dma_start(out=pen[:], in_=pen_d[:]), b_pen, writes=[b_pen])
        P.dma("sp", lambda e: e.dma_start(out=idf[:], in_=id_d[:]), b_id, writes=[b_id])
        P.dma("sp", lambda e: e.dma_start(out=wq[:], in_=w_d[:]), b_wq, writes=[b_wq])
        P.op("pool", lambda e: e.tensor_single_scalar(out=penz[:], in_=pen[:], scalar=-1.0, op=ALU.is_ge), reads=[b_pen], writes=[b_pen])
        P.op("pool", lambda e: e.tensor_copy(out=idb[:], in_=idf[:]), reads=[b_id], writes=[b_id])
        wob = P.sb("wo_b", [64, NH, D], BF16)
        wof = P.sb("wo_f", [64, NH, 128], F32)
        b_wob, b_wof = Buf("wo_b"), Buf("wo_f")
        for m in range(KD):
            P.dma("sp", lambda e, m=m: e.dma_start(out=wof[:], in_=wo_v[:, :, m * 128:(m + 1) * 128]), b_wof, writes=[b_wof])
            P.op("pool", lambda e, m=m: e.tensor_copy(out=wob[:, :, m * 128:(m + 1) * 128], in_=wof[:]), reads=[b_wof], writes=[b_wob])
        score = P.sb("score", [128, SEQ], F32)
        b_score = Buf("score")
        maskT = P.sb("maskT", [128, SEQ // 128, 128], BF16)
        b_maskT = Buf("maskT")
        qh = P.sb("qh", [64, NH, 128], BF16)
        qih = P.sb("qih", [64, NIH, 128], BF16)
        b_qh, b_qih = Buf("qh"), Buf("qih")
        kib = [P.sb(f"kib{i}", [64, 512], BF16) for i in range(2)]
        b_kib = [Buf(f"kib{i}") for i in range(2)]
        tmp = [P.sb(f"itmp{i}", [128, 512], F32) for i in range(2)]
        b_tmp = [Buf(f"itmp{i}") for i in range(2)]
        m8 = P.sb("m8", [128, 8], F32)
        b_m8 = Buf("m8")
        mk = [P.sb(f"mk{i}", [128, 512], BF16) for i in range(2)]
        b_mk = [Buf(f"mk{i}") for i in range(2)]
        kTc = [P.sb(f"kTc{i}", [64, 8, 256], BF16) for i in range(2)]
        vc = [P.sb(f"vc{i}", [128, 2, 512], BF16) for i in range(2)]
        b_kTc = [Buf(f"kTc{i}") for i in range(2)]
        b_vc = [Buf(f"vc{i}") for i in range(2)]
        pT = [P.sb(f"pT{i}", [128, 512], BF16) for i in range(3)]
        b_pT = [Buf(f"pT{i}") for i in range(3)]
        attn = P.sb("attn", [64, NH, 128], BF16)
        b_attn = Buf("attn")
        dsb = [P.sb(f"dsb{i}", [64, 512], F32) for i in range(2)]
        b_dsb = [Buf(f"dsb{i}") for i in range(2)]
        xq = P.sb("xq", [128, KD, 128], F32)
        b_xq = Buf("xq")
        psA = [P.ps(f"psA{i}", [128, 512], F32) for i in range(3)]
        b_psA = [Buf(f"psA{i}") for i in range(3)]
        psT = P.ps("psT", [128, 1024], BF16)
        b_psT = Buf("psT")
        acc = [P.ps(f"acc{i}", [128, 512], F32) for i in range(2)]
        b_acc = [Buf(f"acc{i}") for i in range(2)]
        den = [P.ps(f"den{i}", [128, 512], F32) for i in range(2)]
        b_den = [Buf(f"den{i}") for i in range(2)]
        rr = {"a": 0, "kib": 0, "tmp": 0, "mk": 0, "kv": 0, "pT": 0}

        def nxt(key, n):
            v = rr[key]
            rr[key] = (v + 1) % n
            return v

        toks = []
        for i in range(nblk):
            qs = slice(i * 128, (i + 1) * 128)
            Lk = 1024 * (i + 1)
            nkc = Lk // 128
            n512 = Lk // 512
            P.dma("sp", lambda e, qs=qs: e.dma_start(out=qh[:], in_=qT_v[:, :, qs]), b_qh, writes=[b_qh])
            P.dma("sp", lambda e, qs=qs: e.dma_start(out=qih[:], in_=qiT_v[:, :, qs]), b_qih, writes=[b_qih])
            for j in range(n512):
                cs_ = slice(j * 512, (j + 1) * 512)
                kb = nxt("kib", 2)
                P.dma("sp", lambda e, kb=kb, cs_=cs_: e.dma_start(out=kib[kb][:], in_=kiT_d[:, cs_]), b_kib[kb], writes=[b_kib[kb]])
                for h in range(NIH):
                    a = nxt("a", 3)
                    P.op("pe", lambda e, a=a, h=h, kb=kb: e.matmul(psA[a][:], lhsT=qih[:, h, :], rhs=kib[kb][:], start=True, stop=True),
                         reads=[b_qih, b_kib[kb]], writes=[b_psA[a]])
                    if h == 0:
                        P.op("dve", lambda e, a=a, cs_=cs_, i=i: e.tensor_scalar(
                            out=score[:, cs_], in0=psA[a][:], scalar1=0.0, scalar2=wq[:, i, 0:1], op0=ALU.max, op1=ALU.mult),
                            reads=[b_psA[a], b_wq], writes=[b_score])
                    else:
                        t = nxt("tmp", 2)
                        P.op("dve", lambda e, a=a, t=t, i=i, h=h: e.tensor_scalar(
                            out=tmp[t][:], in0=psA[a][:], scalar1=0.0, scalar2=wq[:, i, h:h + 1], op0=ALU.max, op1=ALU.mult),
                            reads=[b_psA[a], b_wq], writes=[b_tmp[t]])
                        P.op("pool", lambda e, t=t, cs_=cs_: e.tensor_tensor(out=score[:, cs_], in0=score[:, cs_], in1=tmp[t][:], op=ALU.add),
                             reads=[b_tmp[t], b_score], writes=[b_score])
            P.op("pool", lambda e, Lk=Lk: e.tensor_tensor(out=score[:, Lk - 1024:Lk], in0=score[:, Lk - 1024:Lk], in1=pen[:], op=ALU.add),
                 reads=[b_score, b_pen], writes=[b_score])
            for r in range(TOPK // 8):
                P.op("dve", lambda e, Lk=Lk: e.max(out=m8[:], in_=score[:, 0:Lk]), reads=[b_score], writes=[b_m8])
                P.op("dve", lambda e, Lk=Lk: e.match_replace(out=score[:, 0:Lk], in_to_replace=m8[:], in_values=score[:, 0:Lk],
                                                            imm_value=NEG_SEL),
                     reads=[b_score, b_m8], writes=[b_score])
            for j in range(n512):
                cs_ = slice(j * 512, (j + 1) * 512)
                u = nxt("mk", 2)
                P.op("pool", lambda e, u=u, cs_=cs_: e.tensor_single_scalar(out=mk[u][:], in_=score[:, cs_], scalar=-5.0e29, op=ALU.is_le),
                     reads=[b_score], writes=[b_mk[u]])
                if j >= n512 - 2:
                    po = (j - (n512 - 2)) * 512
                    P.op("pool", lambda e, u=u, po=po: e.tensor_tensor(out=mk[u][:], in0=mk[u][:], in1=penz[:, po:po + 512], op=ALU.mult),
                         reads=[b_mk[u], b_pen], writes=[b_mk[u]])
                for jj in range(4):
                    P.op("pe", lambda e, u=u, jj=jj: e.transpose(out=psT[:, jj * 128:(jj + 1) * 128], in_=mk[u][:, jj * 128:(jj + 1) * 128],
                                                                 identity=idb[:]),
                         reads=[b_mk[u], b_id], writes=[b_psT])
                P.op("act", lambda e, j=j: e.activation(out=maskT[:, 4 * j:4 * j + 4, :].rearrange("p a b -> p (a b)"), in_=psT[:, 0:512],
                                                        func=AF.Copy),
                     reads=[b_psT], writes=[b_maskT])
            for half in range(2):
                for kc in range(nkc):
                    kk = kc % 2
                    if kk == 0:
                        s = nxt("kv", 2)
                        r0 = kc * 128
                        P.dma("sp", lambda e, s=s, r0=r0, half=half: e.dma_start(out=kTc[s][:], in_=kT_v[:, half * 8:(half + 1) * 8, r0:r0 + 256]),
                              b_kTc[s], writes=[b_kTc[s]])
                        P.dma("sp", lambda e, s=s, r0=r0, half=half: e.dma_start(
                            out=vc[s][:], in_=v_d[r0:r0 + 256, half * 512:(half + 1) * 512].rearrange("(c p) n -> p c n", p=128)),
                            b_vc[s], writes=[b_vc[s]])
                    for hg in range(2):
                        a = nxt("a", 3)
                        for hh in range(4):
                            h8 = hg * 4 + hh
                            head = half * 8 + h8
                            P.op("pe", lambda e, a=a, hh=hh, h8=h8, head=head, s=s, kk=kk: e.matmul(
                                psA[a][:, hh * 128:(hh + 1) * 128], lhsT=kTc[s][:, h8, kk * 128:(kk + 1) * 128], rhs=qh[:, head, :],
                                start=True, stop=True),
                                reads=[b_kTc[s], b_qh], writes=[b_psA[a]])
                        u = nxt("pT", 3)
                        P.op("act", lambda e, a=a, u=u: e.activation(out=pT[u][:], in_=psA[a][:], func=AF.Exp),
                             reads=[b_psA[a]], writes=[b_pT[u]])
                        P.op("dve", lambda e, u=u, kc=kc: e.tensor_tensor(
                            out=pT[u][:].rearrange("p (h q) -> p h q", h=4), in0=pT[u][:].rearrange("p (h q) -> p h q", h=4),
                            in1=maskT[:, kc, :].unsqueeze(1).to_broadcast([128, 4, 128]), op=ALU.mult),
                            reads=[b_pT[u], b_maskT], writes=[b_pT[u]])
                        for hh in range(4):
                            h8 = hg * 4 + hh
                            P.op("pe", lambda e, hg=hg, hh=hh, h8=h8, s=s, kk=kk, u=u, kc=kc, nkc=nkc: e.matmul(
                                acc[hg][0:64, hh * 128:(hh + 1) * 128], lhsT=vc[s][:, kk, h8 * 64:(h8 + 1) * 64],
                                rhs=pT[u][:, hh * 128:(hh + 1) * 128], start=(kc == 0 and hh == 0), stop=(kc == nkc - 1 and hh == 3),
                                skip_group_check=True),
                                reads=[b_vc[s], b_pT[u]], writes=[b_acc[hg]])
                        P.op("pe", lambda e, hg=hg, u=u, kc=kc, nkc=nkc: e.matmul(
                            den[hg][0:64, :], lhsT=ones[:, 0:64], rhs=pT[u][:], start=(kc == 0), stop=(kc == nkc - 1)),
                            reads=[b_c, b_pT[u]], writes=[b_den[hg]])
                for hg in range(2):
                    h0 = half * 8 + hg * 4
                    P.op("act", lambda e, hg=hg: e.activation(out=dsb[hg][:], in_=den[hg][0:64, :], func=AF.Copy),
                         reads=[b_den[hg]], writes=[b_dsb[hg]])
                    P.op("dve", lambda e, hg=hg: e.reciprocal(out=dsb[hg][:], in_=dsb[hg][:]), reads=[b_dsb[hg]], writes=[b_dsb[hg]])
                    P.op("dve", lambda e, hg=hg, h0=h0: e.tensor_tensor(
                        out=attn[:, h0:h0 + 4, :].rearrange("p a b -> p (a b)"), in0=acc[hg][0:64, :], in1=dsb[hg][:], op=ALU.mult),
                        reads=[b_acc[hg], b_dsb[hg]], writes=[b_attn])
            P.dma("sp", lambda e, qs=qs: e.dma_start(out=xq[:], in_=x_v[:, :, qs]), b_xq, writes=[b_xq])
            for m in range(KD):
                a = nxt("a", 3)
                for h in range(NH):
                    P.op("pe", lambda e, a=a, h=h, m=m: e.matmul(psA[a][:, 0:128], lhsT=wob[:, h, m * 128:(m + 1) * 128], rhs=attn[:, h, :],
                                                                 start=(h == 0), stop=(h == NH - 1)),
                         reads=[b_wob, b_attn], writes=[b_psA[a]])
                P.op("dve", lambda e, a=a, m=m: e.tensor_tensor(out=xq[:, m, :], in0=psA[a][:, 0:128], in1=xq[:, m, :], op=ALU.add),
                     reads=[b_psA[a], b_xq], writes=[b_xq])
            toks.append(P.dma("sp", lambda e, qs=qs: e.dma_start(out=y_v[:, :, qs], in_=xq[:]), b_xq, reads=[b_xq]))
        P.finish(toks[-1:])
        P.emit()
    return nc


def causal_pen(core):
    j = np.arange(1024)[None, :]
    p = np.arange(128)[:, None]
    return np.where(j > 128 * core + p, np.float32(NEG_MASK), np.float32(0.0)).astype(np.float32)


def gather_tokens_T(parts):
    f = parts[0].shape[0]
    out = np.empty((f, SEQ // 128, 128), parts[0].dtype)
    for c, p in enumerate(parts):
        out[:, c::NCORES, :] = p.reshape(f, NT // 128, 128)
    return out.reshape(f, SEQ)


def gather_tokens(parts):
    f = parts[0].shape[1]
    out = np.empty((SEQ // 128, 128, f), parts[0].dtype)
    for c, p in enumerate(parts):
        out[c::NCORES] = p.reshape(NT // 128, 128, f)
    return out.reshape(SEQ, f)


def run_dsa_layer(xT_parts, j, i, inp):
    gm = col_layout(inp["norm_mix"][i])
    qkg = np.stack([np.tile(inp["dsa_q_norm"][j], 2), np.tile(inp["dsa_k_norm"][j], 2)], axis=1).astype(np.float32)
    cm = const_mats()
    maps = [{"xT": xT_parts[c], "w_in": inp["dsa_w_in"][j], "gain": gm, "qk_gain": qkg, "cossin": rope_consts(c), "cmat": cm}
            for c in range(NCORES)]
    pr = _run("dsa_proj", build_dsa_proj_prog, maps)
    kTf = gather_tokens_T([r["kT"] for r in pr])
    kiTf = gather_tokens_T([r["kiT"] for r in pr])
    vf = gather_tokens([r["v"] for r in pr])
    ident = np.eye(128, dtype=np.float32)
    maps = []
    for c in range(NCORES):
        wq = np.ascontiguousarray(pr[c]["w"].reshape(NT // 128, 128, NIH).transpose(1, 0, 2))
        maps.append({"xT": xT_parts[c], "qT": pr[c]["qT"], "qiT": pr[c]["qiT"], "wq": wq, "kTf": kTf, "vf": vf, "kiTf": kiTf,
                     "pen": causal_pen(c), "ident": ident, "w_o": inp["dsa_w_o"][j]})
    ar = _run("dsa_attn", build_dsa_attn_prog, maps)
    gf = col_layout(inp["norm_ffn"][i])
    maps = [{"xT": ar[c]["yT"], "wgu": inp["ffn_w_gate_up"][i], "wd": inp["ffn_w_down"][i], "gain": gf} for c in range(NCORES)]
    fr = _run("ffn", build_ffn_prog, maps)
    return [r["yT"] for r in fr]


def kernel(**inputs):
    inp = {k: np.asarray(v) for k, v in inputs.items()}
    x = np.ascontiguousarray(inp["x"][0], dtype=np.float32)
    parts = [to_core_T(x, c) for c in range(NCORES)]
    for i in range(4):
        if i % 2 == 0:
            parts = run_s5_layer(parts, i // 2, i, inp)
        else:
            parts = run_dsa_layer(parts, i // 2, i, inp)
    return from_core_T(parts)[None].astype(np.float32)
```

```python
import math
from contextlib import ExitStack

import numpy as np
import concourse.bass as bass
import concourse.mybir as mybir
from concourse.bass_utils import run_bass_kernel_spmd

F32 = mybir.dt.float32
BF16 = mybir.dt.bfloat16
ALU = mybir.AluOpType
AF = mybir.ActivationFunctionType
AX = mybir.AxisListType

NCORES = 8
D = 1024
KD = D // 128
SEQ = 16384
NT = SEQ // NCORES
TT = 512
NTT = NT // TT
DFF = 2816
KF = DFF // 128
EPS = 1e-6

ENGS = ("pe", "act", "dve", "pool", "sp")
SAME_ENGINE_SYNC = ("act", "dve", "pool")


class Buf:
    __slots__ = ("name", "w", "r", "dsem", "dcount")

    def __init__(self, name):
        self.name = name
        self.w = None
        self.r = {}
        self.dsem = None
        self.dcount = 0


class Prog:
    def __init__(self, nc, es):
        self.nc = nc
        self.es = es
        self.ops = {e: [] for e in ENGS}
        self.dma_bufs = []
        self.final_tokens = []
        self.bar = []
        self.bar_epoch = 0
        self.eng_epoch = {e: 0 for e in ENGS}

    def sb(self, name, shape, dt, es=None):
        return (es or self.es).enter_context(self.nc.sbuf_tensor(name, list(shape), dt))

    def ps(self, name, shape, dt=F32, es=None):
        return (es or self.es).enter_context(self.nc.psum_tensor(name, list(shape), dt))

    def _deps(self, reads, writes):
        need = []
        for b in reads:
            if b.w is not None:
                need.append(b.w)
        for b in writes:
            if b.w is not None:
                need.append(b.w)
            need.extend(b.r.values())
        return need

    def barrier(self):
        toks = []
        for e in ENGS:
            for idx in range(len(self.ops[e]) - 1, -1, -1):
                if self.ops[e][idx]["dma"] is None:
                    toks.append(("e", e, idx))
                    break
        for b in self.dma_bufs:
            toks.append(("d", b, b.dcount))
        self.bar = toks
        self.bar_epoch += 1

    def _bar_need(self, eng):
        if self.eng_epoch[eng] < self.bar_epoch:
            self.eng_epoch[eng] = self.bar_epoch
            return list(self.bar)
        return []

    def op(self, eng, fn, reads=(), writes=()):
        need = self._deps(reads, writes) + self._bar_need(eng)
        idx = len(self.ops[eng])
        tok = ("e", eng, idx)
        self.ops[eng].append({"need": need, "fn": fn, "dma": None})
        for b in reads:
            b.r[eng] = tok
        for b in writes:
            b.w = tok
            b.r = {}
        return tok

    def dma(self, eng, fn, sembuf, reads=(), writes=()):
        need = self._deps(reads, writes) + self._bar_need(eng)
        if sembuf.dsem is None:
            sembuf.dsem = self.es.enter_context(self.nc.semaphore("d_" + sembuf.name))
            self.dma_bufs.append(sembuf)
        sembuf.dcount += 16
        tok = ("d", sembuf, sembuf.dcount)
        self.ops[eng].append({"need": need, "fn": fn, "dma": sembuf})
        for b in reads:
            b.r[("d", id(sembuf))] = tok
        for b in writes:
            b.w = tok
            b.r = {}
        return tok

    def finish(self, tokens):
        self.final_tokens.extend(tokens)

    def emit(self):
        nc = self.nc
        needed = {e: set() for e in ENGS}
        for e in ENGS:
            for i, o in enumerate(self.ops[e]):
                for t in o["need"]:
                    if t[0] == "e":
                        if t[1] == e and e not in SAME_ENGINE_SYNC:
                            continue
                        needed[t[1]].add(t[2])
        for t in self.final_tokens:
            if t[0] == "e":
                needed[t[1]].add(t[2])
        rank = {}
        for e in ENGS:
            rank[e] = {i: n + 1 for n, i in enumerate(sorted(needed[e]))}
        sems = {e: self.es.enter_context(nc.semaphore("s_" + e)) for e in ENGS}
        final_tokens = self.final_tokens

        def run(e, engine):
            known = {}
            def wait(tok):
                if tok[0] == "e":
                    if tok[1] == e and e not in SAME_ENGINE_SYNC:
                        return
                    key, sem, val = tok[1], sems[tok[1]], rank[tok[1]][tok[2]]
                else:
                    key, sem, val = id(tok[1]), tok[1].dsem, tok[2]
                if known.get(key, 0) >= val:
                    return
                known[key] = val
                engine.wait_ge(sem, val)
            for i, o in enumerate(self.ops[e]):
                for t in o["need"]:
                    wait(t)
                ins = o["fn"](engine)
                if o["dma"] is not None:
                    ins.then_inc(o["dma"].dsem, 16)
                elif i in rank[e]:
                    ins.then_inc(sems[e], 1)
            if e == "sp":
                for t in final_tokens:
                    wait(t)

        with nc.Block() as block:
            @block.tensor
            def _(eng):
                run("pe", eng)

            @block.scalar
            def _(eng):
                run("act", eng)

            @block.vector
            def _(eng):
                run("dve", eng)

            @block.gpsimd
            def _(eng):
                run("pool", eng)

            @block.sync
            def _(eng):
                run("sp", eng)


class Ctx:
    def __init__(self, P):
        self.P = P
        nc = P.nc
        self.ones = P.sb("c_ones", [128, 128], BF16)
        self.b_ones = Buf("ones")
        P.op("pool", lambda g: g.memset(self.ones[:], 1.0), writes=[self.b_ones])
        self.eps_col = P.sb("c_eps", [128, 1], F32)
        P.op("pool", lambda g: g.memset(self.eps_col[:], EPS), writes=[self.b_ones])
        self.psum = [P.ps(f"ps{i}", [128, 512], F32) for i in range(8)]
        self.b_ps = [Buf(f"ps{i}") for i in range(8)]
        self.ps_rr = 0

    def next_ps(self):
        i = self.ps_rr
        self.ps_rr = (self.ps_rr + 1) % 8
        return self.psum[i], self.b_ps[i]


def emit_rmsnorm_T(P, C, xT, b_x, gain, b_gain, gcol0, hT, b_h, tts, es, tag):
    sq = [P.sb(f"{tag}_sq{i}", [128, TT], BF16, es) for i in range(2)]
    b_sq = [Buf(f"{tag}_sq{i}") for i in range(2)]
    rstd = [P.sb(f"{tag}_rstd{i}", [128, TT], F32, es) for i in range(2)]
    b_rstd = [Buf(f"{tag}_rstd{i}") for i in range(2)]
    n = 0
    for j, tt in enumerate(tts):
        ts = slice(tt * TT, (tt + 1) * TT)
        ps, b_ps = C.next_ps()
        for k in range(KD):
            s, bs = sq[n % 2], b_sq[n % 2]
            n += 1
            P.op("act", lambda e, s=s, k=k, ts=ts: e.activation(out=s[:], in_=xT[:, k, ts], func=AF.Square),
                 reads=[b_x[k][tt]], writes=[bs])
            P.op("pe", lambda e, ps=ps, s=s, k=k: e.matmul(ps[:], lhsT=C.ones[:], rhs=s[:],
                                                             start=(k == 0), stop=(k == KD - 1)),
                 reads=[bs, C.b_ones], writes=[b_ps])
        r, br = rstd[j % 2], b_rstd[j % 2]
        P.op("act", lambda e, r=r, ps=ps: e.activation(out=r[:], in_=ps[:], func=AF.Sqrt, scale=1.0 / D, bias=C.eps_col[:]),
             reads=[b_ps, C.b_ones], writes=[br])
        P.op("dve", lambda e, r=r: e.reciprocal(out=r[:], in_=r[:]),
             reads=[br], writes=[br])
        js = slice(j * TT, (j + 1) * TT)
        for k in range(KD):
            P.op("dve", lambda e, k=k, ts=ts, js=js, r=r: e.scalar_tensor_tensor(
                out=hT[:, k, js], in0=xT[:, k, ts], scalar=gain[:, gcol0 + k:gcol0 + k + 1], in1=r[:],
                op0=ALU.mult, op1=ALU.mult),
                reads=[b_x[k][tt], br, b_gain], writes=[b_h[j]])


def emit_ffn(P, C, xT, b_x, wgu, wd, gain, b_gain, gcol0, tag="ffn"):
    nc = P.nc
    with ExitStack() as es:
        HT = 2 * TT
        hT = P.sb(f"{tag}_hT", [128, KD, HT], BF16, es)
        aT = P.sb(f"{tag}_aT", [128, KF, HT], BF16, es)
        wg_f = [P.sb(f"{tag}_wgf{i}", [128, KD, 256], F32, es) for i in range(2)]
        wg_b = [P.sb(f"{tag}_wgb{i}", [128, KD, 256], BF16, es) for i in range(2)]
        wd_f = [P.sb(f"{tag}_wdf{i}", [128, KF, 128], F32, es) for i in range(2)]
        wd_b = [P.sb(f"{tag}_wdb{i}", [128, KF, 128], BF16, es) for i in range(2)]
        sg = [P.sb(f"{tag}_sg{i}", [128, TT], F32, es) for i in range(2)]
        b_hT = [Buf(f"{tag}_hT{j}") for j in range(2)]
        b_aT = [[Buf(f"{tag}_aT{n}_{j}") for j in range(2)] for n in range(KF)]
        b_wgf = [Buf(f"{tag}_wgf{i}") for i in range(2)]
        b_wgb = [Buf(f"{tag}_wgb{i}") for i in range(2)]
        b_wdf = [Buf(f"{tag}_wdf{i}") for i in range(2)]
        b_wdb = [Buf(f"{tag}_wdb{i}") for i in range(2)]
        b_sg = [Buf(f"{tag}_sg{i}") for i in range(2)]
        wgu_v = wgu.rearrange("(k p) n -> p k n", p=128)
        wd_v = wd.rearrange("(k p) n -> p k n", p=128)
        nsg = 0
        nw = 0
        nwd = 0
        for half in range(NT // HT):
            tts = [half * 2, half * 2 + 1]
            emit_rmsnorm_T(P, C, xT, b_x, gain, b_gain, gcol0, hT, b_hT, tts, es, f"{tag}n{half}")
            for n in range(KF):
                s = nw % 2
                nw += 1
                P.dma("sp", lambda e, s=s, n=n: e.dma_start(out=wg_f[s][:, :, 0:128],
                                                            in_=wgu_v[:, :, n * 128:(n + 1) * 128]),
                      b_wgf[s], writes=[b_wgf[s]])
                P.dma("sp", lambda e, s=s, n=n: e.dma_start(out=wg_f[s][:, :, 128:256],
                                                            in_=wgu_v[:, :, DFF + n * 128:DFF + (n + 1) * 128]),
                      b_wgf[s], writes=[])
                b_wgf[s].w = ("d", b_wgf[s], b_wgf[s].dcount)
                P.op("pool", lambda e, s=s: e.tensor_copy(out=wg_b[s][:], in_=wg_f[s][:]),
                     reads=[b_wgf[s]], writes=[b_wgb[s]])
                for j in range(2):
                    js = slice(j * TT, (j + 1) * TT)
                    pg, b_pg = C.next_ps()
                    pu, b_pu = C.next_ps()
                    for k in range(KD):
                        P.op("pe", lambda e, pg=pg, s=s, k=k, js=js: e.matmul(
                            pg[:], lhsT=wg_b[s][:, k, 0:128], rhs=hT[:, k, js], start=(k == 0), stop=(k == KD - 1)),
                            reads=[b_wgb[s], b_hT[j]], writes=[b_pg])
                    for k in range(KD):
                        P.op("pe", lambda e, pu=pu, s=s, k=k, js=js: e.matmul(
                            pu[:], lhsT=wg_b[s][:, k, 128:256], rhs=hT[:, k, js], start=(k == 0), stop=(k == KD - 1)),
                            reads=[b_wgb[s], b_hT[j]], writes=[b_pu])
                    q = nsg % 2
                    nsg += 1
                    P.op("act", lambda e, q=q, pg=pg: e.activation(out=sg[q][:], in_=pg[:], func=AF.Silu),
                         reads=[b_pg], writes=[b_sg[q]])
                    P.op("dve", lambda e, q=q, pu=pu, n=n, js=js: e.tensor_tensor(
                        out=aT[:, n, js], in0=pu[:], in1=sg[q][:], op=ALU.mult),
                        reads=[b_pu, b_sg[q]], writes=[b_aT[n][j]])
            for m in range(KD):
                s = nwd % 2
                nwd += 1
                P.dma("sp", lambda e, s=s, m=m: e.dma_start(out=wd_f[s][:], in_=wd_v[:, :, m * 128:(m + 1) * 128]),
                      b_wdf[s], writes=[b_wdf[s]])
                P.op("pool", lambda e, s=s: e.tensor_copy(out=wd_b[s][:], in_=wd_f[s][:]),
                     reads=[b_wdf[s]], writes=[b_wdb[s]])
                for j in range(2):
                    tt = tts[j]
                    js = slice(j * TT, (j + 1) * TT)
                    ts = slice(tt * TT, (tt + 1) * TT)
                    po, b_po = C.next_ps()
                    for n in range(KF):
                        P.op("pe", lambda e, po=po, s=s, n=n, js=js: e.matmul(
                            po[:], lhsT=wd_b[s][:, n, :], rhs=aT[:, n, js], start=(n == 0), stop=(n == KF - 1)),
                            reads=[b_wdb[s], b_aT[n][j]], writes=[b_po])
                    P.op("dve", lambda e, po=po, m=m, ts=ts: e.tensor_tensor(
                        out=xT[:, m, ts], in0=po[:], in1=xT[:, m, ts], op=ALU.add),
                        reads=[b_po, b_x[m][tt]], writes=[b_x[m][tt]])
    P.barrier()


def load_xT(P, xT, b_x, x_dram, eng="sp"):
    xv = x_dram.rearrange("(k p) t -> p k t", p=128)
    for k in range(KD):
        for tt in range(NTT):
            ts = slice(tt * TT, (tt + 1) * TT)
            P.dma(eng, lambda e, k=k, ts=ts: e.dma_start(out=xT[:, k, ts], in_=xv[:, k, ts]),
                  b_x[k][tt], writes=[b_x[k][tt]])


def store_xT(P, xT, b_x, y_dram, eng="sp"):
    yv = y_dram.rearrange("(k p) t -> p k t", p=128)
    toks = []
    for k in range(KD):
        for tt in range(NTT):
            ts = slice(tt * TT, (tt + 1) * TT)
            toks.append(P.dma(eng, lambda e, k=k, ts=ts: e.dma_start(out=yv[:, k, ts], in_=xT[:, k, ts]),
                              b_x[k][tt], reads=[b_x[k][tt]]))
    P.finish(toks)


def build_ffn_prog():
    nc = bass.Bass("TRN2", target_bir_lowering=False)
    x = nc.dram_tensor("xT", [D, NT], F32, kind="ExternalInput").ap()
    wgu = nc.dram_tensor("wgu", [D, 2 * DFF], F32, kind="ExternalInput").ap()
    wd = nc.dram_tensor("wd", [DFF, D], F32, kind="ExternalInput").ap()
    gain = nc.dram_tensor("gain", [128, KD], F32, kind="ExternalInput").ap()
    y = nc.dram_tensor("yT", [D, NT], F32, kind="ExternalOutput").ap()
    with ExitStack() as es:
        P = Prog(nc, es)
        C = Ctx(P)
        xT = P.sb("xT_sb", [128, KD, NT], F32)
        b_x = [[Buf(f"x{k}_{t}") for t in range(NTT)] for k in range(KD)]
        g_sb = P.sb("gain_sb", [128, KD], F32)
        b_g = Buf("gain")
        P.dma("sp", lambda e: e.dma_start(out=g_sb[:], in_=gain[:]), b_g, writes=[b_g])
        load_xT(P, xT, b_x, x)
        emit_ffn(P, C, xT, b_x, wgu, wd, g_sb, b_g, 0)
        store_xT(P, xT, b_x, y)
        P.emit()
    return nc


def col_layout(v):
    return np.ascontiguousarray(np.asarray(v, np.float32).reshape(-1, 128).T)


def to_core_T(x2d, c):
    blocks = x2d.reshape(SEQ // 128, 128, -1)[c::NCORES]
    return np.ascontiguousarray(blocks.reshape(NT, -1).T)


def from_core_T(parts):
    dd = parts[0].shape[0]
    out = np.empty((SEQ // 128, 128, dd), np.float32)
    for c, p in enumerate(parts):
        out[c::NCORES] = p.T.reshape(NT // 128, 128, dd)
    return out.reshape(SEQ, dd)


def build_prenorm_prog():
    nc = bass.Bass("TRN2", target_bir_lowering=False)
    x = nc.dram_tensor("xT", [D, NT], F32, kind="ExternalInput").ap()
    gain = nc.dram_tensor("gain", [128, KD], F32, kind="ExternalInput").ap()
    y = nc.dram_tensor("hT", [D, NT], F32, kind="ExternalOutput").ap()
    with ExitStack() as es:
        P = Prog(nc, es)
        C = Ctx(P)
        xT = P.sb("xT_sb", [128, KD, NT], F32)
        b_x = [[Buf(f"x{k}_{t}") for t in range(NTT)] for k in range(KD)]
        g_sb = P.sb("gain_sb", [128, KD], F32)
        b_g = Buf("gain")
        P.dma("sp", lambda e: e.dma_start(out=g_sb[:], in_=gain[:]), b_g, writes=[b_g])
        load_xT(P, xT, b_x, x)
        hT = P.sb("hT_sb", [128, KD, NT], F32)
        b_h = [Buf(f"h{t}") for t in range(NTT)]
        emit_rmsnorm_T(P, C, xT, b_x, g_sb, b_g, 0, hT, b_h, list(range(NTT)), es, "pn")
        yv = y.rearrange("(k p) t -> p k t", p=128)
        toks = []
        for tt in range(NTT):
            ts = slice(tt * TT, (tt + 1) * TT)
            toks.append(P.dma("sp", lambda e, ts=ts: e.dma_start(out=yv[:, :, ts], in_=hT[:, :, ts]),
                              b_h[tt], reads=[b_h[tt]]))
        P.finish(toks)
        P.emit()
    return nc


S5T = 512
TWO_PI = 2.0 * math.pi

PI_LO = 3.1415925
CW1 = 6.28125
CW2 = TWO_PI - 6.28125


def emit_range_reduce(P, dst, src, ti, tf, reads, writes):
    rw = list(reads) + list(writes)
    P.op("dve", lambda e: e.tensor_scalar(out=tf, in0=src, scalar1=1.0 / TWO_PI, scalar2=None, op0=ALU.mult),
         reads=rw, writes=writes)
    P.op("dve", lambda e: e.tensor_copy(out=ti, in_=tf), reads=rw, writes=writes)
    P.op("dve", lambda e: e.tensor_copy(out=tf, in_=ti), reads=rw, writes=writes)
    P.op("dve", lambda e: e.scalar_tensor_tensor(out=dst, in0=tf, scalar=-CW1, in1=src, op0=ALU.mult, op1=ALU.add),
         reads=rw, writes=writes)
    P.op("dve", lambda e: e.scalar_tensor_tensor(out=dst, in0=tf, scalar=-CW2, in1=dst, op0=ALU.mult, op1=ALU.add),
         reads=rw, writes=writes)
    P.op("dve", lambda e: e.tensor_scalar(out=tf, in0=dst, scalar1=math.pi, scalar2=-TWO_PI, op0=ALU.is_gt, op1=ALU.mult),
         reads=rw, writes=writes)
    P.op("dve", lambda e: e.tensor_tensor(out=dst, in0=dst, in1=tf, op=ALU.add), reads=rw, writes=writes)
    P.op("dve", lambda e: e.tensor_scalar(out=tf, in0=dst, scalar1=-math.pi, scalar2=TWO_PI, op0=ALU.is_lt, op1=ALU.mult),
         reads=rw, writes=writes)
    P.op("dve", lambda e: e.tensor_tensor(out=dst, in0=dst, in1=tf, op=ALU.add), reads=rw, writes=writes)
    P.op("dve", lambda e: e.tensor_scalar(out=dst, in0=dst, scalar1=PI_LO, scalar2=-PI_LO, op0=ALU.min, op1=ALU.max),
         reads=rw, writes=writes)


def build_s5_prog(debug=0):
    nc = bass.Bass("TRN2", target_bir_lowering=False)
    u_d = nc.dram_tensor("uT", [128, SEQ], F32, kind="ExternalInput").ap()
    par_d = nc.dram_tensor("par", [128, 16], F32, kind="ExternalInput").ap()
    bt_d = nc.dram_tensor("bt", [128, 2, 512], F32, kind="ExternalInput").ap()
    ct_d = nc.dram_tensor("ct", [128, 2, 4, 128], F32, kind="ExternalInput").ap()
    tau_d = nc.dram_tensor("tau", [128, S5T], F32, kind="ExternalInput").ap()
    y_d = nc.dram_tensor("gyT", [128, SEQ], F32, kind="ExternalOutput").ap()
    NCH = SEQ // S5T
    with ExitStack() as es:
        P = Prog(nc, es)
        C = Ctx(P)
        par = P.sb("par_sb", [128, 16], F32)
        bt = P.sb("bt_sb", [128, 2, 512], F32)
        ct = P.sb("ct_sb", [128, 2, 4, 128], F32)
        tau = P.sb("tau_sb", [128, S5T], F32)
        b_par, b_bt, b_ct, b_tau = Buf("par"), Buf("bt"), Buf("ct"), Buf("tau")
        P.dma("sp", lambda e: e.dma_start(out=par[:], in_=par_d[:]), b_par, writes=[b_par])
        P.dma("sp", lambda e: e.dma_start(out=bt[:], in_=bt_d[:]), b_bt, writes=[b_bt])
        P.dma("sp", lambda e: e.dma_start(out=ct[:], in_=ct_d[:]), b_ct, writes=[b_ct])
        P.dma("sp", lambda e: e.dma_start(out=tau[:], in_=tau_d[:]), b_tau, writes=[b_tau])
        sm = P.sb("s5_small", [128, 24, 4], F32)
        b_sm = Buf("s5_small")
        halfpi = P.sb("halfpi", [128, 1], F32)
        P.op("pool", lambda e: e.memset(halfpi[:], math.pi / 2), writes=[b_sm])
        lam_re, lam_im, logdt, dcol = par[:, 0:4], par[:, 4:8], par[:, 8:12], par[:, 12:13]
        (DT, LR, TH, R, THR, ABS, ARE, AIM, NR, NUM_RE, NUM_IM, DEN, KRE, KIM, T1, T2, PHT, CT_, ST_, NST_) = range(20)
        col = lambda i: sm[:, i, :]

        def V(fn, reads=(b_par, b_sm)):
            P.op("dve", fn, reads=list(reads), writes=[b_sm])

        def A(fn):
            P.op("act", fn, reads=[b_par, b_sm], writes=[b_sm])

        def sincos(src, s_out, c_out, shape_ap_abs):
            A(lambda e: e.activation(out=s_out, in_=src, func=AF.Sin))
            A(lambda e: e.activation(out=shape_ap_abs, in_=src, func=AF.Abs))
            A(lambda e: e.activation(out=c_out, in_=shape_ap_abs, func=AF.Sin, scale=-1.0, bias=halfpi[:]))

        def reduce_phase(out, src_fn_desc):
            pass

        A(lambda e: e.activation(out=col(DT), in_=logdt, func=AF.Exp))
        V(lambda e: e.tensor_tensor(out=col(LR), in0=lam_re, in1=col(DT), op=ALU.mult))
        V(lambda e: e.tensor_tensor(out=col(TH), in0=lam_im, in1=col(DT), op=ALU.mult))
        A(lambda e: e.activation(out=col(R), in_=col(LR), func=AF.Exp))
        smi = P.sb("s5_smi", [128, 4], mybir.dt.int32)
        emit_range_reduce(P, col(THR), col(TH), smi[:], col(T1), [b_par], [b_sm])
        sincos(col(THR), col(AIM), col(ARE), col(ABS))
        V(lambda e: e.tensor_tensor(out=col(ARE), in0=col(ARE), in1=col(R), op=ALU.mult))
        V(lambda e: e.tensor_tensor(out=col(AIM), in0=col(AIM), in1=col(R), op=ALU.mult))
        V(lambda e: e.tensor_scalar(out=col(NR), in0=col(ARE), scalar1=-1.0, scalar2=None, op0=ALU.add))
        V(lambda e: e.tensor_tensor(out=col(T1), in0=col(NR), in1=lam_re, op=ALU.mult))
        V(lambda e: e.tensor_tensor(out=col(T2), in0=col(AIM), in1=lam_im, op=ALU.mult))
        V(lambda e: e.tensor_tensor(out=col(NUM_RE), in0=col(T1), in1=col(T2), op=ALU.add))
        V(lambda e: e.tensor_tensor(out=col(T1), in0=col(AIM), in1=lam_re, op=ALU.mult))
        V(lambda e: e.tensor_tensor(out=col(T2), in0=col(NR), in1=lam_im, op=ALU.mult))
        V(lambda e: e.tensor_tensor(out=col(NUM_IM), in0=col(T1), in1=col(T2), op=ALU.subtract))
        V(lambda e: e.tensor_tensor(out=col(T1), in0=lam_re, in1=lam_re, op=ALU.mult))
        V(lambda e: e.tensor_tensor(out=col(T2), in0=lam_im, in1=lam_im, op=ALU.mult))
        V(lambda e: e.tensor_tensor(out=col(DEN), in0=col(T1), in1=col(T2), op=ALU.add))
        V(lambda e: e.reciprocal(out=col(DEN), in_=col(DEN)))
        V(lambda e: e.tensor_tensor(out=col(KRE), in0=col(NUM_RE), in1=col(DEN), op=ALU.mult))
        V(lambda e: e.tensor_tensor(out=col(KIM), in0=col(NUM_IM), in1=col(DEN), op=ALU.mult))
        V(lambda e: e.tensor_scalar(out=col(T2), in0=col(THR), scalar1=float(S5T), scalar2=None, op0=ALU.mult))
        emit_range_reduce(P, col(PHT), col(T2), smi[:], col(T1), [b_par], [b_sm])
        sincos(col(PHT), col(ST_), col(CT_), col(ABS))
        V(lambda e: e.tensor_scalar(out=col(NST_), in0=col(ST_), scalar1=-1.0, scalar2=None, op0=ALU.mult))
        L = P.sb("s5_L", [128, 3, 4, 128], BF16)
        b_L = Buf("s5_L")
        ctmp = P.sb("s5_ctmp", [128, 2, 128], F32)
        b_ctmp = Buf("s5_ctmp")
        for k in range(4):
            kre, kim = sm[:, KRE, k:k + 1], sm[:, KIM, k:k + 1]
            P.op("dve", lambda e, k=k, kim=kim: e.tensor_scalar(out=ctmp[:, 0, :], in0=ct[:, 1, k, :], scalar1=kim,
                                                                 scalar2=-1.0, op0=ALU.mult, op1=ALU.mult),
                 reads=[b_ct, b_sm], writes=[b_ctmp])
            P.op("dve", lambda e, k=k, kre=kre: e.scalar_tensor_tensor(out=ctmp[:, 0, :], in0=ct[:, 0, k, :], scalar=kre,
                                                                        in1=ctmp[:, 0, :], op0=ALU.mult, op1=ALU.add),
                 reads=[b_ct, b_sm, b_ctmp], writes=[b_ctmp])
            P.op("dve", lambda e, k=k, kre=kre: e.tensor_scalar(out=ctmp[:, 1, :], in0=ct[:, 1, k, :], scalar1=kre,
                                                                 scalar2=None, op0=ALU.mult),
                 reads=[b_ct, b_sm], writes=[b_ctmp])
            P.op("dve", lambda e, k=k, kim=kim: e.scalar_tensor_tensor(out=ctmp[:, 1, :], in0=ct[:, 0, k, :], scalar=kim,
                                                                        in1=ctmp[:, 1, :], op0=ALU.mult, op1=ALU.add),
                 reads=[b_ct, b_sm, b_ctmp], writes=[b_ctmp])
            P.op("dve", lambda e, k=k: e.tensor_copy(out=L[:, 0, k, :], in_=ctmp[:, 0, :]), reads=[b_ctmp], writes=[b_L])
            P.op("dve", lambda e, k=k: e.tensor_scalar(out=L[:, 1, k, :], in0=ctmp[:, 0, :], scalar1=-1.0, scalar2=None,
                                                       op0=ALU.mult), reads=[b_ctmp], writes=[b_L])
            P.op("dve", lambda e, k=k: e.tensor_scalar(out=L[:, 2, k, :], in0=ctmp[:, 1, :], scalar1=-1.0, scalar2=None,
                                                       op0=ALU.mult), reads=[b_ctmp], writes=[b_L])
        btb = P.sb("s5_btb", [128, 2, 512], BF16)
        b_btb = Buf("s5_btb")
        P.op("dve", lambda e: e.tensor_copy(out=btb[:], in_=bt[:]), reads=[b_bt], writes=[b_btb])
        cosT = P.sb("s5_cos", [128, 4, S5T], F32)
        sinT = P.sb("s5_sin", [128, 4, S5T], F32)
        rT = P.sb("s5_rT", [128, 4, S5T], F32)
        b_tab = Buf("s5_tab")
        ph = P.sb("s5_ph", [128, S5T], F32)
        pha = P.sb("s5_pha", [128, S5T], F32)
        phx = P.sb("s5_phx", [128, S5T], F32)
        phi = P.sb("s5_phi", [128, S5T], mybir.dt.int32)
        b_ph = Buf("s5_ph")
        for k in range(4):
            P.op("dve", lambda e, k=k: e.tensor_scalar(out=phx[:], in0=tau[:], scalar1=sm[:, THR, k:k + 1],
                                                       scalar2=None, op0=ALU.mult),
                 reads=[b_tau, b_sm, b_tab], writes=[b_ph])
            emit_range_reduce(P, ph[:], phx[:], phi[:], pha[:], [b_sm], [b_ph])
            P.op("act", lambda e, k=k: e.activation(out=sinT[:, k, :], in_=ph[:], func=AF.Sin),
                 reads=[b_ph], writes=[b_tab])
            P.op("act", lambda e: e.activation(out=pha[:], in_=ph[:], func=AF.Abs),
                 reads=[b_ph], writes=[b_ph])
            P.op("act", lambda e, k=k: e.activation(out=cosT[:, k, :], in_=pha[:], func=AF.Sin, scale=-1.0, bias=halfpi[:]),
                 reads=[b_ph, b_sm], writes=[b_tab])
            P.op("dve", lambda e, k=k: e.tensor_scalar(out=rT[:, k, :], in0=tau[:], scalar1=0.0, scalar2=sm[:, R, k:k + 1],
                                                       op0=ALU.mult, op1=ALU.add),
                 reads=[b_tau, b_sm], writes=[b_tab])
        if debug == 2:
            tk = [P.dma("sp", lambda e: e.dma_start(out=y_d[:, 0:96], in_=sm[:].rearrange("p a b -> p (a b)")), b_sm, reads=[b_sm]),
                  P.dma("sp", lambda e: e.dma_start(out=y_d[:, 1024:3072], in_=cosT[:].rearrange("p a b -> p (a b)")), b_tab, reads=[b_tab]),
                  P.dma("sp", lambda e: e.dma_start(out=y_d[:, 3072:5120], in_=sinT[:].rearrange("p a b -> p (a b)")), b_tab, reads=[b_tab]),
                  P.dma("sp", lambda e: e.dma_start(out=y_d[:, 5120:7168], in_=rT[:].rearrange("p a b -> p (a b)")), b_tab, reads=[b_tab])]
            P.finish(tk)
            P.emit()
            return nc
        NB = 2
        uf = [P.sb(f"s5_uf{i}", [128, S5T], F32) for i in range(NB)]
        ub = [P.sb(f"s5_ub{i}", [128, S5T], BF16) for i in range(NB)]
        b_uf = [Buf(f"s5_uf{i}") for i in range(NB)]
        b_ub = [Buf(f"s5_ub{i}") for i in range(NB)]
        sA = [P.sb(f"s5_sA{i}", [128, S5T], F32) for i in range(2)]
        sB = [P.sb(f"s5_sB{i}", [128, S5T], F32) for i in range(2)]
        b_sA = [Buf(f"s5_sA{i}") for i in range(2)]
        b_sB = [Buf(f"s5_sB{i}") for i in range(2)]
        t_ = [[P.sb(f"s5_t{j}_{i}", [128, S5T], F32) for i in range(2)] for j in range(4)]
        b_t = [[Buf(f"s5_t{j}_{i}") for i in range(2)] for j in range(4)]
        bre = [P.sb(f"s5_bre{i}", [128, S5T], F32) for i in range(2)]
        bim = [P.sb(f"s5_bim{i}", [128, S5T], F32) for i in range(2)]
        b_bre = [Buf(f"s5_bre{i}") for i in range(2)]
        b_bim = [Buf(f"s5_bim{i}") for i in range(2)]
        zre = [P.sb(f"s5_zre{i}", [128, S5T], F32) for i in range(2)]
        zim = [P.sb(f"s5_zim{i}", [128, S5T], F32) for i in range(2)]
        b_zre = [Buf(f"s5_zre{i}") for i in range(2)]
        b_zim = [Buf(f"s5_zim{i}") for i in range(2)]
        pp = [[P.sb(f"s5_p{j}_{i}", [128, S5T], BF16) for i in range(2)] for j in range(4)]
        b_pp = [[Buf(f"s5_p{j}_{i}") for i in range(2)] for j in range(4)]
        init = P.sb("s5_init", [128, 2, 4], F32)
        itmp = P.sb("s5_itmp", [128, 2, 4], F32)
        b_init = [Buf(f"s5_init{k}") for k in range(4)]
        P.op("dve", lambda e: e.memset(init[:], 0.0), writes=b_init)
        ysb = [P.sb(f"s5_y{i}", [128, S5T], F32) for i in range(2)]
        g1 = [P.sb(f"s5_g1{i}", [128, S5T], F32) for i in range(2)]
        g2 = [P.sb(f"s5_g2{i}", [128, S5T], F32) for i in range(2)]
        b_y = [Buf(f"s5_y{i}") for i in range(2)]
        b_g1 = [Buf(f"s5_g1{i}") for i in range(2)]
        b_g2 = [Buf(f"s5_g2{i}") for i in range(2)]
        toks = []
        it = 0
        for c in range(NCH):
            cs = slice(c * S5T, (c + 1) * S5T)
            ui = c % NB
            P.dma("sp", lambda e, ui=ui, cs=cs: e.dma_start(out=uf[ui][:], in_=u_d[:, cs]), b_uf[ui], writes=[b_uf[ui]])
            P.op("act", lambda e, ui=ui: e.activation(out=ub[ui][:], in_=uf[ui][:], func=AF.Copy),
                 reads=[b_uf[ui]], writes=[b_ub[ui]])
            yps, b_yps = C.psum[6 + c % 2], C.b_ps[6 + c % 2]
            for k in range(4):
                s = it % 2
                pa, b_pa = C.psum[(2 * it) % 6], C.b_ps[(2 * it) % 6]
                pb, b_pb = C.psum[(2 * it + 1) % 6], C.b_ps[(2 * it + 1) % 6]
                it += 1
                ks = slice(k * 128, (k + 1) * 128)
                P.op("pe", lambda e, pa=pa, ks=ks, ui=ui: e.matmul(pa[:], lhsT=btb[:, 0, ks], rhs=ub[ui][:], start=True, stop=True),
                     reads=[b_btb, b_ub[ui]], writes=[b_pa])
                P.op("pe", lambda e, pb=pb, ks=ks, ui=ui: e.matmul(pb[:], lhsT=btb[:, 1, ks], rhs=ub[ui][:], start=True, stop=True),
                     reads=[b_btb, b_ub[ui]], writes=[b_pb])
                P.op("act", lambda e, s=s, pa=pa: e.activation(out=sA[s][:], in_=pa[:], func=AF.Copy), reads=[b_pa], writes=[b_sA[s]])
                P.op("act", lambda e, s=s, pb=pb: e.activation(out=sB[s][:], in_=pb[:], func=AF.Copy), reads=[b_pb], writes=[b_sB[s]])
                P.op("pool", lambda e, s=s, k=k: e.tensor_tensor(out=t_[0][s][:], in0=sA[s][:], in1=cosT[:, k, :], op=ALU.mult),
                     reads=[b_sA[s], b_tab], writes=[b_t[0][s]])
                P.op("dve", lambda e, s=s, k=k: e.tensor_tensor(out=t_[1][s][:], in0=sB[s][:], in1=sinT[:, k, :], op=ALU.mult),
                     reads=[b_sB[s], b_tab], writes=[b_t[1][s]])
                P.op("pool", lambda e, s=s, k=k: e.tensor_tensor(out=t_[2][s][:], in0=sB[s][:], in1=cosT[:, k, :], op=ALU.mult),
                     reads=[b_sB[s], b_tab], writes=[b_t[2][s]])
                P.op("dve", lambda e, s=s, k=k: e.tensor_tensor(out=t_[3][s][:], in0=sA[s][:], in1=sinT[:, k, :], op=ALU.mult),
                     reads=[b_sA[s], b_tab], writes=[b_t[3][s]])
                P.op("pool", lambda e, s=s: e.tensor_tensor(out=bre[s][:], in0=t_[0][s][:], in1=t_[1][s][:], op=ALU.add),
                     reads=[b_t[0][s], b_t[1][s]], writes=[b_bre[s]])
                P.op("pool", lambda e, s=s: e.tensor_tensor(out=bim[s][:], in0=t_[2][s][:], in1=t_[3][s][:], op=ALU.subtract),
                     reads=[b_t[2][s], b_t[3][s]], writes=[b_bim[s]])
                P.op("dve", lambda e, s=s, k=k: e.tensor_tensor_scan(out=zre[s][:], data0=rT[:, k, :], data1=bre[s][:],
                                                                      initial=init[:, 0, k:k + 1], op0=ALU.mult, op1=ALU.add),
                     reads=[b_tab, b_bre[s], b_init[k]], writes=[b_zre[s]])
                P.op("dve", lambda e, s=s, k=k: e.tensor_tensor_scan(out=zim[s][:], data0=rT[:, k, :], data1=bim[s][:],
                                                                      initial=init[:, 1, k:k + 1], op0=ALU.mult, op1=ALU.add),
                     reads=[b_tab, b_bim[s], b_init[k]], writes=[b_zim[s]])
                zlr, zli = zre[s][:, S5T - 1:S5T], zim[s][:, S5T - 1:S5T]
                cT_, sT_, nsT_ = sm[:, CT_, k:k + 1], sm[:, ST_, k:k + 1], sm[:, NST_, k:k + 1]
                P.op("dve", lambda e, k=k, zlr=zlr, cT_=cT_: e.tensor_tensor(out=itmp[:, 0, k:k + 1], in0=zlr, in1=cT_, op=ALU.mult),
                     reads=[b_zre[s], b_sm], writes=[b_init[k]])
                P.op("dve", lambda e, k=k, zlr=zlr, sT_=sT_: e.tensor_tensor(out=itmp[:, 1, k:k + 1], in0=zlr, in1=sT_, op=ALU.mult),
                     reads=[b_zre[s], b_sm], writes=[b_init[k]])
                P.op("dve", lambda e, k=k, zli=zli, nsT_=nsT_: e.scalar_tensor_tensor(
                    out=init[:, 0, k:k + 1], in0=zli, scalar=nsT_, in1=itmp[:, 0, k:k + 1], op0=ALU.mult, op1=ALU.add),
                    reads=[b_zim[s], b_sm], writes=[b_init[k]])
                P.op("dve", lambda e, k=k, zli=zli, cT_=cT_: e.scalar_tensor_tensor(
                    out=init[:, 1, k:k + 1], in0=zli, scalar=cT_, in1=itmp[:, 1, k:k + 1], op0=ALU.mult, op1=ALU.add),
                    reads=[b_zim[s], b_sm], writes=[b_init[k]])
                P.op("pool", lambda e, s=s, k=k: e.tensor_tensor(out=pp[0][s][:], in0=zre[s][:], in1=cosT[:, k, :], op=ALU.mult),
                     reads=[b_zre[s], b_tab], writes=[b_pp[0][s]])
                P.op("pool", lambda e, s=s, k=k: e.tensor_tensor(out=pp[1][s][:], in0=zim[s][:], in1=sinT[:, k, :], op=ALU.mult),
                     reads=[b_zim[s], b_tab], writes=[b_pp[1][s]])
                P.op("dve", lambda e, s=s, k=k: e.tensor_tensor(out=pp[2][s][:], in0=zre[s][:], in1=sinT[:, k, :], op=ALU.mult),
                     reads=[b_zre[s], b_tab], writes=[b_pp[2][s]])
                P.op("pool", lambda e, s=s, k=k: e.tensor_tensor(out=pp[3][s][:], in0=zim[s][:], in1=cosT[:, k, :], op=ALU.mult),
                     reads=[b_zim[s], b_tab], writes=[b_pp[3][s]])
                for j, li in enumerate((0, 1, 2, 2)):
                    P.op("pe", lambda e, yps=yps, li=li, k=k, j=j, s=s: e.matmul(
                        yps[:], lhsT=L[:, li, k, :], rhs=pp[j][s][:], start=(k == 0 and j == 0), stop=(k == 3 and j == 3)),
                        reads=[b_L, b_pp[j][s]], writes=[b_yps])
            q = c % 2
            P.op("dve", lambda e, q=q, ui=ui, yps=yps: e.scalar_tensor_tensor(
                out=ysb[q][:], in0=uf[ui][:], scalar=dcol, in1=yps[:], op0=ALU.mult, op1=ALU.add),
                reads=[b_uf[ui], b_par, b_yps], writes=[b_y[q]])
            P.op("pool", lambda e, q=q: e.tensor_tensor(out=g1[q][:], in0=ysb[q][:], in1=ysb[q][:], op=ALU.mult),
                 reads=[b_y[q]], writes=[b_g1[q]])
            P.op("pool", lambda e, q=q: e.tensor_scalar(out=g1[q][:], in0=g1[q][:], scalar1=0.044715, scalar2=1.0,
                                                        op0=ALU.mult, op1=ALU.add),
                 reads=[b_g1[q]], writes=[b_g1[q]])
            P.op("pool", lambda e, q=q: e.tensor_tensor(out=g1[q][:], in0=g1[q][:], in1=ysb[q][:], op=ALU.mult),
                 reads=[b_g1[q], b_y[q]], writes=[b_g1[q]])
            P.op("act", lambda e, q=q: e.activation(out=g2[q][:], in_=g1[q][:], func=AF.Sigmoid, scale=1.5957691216057308),
                 reads=[b_g1[q]], writes=[b_g2[q]])
            P.op("pool", lambda e, q=q: e.tensor_tensor(out=g2[q][:], in0=g2[q][:], in1=ysb[q][:], op=ALU.mult),
                 reads=[b_g2[q], b_y[q]], writes=[b_g2[q]])
            if debug == 1:
                toks.append(P.dma("sp", lambda e, q=q, cs=cs: e.dma_start(out=y_d[:, cs], in_=ysb[q][:]), b_g2[q], reads=[b_g2[q], b_y[q]]))
                continue
            toks.append(P.dma("sp", lambda e, q=q, cs=cs: e.dma_start(out=y_d[:, cs], in_=g2[q][:]), b_g2[q], reads=[b_g2[q]]))
        P.finish(toks[-2:])
        P.emit()
    return nc


def s5_host_layout(lam_re, lam_im, log_dt, b_re, b_im, c_re, c_im, d, core):
    g0 = core * 8
    par = np.zeros((128, 16), np.float32)
    bt = np.zeros((128, 2, 512), np.float32)
    ct = np.zeros((128, 2, 4, 128), np.float32)
    for gl in range(8):
        g = g0 + gl
        k, p0 = gl // 2, (gl % 2) * 64
        par[p0:p0 + 64, 0 + k] = lam_re[g]
        par[p0:p0 + 64, 4 + k] = lam_im[g]
        par[p0:p0 + 64, 8 + k] = log_dt[g]
        bt[16 * gl:16 * gl + 16, 0, 64 * gl:64 * gl + 64] = b_re[g].T
        bt[16 * gl:16 * gl + 16, 1, 64 * gl:64 * gl + 64] = b_im[g].T
        ct[p0:p0 + 64, 0, k, 16 * gl:16 * gl + 16] = c_re[g].T
        ct[p0:p0 + 64, 1, k, 16 * gl:16 * gl + 16] = c_im[g].T
    par[:, 12] = d[core * 128:(core + 1) * 128]
    return par, bt, ct


def emit_glu(P, C, xT, b_x, gy_dram, wglu):
    with ExitStack() as es:
        gyb = P.sb("glu_gyb", [128, KD, NT], BF16, es)
        b_gyb = [Buf(f"glu_gyb{t}") for t in range(NTT)]
        st = [P.sb(f"glu_st{i}", [128, TT], F32, es) for i in range(2)]
        b_st = [Buf(f"glu_st{i}") for i in range(2)]
        gv = gy_dram.rearrange("(k p) t -> p k t", p=128)
        n = 0
        for tt in range(NTT):
            ts = slice(tt * TT, (tt + 1) * TT)
            for k in range(KD):
                s = n % 2
                n += 1
                P.dma("sp", lambda e, s=s, k=k, ts=ts: e.dma_start(out=st[s][:], in_=gv[:, k, ts]), b_st[s], writes=[b_st[s]])
                P.op("pool", lambda e, s=s, k=k, ts=ts: e.tensor_copy(out=gyb[:, k, ts], in_=st[s][:]),
                     reads=[b_st[s]], writes=[b_gyb[tt]])
        w_f = [P.sb(f"glu_wf{i}", [128, KD, 256], F32, es) for i in range(2)]
        w_b = [P.sb(f"glu_wb{i}", [128, KD, 256], BF16, es) for i in range(2)]
        b_wf = [Buf(f"glu_wf{i}") for i in range(2)]
        b_wb = [Buf(f"glu_wb{i}") for i in range(2)]
        sg = [P.sb(f"glu_sg{i}", [128, TT], F32, es) for i in range(2)]
        b_sg = [Buf(f"glu_sg{i}") for i in range(2)]
        wv = wglu.rearrange("(k p) n -> p k n", p=128)
        q = 0
        for m in range(KD):
            s = m % 2
            P.dma("sp", lambda e, s=s, m=m: e.dma_start(out=w_f[s][:, :, 0:128], in_=wv[:, :, m * 128:(m + 1) * 128]),
                  b_wf[s], writes=[b_wf[s]])
            P.dma("sp", lambda e, s=s, m=m: e.dma_start(out=w_f[s][:, :, 128:256], in_=wv[:, :, D + m * 128:D + (m + 1) * 128]),
                  b_wf[s], writes=[])
            b_wf[s].w = ("d", b_wf[s], b_wf[s].dcount)
            P.op("pool", lambda e, s=s: e.tensor_copy(out=w_b[s][:], in_=w_f[s][:]), reads=[b_wf[s]], writes=[b_wb[s]])
            for tt in range(NTT):
                ts = slice(tt * TT, (tt + 1) * TT)
                pv, b_pv = C.next_ps()
                pg, b_pg = C.next_ps()
                for k in range(KD):
                    P.op("pe", lambda e, pv=pv, s=s, k=k, ts=ts: e.matmul(pv[:], lhsT=w_b[s][:, k, 0:128], rhs=gyb[:, k, ts],
                                                                         start=(k == 0), stop=(k == KD - 1)),
                         reads=[b_wb[s], b_gyb[tt]], writes=[b_pv])
                for k in range(KD):
                    P.op("pe", lambda e, pg=pg, s=s, k=k, ts=ts: e.matmul(pg[:], lhsT=w_b[s][:, k, 128:256], rhs=gyb[:, k, ts],
                                                                         start=(k == 0), stop=(k == KD - 1)),
                         reads=[b_wb[s], b_gyb[tt]], writes=[b_pg])
                qq = q % 2
                q += 1
                P.op("act", lambda e, qq=qq, pg=pg: e.activation(out=sg[qq][:], in_=pg[:], func=AF.Sigmoid),
                     reads=[b_pg], writes=[b_sg[qq]])
                P.op("dve", lambda e, qq=qq, pv=pv: e.tensor_tensor(out=sg[qq][:], in0=pv[:], in1=sg[qq][:], op=ALU.mult),
                     reads=[b_pv, b_sg[qq]], writes=[b_sg[qq]])
                P.op("pool", lambda e, qq=qq, m=m, ts=ts: e.tensor_tensor(out=xT[:, m, ts], in0=xT[:, m, ts], in1=sg[qq][:], op=ALU.add),
                     reads=[b_sg[qq], b_x[m][tt]], writes=[b_x[m][tt]])
    P.barrier()


def build_glu_ffn_prog():
    nc = bass.Bass("TRN2", target_bir_lowering=False)
    x = nc.dram_tensor("xT", [D, NT], F32, kind="ExternalInput").ap()
    gy = nc.dram_tensor("gyT", [D, NT], F32, kind="ExternalInput").ap()
    wglu = nc.dram_tensor("wglu", [D, 2 * D], F32, kind="ExternalInput").ap()
    wgu = nc.dram_tensor("wgu", [D, 2 * DFF], F32, kind="ExternalInput").ap()
    wd = nc.dram_tensor("wd", [DFF, D], F32, kind="ExternalInput").ap()
    gain = nc.dram_tensor("gain", [128, KD], F32, kind="ExternalInput").ap()
    y = nc.dram_tensor("yT", [D, NT], F32, kind="ExternalOutput").ap()
    with ExitStack() as es:
        P = Prog(nc, es)
        C = Ctx(P)
        xT = P.sb("xT_sb", [128, KD, NT], F32)
        b_x = [[Buf(f"x{k}_{t}") for t in range(NTT)] for k in range(KD)]
        g_sb = P.sb("gain_sb", [128, KD], F32)
        b_g = Buf("gain")
        P.dma("sp", lambda e: e.dma_start(out=g_sb[:], in_=gain[:]), b_g, writes=[b_g])
        load_xT(P, xT, b_x, x)
        emit_glu(P, C, xT, b_x, gy, wglu)
        emit_ffn(P, C, xT, b_x, wgu, wd, g_sb, b_g, 0)
        store_xT(P, xT, b_x, y)
        P.emit()
    return nc


_PROGS = {}


def _prog(name, builder):
    if name not in _PROGS:
        _PROGS[name] = builder()
    return _PROGS[name]


def _run(name, builder, in_maps):
    import time
    t0 = time.time()
    nc = _prog(name, builder)
    t1 = time.time()
    res = run_bass_kernel_spmd(nc, in_maps, core_ids=list(range(NCORES))).results
    nb = sum(v.nbytes for m in in_maps for v in m.values())
    print(f"[launch {name}] build {t1 - t0:.1f}s run {time.time() - t1:.1f}s in_bytes {nb / 1e6:.0f}MB", flush=True)
    return res


def run_s5_layer(xT_parts, j, i, inp):
    gm = col_layout(inp["norm_mix"][i])
    res = _run("prenorm", build_prenorm_prog, [{"xT": xT_parts[c], "gain": gm} for c in range(NCORES)])
    h_full = from_core_T([r["hT"] for r in res])
    tau = np.tile(np.arange(S5T, dtype=np.float32)[None], (128, 1))
    maps = []
    for c in range(NCORES):
        par, bt, ct = s5_host_layout(inp["s5_lambda_re"][j], inp["s5_lambda_im"][j], inp["s5_log_dt"][j],
                                     inp["s5_b_re"][j], inp["s5_b_im"][j], inp["s5_c_re"][j], inp["s5_c_im"][j],
                                     inp["s5_d"][j], c)
        maps.append({"uT": np.ascontiguousarray(h_full[:, c * 128:(c + 1) * 128].T), "par": par, "bt": bt, "ct": ct, "tau": tau})
    res = _run("s5", build_s5_prog, maps)
    gy_full = np.concatenate([r["gyT"] for r in res], axis=0).T
    gf = col_layout(inp["norm_ffn"][i])
    maps = [{"xT": xT_parts[c], "gyT": to_core_T(gy_full, c), "wglu": inp["s5_w_glu"][j],
             "wgu": inp["ffn_w_gate_up"][i], "wd": inp["ffn_w_down"][i], "gain": gf} for c in range(NCORES)]
    res = _run("glu_ffn", build_glu_ffn_prog, maps)
    return [r["yT"] for r in res]


NH = 16
HD = 64
NIH = 8
PROJ = 3 * D + NIH * HD + HD + NIH


def build_dsa_proj_prog():
    nc = bass.Bass("TRN2", target_bir_lowering=False)
    x = nc.dram_tensor("xT", [D, NT], F32, kind="ExternalInput").ap()
    w_in = nc.dram_tensor("w_in", [D, PROJ], F32, kind="ExternalInput").ap()
    gain = nc.dram_tensor("gain", [128, KD], F32, kind="ExternalInput").ap()
    qk_g = nc.dram_tensor("qk_gain", [128, 2], F32, kind="ExternalInput").ap()
    cs_d = nc.dram_tensor("cossin", [128, 2, NT], F32, kind="ExternalInput").ap()
    cm_d = nc.dram_tensor("cmat", [128, 2, 128], F32, kind="ExternalInput").ap()
    qT_d = nc.dram_tensor("qT", [D, NT], BF16, kind="ExternalOutput").ap()
    kT_d = nc.dram_tensor("kT", [D, NT], BF16, kind="ExternalOutput").ap()
    v_d = nc.dram_tensor("v", [NT, D], BF16, kind="ExternalOutput").ap()
    qiT_d = nc.dram_tensor("qiT", [NIH * HD, NT], BF16, kind="ExternalOutput").ap()
    kiT_d = nc.dram_tensor("kiT", [HD, NT], BF16, kind="ExternalOutput").ap()
    w_d = nc.dram_tensor("w", [NT, NIH], F32, kind="ExternalOutput").ap()
    with ExitStack() as es:
        P = Prog(nc, es)
        C = Ctx(P)
        xT = P.sb("xT_sb", [128, KD, NT], F32)
        b_x = [[Buf(f"x{k}_{t}") for t in range(NTT)] for k in range(KD)]
        g_sb = P.sb("gain_sb", [128, KD], F32)
        qkg = P.sb("qkg_sb", [128, 2], F32)
        cs = P.sb("cs_sb", [128, 2, NT], F32)
        cm = P.sb("cm_sb", [128, 2, 128], F32)
        b_g, b_qkg, b_cs, b_cm = Buf("gain"), Buf("qkg"), Buf("cs"), Buf("cm")
        P.dma("sp", lambda e: e.dma_start(out=g_sb[:], in_=gain[:]), b_g, writes=[b_g])
        P.dma("sp", lambda e: e.dma_start(out=qkg[:], in_=qk_g[:]), b_qkg, writes=[b_qkg])
        P.dma("sp", lambda e: e.dma_start(out=cm[:], in_=cm_d[:]), b_cm, writes=[b_cm])
        P.dma("sp", lambda e: e.dma_start(out=cs[:, 0, :], in_=cs_d[:, 0, :]), b_cs, writes=[b_cs])
        P.dma("sp", lambda e: e.dma_start(out=cs[:, 1, :], in_=cs_d[:, 1, :]), b_cs, writes=[])
        b_cs.w = ("d", b_cs, b_cs.dcount)
        load_xT(P, xT, b_x, x)
        bones = P.sb("bones_bf", [128, 128], BF16)
        b_bones = Buf("bones")
        P.op("dve", lambda e: e.tensor_copy(out=bones[:], in_=cm[:, 0, :]), reads=[b_cm], writes=[b_bones])
        P.op("dve", lambda e: e.tensor_scalar(out=qkg[:, 0:1], in0=qkg[:, 0:1], scalar1=HD ** -0.5, scalar2=None, op0=ALU.mult),
             reads=[b_qkg], writes=[b_qkg])
        hT = P.sb("hT_sb", [128, KD, NT], BF16)
        b_h = [Buf(f"h{t}") for t in range(NTT)]
        emit_rmsnorm_T(P, C, xT, b_x, g_sb, b_g, 0, hT, b_h, list(range(NTT)), es, "pn")
        wv_ = w_in.rearrange("(k p) n -> p k n", p=128)
        w_f = [P.sb(f"pj_wf{i}", [128, KD, 128], F32) for i in range(2)]
        w_b = [P.sb(f"pj_wb{i}", [128, KD, 128], BF16) for i in range(2)]
        b_wf = [Buf(f"pj_wf{i}") for i in range(2)]
        b_wb = [Buf(f"pj_wb{i}") for i in range(2)]
        sq = [P.sb(f"pj_sq{i}", [128, TT], BF16) for i in range(2)]
        rs = [P.sb(f"pj_rs{i}", [128, TT], F32) for i in range(2)]
        tf = [P.sb(f"pj_t{i}", [128, TT], F32) for i in range(2)]
        o1 = [P.sb(f"pj_o1{i}", [128, TT], F32) for i in range(2)]
        o2 = [P.sb(f"pj_o2{i}", [128, TT], F32) for i in range(2)]
        ob = [P.sb(f"pj_ob{i}", [128, TT], BF16) for i in range(2)]
        b_sq = [Buf(f"pj_sq{i}") for i in range(2)]
        b_rs = [Buf(f"pj_rs{i}") for i in range(2)]
        b_tf = [Buf(f"pj_t{i}") for i in range(2)]
        b_o1 = [Buf(f"pj_o1{i}") for i in range(2)]
        b_o2 = [Buf(f"pj_o2{i}") for i in range(2)]
        b_ob = [Buf(f"pj_ob{i}") for i in range(2)]
        toks = []
        tiles = []
        for m in range(8):
            tiles.append((m * 128, 128, "norm", qT_d, m * 128, 0))
        for m in range(8):
            tiles.append((D + m * 128, 128, "norm", kT_d, m * 128, 1))
        for m in range(4):
            tiles.append((3 * D + m * 128, 128, "plain", qiT_d, m * 128, None))
        tiles.append((3 * D + NIH * HD, 64, "normnog", kiT_d, 0, None))
        it = 0
        for ti, (c0, M, kind, od, r0, gc) in enumerate(tiles):
            s = ti % 2
            P.dma("sp", lambda e, s=s, c0=c0, M=M: e.dma_start(out=w_f[s][:, :, 0:M], in_=wv_[:, :, c0:c0 + M]),
                  b_wf[s], writes=[b_wf[s]])
            P.op("pool", lambda e, s=s, M=M: e.tensor_copy(out=w_b[s][:, :, 0:M], in_=w_f[s][:, :, 0:M]),
                 reads=[b_wf[s]], writes=[b_wb[s]])
            for tt in range(NTT):
                ts = slice(tt * TT, (tt + 1) * TT)
                u = it % 2
                it += 1
                ps, b_ps = C.next_ps()
                for k in range(KD):
                    P.op("pe", lambda e, ps=ps, s=s, k=k, ts=ts, M=M: e.matmul(ps[0:M, :], lhsT=w_b[s][:, k, 0:M], rhs=hT[:, k, ts],
                                                                              start=(k == 0), stop=(k == KD - 1)),
                         reads=[b_wb[s], b_h[tt]], writes=[b_ps])
                if kind == "plain":
                    P.op("act", lambda e, u=u, ps=ps, M=M: e.activation(out=tf[u][0:M, :], in_=ps[0:M, :], func=AF.Copy),
                         reads=[b_ps], writes=[b_tf[u]])
                else:
                    P.op("act", lambda e, u=u, ps=ps, M=M: e.activation(out=sq[u][0:M, :], in_=ps[0:M, :], func=AF.Square),
                         reads=[b_ps], writes=[b_sq[u]])
                    p2, b_p2 = C.next_ps()
                    P.op("pe", lambda e, p2=p2, u=u, M=M: e.matmul(p2[0:M, :], lhsT=bones[0:M, 0:M], rhs=sq[u][0:M, :], start=True, stop=True),
                         reads=[b_bones, b_sq[u]], writes=[b_p2])
                    P.op("act", lambda e, u=u, p2=p2, M=M: e.activation(out=rs[u][0:M, :], in_=p2[0:M, :], func=AF.Sqrt, scale=1.0 / HD,
                                                                   bias=C.eps_col[0:M, :]),
                         reads=[b_p2, C.b_ones], writes=[b_rs[u]])
                    P.op("dve", lambda e, u=u, M=M: e.reciprocal(out=rs[u][0:M, :], in_=rs[u][0:M, :]), reads=[b_rs[u]], writes=[b_rs[u]])
                    if kind == "norm":
                        P.op("dve", lambda e, u=u, ps=ps, gc=gc, M=M: e.scalar_tensor_tensor(
                            out=tf[u][0:M, :], in0=ps[0:M, :], scalar=qkg[0:M, gc:gc + 1], in1=rs[u][0:M, :], op0=ALU.mult, op1=ALU.mult),
                            reads=[b_ps, b_qkg, b_rs[u]], writes=[b_tf[u]])
                    else:
                        P.op("dve", lambda e, u=u, ps=ps, M=M: e.tensor_tensor(out=tf[u][0:M, :], in0=ps[0:M, :], in1=rs[u][0:M, :], op=ALU.mult),
                             reads=[b_ps, b_rs[u]], writes=[b_tf[u]])
                p3, b_p3 = C.next_ps()
                P.op("pe", lambda e, p3=p3, u=u, M=M: e.matmul(p3[0:M, :], lhsT=cm[0:M, 1, 0:M], rhs=tf[u][0:M, :], start=True, stop=True),
                     reads=[b_cm, b_tf[u]], writes=[b_p3])
                P.op("pool", lambda e, u=u, ts=ts, M=M: e.tensor_tensor(out=o1[u][0:M, :], in0=tf[u][0:M, :], in1=cs[0:M, 0, ts], op=ALU.mult),
                     reads=[b_tf[u], b_cs], writes=[b_o1[u]])
                P.op("dve", lambda e, u=u, ts=ts, p3=p3, M=M: e.tensor_tensor(out=o2[u][0:M, :], in0=p3[0:M, :], in1=cs[0:M, 1, ts], op=ALU.mult),
                     reads=[b_p3, b_cs], writes=[b_o2[u]])
                P.op("pool", lambda e, u=u, M=M: e.tensor_tensor(out=ob[u][0:M, :], in0=o1[u][0:M, :], in1=o2[u][0:M, :], op=ALU.add),
                     reads=[b_o1[u], b_o2[u]], writes=[b_ob[u]])
                toks.append(P.dma("sp", lambda e, u=u, od=od, r0=r0, ts=ts, M=M: e.dma_start(out=od[r0:r0 + M, ts], in_=ob[u][0:M, :]),
                                  b_ob[u], reads=[b_ob[u]]))
        wvf = [P.sb(f"pj_vf{i}", [128, KD, 512], F32) for i in range(1)]
        wvb = [P.sb(f"pj_vb{i}", [128, KD, 512], BF16) for i in range(2)]
        b_wvf = [Buf("pj_vf0")]
        b_wvb = [Buf(f"pj_vb{i}") for i in range(2)]
        vo = [P.sb(f"pj_vo{i}", [128, 512], BF16) for i in range(2)]
        b_vo = [Buf(f"pj_vo{i}") for i in range(2)]
        for hf in range(2):
            P.dma("sp", lambda e, hf=hf: e.dma_start(out=wvf[0][:], in_=wv_[:, :, 2 * D + hf * 512:2 * D + (hf + 1) * 512]),
                  b_wvf[0], writes=[b_wvf[0]])
            P.op("pool", lambda e, hf=hf: e.tensor_copy(out=wvb[hf][:], in_=wvf[0][:]), reads=[b_wvf[0]], writes=[b_wvb[hf]])
        ww_f = P.sb("pj_wwf", [128, KD, NIH], F32)
        ww_b = P.sb("pj_wwb", [128, KD, NIH], BF16)
        b_wwf, b_wwb = Buf("pj_wwf"), Buf("pj_wwb")
        P.dma("sp", lambda e: e.dma_start(out=ww_f[:], in_=wv_[:, :, PROJ - NIH:PROJ]), b_wwf, writes=[b_wwf])
        P.op("pool", lambda e: e.tensor_copy(out=ww_b[:], in_=ww_f[:]), reads=[b_wwf], writes=[b_wwb])
        wo_sb = P.sb("pj_wo", [128, NT // 128, NIH], F32)
        b_wo = Buf("pj_wo")
        n = 0
        for blk in range(NT // 128):
            tt = blk // 4
            bs = slice(blk * 128, (blk + 1) * 128)
            for hf in range(2):
                u = n % 2
                n += 1
                ps, b_ps = C.next_ps()
                for k in range(KD):
                    P.op("pe", lambda e, ps=ps, k=k, bs=bs, hf=hf: e.matmul(ps[:], lhsT=hT[:, k, bs], rhs=wvb[hf][:, k, :],
                                                                           start=(k == 0), stop=(k == KD - 1)),
                         reads=[b_wvb[hf], b_h[tt]], writes=[b_ps])
                P.op("act", lambda e, u=u, ps=ps: e.activation(out=vo[u][:], in_=ps[:], func=AF.Copy), reads=[b_ps], writes=[b_vo[u]])
                toks.append(P.dma("sp", lambda e, u=u, bs=bs, hf=hf: e.dma_start(out=v_d[bs, hf * 512:(hf + 1) * 512], in_=vo[u][:]),
                                  b_vo[u], reads=[b_vo[u]]))
            ps, b_ps = C.next_ps()
            for k in range(KD):
                P.op("pe", lambda e, ps=ps, k=k, bs=bs: e.matmul(ps[:, 0:NIH], lhsT=hT[:, k, bs], rhs=ww_b[:, k, :],
                                                                 start=(k == 0), stop=(k == KD - 1)),
                     reads=[b_wwb, b_h[tt]], writes=[b_ps])
            P.op("act", lambda e, ps=ps, blk=blk: e.activation(out=wo_sb[:, blk, :], in_=ps[:, 0:NIH], func=AF.Copy,
                                                               scale=(NIH ** -0.5) * (HD ** -0.5)),
                 reads=[b_ps], writes=[b_wo])
        toks.append(P.dma("sp", lambda e: e.dma_start(out=w_d.rearrange("(b p) h -> p b h", p=128), in_=wo_sb[:]), b_wo, reads=[b_wo]))
        P.finish(toks)
        P.emit()
    return nc


def rope_consts(core):
    blocks = np.arange(SEQ // 128)[core::NCORES]
    pos = (blocks[:, None] * 128 + np.arange(128)[None, :]).reshape(-1).astype(np.float32)
    inv_freq = (10000.0 ** (-np.arange(0, HD, 2, dtype=np.float32) / HD)).astype(np.float32)
    ang = pos[None, :] * inv_freq[:, None]
    cos, sin = np.cos(ang).astype(np.float32), np.sin(ang).astype(np.float32)
    cs = np.empty((128, 2, NT), np.float32)
    for p in range(128):
        cs[p, 0] = cos[p % 32]
        cs[p, 1] = sin[p % 32]
    return cs


def const_mats():
    cm = np.zeros((128, 2, 128), np.float32)
    for p in range(128):
        for m in range(128):
            if p // 64 == m // 64:
                cm[p, 0, m] = 1.0
    for m in range(128):
        if (m % 64) < 32:
            cm[m + 32, 1, m] = -1.0
        else:
            cm[m - 32, 1, m] = 1.0
    return cm


TOPK = 256
NEG_SEL = -1.0e30
NEG_MASK = -2.0e30


def build_dsa_attn_prog(nblk=NT // 128):
    U8 = mybir.dt.uint8
    NBIS = 16
    nc = bass.Bass("TRN2", target_bir_lowering=False)
    x_d = nc.dram_tensor("xT", [D, NT], F32, kind="ExternalInput").ap()
    qT_d = nc.dram_tensor("qT", [D, NT], BF16, kind="ExternalInput").ap()
    qiT_d = nc.dram_tensor("qiT", [NIH * HD, NT], BF16, kind="ExternalInput").ap()
    w_d = nc.dram_tensor("wq", [128, NT // 128, NIH], F32, kind="ExternalInput").ap()
    kT_d = nc.dram_tensor("kTf", [D, SEQ], BF16, kind="ExternalInput").ap()
    v_d = nc.dram_tensor("vf", [SEQ, D], BF16, kind="ExternalInput").ap()
    kiT_d = nc.dram_tensor("kiTf", [HD, SEQ], BF16, kind="ExternalInput").ap()
    pen_d = nc.dram_tensor("pen", [128, 1024], F32, kind="ExternalInput").ap()
    id_d = nc.dram_tensor("ident", [128, 128], F32, kind="ExternalInput").ap()
    wo_d = nc.dram_tensor("w_o", [D, D], F32, kind="ExternalInput").ap()
    y_d = nc.dram_tensor("yT", [D, NT], F32, kind="ExternalOutput").ap()
    qT_v = qT_d.rearrange("(h p) t -> p h t", p=64)
    qiT_v = qiT_d.rearrange("(h p) t -> p h t", p=64)
    kT_v = kT_d.rearrange("(h p) t -> p h t", p=64)
    x_v = x_d.rearrange("(k p) t -> p k t", p=128)
    y_v = y_d.rearrange("(k p) t -> p k t", p=128)
    wo_v = wo_d.rearrange("(h p) n -> p h n", p=64)
    with ExitStack() as es:
        P = Prog(nc, es)
        ones = P.sb("c_ones", [128, 64], BF16)
        half_c = P.sb("c_half", [128, 1], F32)
        b_c = Buf("consts")
        P.op("pool", lambda e: e.memset(ones[:], 1.0), writes=[b_c])
        P.op("pool", lambda e: e.memset(half_c[:], 0.5), writes=[b_c])
        pen = P.sb("pen_sb", [128, 1024], F32)
        idf = P.sb("id_f", [128, 128], F32)
        idb = P.sb("id_b", [128, 128], BF16)
        wq = P.sb("wq_sb", [128, NT // 128, NIH], F32)
        b_pen, b_id, b_wq = Buf("pen"), Buf("ident"), Buf("wq")
        P.dma("sp", lambda e: e.dma_start(out=pen[:], in_=pen_d[:]), b_pen, writes=[b_pen])
        P.dma("sp", lambda e: e.dma_start(out=idf[:], in_=id_d[:]), b_id, writes=[b_id])
        P.dma("sp", lambda e: e.dma_start(out=wq[:], in_=w_d[:]), b_wq, writes=[b_wq])
        P.op("pool", lambda e: e.tensor_copy(out=idb[:], in_=idf[:]), reads=[b_id], writes=[b_id])
        wob = P.sb("wo_b", [64, NH, D], BF16)
        wof = P.sb("wo_f", [64, NH, 64], F32)
        b_wob, b_wof = Buf("wo_b"), Buf("wo_f")
        for m in range(2 * KD):
            P.dma("sp", lambda e, m=m: e.dma_start(out=wof[:], in_=wo_v[:, :, m * 64:(m + 1) * 64]), b_wof, writes=[b_wof])
            P.op("pool", lambda e, m=m: e.tensor_copy(out=wob[:, :, m * 64:(m + 1) * 64], in_=wof[:]), reads=[b_wof], writes=[b_wob])
        score = P.sb("score", [128, SEQ], F32)
        b_score = Buf("score")
        junk = P.sb("junk", [128, SEQ], U8)
        b_junk = Buf("junk")
        maskT = P.sb("maskT", [128, SEQ // 128, 128], U8)
        b_maskT = Buf("maskT")
        qh = P.sb("qh", [64, NH, 128], BF16)
        qih = P.sb("qih", [64, NIH, 128], BF16)
        b_qh, b_qih = Buf("qh"), Buf("qih")
        kib = [P.sb(f"kib{i}", [64, 512], BF16) for i in range(2)]
        b_kib = [Buf(f"kib{i}") for i in range(2)]
        tmp = [P.sb(f"itmp{i}", [128, 512], F32) for i in range(2)]
        b_tmp = [Buf(f"itmp{i}") for i in range(2)]
        m8 = P.sb("m8", [128, 8], F32)
        bis = P.sb("bis", [128, 8], F32)
        b_bis = Buf("bis")
        LO, HI, MID, CNT, GE, D1, D2 = (bis[:, j:j + 1] for j in range(7))
        mk = [P.sb(f"mk{i}", [128, 512], BF16) for i in range(2)]
        b_mk = [Buf(f"mk{i}") for i in range(2)]
        kTc = [P.sb(f"kTc{i}", [64, 8, 256], BF16) for i in range(2)]
        vc = [P.sb(f"vc{i}", [128, 2, 512], BF16) for i in range(2)]
        b_kTc = [Buf(f"kTc{i}") for i in range(2)]
        b_vc = [Buf(f"vc{i}") for i in range(2)]
        pT = [P.sb(f"pT{i}", [128, 512], BF16) for i in range(3)]
        b_pT = [Buf(f"pT{i}") for i in range(3)]
        attn = P.sb("attn", [64, NH, 128], BF16)
        b_attn = Buf("attn")
        dsb = [P.sb(f"dsb{i}", [64, 512], F32) for i in range(2)]
        b_dsb = [Buf(f"dsb{i}") for i in range(2)]
        xq = P.sb("xq", [128, KD, 128], F32)
        b_xq = Buf("xq")
        psA = [P.ps(f"psA{i}", [128, 512], F32) for i in range(3)]
        b_psA = [Buf(f"psA{i}") for i in range(3)]
        psT = P.ps("psT", [128, 1024], BF16)
        b_psT = Buf("psT")
        acc = [P.ps(f"acc{i}", [128, 512], F32) for i in range(2)]
        b_acc = [Buf(f"acc{i}") for i in range(2)]
        den = [P.ps(f"den{i}", [128, 512], F32) for i in range(2)]
        b_den = [Buf(f"den{i}") for i in range(2)]
        rr = {"a": 0, "kib": 0, "tmp": 0, "mk": 0, "kv": 0, "pT": 0}

        def nxt(key, n):
            v = rr[key]
            rr[key] = (v + 1) % n
            return v

        def phase_A(i):
            qs = slice(i * 128, (i + 1) * 128)
            Lk = 1024 * (i + 1)
            P.dma("sp", lambda e: e.dma_start(out=qih[:], in_=qiT_v[:, :, qs]), b_qih, writes=[b_qih])
            for j in range(Lk // 512):
                cs_ = slice(j * 512, (j + 1) * 512)
                kb = nxt("kib", 2)
                P.dma("sp", lambda e, kb=kb, cs_=cs_: e.dma_start(out=kib[kb][:], in_=kiT_d[:, cs_]), b_kib[kb], writes=[b_kib[kb]])
                for h in range(NIH):
                    a = nxt("a", 3)
                    P.op("pe", lambda e, a=a, h=h, kb=kb: e.matmul(psA[a][:], lhsT=qih[:, h, :], rhs=kib[kb][:], start=True, stop=True),
                         reads=[b_qih, b_kib[kb]], writes=[b_psA[a]])
                    if h == 0:
                        P.op("dve", lambda e, a=a, cs_=cs_: e.tensor_scalar(
                            out=score[:, cs_], in0=psA[a][:], scalar1=0.0, scalar2=wq[:, i, 0:1], op0=ALU.max, op1=ALU.mult),
                            reads=[b_psA[a], b_wq], writes=[b_score])
                    else:
                        t = nxt("tmp", 2)
                        P.op("dve", lambda e, a=a, t=t, h=h: e.tensor_scalar(
                            out=tmp[t][:], in0=psA[a][:], scalar1=0.0, scalar2=wq[:, i, h:h + 1], op0=ALU.max, op1=ALU.mult),
                            reads=[b_psA[a], b_wq], writes=[b_tmp[t]])
                        P.op("pool", lambda e, t=t, cs_=cs_: e.tensor_tensor(out=score[:, cs_], in0=score[:, cs_], in1=tmp[t][:], op=ALU.add),
                             reads=[b_tmp[t], b_score], writes=[b_score])
            P.op("dve", lambda e: e.tensor_reduce(out=LO, in_=score[:, 0:Lk], axis=AX.X, op=ALU.min), reads=[b_score], writes=[b_bis])
            P.op("pool", lambda e: e.tensor_tensor(out=score[:, Lk - 1024:Lk], in0=score[:, Lk - 1024:Lk], in1=pen[:], op=ALU.add),
                 reads=[b_score, b_pen], writes=[b_score])
            P.op("dve", lambda e: e.max(out=m8[:], in_=score[:, 0:Lk]), reads=[b_score], writes=[b_bis])
            P.op("dve", lambda e: e.tensor_copy(out=HI, in_=m8[:, 0:1]), reads=[b_bis], writes=[b_bis])

        def bis_iter(i):
            Lk = 1024 * (i + 1)
            V_ = lambda fn, rd=(): P.op("dve", fn, reads=[b_bis, b_c] + list(rd), writes=[b_bis])
            V_(lambda e: e.scalar_tensor_tensor(out=MID, in0=LO, scalar=HI, in1=half_c[:], op0=ALU.add, op1=ALU.mult))
            P.op("dve", lambda e: e.tensor_scalar(out=junk[:, 0:Lk], in0=score[:, 0:Lk], scalar1=MID, scalar2=0.0,
                                                  op0=ALU.is_ge, op1=ALU.add, accum_out=CNT),
                 reads=[b_score, b_bis], writes=[b_junk, b_bis])
            V_(lambda e: e.tensor_single_scalar(out=GE, in_=CNT, scalar=TOPK - 0.5, op=ALU.is_ge))
            V_(lambda e: e.tensor_tensor(out=D1, in0=MID, in1=LO, op=ALU.subtract))
            V_(lambda e: e.tensor_tensor(out=D2, in0=HI, in1=MID, op=ALU.subtract))
            V_(lambda e: e.scalar_tensor_tensor(out=LO, in0=D1, scalar=GE, in1=LO, op0=ALU.mult, op1=ALU.add))
            V_(lambda e: e.scalar_tensor_tensor(out=HI, in0=D2, scalar=GE, in1=MID, op0=ALU.mult, op1=ALU.add))

        def phase_C(i):
            Lk = 1024 * (i + 1)
            for j in range(Lk // 512):
                cs_ = slice(j * 512, (j + 1) * 512)
                u = nxt("mk", 2)
                P.op("pool", lambda e, u=u, cs_=cs_: e.tensor_scalar(out=mk[u][:], in0=score[:, cs_], scalar1=LO, scalar2=None, op0=ALU.is_ge),
                     reads=[b_score, b_bis], writes=[b_mk[u]])
                for jj in range(4):
                    P.op("pe", lambda e, u=u, jj=jj: e.transpose(out=psT[:, jj * 128:(jj + 1) * 128], in_=mk[u][:, jj * 128:(jj + 1) * 128],
                                                                 identity=idb[:]),
                         reads=[b_mk[u], b_id], writes=[b_psT])
                P.op("act", lambda e, j=j: e.activation(out=maskT[:, 4 * j:4 * j + 4, :].rearrange("p a b -> p (a b)"), in_=psT[:, 0:512],
                                                        func=AF.Copy),
                     reads=[b_psT], writes=[b_maskT])

        def phase_D(i, side):
            qs = slice(i * 128, (i + 1) * 128)
            nkc = 8 * (i + 1)
            P.dma("sp", lambda e: e.dma_start(out=qh[:], in_=qT_v[:, :, qs]), b_qh, writes=[b_qh])
            ngroups = 2 * nkc
            done = 0
            g = 0
            for half in range(2):
                for kc in range(nkc):
                    kk = kc % 2
                    if kk == 0:
                        s = nxt("kv", 2)
                        r0 = kc * 128
                        P.dma("sp", lambda e, s=s, r0=r0, half=half: e.dma_start(out=kTc[s][:], in_=kT_v[:, half * 8:(half + 1) * 8, r0:r0 + 256]),
                              b_kTc[s], writes=[b_kTc[s]])
                        P.dma("sp", lambda e, s=s, r0=r0, half=half: e.dma_start(
                            out=vc[s][:], in_=v_d[r0:r0 + 256, half * 512:(half + 1) * 512].rearrange("(c p) n -> p c n", p=128)),
                            b_vc[s], writes=[b_vc[s]])
                    for hg in range(2):
                        a = nxt("a", 3)
                        for hh in range(4):
                            h8 = hg * 4 + hh
                            head = half * 8 + h8
                            P.op("pe", lambda e, a=a, hh=hh, h8=h8, head=head, s=s, kk=kk: e.matmul(
                                psA[a][:, hh * 128:(hh + 1) * 128], lhsT=kTc[s][:, h8, kk * 128:(kk + 1) * 128], rhs=qh[:, head, :],
                                start=True, stop=True),
                                reads=[b_kTc[s], b_qh], writes=[b_psA[a]])
                        u = nxt("pT", 3)
                        P.op("act", lambda e, a=a, u=u: e.activation(out=pT[u][:], in_=psA[a][:], func=AF.Exp),
                             reads=[b_psA[a]], writes=[b_pT[u]])
                        P.op("dve", lambda e, u=u, kc=kc: e.tensor_tensor(
                            out=pT[u][:].rearrange("p (h q) -> p h q", h=4), in0=pT[u][:].rearrange("p (h q) -> p h q", h=4),
                            in1=maskT[:, kc, :].unsqueeze(1).to_broadcast([128, 4, 128]), op=ALU.mult),
                            reads=[b_pT[u], b_maskT], writes=[b_pT[u]])
                        for hh in range(4):
                            h8 = hg * 4 + hh
                            P.op("pe", lambda e, hg=hg, hh=hh, h8=h8, s=s, kk=kk, u=u, kc=kc: e.matmul(
                                acc[hg][0:64, hh * 128:(hh + 1) * 128], lhsT=vc[s][:, kk, h8 * 64:(h8 + 1) * 64],
                                rhs=pT[u][:, hh * 128:(hh + 1) * 128], start=(kc == 0 and hh == 0), stop=(kc == nkc - 1 and hh == 3),
                                skip_group_check=True),
                                reads=[b_vc[s], b_pT[u]], writes=[b_acc[hg]])
                        P.op("pe", lambda e, hg=hg, u=u, kc=kc: e.matmul(
                            den[hg][0:64, :], lhsT=ones[:, 0:64], rhs=pT[u][:], start=(kc == 0), stop=(kc == nkc - 1)),
                            reads=[b_c, b_pT[u]], writes=[b_den[hg]])
                    g += 1
                    want = (len(side) * g) // ngroups
                    while done < want:
                        side[done]()
                        done += 1
                for hg in range(2):
                    h0 = half * 8 + hg * 4
                    P.op("act", lambda e, hg=hg: e.activation(out=dsb[hg][:], in_=den[hg][0:64, :], func=AF.Copy),
                         reads=[b_den[hg]], writes=[b_dsb[hg]])
                    P.op("dve", lambda e, hg=hg: e.reciprocal(out=dsb[hg][:], in_=dsb[hg][:]), reads=[b_dsb[hg]], writes=[b_dsb[hg]])
                    P.op("dve", lambda e, hg=hg, h0=h0: e.tensor_tensor(
                        out=attn[:, h0:h0 + 4, :].rearrange("p a b -> p (a b)"), in0=acc[hg][0:64, :], in1=dsb[hg][:], op=ALU.mult),
                        reads=[b_acc[hg], b_dsb[hg]], writes=[b_attn])
            while done < len(side):
                side[done]()
                done += 1

        def phase_E(i):
            qs = slice(i * 128, (i + 1) * 128)
            P.dma("sp", lambda e: e.dma_start(out=xq[:], in_=x_v[:, :, qs]), b_xq, writes=[b_xq])
            for m in range(KD):
                a = nxt("a", 3)
                for h in range(NH):
                    P.op("pe", lambda e, a=a, h=h, m=m: e.matmul(psA[a][:, 0:128], lhsT=wob[:, h, m * 128:(m + 1) * 128], rhs=attn[:, h, :],
                                                                 start=(h == 0), stop=(h == NH - 1)),
                         reads=[b_wob, b_attn], writes=[b_psA[a]])
                P.op("dve", lambda e, a=a, m=m: e.tensor_tensor(out=xq[:, m, :], in0=psA[a][:, 0:128], in1=xq[:, m, :], op=ALU.add),
                     reads=[b_psA[a], b_xq], writes=[b_xq])
            return P.dma("sp", lambda e: e.dma_start(out=y_v[:, :, qs], in_=xq[:]), b_xq, reads=[b_xq])

        phase_A(0)
        for _ in range(NBIS):
            bis_iter(0)
        phase_C(0)
        tok = None
        for i in range(nblk):
            side = []
            if i + 1 < nblk:
                phase_A(i + 1)
                side = [(lambda n=i + 1: bis_iter(n)) for _ in range(NBIS)]
            phase_D(i, side)
            tok = phase_E(i)
            if i + 1 < nblk:
                phase_C(i + 1)
        P.finish([tok])
        P.emit()
    return nc


def causal_pen(core):
    j = np.arange(1024)[None, :]
    p = np.arange(128)[:, None]
    return np.where(j > 128 * core + p, np.float32(NEG_MASK), np.float32(0.0)).astype(np.float32)


def gather_tokens_T(parts):
    f = parts[0].shape[0]
    out = np.empty((f, SEQ // 128, 128), parts[0].dtype)
    for c, p in enumerate(parts):
        out[:, c::NCORES, :] = p.reshape(f, NT // 128, 128)
    return out.reshape(f, SEQ)


def gather_tokens(parts):
    f = parts[0].shape[1]
    out = np.empty((SEQ // 128, 128, f), parts[0].dtype)
    for c, p in enumerate(parts):
        out[c::NCORES] = p.reshape(NT // 128, 128, f)
    return out.reshape(SEQ, f)


def run_dsa_layer(xT_parts, j, i, inp):
    gm = col_layout(inp["norm_mix"][i])
    qkg = np.stack([np.tile(inp["dsa_q_norm"][j], 2), np.tile(inp["dsa_k_norm"][j], 2)], axis=1).astype(np.float32)
    cm = const_mats()
    maps = [{"xT": xT_parts[c], "w_in": inp["dsa_w_in"][j], "gain": gm, "qk_gain": qkg, "cossin": rope_consts(c), "cmat": cm}
            for c in range(NCORES)]
    pr = _run("dsa_proj", build_dsa_proj_prog, maps)
    kTf = gather_tokens_T([r["kT"] for r in pr])
    kiTf = gather_tokens_T([r["kiT"] for r in pr])
    vf = gather_tokens([r["v"] for r in pr])
    ident = np.eye(128, dtype=np.float32)
    maps = []
    for c in range(NCORES):
        wq = np.ascontiguousarray(pr[c]["w"].reshape(NT // 128, 128, NIH).transpose(1, 0, 2))
        maps.append({"xT": xT_parts[c], "qT": pr[c]["qT"], "qiT": pr[c]["qiT"], "wq": wq, "kTf": kTf, "vf": vf, "kiTf": kiTf,
                     "pen": causal_pen(c), "ident": ident, "w_o": inp["dsa_w_o"][j]})
    ar = _run("dsa_attn", build_dsa_attn_prog, maps)
    gf = col_layout(inp["norm_ffn"][i])
    maps = [{"xT": ar[c]["yT"], "wgu": inp["ffn_w_gate_up"][i], "wd": inp["ffn_w_down"][i], "gain": gf} for c in range(NCORES)]
    fr = _run("ffn", build_ffn_prog, maps)
    return [r["yT"] for r in fr]


def kernel(**inputs):
    inp = {k: np.asarray(v) for k, v in inputs.items()}
    x = np.ascontiguousarray(inp["x"][0], dtype=np.float32)
    parts = [to_core_T(x, c) for c in range(NCORES)]
    for i in range(4):
        if i % 2 == 0:
            parts = run_s5_layer(parts, i // 2, i, inp)
        else:
            parts = run_dsa_layer(parts, i // 2, i, inp)
    return from_core_T(parts)[None].astype(np.float32)
```

```python
import math
from contextlib import ExitStack

import numpy as np
import concourse.bass as bass
import concourse.mybir as mybir
from concourse.bass_utils import run_bass_kernel_spmd

F32 = mybir.dt.float32
BF16 = mybir.dt.bfloat16
ALU = mybir.AluOpType
AF = mybir.ActivationFunctionType
AX = mybir.AxisListType

NCORES = 8
D = 1024
KD = D // 128
SEQ = 16384
NT = SEQ // NCORES
TT = 512
NTT = NT // TT
DFF = 2816
KF = DFF // 128
EPS = 1e-6

ENGS = ("pe", "act", "dve", "pool", "sp")
SAME_ENGINE_SYNC = ("act", "dve", "pool")


class Buf:
    __slots__ = ("name", "w", "r", "dsem", "dcount")

    def __init__(self, name):
        self.name = name
        self.w = None
        self.r = {}
        self.dsem = None
        self.dcount = 0


class Prog:
    def __init__(self, nc, es):
        self.nc = nc
        self.es = es
        self.ops = {e: [] for e in ENGS}
        self.dma_bufs = []
        self.final_tokens = []
        self.bar = []
        self.bar_epoch = 0
        self.eng_epoch = {e: 0 for e in ENGS}

    def sb(self, name, shape, dt, es=None):
        return (es or self.es).enter_context(self.nc.sbuf_tensor(name, list(shape), dt))

    def ps(self, name, shape, dt=F32, es=None):
        return (es or self.es).enter_context(self.nc.psum_tensor(name, list(shape), dt))

    def _deps(self, reads, writes):
        need = []
        for b in reads:
            if b.w is not None:
                need.append(b.w)
        for b in writes:
            if b.w is not None:
                need.append(b.w)
            need.extend(b.r.values())
        return need

    def barrier(self):
        toks = []
        for e in ENGS:
            for idx in range(len(self.ops[e]) - 1, -1, -1):
                if self.ops[e][idx]["dma"] is None:
                    toks.append(("e", e, idx))
                    break
        for b in self.dma_bufs:
            toks.append(("d", b, b.dcount))
        self.bar = toks
        self.bar_epoch += 1

    def _bar_need(self, eng):
        if self.eng_epoch[eng] < self.bar_epoch:
            self.eng_epoch[eng] = self.bar_epoch
            return list(self.bar)
        return []

    def op(self, eng, fn, reads=(), writes=()):
        need = self._deps(reads, writes) + self._bar_need(eng)
        idx = len(self.ops[eng])
        tok = ("e", eng, idx)
        self.ops[eng].append({"need": need, "fn": fn, "dma": None})
        for b in reads:
            b.r[eng] = tok
        for b in writes:
            b.w = tok
            b.r = {}
        return tok

    def dma(self, eng, fn, sembuf, reads=(), writes=()):
        need = self._deps(reads, writes) + self._bar_need(eng)
        if sembuf.dsem is None:
            sembuf.dsem = self.es.enter_context(self.nc.semaphore("d_" + sembuf.name))
            self.dma_bufs.append(sembuf)
        sembuf.dcount += 16
        tok = ("d", sembuf, sembuf.dcount)
        self.ops[eng].append({"need": need, "fn": fn, "dma": sembuf})
        for b in reads:
            b.r[("d", id(sembuf))] = tok
        for b in writes:
            b.w = tok
            b.r = {}
        return tok

    def finish(self, tokens):
        self.final_tokens.extend(tokens)

    def emit(self):
        nc = self.nc
        needed = {e: set() for e in ENGS}
        for e in ENGS:
            for i, o in enumerate(self.ops[e]):
                for t in o["need"]:
                    if t[0] == "e":
                        if t[1] == e and e not in SAME_ENGINE_SYNC:
                            continue
                        needed[t[1]].add(t[2])
        for t in self.final_tokens:
            if t[0] == "e":
                needed[t[1]].add(t[2])
        rank = {}
        for e in ENGS:
            rank[e] = {i: n + 1 for n, i in enumerate(sorted(needed[e]))}
        sems = {e: self.es.enter_context(nc.semaphore("s_" + e)) for e in ENGS}
        final_tokens = self.final_tokens

        def run(e, engine):
            known = {}
            def wait(tok):
                if tok[0] == "e":
                    if tok[1] == e and e not in SAME_ENGINE_SYNC:
                        return
                    key, sem, val = tok[1], sems[tok[1]], rank[tok[1]][tok[2]]
                else:
                    key, sem, val = id(tok[1]), tok[1].dsem, tok[2]
                if known.get(key, 0) >= val:
                    return
                known[key] = val
                engine.wait_ge(sem, val)
            for i, o in enumerate(self.ops[e]):
                for t in o["need"]:
                    wait(t)
                ins = o["fn"](engine)
                if o["dma"] is not None:
                    ins.then_inc(o["dma"].dsem, 16)
                elif i in rank[e]:
                    ins.then_inc(sems[e], 1)
            if e == "sp":
                for t in final_tokens:
                    wait(t)

        with nc.Block() as block:
            @block.tensor
            def _(eng):
                run("pe", eng)

            @block.scalar
            def _(eng):
                run("act", eng)

            @block.vector
            def _(eng):
                run("dve", eng)

            @block.gpsimd
            def _(eng):
                run("pool", eng)

            @block.sync
            def _(eng):
                run("sp", eng)


class Ctx:
    def __init__(self, P):
        self.P = P
        nc = P.nc
        self.ones = P.sb("c_ones", [128, 128], BF16)
        self.b_ones = Buf("ones")
        P.op("pool", lambda g: g.memset(self.ones[:], 1.0), writes=[self.b_ones])
        self.eps_col = P.sb("c_eps", [128, 1], F32)
        P.op("pool", lambda g: g.memset(self.eps_col[:], EPS), writes=[self.b_ones])
        self.psum = [P.ps(f"ps{i}", [128, 512], F32) for i in range(8)]
        self.b_ps = [Buf(f"ps{i}") for i in range(8)]
        self.ps_rr = 0

    def next_ps(self):
        i = self.ps_rr
        self.ps_rr = (self.ps_rr + 1) % 8
        return self.psum[i], self.b_ps[i]


def emit_rmsnorm_T(P, C, xT, b_x, gain, b_gain, gcol0, hT, b_h, tts, es, tag):
    sq = [P.sb(f"{tag}_sq{i}", [128, TT], BF16, es) for i in range(2)]
    b_sq = [Buf(f"{tag}_sq{i}") for i in range(2)]
    rstd = [P.sb(f"{tag}_rstd{i}", [128, TT], F32, es) for i in range(2)]
    b_rstd = [Buf(f"{tag}_rstd{i}") for i in range(2)]
    n = 0
    for j, tt in enumerate(tts):
        ts = slice(tt * TT, (tt + 1) * TT)
        ps, b_ps = C.next_ps()
        for k in range(KD):
            s, bs = sq[n % 2], b_sq[n % 2]
            n += 1
            P.op("act", lambda e, s=s, k=k, ts=ts: e.activation(out=s[:], in_=xT[:, k, ts], func=AF.Square),
                 reads=[b_x[k][tt]], writes=[bs])
            P.op("pe", lambda e, ps=ps, s=s, k=k: e.matmul(ps[:], lhsT=C.ones[:], rhs=s[:],
                                                             start=(k == 0), stop=(k == KD - 1)),
                 reads=[bs, C.b_ones], writes=[b_ps])
        r, br = rstd[j % 2], b_rstd[j % 2]
        P.op("act", lambda e, r=r, ps=ps: e.activation(out=r[:], in_=ps[:], func=AF.Sqrt, scale=1.0 / D, bias=C.eps_col[:]),
             reads=[b_ps, C.b_ones], writes=[br])
        P.op("dve", lambda e, r=r: e.reciprocal(out=r[:], in_=r[:]),
             reads=[br], writes=[br])
        js = slice(j * TT, (j + 1) * TT)
        for k in range(KD):
            P.op("dve", lambda e, k=k, ts=ts, js=js, r=r: e.scalar_tensor_tensor(
                out=hT[:, k, js], in0=xT[:, k, ts], scalar=gain[:, gcol0 + k:gcol0 + k + 1], in1=r[:],
                op0=ALU.mult, op1=ALU.mult),
                reads=[b_x[k][tt], br, b_gain], writes=[b_h[j]])


def emit_ffn(P, C, xT, b_x, wgu, wd, gain, b_gain, gcol0, tag="ffn"):
    nc = P.nc
    with ExitStack() as es:
        HT = 2 * TT
        hT = P.sb(f"{tag}_hT", [128, KD, HT], BF16, es)
        aT = P.sb(f"{tag}_aT", [128, KF, HT], BF16, es)
        wg_f = [P.sb(f"{tag}_wgf{i}", [128, KD, 256], F32, es) for i in range(2)]
        wg_b = [P.sb(f"{tag}_wgb{i}", [128, KD, 256], BF16, es) for i in range(2)]
        wd_f = [P.sb(f"{tag}_wdf{i}", [128, KF, 128], F32, es) for i in range(2)]
        wd_b = [P.sb(f"{tag}_wdb{i}", [128, KF, 128], BF16, es) for i in range(2)]
        sg = [P.sb(f"{tag}_sg{i}", [128, TT], F32, es) for i in range(2)]
        b_hT = [Buf(f"{tag}_hT{j}") for j in range(2)]
        b_aT = [[Buf(f"{tag}_aT{n}_{j}") for j in range(2)] for n in range(KF)]
        b_wgf = [Buf(f"{tag}_wgf{i}") for i in range(2)]
        b_wgb = [Buf(f"{tag}_wgb{i}") for i in range(2)]
        b_wdf = [Buf(f"{tag}_wdf{i}") for i in range(2)]
        b_wdb = [Buf(f"{tag}_wdb{i}") for i in range(2)]
        b_sg = [Buf(f"{tag}_sg{i}") for i in range(2)]
        wgu_v = wgu.rearrange("(k p) n -> p k n", p=128)
        wd_v = wd.rearrange("(k p) n -> p k n", p=128)
        nsg = 0
        nw = 0
        nwd = 0
        for half in range(NT // HT):
            tts = [half * 2, half * 2 + 1]
            emit_rmsnorm_T(P, C, xT, b_x, gain, b_gain, gcol0, hT, b_hT, tts, es, f"{tag}n{half}")
            for n in range(KF):
                s = nw % 2
                nw += 1
                P.dma("sp", lambda e, s=s, n=n: e.dma_start(out=wg_f[s][:, :, 0:128],
                                                            in_=wgu_v[:, :, n * 128:(n + 1) * 128]),
                      b_wgf[s], writes=[b_wgf[s]])
                P.dma("sp", lambda e, s=s, n=n: e.dma_start(out=wg_f[s][:, :, 128:256],
                                                            in_=wgu_v[:, :, DFF + n * 128:DFF + (n + 1) * 128]),
                      b_wgf[s], writes=[])
                b_wgf[s].w = ("d", b_wgf[s], b_wgf[s].dcount)
                P.op("pool", lambda e, s=s: e.tensor_copy(out=wg_b[s][:], in_=wg_f[s][:]),
                     reads=[b_wgf[s]], writes=[b_wgb[s]])
                for j in range(2):
                    js = slice(j * TT, (j + 1) * TT)
                    pg, b_pg = C.next_ps()
                    pu, b_pu = C.next_ps()
                    for k in range(KD):
                        P.op("pe", lambda e, pg=pg, s=s, k=k, js=js: e.matmul(
                            pg[:], lhsT=wg_b[s][:, k, 0:128], rhs=hT[:, k, js], start=(k == 0), stop=(k == KD - 1)),
                            reads=[b_wgb[s], b_hT[j]], writes=[b_pg])
                    for k in range(KD):
                        P.op("pe", lambda e, pu=pu, s=s, k=k, js=js: e.matmul(
                            pu[:], lhsT=wg_b[s][:, k, 128:256], rhs=hT[:, k, js], start=(k == 0), stop=(k == KD - 1)),
                            reads=[b_wgb[s], b_hT[j]], writes=[b_pu])
                    q = nsg % 2
                    nsg += 1
                    P.op("act", lambda e, q=q, pg=pg: e.activation(out=sg[q][:], in_=pg[:], func=AF.Silu),
                         reads=[b_pg], writes=[b_sg[q]])
                    P.op("dve", lambda e, q=q, pu=pu, n=n, js=js: e.tensor_tensor(
                        out=aT[:, n, js], in0=pu[:], in1=sg[q][:], op=ALU.mult),
                        reads=[b_pu, b_sg[q]], writes=[b_aT[n][j]])
            for m in range(KD):
                s = nwd % 2
                nwd += 1
                P.dma("sp", lambda e, s=s, m=m: e.dma_start(out=wd_f[s][:], in_=wd_v[:, :, m * 128:(m + 1) * 128]),
                      b_wdf[s], writes=[b_wdf[s]])
                P.op("pool", lambda e, s=s: e.tensor_copy(out=wd_b[s][:], in_=wd_f[s][:]),
                     reads=[b_wdf[s]], writes=[b_wdb[s]])
                for j in range(2):
                    tt = tts[j]
                    js = slice(j * TT, (j + 1) * TT)
                    ts = slice(tt * TT, (tt + 1) * TT)
                    po, b_po = C.next_ps()
                    for n in range(KF):
                        P.op("pe", lambda e, po=po, s=s, n=n, js=js: e.matmul(
                            po[:], lhsT=wd_b[s][:, n, :], rhs=aT[:, n, js], start=(n == 0), stop=(n == KF - 1)),
                            reads=[b_wdb[s], b_aT[n][j]], writes=[b_po])
                    P.op("dve", lambda e, po=po, m=m, ts=ts: e.tensor_tensor(
                        out=xT[:, m, ts], in0=po[:], in1=xT[:, m, ts], op=ALU.add),
                        reads=[b_po, b_x[m][tt]], writes=[b_x[m][tt]])
    P.barrier()


def load_xT(P, xT, b_x, x_dram, eng="sp"):
    xv = x_dram.rearrange("(k p) t -> p k t", p=128)
    for k in range(KD):
        for tt in range(NTT):
            ts = slice(tt * TT, (tt + 1) * TT)
            P.dma(eng, lambda e, k=k, ts=ts: e.dma_start(out=xT[:, k, ts], in_=xv[:, k, ts]),
                  b_x[k][tt], writes=[b_x[k][tt]])


def store_xT(P, xT, b_x, y_dram, eng="sp"):
    yv = y_dram.rearrange("(k p) t -> p k t", p=128)
    toks = []
    for k in range(KD):
        for tt in range(NTT):
            ts = slice(tt * TT, (tt + 1) * TT)
            toks.append(P.dma(eng, lambda e, k=k, ts=ts: e.dma_start(out=yv[:, k, ts], in_=xT[:, k, ts]),
                              b_x[k][tt], reads=[b_x[k][tt]]))
    P.finish(toks)


def build_ffn_prog():
    nc = bass.Bass("TRN2", target_bir_lowering=False)
    x = nc.dram_tensor("xT", [D, NT], F32, kind="ExternalInput").ap()
    wgu = nc.dram_tensor("wgu", [D, 2 * DFF], F32, kind="ExternalInput").ap()
    wd = nc.dram_tensor("wd", [DFF, D], F32, kind="ExternalInput").ap()
    gain = nc.dram_tensor("gain", [128, KD], F32, kind="ExternalInput").ap()
    y = nc.dram_tensor("yT", [D, NT], F32, kind="ExternalOutput").ap()
    with ExitStack() as es:
        P = Prog(nc, es)
        C = Ctx(P)
        xT = P.sb("xT_sb", [128, KD, NT], F32)
        b_x = [[Buf(f"x{k}_{t}") for t in range(NTT)] for k in range(KD)]
        g_sb = P.sb("gain_sb", [128, KD], F32)
        b_g = Buf("gain")
        P.dma("sp", lambda e: e.dma_start(out=g_sb[:], in_=gain[:]), b_g, writes=[b_g])
        load_xT(P, xT, b_x, x)
        emit_ffn(P, C, xT, b_x, wgu, wd, g_sb, b_g, 0)
        store_xT(P, xT, b_x, y)
        P.emit()
    return nc


def col_layout(v):
    return np.ascontiguousarray(np.asarray(v, np.float32).reshape(-1, 128).T)


def to_core_T(x2d, c):
    blocks = x2d.reshape(SEQ // 128, 128, -1)[c::NCORES]
    return np.ascontiguousarray(blocks.reshape(NT, -1).T)


def from_core_T(parts):
    dd = parts[0].shape[0]
    out = np.empty((SEQ // 128, 128, dd), np.float32)
    for c, p in enumerate(parts):
        out[c::NCORES] = p.T.reshape(NT // 128, 128, dd)
    return out.reshape(SEQ, dd)


def build_prenorm_prog():
    nc = bass.Bass("TRN2", target_bir_lowering=False)
    x = nc.dram_tensor("xT", [D, NT], F32, kind="ExternalInput").ap()
    gain = nc.dram_tensor("gain", [128, KD], F32, kind="ExternalInput").ap()
    y = nc.dram_tensor("hT", [D, NT], F32, kind="ExternalOutput").ap()
    with ExitStack() as es:
        P = Prog(nc, es)
        C = Ctx(P)
        xT = P.sb("xT_sb", [128, KD, NT], F32)
        b_x = [[Buf(f"x{k}_{t}") for t in range(NTT)] for k in range(KD)]
        g_sb = P.sb("gain_sb", [128, KD], F32)
        b_g = Buf("gain")
        P.dma("sp", lambda e: e.dma_start(out=g_sb[:], in_=gain[:]), b_g, writes=[b_g])
        load_xT(P, xT, b_x, x)
        hT = P.sb("hT_sb", [128, KD, NT], F32)
        b_h = [Buf(f"h{t}") for t in range(NTT)]
        emit_rmsnorm_T(P, C, xT, b_x, g_sb, b_g, 0, hT, b_h, list(range(NTT)), es, "pn")
        yv = y.rearrange("(k p) t -> p k t", p=128)
        toks = []
        for tt in range(NTT):
            ts = slice(tt * TT, (tt + 1) * TT)
            toks.append(P.dma("sp", lambda e, ts=ts: e.dma_start(out=yv[:, :, ts], in_=hT[:, :, ts]),
                              b_h[tt], reads=[b_h[tt]]))
        P.finish(toks)
        P.emit()
    return nc


S5T = 512
TWO_PI = 2.0 * math.pi

PI_LO = 3.1415925
CW1 = 6.28125
CW2 = TWO_PI - 6.28125


def emit_range_reduce(P, dst, src, ti, tf, reads, writes):
    rw = list(reads) + list(writes)
    P.op("dve", lambda e: e.tensor_scalar(out=tf, in0=src, scalar1=1.0 / TWO_PI, scalar2=None, op0=ALU.mult),
         reads=rw, writes=writes)
    P.op("dve", lambda e: e.tensor_copy(out=ti, in_=tf), reads=rw, writes=writes)
    P.op("dve", lambda e: e.tensor_copy(out=tf, in_=ti), reads=rw, writes=writes)
    P.op("dve", lambda e: e.scalar_tensor_tensor(out=dst, in0=tf, scalar=-CW1, in1=src, op0=ALU.mult, op1=ALU.add),
         reads=rw, writes=writes)
    P.op("dve", lambda e: e.scalar_tensor_tensor(out=dst, in0=tf, scalar=-CW2, in1=dst, op0=ALU.mult, op1=ALU.add),
         reads=rw, writes=writes)
    P.op("dve", lambda e: e.tensor_scalar(out=tf, in0=dst, scalar1=math.pi, scalar2=-TWO_PI, op0=ALU.is_gt, op1=ALU.mult),
         reads=rw, writes=writes)
    P.op("dve", lambda e: e.tensor_tensor(out=dst, in0=dst, in1=tf, op=ALU.add), reads=rw, writes=writes)
    P.op("dve", lambda e: e.tensor_scalar(out=tf, in0=dst, scalar1=-math.pi, scalar2=TWO_PI, op0=ALU.is_lt, op1=ALU.mult),
         reads=rw, writes=writes)
    P.op("dve", lambda e: e.tensor_tensor(out=dst, in0=dst, in1=tf, op=ALU.add), reads=rw, writes=writes)
    P.op("dve", lambda e: e.tensor_scalar(out=dst, in0=dst, scalar1=PI_LO, scalar2=-PI_LO, op0=ALU.min, op1=ALU.max),
         reads=rw, writes=writes)


def build_s5_prog(debug=0):
    nc = bass.Bass("TRN2", target_bir_lowering=False)
    u_d = nc.dram_tensor("uT", [128, SEQ], F32, kind="ExternalInput").ap()
    par_d = nc.dram_tensor("par", [128, 16], F32, kind="ExternalInput").ap()
    bt_d = nc.dram_tensor("bt", [128, 2, 512], F32, kind="ExternalInput").ap()
    ct_d = nc.dram_tensor("ct", [128, 2, 4, 128], F32, kind="ExternalInput").ap()
    tau_d = nc.dram_tensor("tau", [128, S5T], F32, kind="ExternalInput").ap()
    y_d = nc.dram_tensor("gyT", [128, SEQ], F32, kind="ExternalOutput").ap()
    NCH = SEQ // S5T
    with ExitStack() as es:
        P = Prog(nc, es)
        C = Ctx(P)
        par = P.sb("par_sb", [128, 16], F32)
        bt = P.sb("bt_sb", [128, 2, 512], F32)
        ct = P.sb("ct_sb", [128, 2, 4, 128], F32)
        tau = P.sb("tau_sb", [128, S5T], F32)
        b_par, b_bt, b_ct, b_tau = Buf("par"), Buf("bt"), Buf("ct"), Buf("tau")
        P.dma("sp", lambda e: e.dma_start(out=par[:], in_=par_d[:]), b_par, writes=[b_par])
        P.dma("sp", lambda e: e.dma_start(out=bt[:], in_=bt_d[:]), b_bt, writes=[b_bt])
        P.dma("sp", lambda e: e.dma_start(out=ct[:], in_=ct_d[:]), b_ct, writes=[b_ct])
        P.dma("sp", lambda e: e.dma_start(out=tau[:], in_=tau_d[:]), b_tau, writes=[b_tau])
        sm = P.sb("s5_small", [128, 24, 4], F32)
        b_sm = Buf("s5_small")
        halfpi = P.sb("halfpi", [128, 1], F32)
        P.op("pool", lambda e: e.memset(halfpi[:], math.pi / 2), writes=[b_sm])
        lam_re, lam_im, logdt, dcol = par[:, 0:4], par[:, 4:8], par[:, 8:12], par[:, 12:13]
        (DT, LR, TH, R, THR, ABS, ARE, AIM, NR, NUM_RE, NUM_IM, DEN, KRE, KIM, T1, T2, PHT, CT_, ST_, NST_) = range(20)
        col = lambda i: sm[:, i, :]

        def V(fn, reads=(b_par, b_sm)):
            P.op("dve", fn, reads=list(reads), writes=[b_sm])

        def A(fn):
            P.op("act", fn, reads=[b_par, b_sm], writes=[b_sm])

        def sincos(src, s_out, c_out, shape_ap_abs):
            A(lambda e: e.activation(out=s_out, in_=src, func=AF.Sin))
            A(lambda e: e.activation(out=shape_ap_abs, in_=src, func=AF.Abs))
            A(lambda e: e.activation(out=c_out, in_=shape_ap_abs, func=AF.Sin, scale=-1.0, bias=halfpi[:]))

        def reduce_phase(out, src_fn_desc):
            pass

        A(lambda e: e.activation(out=col(DT), in_=logdt, func=AF.Exp))
        V(lambda e: e.tensor_tensor(out=col(LR), in0=lam_re, in1=col(DT), op=ALU.mult))
        V(lambda e: e.tensor_tensor(out=col(TH), in0=lam_im, in1=col(DT), op=ALU.mult))
        A(lambda e: e.activation(out=col(R), in_=col(LR), func=AF.Exp))
        smi = P.sb("s5_smi", [128, 4], mybir.dt.int32)
        emit_range_reduce(P, col(THR), col(TH), smi[:], col(T1), [b_par], [b_sm])
        sincos(col(THR), col(AIM), col(ARE), col(ABS))
        V(lambda e: e.tensor_tensor(out=col(ARE), in0=col(ARE), in1=col(R), op=ALU.mult))
        V(lambda e: e.tensor_tensor(out=col(AIM), in0=col(AIM), in1=col(R), op=ALU.mult))
        V(lambda e: e.tensor_scalar(out=col(NR), in0=col(ARE), scalar1=-1.0, scalar2=None, op0=ALU.add))
        V(lambda e: e.tensor_tensor(out=col(T1), in0=col(NR), in1=lam_re, op=ALU.mult))
        V(lambda e: e.tensor_tensor(out=col(T2), in0=col(AIM), in1=lam_im, op=ALU.mult))
        V(lambda e: e.tensor_tensor(out=col(NUM_RE), in0=col(T1), in1=col(T2), op=ALU.add))
        V(lambda e: e.tensor_tensor(out=col(T1), in0=col(AIM), in1=lam_re, op=ALU.mult))
        V(lambda e: e.tensor_tensor(out=col(T2), in0=col(NR), in1=lam_im, op=ALU.mult))
        V(lambda e: e.tensor_tensor(out=col(NUM_IM), in0=col(T1), in1=col(T2), op=ALU.subtract))
        V(lambda e: e.tensor_tensor(out=col(T1), in0=lam_re, in1=lam_re, op=ALU.mult))
        V(lambda e: e.tensor_tensor(out=col(T2), in0=lam_im, in1=lam_im, op=ALU.mult))
        V(lambda e: e.tensor_tensor(out=col(DEN), in0=col(T1), in1=col(T2), op=ALU.add))
        V(lambda e: e.reciprocal(out=col(DEN), in_=col(DEN)))
        V(lambda e: e.tensor_tensor(out=col(KRE), in0=col(NUM_RE), in1=col(DEN), op=ALU.mult))
        V(lambda e: e.tensor_tensor(out=col(KIM), in0=col(NUM_IM), in1=col(DEN), op=ALU.mult))
        V(lambda e: e.tensor_scalar(out=col(T2), in0=col(THR), scalar1=float(S5T), scalar2=None, op0=ALU.mult))
        emit_range_reduce(P, col(PHT), col(T2), smi[:], col(T1), [b_par], [b_sm])
        sincos(col(PHT), col(ST_), col(CT_), col(ABS))
        V(lambda e: e.tensor_scalar(out=col(NST_), in0=col(ST_), scalar1=-1.0, scalar2=None, op0=ALU.mult))
        L = P.sb("s5_L", [128, 3, 4, 128], BF16)
        b_L = Buf("s5_L")
        ctmp = P.sb("s5_ctmp", [128, 2, 128], F32)
        b_ctmp = Buf("s5_ctmp")
        for k in range(4):
            kre, kim = sm[:, KRE, k:k + 1], sm[:, KIM, k:k + 1]
            P.op("dve", lambda e, k=k, kim=kim: e.tensor_scalar(out=ctmp[:, 0, :], in0=ct[:, 1, k, :], scalar1=kim,
                                                                 scalar2=-1.0, op0=ALU.mult, op1=ALU.mult),
                 reads=[b_ct, b_sm], writes=[b_ctmp])
            P.op("dve", lambda e, k=k, kre=kre: e.scalar_tensor_tensor(out=ctmp[:, 0, :], in0=ct[:, 0, k, :], scalar=kre,
                                                                        in1=ctmp[:, 0, :], op0=ALU.mult, op1=ALU.add),
                 reads=[b_ct, b_sm, b_ctmp], writes=[b_ctmp])
            P.op("dve", lambda e, k=k, kre=kre: e.tensor_scalar(out=ctmp[:, 1, :], in0=ct[:, 1, k, :], scalar1=kre,
                                                                 scalar2=None, op0=ALU.mult),
                 reads=[b_ct, b_sm], writes=[b_ctmp])
            P.op("dve", lambda e, k=k, kim=kim: e.scalar_tensor_tensor(out=ctmp[:, 1, :], in0=ct[:, 0, k, :], scalar=kim,
                                                                        in1=ctmp[:, 1, :], op0=ALU.mult, op1=ALU.add),
                 reads=[b_ct, b_sm, b_ctmp], writes=[b_ctmp])
            P.op("dve", lambda e, k=k: e.tensor_copy(out=L[:, 0, k, :], in_=ctmp[:, 0, :]), reads=[b_ctmp], writes=[b_L])
            P.op("dve", lambda e, k=k: e.tensor_scalar(out=L[:, 1, k, :], in0=ctmp[:, 0, :], scalar1=-1.0, scalar2=None,
                                                       op0=ALU.mult), reads=[b_ctmp], writes=[b_L])
            P.op("dve", lambda e, k=k: e.tensor_scalar(out=L[:, 2, k, :], in0=ctmp[:, 1, :], scalar1=-1.0, scalar2=None,
                                                       op0=ALU.mult), reads=[b_ctmp], writes=[b_L])
        btb = P.sb("s5_btb", [128, 2, 512], BF16)
        b_btb = Buf("s5_btb")
        P.op("dve", lambda e: e.tensor_copy(out=btb[:], in_=bt[:]), reads=[b_bt], writes=[b_btb])
        cosT = P.sb("s5_cos", [128, 4, S5T], F32)
        sinT = P.sb("s5_sin", [128, 4, S5T], F32)
        rT = P.sb("s5_rT", [128, 4, S5T], F32)
        b_tab = Buf("s5_tab")
        ph = P.sb("s5_ph", [128, S5T], F32)
        pha = P.sb("s5_pha", [128, S5T], F32)
        phx = P.sb("s5_phx", [128, S5T], F32)
        phi = P.sb("s5_phi", [128, S5T], mybir.dt.int32)
        b_ph = Buf("s5_ph")
        for k in range(4):
            P.op("dve", lambda e, k=k: e.tensor_scalar(out=phx[:], in0=tau[:], scalar1=sm[:, THR, k:k + 1],
                                                       scalar2=None, op0=ALU.mult),
                 reads=[b_tau, b_sm, b_tab], writes=[b_ph])
            emit_range_reduce(P, ph[:], phx[:], phi[:], pha[:], [b_sm], [b_ph])
            P.op("act", lambda e, k=k: e.activation(out=sinT[:, k, :], in_=ph[:], func=AF.Sin),
                 reads=[b_ph], writes=[b_tab])
            P.op("act", lambda e: e.activation(out=pha[:], in_=ph[:], func=AF.Abs),
                 reads=[b_ph], writes=[b_ph])
            P.op("act", lambda e, k=k: e.activation(out=cosT[:, k, :], in_=pha[:], func=AF.Sin, scale=-1.0, bias=halfpi[:]),
                 reads=[b_ph, b_sm], writes=[b_tab])
            P.op("dve", lambda e, k=k: e.tensor_scalar(out=rT[:, k, :], in0=tau[:], scalar1=0.0, scalar2=sm[:, R, k:k + 1],
                                                       op0=ALU.mult, op1=ALU.add),
                 reads=[b_tau, b_sm], writes=[b_tab])
        if debug == 2:
            tk = [P.dma("sp", lambda e: e.dma_start(out=y_d[:, 0:96], in_=sm[:].rearrange("p a b -> p (a b)")), b_sm, reads=[b_sm]),
                  P.dma("sp", lambda e: e.dma_start(out=y_d[:, 1024:3072], in_=cosT[:].rearrange("p a b -> p (a b)")), b_tab, reads=[b_tab]),
                  P.dma("sp", lambda e: e.dma_start(out=y_d[:, 3072:5120], in_=sinT[:].rearrange("p a b -> p (a b)")), b_tab, reads=[b_tab]),
                  P.dma("sp", lambda e: e.dma_start(out=y_d[:, 5120:7168], in_=rT[:].rearrange("p a b -> p (a b)")), b_tab, reads=[b_tab])]
            P.finish(tk)
            P.emit()
            return nc
        NB = 2
        uf = [P.sb(f"s5_uf{i}", [128, S5T], F32) for i in range(NB)]
        ub = [P.sb(f"s5_ub{i}", [128, S5T], BF16) for i in range(NB)]
        b_uf = [Buf(f"s5_uf{i}") for i in range(NB)]
        b_ub = [Buf(f"s5_ub{i}") for i in range(NB)]
        sA = [P.sb(f"s5_sA{i}", [128, S5T], F32) for i in range(2)]
        sB = [P.sb(f"s5_sB{i}", [128, S5T], F32) for i in range(2)]
        b_sA = [Buf(f"s5_sA{i}") for i in range(2)]
        b_sB = [Buf(f"s5_sB{i}") for i in range(2)]
        t_ = [[P.sb(f"s5_t{j}_{i}", [128, S5T], F32) for i in range(2)] for j in range(4)]
        b_t = [[Buf(f"s5_t{j}_{i}") for i in range(2)] for j in range(4)]
        bre = [P.sb(f"s5_bre{i}", [128, S5T], F32) for i in range(2)]
        bim = [P.sb(f"s5_bim{i}", [128, S5T], F32) for i in range(2)]
        b_bre = [Buf(f"s5_bre{i}") for i in range(2)]
        b_bim = [Buf(f"s5_bim{i}") for i in range(2)]
        zre = [P.sb(f"s5_zre{i}", [128, S5T], F32) for i in range(2)]
        zim = [P.sb(f"s5_zim{i}", [128, S5T], F32) for i in range(2)]
        b_zre = [Buf(f"s5_zre{i}") for i in range(2)]
        b_zim = [Buf(f"s5_zim{i}") for i in range(2)]
        pp = [[P.sb(f"s5_p{j}_{i}", [128, S5T], BF16) for i in range(2)] for j in range(4)]
        b_pp = [[Buf(f"s5_p{j}_{i}") for i in range(2)] for j in range(4)]
        init = P.sb("s5_init", [128, 2, 4], F32)
        itmp = P.sb("s5_itmp", [128, 2, 4], F32)
        b_init = [Buf(f"s5_init{k}") for k in range(4)]
        P.op("dve", lambda e: e.memset(init[:], 0.0), writes=b_init)
        ysb = [P.sb(f"s5_y{i}", [128, S5T], F32) for i in range(2)]
        g1 = [P.sb(f"s5_g1{i}", [128, S5T], F32) for i in range(2)]
        g2 = [P.sb(f"s5_g2{i}", [128, S5T], F32) for i in range(2)]
        b_y = [Buf(f"s5_y{i}") for i in range(2)]
        b_g1 = [Buf(f"s5_g1{i}") for i in range(2)]
        b_g2 = [Buf(f"s5_g2{i}") for i in range(2)]
        toks = []
        it = 0
        for c in range(NCH):
            cs = slice(c * S5T, (c + 1) * S5T)
            ui = c % NB
            P.dma("sp", lambda e, ui=ui, cs=cs: e.dma_start(out=uf[ui][:], in_=u_d[:, cs]), b_uf[ui], writes=[b_uf[ui]])
            P.op("act", lambda e, ui=ui: e.activation(out=ub[ui][:], in_=uf[ui][:], func=AF.Copy),
                 reads=[b_uf[ui]], writes=[b_ub[ui]])
            yps, b_yps = C.psum[6 + c % 2], C.b_ps[6 + c % 2]
            for k in range(4):
                s = it % 2
                pa, b_pa = C.psum[(2 * it) % 6], C.b_ps[(2 * it) % 6]
                pb, b_pb = C.psum[(2 * it + 1) % 6], C.b_ps[(2 * it + 1) % 6]
                it += 1
                ks = slice(k * 128, (k + 1) * 128)
                P.op("pe", lambda e, pa=pa, ks=ks, ui=ui: e.matmul(pa[:], lhsT=btb[:, 0, ks], rhs=ub[ui][:], start=True, stop=True),
                     reads=[b_btb, b_ub[ui]], writes=[b_pa])
                P.op("pe", lambda e, pb=pb, ks=ks, ui=ui: e.matmul(pb[:], lhsT=btb[:, 1, ks], rhs=ub[ui][:], start=True, stop=True),
                     reads=[b_btb, b_ub[ui]], writes=[b_pb])
                P.op("act", lambda e, s=s, pa=pa: e.activation(out=sA[s][:], in_=pa[:], func=AF.Copy), reads=[b_pa], writes=[b_sA[s]])
                P.op("act", lambda e, s=s, pb=pb: e.activation(out=sB[s][:], in_=pb[:], func=AF.Copy), reads=[b_pb], writes=[b_sB[s]])
                P.op("pool", lambda e, s=s, k=k: e.tensor_tensor(out=t_[0][s][:], in0=sA[s][:], in1=cosT[:, k, :], op=ALU.mult),
                     reads=[b_sA[s], b_tab], writes=[b_t[0][s]])
                P.op("dve", lambda e, s=s, k=k: e.tensor_tensor(out=t_[1][s][:], in0=sB[s][:], in1=sinT[:, k, :], op=ALU.mult),
                     reads=[b_sB[s], b_tab], writes=[b_t[1][s]])
                P.op("pool", lambda e, s=s, k=k: e.tensor_tensor(out=t_[2][s][:], in0=sB[s][:], in1=cosT[:, k, :], op=ALU.mult),
                     reads=[b_sB[s], b_tab], writes=[b_t[2][s]])
                P.op("dve", lambda e, s=s, k=k: e.tensor_tensor(out=t_[3][s][:], in0=sA[s][:], in1=sinT[:, k, :], op=ALU.mult),
                     reads=[b_sA[s], b_tab], writes=[b_t[3][s]])
                P.op("pool", lambda e, s=s: e.tensor_tensor(out=bre[s][:], in0=t_[0][s][:], in1=t_[1][s][:], op=ALU.add),
                     reads=[b_t[0][s], b_t[1][s]], writes=[b_bre[s]])
                P.op("pool", lambda e, s=s: e.tensor_tensor(out=bim[s][:], in0=t_[2][s][:], in1=t_[3][s][:], op=ALU.subtract),
                     reads=[b_t[2][s], b_t[3][s]], writes=[b_bim[s]])
                P.op("dve", lambda e, s=s, k=k: e.tensor_tensor_scan(out=zre[s][:], data0=rT[:, k, :], data1=bre[s][:],
                                                                      initial=init[:, 0, k:k + 1], op0=ALU.mult, op1=ALU.add),
                     reads=[b_tab, b_bre[s], b_init[k]], writes=[b_zre[s]])
                P.op("dve", lambda e, s=s, k=k: e.tensor_tensor_scan(out=zim[s][:], data0=rT[:, k, :], data1=bim[s][:],
                                                                      initial=init[:, 1, k:k + 1], op0=ALU.mult, op1=ALU.add),
                     reads=[b_tab, b_bim[s], b_init[k]], writes=[b_zim[s]])
                zlr, zli = zre[s][:, S5T - 1:S5T], zim[s][:, S5T - 1:S5T]
                cT_, sT_, nsT_ = sm[:, CT_, k:k + 1], sm[:, ST_, k:k + 1], sm[:, NST_, k:k + 1]
                P.op("dve", lambda e, k=k, zlr=zlr, cT_=cT_: e.tensor_tensor(out=itmp[:, 0, k:k + 1], in0=zlr, in1=cT_, op=ALU.mult),
                     reads=[b_zre[s], b_sm], writes=[b_init[k]])
                P.op("dve", lambda e, k=k, zlr=zlr, sT_=sT_: e.tensor_tensor(out=itmp[:, 1, k:k + 1], in0=zlr, in1=sT_, op=ALU.mult),
                     reads=[b_zre[s], b_sm], writes=[b_init[k]])
                P.op("dve", lambda e, k=k, zli=zli, nsT_=nsT_: e.scalar_tensor_tensor(
                    out=init[:, 0, k:k + 1], in0=zli, scalar=nsT_, in1=itmp[:, 0, k:k + 1], op0=ALU.mult, op1=ALU.add),
                    reads=[b_zim[s], b_sm], writes=[b_init[k]])
                P.op("dve", lambda e, k=k, zli=zli, cT_=cT_: e.scalar_tensor_tensor(
                    out=init[:, 1, k:k + 1], in0=zli, scalar=cT_, in1=itmp[:, 1, k:k + 1], op0=ALU.mult, op1=ALU.add),
                    reads=[b_zim[s], b_sm], writes=[b_init[k]])
                P.op("pool", lambda e, s=s, k=k: e.tensor_tensor(out=pp[0][s][:], in0=zre[s][:], in1=cosT[:, k, :], op=ALU.mult),
                     reads=[b_zre[s], b_tab], writes=[b_pp[0][s]])
                P.op("pool", lambda e, s=s, k=k: e.tensor_tensor(out=pp[1][s][:], in0=zim[s][:], in1=sinT[:, k, :], op=ALU.mult),
                     reads=[b_zim[s], b_tab], writes=[b_pp[1][s]])
                P.op("dve", lambda e, s=s, k=k: e.tensor_tensor(out=pp[2][s][:], in0=zre[s][:], in1=sinT[:, k, :], op=ALU.mult),
                     reads=[b_zre[s], b_tab], writes=[b_pp[2][s]])
                P.op("pool", lambda e, s=s, k=k: e.tensor_tensor(out=pp[3][s][:], in0=zim[s][:], in1=cosT[:, k, :], op=ALU.mult),
                     reads=[b_zim[s], b_tab], writes=[b_pp[3][s]])
                for j, li in enumerate((0, 1, 2, 2)):
                    P.op("pe", lambda e, yps=yps, li=li, k=k, j=j, s=s: e.matmul(
                        yps[:], lhsT=L[:, li, k, :], rhs=pp[j][s][:], start=(k == 0 and j == 0), stop=(k == 3 and j == 3)),
                        reads=[b_L, b_pp[j][s]], writes=[b_yps])
            q = c % 2
            P.op("dve", lambda e, q=q, ui=ui, yps=yps: e.scalar_tensor_tensor(
                out=ysb[q][:], in0=uf[ui][:], scalar=dcol, in1=yps[:], op0=ALU.mult, op1=ALU.add),
                reads=[b_uf[ui], b_par, b_yps], writes=[b_y[q]])
            P.op("pool", lambda e, q=q: e.tensor_tensor(out=g1[q][:], in0=ysb[q][:], in1=ysb[q][:], op=ALU.mult),
                 reads=[b_y[q]], writes=[b_g1[q]])
            P.op("pool", lambda e, q=q: e.tensor_scalar(out=g1[q][:], in0=g1[q][:], scalar1=0.044715, scalar2=1.0,
                                                        op0=ALU.mult, op1=ALU.add),
                 reads=[b_g1[q]], writes=[b_g1[q]])
            P.op("pool", lambda e, q=q: e.tensor_tensor(out=g1[q][:], in0=g1[q][:], in1=ysb[q][:], op=ALU.mult),
                 reads=[b_g1[q], b_y[q]], writes=[b_g1[q]])
            P.op("act", lambda e, q=q: e.activation(out=g2[q][:], in_=g1[q][:], func=AF.Sigmoid, scale=1.5957691216057308),
                 reads=[b_g1[q]], writes=[b_g2[q]])
            P.op("pool", lambda e, q=q: e.tensor_tensor(out=g2[q][:], in0=g2[q][:], in1=ysb[q][:], op=ALU.mult),
                 reads=[b_g2[q], b_y[q]], writes=[b_g2[q]])
            if debug == 1:
                toks.append(P.dma("sp", lambda e, q=q, cs=cs: e.dma_start(out=y_d[:, cs], in_=ysb[q][:]), b_g2[q], reads=[b_g2[q], b_y[q]]))
                continue
            toks.append(P.dma("sp", lambda e, q=q, cs=cs: e.dma_start(out=y_d[:, cs], in_=g2[q][:]), b_g2[q], reads=[b_g2[q]]))
        P.finish(toks[-2:])
        P.emit()
    return nc


def s5_host_layout(lam_re, lam_im, log_dt, b_re, b_im, c_re, c_im, d, core):
    g0 = core * 8
    par = np.zeros((128, 16), np.float32)
    bt = np.zeros((128, 2, 512), np.float32)
    ct = np.zeros((128, 2, 4, 128), np.float32)
    for gl in range(8):
        g = g0 + gl
        k, p0 = gl // 2, (gl % 2) * 64
        par[p0:p0 + 64, 0 + k] = lam_re[g]
        par[p0:p0 + 64, 4 + k] = lam_im[g]
        par[p0:p0 + 64, 8 + k] = log_dt[g]
        bt[16 * gl:16 * gl + 16, 0, 64 * gl:64 * gl + 64] = b_re[g].T
        bt[16 * gl:16 * gl + 16, 1, 64 * gl:64 * gl + 64] = b_im[g].T
        ct[p0:p0 + 64, 0, k, 16 * gl:16 * gl + 16] = c_re[g].T
        ct[p0:p0 + 64, 1, k, 16 * gl:16 * gl + 16] = c_im[g].T
    par[:, 12] = d[core * 128:(core + 1) * 128]
    return par, bt, ct


def emit_glu(P, C, xT, b_x, gy_dram, wglu):
    with ExitStack() as es:
        gyb = P.sb("glu_gyb", [128, KD, NT], BF16, es)
        b_gyb = [Buf(f"glu_gyb{t}") for t in range(NTT)]
        st = [P.sb(f"glu_st{i}", [128, TT], F32, es) for i in range(2)]
        b_st = [Buf(f"glu_st{i}") for i in range(2)]
        gv = gy_dram.rearrange("(k p) t -> p k t", p=128)
        n = 0
        for tt in range(NTT):
            ts = slice(tt * TT, (tt + 1) * TT)
            for k in range(KD):
                s = n % 2
                n += 1
                P.dma("sp", lambda e, s=s, k=k, ts=ts: e.dma_start(out=st[s][:], in_=gv[:, k, ts]), b_st[s], writes=[b_st[s]])
                P.op("pool", lambda e, s=s, k=k, ts=ts: e.tensor_copy(out=gyb[:, k, ts], in_=st[s][:]),
                     reads=[b_st[s]], writes=[b_gyb[tt]])
        w_f = [P.sb(f"glu_wf{i}", [128, KD, 256], F32, es) for i in range(2)]
        w_b = [P.sb(f"glu_wb{i}", [128, KD, 256], BF16, es) for i in range(2)]
        b_wf = [Buf(f"glu_wf{i}") for i in range(2)]
        b_wb = [Buf(f"glu_wb{i}") for i in range(2)]
        sg = [P.sb(f"glu_sg{i}", [128, TT], F32, es) for i in range(2)]
        b_sg = [Buf(f"glu_sg{i}") for i in range(2)]
        wv = wglu.rearrange("(k p) n -> p k n", p=128)
        q = 0
        for m in range(KD):
            s = m % 2
            P.dma("sp", lambda e, s=s, m=m: e.dma_start(out=w_f[s][:, :, 0:128], in_=wv[:, :, m * 128:(m + 1) * 128]),
                  b_wf[s], writes=[b_wf[s]])
            P.dma("sp", lambda e, s=s, m=m: e.dma_start(out=w_f[s][:, :, 128:256], in_=wv[:, :, D + m * 128:D + (m + 1) * 128]),
                  b_wf[s], writes=[])
            b_wf[s].w = ("d", b_wf[s], b_wf[s].dcount)
            P.op("pool", lambda e, s=s: e.tensor_copy(out=w_b[s][:], in_=w_f[s][:]), reads=[b_wf[s]], writes=[b_wb[s]])
            for tt in range(NTT):
                ts = slice(tt * TT, (tt + 1) * TT)
                pv, b_pv = C.next_ps()
                pg, b_pg = C.next_ps()
                for k in range(KD):
                    P.op("pe", lambda e, pv=pv, s=s, k=k, ts=ts: e.matmul(pv[:], lhsT=w_b[s][:, k, 0:128], rhs=gyb[:, k, ts],
                                                                         start=(k == 0), stop=(k == KD - 1)),
                         reads=[b_wb[s], b_gyb[tt]], writes=[b_pv])
                for k in range(KD):
                    P.op("pe", lambda e, pg=pg, s=s, k=k, ts=ts: e.matmul(pg[:], lhsT=w_b[s][:, k, 128:256], rhs=gyb[:, k, ts],
                                                                         start=(k == 0), stop=(k == KD - 1)),
                         reads=[b_wb[s], b_gyb[tt]], writes=[b_pg])
                qq = q % 2
                q += 1
                P.op("act", lambda e, qq=qq, pg=pg: e.activation(out=sg[qq][:], in_=pg[:], func=AF.Sigmoid),
                     reads=[b_pg], writes=[b_sg[qq]])
                P.op("dve", lambda e, qq=qq, pv=pv: e.tensor_tensor(out=sg[qq][:], in0=pv[:], in1=sg[qq][:], op=ALU.mult),
                     reads=[b_pv, b_sg[qq]], writes=[b_sg[qq]])
                P.op("pool", lambda e, qq=qq, m=m, ts=ts: e.tensor_tensor(out=xT[:, m, ts], in0=xT[:, m, ts], in1=sg[qq][:], op=ALU.add),
                     reads=[b_sg[qq], b_x[m][tt]], writes=[b_x[m][tt]])
    P.barrier()


def build_glu_ffn_prog():
    nc = bass.Bass("TRN2", target_bir_lowering=False)
    x = nc.dram_tensor("xT", [D, NT], F32, kind="ExternalInput").ap()
    gy = nc.dram_tensor("gyT", [D, NT], F32, kind="ExternalInput").ap()
    wglu = nc.dram_tensor("wglu", [D, 2 * D], F32, kind="ExternalInput").ap()
    wgu = nc.dram_tensor("wgu", [D, 2 * DFF], F32, kind="ExternalInput").ap()
    wd = nc.dram_tensor("wd", [DFF, D], F32, kind="ExternalInput").ap()
    gain = nc.dram_tensor("gain", [128, KD], F32, kind="ExternalInput").ap()
    y = nc.dram_tensor("yT", [D, NT], F32, kind="ExternalOutput").ap()
    with ExitStack() as es:
        P = Prog(nc, es)
        C = Ctx(P)
        xT = P.sb("xT_sb", [128, KD, NT], F32)
        b_x = [[Buf(f"x{k}_{t}") for t in range(NTT)] for k in range(KD)]
        g_sb = P.sb("gain_sb", [128, KD], F32)
        b_g = Buf("gain")
        P.dma("sp", lambda e: e.dma_start(out=g_sb[:], in_=gain[:]), b_g, writes=[b_g])
        load_xT(P, xT, b_x, x)
        emit_glu(P, C, xT, b_x, gy, wglu)
        emit_ffn(P, C, xT, b_x, wgu, wd, g_sb, b_g, 0)
        store_xT(P, xT, b_x, y)
        P.emit()
    return nc


_PROGS = {}


def _prog(name, builder):
    if name not in _PROGS:
        _PROGS[name] = builder()
    return _PROGS[name]


def _run(name, builder, in_maps):
    import time
    t0 = time.time()
    nc = _prog(name, builder)
    t1 = time.time()
    import os
    r = run_bass_kernel_spmd(nc, in_maps, core_ids=list(range(NCORES)), **({"trace": True} if os.environ.get("KPROF") else {}))
    res = r.results
    nb = sum(v.nbytes for m in in_maps for v in m.values())
    print(f"[launch {name}] build {t1 - t0:.1f}s run {time.time() - t1:.1f}s in_bytes {nb / 1e6:.0f}MB exec_ns {r.exec_time_ns}", flush=True)
    return res


def run_s5_layer(xT_parts, j, i, inp):
    gm = col_layout(inp["norm_mix"][i])
    res = _run("prenorm", build_prenorm_prog, [{"xT": xT_parts[c], "gain": gm} for c in range(NCORES)])
    h_full = from_core_T([r["hT"] for r in res])
    tau = np.tile(np.arange(S5T, dtype=np.float32)[None], (128, 1))
    maps = []
    for c in range(NCORES):
        par, bt, ct = s5_host_layout(inp["s5_lambda_re"][j], inp["s5_lambda_im"][j], inp["s5_log_dt"][j],
                                     inp["s5_b_re"][j], inp["s5_b_im"][j], inp["s5_c_re"][j], inp["s5_c_im"][j],
                                     inp["s5_d"][j], c)
        maps.append({"uT": np.ascontiguousarray(h_full[:, c * 128:(c + 1) * 128].T), "par": par, "bt": bt, "ct": ct, "tau": tau})
    res = _run("s5", build_s5_prog, maps)
    gy_full = np.concatenate([r["gyT"] for r in res], axis=0).T
    gf = col_layout(inp["norm_ffn"][i])
    maps = [{"xT": xT_parts[c], "gyT": to_core_T(gy_full, c), "wglu": inp["s5_w_glu"][j],
             "wgu": inp["ffn_w_gate_up"][i], "wd": inp["ffn_w_down"][i], "gain": gf} for c in range(NCORES)]
    res = _run("glu_ffn", build_glu_ffn_prog, maps)
    return [r["yT"] for r in res]


NH = 16
HD = 64
NIH = 8
PROJ = 3 * D + NIH * HD + HD + NIH


def build_dsa_proj_prog():
    nc = bass.Bass("TRN2", target_bir_lowering=False)
    x = nc.dram_tensor("xT", [D, NT], F32, kind="ExternalInput").ap()
    w_in = nc.dram_tensor("w_in", [D, PROJ], F32, kind="ExternalInput").ap()
    gain = nc.dram_tensor("gain", [128, KD], F32, kind="ExternalInput").ap()
    qk_g = nc.dram_tensor("qk_gain", [128, 2], F32, kind="ExternalInput").ap()
    cs_d = nc.dram_tensor("cossin", [128, 2, NT], F32, kind="ExternalInput").ap()
    cm_d = nc.dram_tensor("cmat", [128, 2, 128], F32, kind="ExternalInput").ap()
    qT_d = nc.dram_tensor("qT", [D, NT], BF16, kind="ExternalOutput").ap()
    kT_d = nc.dram_tensor("kT", [D, NT], BF16, kind="ExternalOutput").ap()
    v_d = nc.dram_tensor("v", [NT, D], BF16, kind="ExternalOutput").ap()
    qiT_d = nc.dram_tensor("qiT", [NIH * HD, NT], BF16, kind="ExternalOutput").ap()
    kiT_d = nc.dram_tensor("kiT", [HD, NT], BF16, kind="ExternalOutput").ap()
    w_d = nc.dram_tensor("w", [NT, NIH], F32, kind="ExternalOutput").ap()
    with ExitStack() as es:
        P = Prog(nc, es)
        C = Ctx(P)
        xT = P.sb("xT_sb", [128, KD, NT], F32)
        b_x = [[Buf(f"x{k}_{t}") for t in range(NTT)] for k in range(KD)]
        g_sb = P.sb("gain_sb", [128, KD], F32)
        qkg = P.sb("qkg_sb", [128, 2], F32)
        cs = P.sb("cs_sb", [128, 2, NT], F32)
        cm = P.sb("cm_sb", [128, 2, 128], F32)
        b_g, b_qkg, b_cs, b_cm = Buf("gain"), Buf("qkg"), Buf("cs"), Buf("cm")
        P.dma("sp", lambda e: e.dma_start(out=g_sb[:], in_=gain[:]), b_g, writes=[b_g])
        P.dma("sp", lambda e: e.dma_start(out=qkg[:], in_=qk_g[:]), b_qkg, writes=[b_qkg])
        P.dma("sp", lambda e: e.dma_start(out=cm[:], in_=cm_d[:]), b_cm, writes=[b_cm])
        P.dma("sp", lambda e: e.dma_start(out=cs[:, 0, :], in_=cs_d[:, 0, :]), b_cs, writes=[b_cs])
        P.dma("sp", lambda e: e.dma_start(out=cs[:, 1, :], in_=cs_d[:, 1, :]), b_cs, writes=[])
        b_cs.w = ("d", b_cs, b_cs.dcount)
        load_xT(P, xT, b_x, x)
        bones = P.sb("bones_bf", [128, 128], BF16)
        b_bones = Buf("bones")
        P.op("dve", lambda e: e.tensor_copy(out=bones[:], in_=cm[:, 0, :]), reads=[b_cm], writes=[b_bones])
        P.op("dve", lambda e: e.tensor_scalar(out=qkg[:, 0:1], in0=qkg[:, 0:1], scalar1=HD ** -0.5, scalar2=None, op0=ALU.mult),
             reads=[b_qkg], writes=[b_qkg])
        hT = P.sb("hT_sb", [128, KD, NT], BF16)
        b_h = [Buf(f"h{t}") for t in range(NTT)]
        emit_rmsnorm_T(P, C, xT, b_x, g_sb, b_g, 0, hT, b_h, list(range(NTT)), es, "pn")
        wv_ = w_in.rearrange("(k p) n -> p k n", p=128)
        w_f = [P.sb(f"pj_wf{i}", [128, KD, 128], F32) for i in range(2)]
        w_b = [P.sb(f"pj_wb{i}", [128, KD, 128], BF16) for i in range(2)]
        b_wf = [Buf(f"pj_wf{i}") for i in range(2)]
        b_wb = [Buf(f"pj_wb{i}") for i in range(2)]
        sq = [P.sb(f"pj_sq{i}", [128, TT], BF16) for i in range(2)]
        rs = [P.sb(f"pj_rs{i}", [128, TT], F32) for i in range(2)]
        tf = [P.sb(f"pj_t{i}", [128, TT], F32) for i in range(2)]
        o1 = [P.sb(f"pj_o1{i}", [128, TT], F32) for i in range(2)]
        o2 = [P.sb(f"pj_o2{i}", [128, TT], F32) for i in range(2)]
        ob = [P.sb(f"pj_ob{i}", [128, TT], BF16) for i in range(2)]
        b_sq = [Buf(f"pj_sq{i}") for i in range(2)]
        b_rs = [Buf(f"pj_rs{i}") for i in range(2)]
        b_tf = [Buf(f"pj_t{i}") for i in range(2)]
        b_o1 = [Buf(f"pj_o1{i}") for i in range(2)]
        b_o2 = [Buf(f"pj_o2{i}") for i in range(2)]
        b_ob = [Buf(f"pj_ob{i}") for i in range(2)]
        toks = []
        tiles = []
        for m in range(8):
            tiles.append((m * 128, 128, "norm", qT_d, m * 128, 0))
        for m in range(8):
            tiles.append((D + m * 128, 128, "norm", kT_d, m * 128, 1))
        for m in range(4):
            tiles.append((3 * D + m * 128, 128, "plain", qiT_d, m * 128, None))
        tiles.append((3 * D + NIH * HD, 64, "normnog", kiT_d, 0, None))
        it = 0
        for ti, (c0, M, kind, od, r0, gc) in enumerate(tiles):
            s = ti % 2
            P.dma("sp", lambda e, s=s, c0=c0, M=M: e.dma_start(out=w_f[s][:, :, 0:M], in_=wv_[:, :, c0:c0 + M]),
                  b_wf[s], writes=[b_wf[s]])
            P.op("pool", lambda e, s=s, M=M: e.tensor_copy(out=w_b[s][:, :, 0:M], in_=w_f[s][:, :, 0:M]),
                 reads=[b_wf[s]], writes=[b_wb[s]])
            for tt in range(NTT):
                ts = slice(tt * TT, (tt + 1) * TT)
                u = it % 2
                it += 1
                ps, b_ps = C.next_ps()
                for k in range(KD):
                    P.op("pe", lambda e, ps=ps, s=s, k=k, ts=ts, M=M: e.matmul(ps[0:M, :], lhsT=w_b[s][:, k, 0:M], rhs=hT[:, k, ts],
                                                                              start=(k == 0), stop=(k == KD - 1)),
                         reads=[b_wb[s], b_h[tt]], writes=[b_ps])
                if kind == "plain":
                    P.op("act", lambda e, u=u, ps=ps, M=M: e.activation(out=tf[u][0:M, :], in_=ps[0:M, :], func=AF.Copy),
                         reads=[b_ps], writes=[b_tf[u]])
                else:
                    P.op("act", lambda e, u=u, ps=ps, M=M: e.activation(out=sq[u][0:M, :], in_=ps[0:M, :], func=AF.Square),
                         reads=[b_ps], writes=[b_sq[u]])
                    p2, b_p2 = C.next_ps()
                    P.op("pe", lambda e, p2=p2, u=u, M=M: e.matmul(p2[0:M, :], lhsT=bones[0:M, 0:M], rhs=sq[u][0:M, :], start=True, stop=True),
                         reads=[b_bones, b_sq[u]], writes=[b_p2])
                    P.op("act", lambda e, u=u, p2=p2, M=M: e.activation(out=rs[u][0:M, :], in_=p2[0:M, :], func=AF.Sqrt, scale=1.0 / HD,
                                                                   bias=C.eps_col[0:M, :]),
                         reads=[b_p2, C.b_ones], writes=[b_rs[u]])
                    P.op("dve", lambda e, u=u, M=M: e.reciprocal(out=rs[u][0:M, :], in_=rs[u][0:M, :]), reads=[b_rs[u]], writes=[b_rs[u]])
                    if kind == "norm":
                        P.op("dve", lambda e, u=u, ps=ps, gc=gc, M=M: e.scalar_tensor_tensor(
                            out=tf[u][0:M, :], in0=ps[0:M, :], scalar=qkg[0:M, gc:gc + 1], in1=rs[u][0:M, :], op0=ALU.mult, op1=ALU.mult),
                            reads=[b_ps, b_qkg, b_rs[u]], writes=[b_tf[u]])
                    else:
                        P.op("dve", lambda e, u=u, ps=ps, M=M: e.tensor_tensor(out=tf[u][0:M, :], in0=ps[0:M, :], in1=rs[u][0:M, :], op=ALU.mult),
                             reads=[b_ps, b_rs[u]], writes=[b_tf[u]])
                p3, b_p3 = C.next_ps()
                P.op("pe", lambda e, p3=p3, u=u, M=M: e.matmul(p3[0:M, :], lhsT=cm[0:M, 1, 0:M], rhs=tf[u][0:M, :], start=True, stop=True),
                     reads=[b_cm, b_tf[u]], writes=[b_p3])
                P.op("pool", lambda e, u=u, ts=ts, M=M: e.tensor_tensor(out=o1[u][0:M, :], in0=tf[u][0:M, :], in1=cs[0:M, 0, ts], op=ALU.mult),
                     reads=[b_tf[u], b_cs], writes=[b_o1[u]])
                P.op("dve", lambda e, u=u, ts=ts, p3=p3, M=M: e.tensor_tensor(out=o2[u][0:M, :], in0=p3[0:M, :], in1=cs[0:M, 1, ts], op=ALU.mult),
                     reads=[b_p3, b_cs], writes=[b_o2[u]])
                P.op("pool", lambda e, u=u, M=M: e.tensor_tensor(out=ob[u][0:M, :], in0=o1[u][0:M, :], in1=o2[u][0:M, :], op=ALU.add),
                     reads=[b_o1[u], b_o2[u]], writes=[b_ob[u]])
                toks.append(P.dma("sp", lambda e, u=u, od=od, r0=r0, ts=ts, M=M: e.dma_start(out=od[r0:r0 + M, ts], in_=ob[u][0:M, :]),
                                  b_ob[u], reads=[b_ob[u]]))
        wvf = [P.sb(f"pj_vf{i}", [128, KD, 512], F32) for i in range(1)]
        wvb = [P.sb(f"pj_vb{i}", [128, KD, 512], BF16) for i in range(2)]
        b_wvf = [Buf("pj_vf0")]
        b_wvb = [Buf(f"pj_vb{i}") for i in range(2)]
        vo = [P.sb(f"pj_vo{i}", [128, 512], BF16) for i in range(2)]
        b_vo = [Buf(f"pj_vo{i}") for i in range(2)]
        for hf in range(2):
            P.dma("sp", lambda e, hf=hf: e.dma_start(out=wvf[0][:], in_=wv_[:, :, 2 * D + hf * 512:2 * D + (hf + 1) * 512]),
                  b_wvf[0], writes=[b_wvf[0]])
            P.op("pool", lambda e, hf=hf: e.tensor_copy(out=wvb[hf][:], in_=wvf[0][:]), reads=[b_wvf[0]], writes=[b_wvb[hf]])
        ww_f = P.sb("pj_wwf", [128, KD, NIH], F32)
        ww_b = P.sb("pj_wwb", [128, KD, NIH], BF16)
        b_wwf, b_wwb = Buf("pj_wwf"), Buf("pj_wwb")
        P.dma("sp", lambda e: e.dma_start(out=ww_f[:], in_=wv_[:, :, PROJ - NIH:PROJ]), b_wwf, writes=[b_wwf])
        P.op("pool", lambda e: e.tensor_copy(out=ww_b[:], in_=ww_f[:]), reads=[b_wwf], writes=[b_wwb])
        wo_sb = P.sb("pj_wo", [128, NT // 128, NIH], F32)
        b_wo = Buf("pj_wo")
        n = 0
        for blk in range(NT // 128):
            tt = blk // 4
            bs = slice(blk * 128, (blk + 1) * 128)
            for hf in range(2):
                u = n % 2
                n += 1
                ps, b_ps = C.next_ps()
                for k in range(KD):
                    P.op("pe", lambda e, ps=ps, k=k, bs=bs, hf=hf: e.matmul(ps[:], lhsT=hT[:, k, bs], rhs=wvb[hf][:, k, :],
                                                                           start=(k == 0), stop=(k == KD - 1)),
                         reads=[b_wvb[hf], b_h[tt]], writes=[b_ps])
                P.op("act", lambda e, u=u, ps=ps: e.activation(out=vo[u][:], in_=ps[:], func=AF.Copy), reads=[b_ps], writes=[b_vo[u]])
                toks.append(P.dma("sp", lambda e, u=u, bs=bs, hf=hf: e.dma_start(out=v_d[bs, hf * 512:(hf + 1) * 512], in_=vo[u][:]),
                                  b_vo[u], reads=[b_vo[u]]))
            ps, b_ps = C.next_ps()
            for k in range(KD):
                P.op("pe", lambda e, ps=ps, k=k, bs=bs: e.matmul(ps[:, 0:NIH], lhsT=hT[:, k, bs], rhs=ww_b[:, k, :],
                                                                 start=(k == 0), stop=(k == KD - 1)),
                     reads=[b_wwb, b_h[tt]], writes=[b_ps])
            P.op("act", lambda e, ps=ps, blk=blk: e.activation(out=wo_sb[:, blk, :], in_=ps[:, 0:NIH], func=AF.Copy,
                                                               scale=(NIH ** -0.5) * (HD ** -0.5)),
                 reads=[b_ps], writes=[b_wo])
        toks.append(P.dma("sp", lambda e: e.dma_start(out=w_d.rearrange("(b p) h -> p b h", p=128), in_=wo_sb[:]), b_wo, reads=[b_wo]))
        P.finish(toks)
        P.emit()
    return nc


def rope_consts(core):
    blocks = np.arange(SEQ // 128)[core::NCORES]
    pos = (blocks[:, None] * 128 + np.arange(128)[None, :]).reshape(-1).astype(np.float32)
    inv_freq = (10000.0 ** (-np.arange(0, HD, 2, dtype=np.float32) / HD)).astype(np.float32)
    ang = pos[None, :] * inv_freq[:, None]
    cos, sin = np.cos(ang).astype(np.float32), np.sin(ang).astype(np.float32)
    cs = np.empty((128, 2, NT), np.float32)
    for p in range(128):
        cs[p, 0] = cos[p % 32]
        cs[p, 1] = sin[p % 32]
    return cs


def const_mats():
    cm = np.zeros((128, 2, 128), np.float32)
    for p in range(128):
        for m in range(128):
            if p // 64 == m // 64:
                cm[p, 0, m] = 1.0
    for m in range(128):
        if (m % 64) < 32:
            cm[m + 32, 1, m] = -1.0
        else:
            cm[m - 32, 1, m] = 1.0
    return cm


TOPK = 256
NEG_SEL = -1.0e30
NEG_MASK = -2.0e30


def build_dsa_attn_prog(nblk=NT // 128):
    U8 = mybir.dt.uint8
    NBIS = 16
    nc = bass.Bass("TRN2", target_bir_lowering=False)
    x_d = nc.dram_tensor("xT", [D, NT], F32, kind="ExternalInput").ap()
    qT_d = nc.dram_tensor("qT", [D, NT], BF16, kind="ExternalInput").ap()
    qiT_d = nc.dram_tensor("qiT", [NIH * HD, NT], BF16, kind="ExternalInput").ap()
    w_d = nc.dram_tensor("wq", [128, NT // 128, NIH], F32, kind="ExternalInput").ap()
    kT_d = nc.dram_tensor("kTf", [D, SEQ], BF16, kind="ExternalInput").ap()
    v_d = nc.dram_tensor("vf", [SEQ, D], BF16, kind="ExternalInput").ap()
    kiT_d = nc.dram_tensor("kiTf", [HD, SEQ], BF16, kind="ExternalInput").ap()
    pen_d = nc.dram_tensor("pen", [128, 1024], F32, kind="ExternalInput").ap()
    id_d = nc.dram_tensor("ident", [128, 128], F32, kind="ExternalInput").ap()
    wo_d = nc.dram_tensor("w_o", [D, D], F32, kind="ExternalInput").ap()
    y_d = nc.dram_tensor("yT", [D, NT], F32, kind="ExternalOutput").ap()
    qT_v = qT_d.rearrange("(h p) t -> p h t", p=64)
    qiT_v = qiT_d.rearrange("(h p) t -> p h t", p=64)
    kT_v = kT_d.rearrange("(pr p) t -> p pr t", p=128)
    qT_pv = qT_d.rearrange("(pr two p) t -> two p pr t", two=2, p=64)
    x_v = x_d.rearrange("(k p) t -> p k t", p=128)
    y_v = y_d.rearrange("(k p) t -> p k t", p=128)
    wo_v = wo_d.rearrange("(h p) n -> p h n", p=64)
    with ExitStack() as es:
        P = Prog(nc, es)
        ones = P.sb("c_ones", [128, 64], BF16)
        half_c = P.sb("c_half", [128, 1], F32)
        b_c = Buf("consts")
        P.op("pool", lambda e: e.memset(ones[:], 1.0), writes=[b_c])
        P.op("pool", lambda e: e.memset(half_c[:], 0.5), writes=[b_c])
        pen = P.sb("pen_sb", [128, 1024], F32)
        idf = P.sb("id_f", [128, 128], F32)
        idb = P.sb("id_b", [128, 128], BF16)
        wq = P.sb("wq_sb", [128, NT // 128, NIH], F32)
        b_pen, b_id, b_wq = Buf("pen"), Buf("ident"), Buf("wq")
        P.dma("sp", lambda e: e.dma_start(out=pen[:], in_=pen_d[:]), b_pen, writes=[b_pen])
        P.dma("sp", lambda e: e.dma_start(out=idf[:], in_=id_d[:]), b_id, writes=[b_id])
        P.dma("sp", lambda e: e.dma_start(out=wq[:], in_=w_d[:]), b_wq, writes=[b_wq])
        P.op("pool", lambda e: e.tensor_copy(out=idb[:], in_=idf[:]), reads=[b_id], writes=[b_id])
        wob = P.sb("wo_b", [64, NH, D], BF16)
        wof = P.sb("wo_f", [64, NH, 64], F32)
        b_wob, b_wof = Buf("wo_b"), Buf("wo_f")
        for m in range(2 * KD):
            P.dma("sp", lambda e, m=m: e.dma_start(out=wof[:], in_=wo_v[:, :, m * 64:(m + 1) * 64]), b_wof, writes=[b_wof])
            P.op("pool", lambda e, m=m: e.tensor_copy(out=wob[:, :, m * 64:(m + 1) * 64], in_=wof[:]), reads=[b_wof], writes=[b_wob])
        score = P.sb("score", [128, SEQ], F32)
        b_score = Buf("score")
        junk = P.sb("junk", [128, SEQ], U8)
        b_junk = Buf("junk")
        maskT = P.sb("maskT", [128, SEQ // 128, 128], U8)
        b_maskT = Buf("maskT")
        qbd = P.sb("qbd", [128, NH // 2, 256], BF16)
        qih = P.sb("qih", [64, NIH, 128], BF16)
        b_qh, b_qih = Buf("qh"), Buf("qih")
        P.op("pool", lambda e: e.memset(qbd[:], 0.0), writes=[b_qh])
        kib = [P.sb(f"kib{i}", [64, 512], BF16) for i in range(2)]
        b_kib = [Buf(f"kib{i}") for i in range(2)]
        tmp = [P.sb(f"itmp{i}", [128, 512], F32) for i in range(2)]
        b_tmp = [Buf(f"itmp{i}") for i in range(2)]
        m8 = P.sb("m8", [128, 8], F32)
        bis = P.sb("bis", [128, 8], F32)
        b_bis = Buf("bis")
        LO, HI, MID, CNT, GE, D1, D2 = (bis[:, j:j + 1] for j in range(7))
        mk = [P.sb(f"mk{i}", [128, 512], BF16) for i in range(2)]
        b_mk = [Buf(f"mk{i}") for i in range(2)]
        kTc = [P.sb(f"kTc{i}", [128, 4, 256], BF16) for i in range(2)]
        vc = [P.sb(f"vc{i}", [128, 2, 512], BF16) for i in range(2)]
        b_kTc = [Buf(f"kTc{i}") for i in range(2)]
        b_vc = [Buf(f"vc{i}") for i in range(2)]
        pT = [P.sb(f"pT{i}", [128, 512], BF16) for i in range(3)]
        b_pT = [Buf(f"pT{i}") for i in range(3)]
        attn = P.sb("attn", [64, NH, 128], BF16)
        b_attn = Buf("attn")
        dsb = [P.sb(f"dsb{i}", [64, 512], F32) for i in range(2)]
        b_dsb = [Buf(f"dsb{i}") for i in range(2)]
        xq = P.sb("xq", [128, KD, 128], F32)
        b_xq = Buf("xq")
        psA = [P.ps(f"psA{i}", [128, 512], F32) for i in range(3)]
        b_psA = [Buf(f"psA{i}") for i in range(3)]
        psT = P.ps("psT", [128, 1024], BF16)
        b_psT = Buf("psT")
        acc = [P.ps(f"acc{i}", [128, 512], F32) for i in range(2)]
        b_acc = [Buf(f"acc{i}") for i in range(2)]
        den = [P.ps(f"den{i}", [128, 512], F32) for i in range(2)]
        b_den = [Buf(f"den{i}") for i in range(2)]
        rr = {"a": 0, "kib": 0, "tmp": 0, "mk": 0, "kv": 0, "pT": 0}

        def nxt(key, n):
            v = rr[key]
            rr[key] = (v + 1) % n
            return v

        def phase_A(i):
            qs = slice(i * 128, (i + 1) * 128)
            Lk = 1024 * (i + 1)
            P.dma("sp", lambda e: e.dma_start(out=qih[:], in_=qiT_v[:, :, qs]), b_qih, writes=[b_qih])
            for j in range(Lk // 512):
                cs_ = slice(j * 512, (j + 1) * 512)
                kb = nxt("kib", 2)
                P.dma("sp", lambda e, kb=kb, cs_=cs_: e.dma_start(out=kib[kb][:], in_=kiT_d[:, cs_]), b_kib[kb], writes=[b_kib[kb]])
                for h in range(NIH):
                    a = nxt("a", 3)
                    P.op("pe", lambda e, a=a, h=h, kb=kb: e.matmul(psA[a][:], lhsT=qih[:, h, :], rhs=kib[kb][:], start=True, stop=True),
                         reads=[b_qih, b_kib[kb]], writes=[b_psA[a]])
                    if h == 0:
                        P.op("dve", lambda e, a=a, cs_=cs_: e.tensor_scalar(
                            out=score[:, cs_], in0=psA[a][:], scalar1=0.0, scalar2=wq[:, i, 0:1], op0=ALU.max, op1=ALU.mult),
                            reads=[b_psA[a], b_wq], writes=[b_score])
                    else:
                        t = nxt("tmp", 2)
                        P.op("dve", lambda e, a=a, t=t, h=h: e.tensor_scalar(
                            out=tmp[t][:], in0=psA[a][:], scalar1=0.0, scalar2=wq[:, i, h:h + 1], op0=ALU.max, op1=ALU.mult),
                            reads=[b_psA[a], b_wq], writes=[b_tmp[t]])
                        P.op("pool", lambda e, t=t, cs_=cs_: e.tensor_tensor(out=score[:, cs_], in0=score[:, cs_], in1=tmp[t][:], op=ALU.add),
                             reads=[b_tmp[t], b_score], writes=[b_score])
            P.op("dve", lambda e: e.tensor_reduce(out=LO, in_=score[:, 0:Lk], axis=AX.X, op=ALU.min), reads=[b_score], writes=[b_bis])
            P.op("pool", lambda e: e.tensor_tensor(out=score[:, Lk - 1024:Lk], in0=score[:, Lk - 1024:Lk], in1=pen[:], op=ALU.add),
                 reads=[b_score, b_pen], writes=[b_score])
            P.op("dve", lambda e: e.max(out=m8[:], in_=score[:, 0:Lk]), reads=[b_score], writes=[b_bis])
            P.op("dve", lambda e: e.tensor_copy(out=HI, in_=m8[:, 0:1]), reads=[b_bis], writes=[b_bis])

        def bis_iter(i):
            Lk = 1024 * (i + 1)
            V_ = lambda fn, rd=(): P.op("dve", fn, reads=[b_bis, b_c] + list(rd), writes=[b_bis])
            V_(lambda e: e.scalar_tensor_tensor(out=MID, in0=LO, scalar=HI, in1=half_c[:], op0=ALU.add, op1=ALU.mult))
            P.op("dve", lambda e: e.tensor_scalar(out=junk[:, 0:Lk], in0=score[:, 0:Lk], scalar1=MID, scalar2=0.0,
                                                  op0=ALU.is_ge, op1=ALU.add, accum_out=CNT),
                 reads=[b_score, b_bis], writes=[b_junk, b_bis])
            V_(lambda e: e.tensor_single_scalar(out=GE, in_=CNT, scalar=TOPK - 0.5, op=ALU.is_ge))
            V_(lambda e: e.tensor_tensor(out=D1, in0=MID, in1=LO, op=ALU.subtract))
            V_(lambda e: e.tensor_tensor(out=D2, in0=HI, in1=MID, op=ALU.subtract))
            V_(lambda e: e.scalar_tensor_tensor(out=LO, in0=D1, scalar=GE, in1=LO, op0=ALU.mult, op1=ALU.add))
            V_(lambda e: e.scalar_tensor_tensor(out=HI, in0=D2, scalar=GE, in1=MID, op0=ALU.mult, op1=ALU.add))

        def phase_C(i):
            Lk = 1024 * (i + 1)
            for j in range(Lk // 512):
                cs_ = slice(j * 512, (j + 1) * 512)
                u = nxt("mk", 2)
                P.op("pool", lambda e, u=u, cs_=cs_: e.tensor_scalar(out=mk[u][:], in0=score[:, cs_], scalar1=LO, scalar2=None, op0=ALU.is_ge),
                     reads=[b_score, b_bis], writes=[b_mk[u]])
                for jj in range(4):
                    P.op("pe", lambda e, u=u, jj=jj: e.transpose(out=psT[:, jj * 128:(jj + 1) * 128], in_=mk[u][:, jj * 128:(jj + 1) * 128],
                                                                 identity=idb[:]),
                         reads=[b_mk[u], b_id], writes=[b_psT])
                P.op("act", lambda e, j=j: e.activation(out=maskT[:, 4 * j:4 * j + 4, :].rearrange("p a b -> p (a b)"), in_=psT[:, 0:512],
                                                        func=AF.Copy),
                     reads=[b_psT], writes=[b_maskT])

        def phase_D(i, side):
            qs = slice(i * 128, (i + 1) * 128)
            nkc = 8 * (i + 1)
            P.dma("sp", lambda e: e.dma_start(out=qbd[0:64, :, 0:128], in_=qT_pv[0][:, :, qs]), b_qh, writes=[b_qh])
            P.dma("sp", lambda e: e.dma_start(out=qbd[64:128, :, 128:256], in_=qT_pv[1][:, :, qs]), b_qh, writes=[])
            b_qh.w = ("d", b_qh, b_qh.dcount)
            groups = [(half, kc, hg) for half in range(2) for kc in range(nkc) for hg in range(2)]
            st = {}

            def emit_S(gi):
                half, kc, hg = groups[gi]
                kk = kc % 2
                if kk == 0 and hg == 0:
                    s_ = nxt("kv", 2)
                    r0 = kc * 128
                    P.dma("sp", lambda e: e.dma_start(out=kTc[s_][:], in_=kT_v[:, half * 4:(half + 1) * 4, r0:r0 + 256]),
                          b_kTc[s_], writes=[b_kTc[s_]])
                    P.dma("sp", lambda e: e.dma_start(
                        out=vc[s_][:], in_=v_d[r0:r0 + 256, half * 512:(half + 1) * 512].rearrange("(c p) n -> p c n", p=128)),
                        b_vc[s_], writes=[b_vc[s_]])
                    st["slot", half, kc // 2] = s_
                s_ = st["slot", half, kc // 2]
                a = nxt("a", 3)
                for pl2 in range(2):
                    pl = hg * 2 + pl2
                    pair = half * 4 + pl
                    P.op("pe", lambda e, pl2=pl2, pl=pl, pair=pair: e.matmul(
                        psA[a][:, pl2 * 256:(pl2 + 1) * 256], lhsT=kTc[s_][:, pl, kk * 128:(kk + 1) * 128], rhs=qbd[:, pair, :],
                        start=True, stop=True),
                        reads=[b_kTc[s_], b_qh], writes=[b_psA[a]])
                st["a", gi] = a

            def emit_rest(gi):
                half, kc, hg = groups[gi]
                kk = kc % 2
                s_ = st["slot", half, kc // 2]
                a = st["a", gi]
                u = nxt("pT", 3)
                P.op("act", lambda e: e.activation(out=pT[u][:], in_=psA[a][:], func=AF.Exp),
                     reads=[b_psA[a]], writes=[b_pT[u]])
                P.op("dve", lambda e: e.tensor_tensor(
                    out=pT[u][:].rearrange("p (h q) -> p h q", h=4), in0=pT[u][:].rearrange("p (h q) -> p h q", h=4),
                    in1=maskT[:, kc, :].unsqueeze(1).to_broadcast([128, 4, 128]), op=ALU.mult),
                    reads=[b_pT[u], b_maskT], writes=[b_pT[u]])
                for hh in range(4):
                    h8 = hg * 4 + hh
                    P.op("pe", lambda e, hh=hh, h8=h8: e.matmul(
                        acc[hg][0:64, hh * 128:(hh + 1) * 128], lhsT=vc[s_][:, kk, h8 * 64:(h8 + 1) * 64],
                        rhs=pT[u][:, hh * 128:(hh + 1) * 128], start=(kc == 0 and hh == 0), stop=(kc == nkc - 1 and hh == 3),
                        skip_group_check=True),
                        reads=[b_vc[s_], b_pT[u]], writes=[b_acc[hg]])
                P.op("pe", lambda e: e.matmul(
                    den[hg][0:64, :], lhsT=ones[:, 0:64], rhs=pT[u][:], start=(kc == 0), stop=(kc == nkc - 1)),
                    reads=[b_c, b_pT[u]], writes=[b_den[hg]])
                if kc == nkc - 1:
                    h0 = half * 8 + hg * 4
                    P.op("act", lambda e: e.activation(out=dsb[hg][:], in_=den[hg][0:64, :], func=AF.Copy),
                         reads=[b_den[hg]], writes=[b_dsb[hg]])
                    P.op("dve", lambda e: e.reciprocal(out=dsb[hg][:], in_=dsb[hg][:]), reads=[b_dsb[hg]], writes=[b_dsb[hg]])
                    P.op("dve", lambda e: e.tensor_tensor(
                        out=attn[:, h0:h0 + 4, :].rearrange("p a b -> p (a b)"), in0=acc[hg][0:64, :], in1=dsb[hg][:], op=ALU.mult),
                        reads=[b_acc[hg], b_dsb[hg]], writes=[b_attn])

            G = len(groups)
            done = 0
            emit_S(0)
            for gi in range(G):
                if gi + 1 < G:
                    emit_S(gi + 1)
                emit_rest(gi)
                want = (len(side) * (gi + 1)) // G
                while done < want:
                    side[done]()
                    done += 1
            while done < len(side):
                side[done]()
                done += 1

        def phase_E(i):
            qs = slice(i * 128, (i + 1) * 128)
            P.dma("sp", lambda e: e.dma_start(out=xq[:], in_=x_v[:, :, qs]), b_xq, writes=[b_xq])
            for m in range(KD):
                a = nxt("a", 3)
                for h in range(NH):
                    P.op("pe", lambda e, a=a, h=h, m=m: e.matmul(psA[a][:, 0:128], lhsT=wob[:, h, m * 128:(m + 1) * 128], rhs=attn[:, h, :],
                                                                 start=(h == 0), stop=(h == NH - 1)),
                         reads=[b_wob, b_attn], writes=[b_psA[a]])
                P.op("dve", lambda e, a=a, m=m: e.tensor_tensor(out=xq[:, m, :], in0=psA[a][:, 0:128], in1=xq[:, m, :], op=ALU.add),
                     reads=[b_psA[a], b_xq], writes=[b_xq])
            return P.dma("sp", lambda e: e.dma_start(out=y_v[:, :, qs], in_=xq[:]), b_xq, reads=[b_xq])

        phase_A(0)
        for _ in range(NBIS):
            bis_iter(0)
        phase_C(0)
        tok = None
        for i in range(nblk):
            side = []
            if i + 1 < nblk:
                phase_A(i + 1)
                side = [(lambda n=i + 1: bis_iter(n)) for _ in range(NBIS)]
            phase_D(i, side)
            tok = phase_E(i)
            if i + 1 < nblk:
                phase_C(i + 1)
        P.finish([tok])
        P.emit()
    return nc


def causal_pen(core):
    j = np.arange(1024)[None, :]
    p = np.arange(128)[:, None]
    return np.where(j > 128 * core + p, np.float32(NEG_MASK), np.float32(0.0)).astype(np.float32)


def gather_tokens_T(parts):
    f = parts[0].shape[0]
    out = np.empty((f, SEQ // 128, 128), parts[0].dtype)
    for c, p in enumerate(parts):
        out[:, c::NCORES, :] = p.reshape(f, NT // 128, 128)
    return out.reshape(f, SEQ)


def gather_tokens(parts):
    f = parts[0].shape[1]
    out = np.empty((SEQ // 128, 128, f), parts[0].dtype)
    for c, p in enumerate(parts):
        out[c::NCORES] = p.reshape(NT // 128, 128, f)
    return out.reshape(SEQ, f)


def run_dsa_layer(xT_parts, j, i, inp):
    gm = col_layout(inp["norm_mix"][i])
    qkg = np.stack([np.tile(inp["dsa_q_norm"][j], 2), np.tile(inp["dsa_k_norm"][j], 2)], axis=1).astype(np.float32)
    cm = const_mats()
    maps = [{"xT": xT_parts[c], "w_in": inp["dsa_w_in"][j], "gain": gm, "qk_gain": qkg, "cossin": rope_consts(c), "cmat": cm}
            for c in range(NCORES)]
    pr = _run("dsa_proj", build_dsa_proj_prog, maps)
    kTf = gather_tokens_T([r["kT"] for r in pr])
    kiTf = gather_tokens_T([r["kiT"] for r in pr])
    vf = gather_tokens([r["v"] for r in pr])
    ident = np.eye(128, dtype=np.float32)
    maps = []
    for c in range(NCORES):
        wq = np.ascontiguousarray(pr[c]["w"].reshape(NT // 128, 128, NIH).transpose(1, 0, 2))
        maps.append({"xT": xT_parts[c], "qT": pr[c]["qT"], "qiT": pr[c]["qiT"], "wq": wq, "kTf": kTf, "vf": vf, "kiTf": kiTf,
                     "pen": causal_pen(c), "ident": ident, "w_o": inp["dsa_w_o"][j]})
    ar = _run("dsa_attn", build_dsa_attn_prog, maps)
    gf = col_layout(inp["norm_ffn"][i])
    maps = [{"xT": ar[c]["yT"], "wgu": inp["ffn_w_gate_up"][i], "wd": inp["ffn_w_down"][i], "gain": gf} for c in range(NCORES)]
    fr = _run("ffn", build_ffn_prog, maps)
    return [r["yT"] for r in fr]


def kernel(**inputs):
    inp = {k: np.asarray(v) for k, v in inputs.items()}
    x = np.ascontiguousarray(inp["x"][0], dtype=np.float32)
    parts = [to_core_T(x, c) for c in range(NCORES)]
    for i in range(4):
        if i % 2 == 0:
            parts = run_s5_layer(parts, i // 2, i, inp)
        else:
            parts = run_dsa_layer(parts, i // 2, i, inp)
    return from_core_T(parts)[None].astype(np.float32)
```

```python
import math
from contextlib import ExitStack

import numpy as np
import concourse.bass as bass
import concourse.mybir as mybir
from concourse.bass_utils import run_bass_kernel_spmd

F32 = mybir.dt.float32
BF16 = mybir.dt.bfloat16
ALU = mybir.AluOpType
AF = mybir.ActivationFunctionType
AX = mybir.AxisListType

NCORES = 8
D = 1024
KD = D // 128
SEQ = 16384
NT = SEQ // NCORES
TT = 512
NTT = NT // TT
DFF = 2816
KF = DFF // 128
EPS = 1e-6

ENGS = ("pe", "act", "dve", "pool", "sp")
SAME_ENGINE_SYNC = ("act", "dve", "pool")


class Buf:
    __slots__ = ("name", "w", "r", "dsem", "dcount")

    def __init__(self, name):
        self.name = name
        self.w = None
        self.r = {}
        self.dsem = None
        self.dcount = 0


class Prog:
    def __init__(self, nc, es):
        self.nc = nc
        self.es = es
        self.ops = {e: [] for e in ENGS}
        self.dma_bufs = []
        self.final_tokens = []
        self.bar = []
        self.bar_epoch = 0
        self.eng_epoch = {e: 0 for e in ENGS}

    def sb(self, name, shape, dt, es=None):
        return (es or self.es).enter_context(self.nc.sbuf_tensor(name, list(shape), dt))

    def ps(self, name, shape, dt=F32, es=None):
        return (es or self.es).enter_context(self.nc.psum_tensor(name, list(shape), dt))

    def _deps(self, reads, writes):
        need = []
        for b in reads:
            if b.w is not None:
                need.append(b.w)
        for b in writes:
            if b.w is not None:
                need.append(b.w)
            need.extend(b.r.values())
        return need

    def barrier(self):
        toks = []
        for e in ENGS:
            for idx in range(len(self.ops[e]) - 1, -1, -1):
                if self.ops[e][idx]["dma"] is None:
                    toks.append(("e", e, idx))
                    break
        for b in self.dma_bufs:
            toks.append(("d", b, b.dcount))
        self.bar = toks
        self.bar_epoch += 1

    def _bar_need(self, eng):
        if self.eng_epoch[eng] < self.bar_epoch:
            self.eng_epoch[eng] = self.bar_epoch
            return list(self.bar)
        return []

    def op(self, eng, fn, reads=(), writes=()):
        need = self._deps(reads, writes) + self._bar_need(eng)
        idx = len(self.ops[eng])
        tok = ("e", eng, idx)
        self.ops[eng].append({"need": need, "fn": fn, "dma": None})
        for b in reads:
            b.r[eng] = tok
        for b in writes:
            b.w = tok
            b.r = {}
        return tok

    def dma(self, eng, fn, sembuf, reads=(), writes=()):
        need = self._deps(reads, writes) + self._bar_need(eng)
        if sembuf.dsem is None:
            sembuf.dsem = self.es.enter_context(self.nc.semaphore("d_" + sembuf.name))
            self.dma_bufs.append(sembuf)
        sembuf.dcount += 16
        tok = ("d", sembuf, sembuf.dcount)
        self.ops[eng].append({"need": need, "fn": fn, "dma": sembuf})
        for b in reads:
            b.r[("d", id(sembuf))] = tok
        for b in writes:
            b.w = tok
            b.r = {}
        return tok

    def finish(self, tokens):
        self.final_tokens.extend(tokens)

    def emit(self):
        nc = self.nc
        needed = {e: set() for e in ENGS}
        for e in ENGS:
            for i, o in enumerate(self.ops[e]):
                for t in o["need"]:
                    if t[0] == "e":
                        if t[1] == e and e not in SAME_ENGINE_SYNC:
                            continue
                        needed[t[1]].add(t[2])
        for t in self.final_tokens:
            if t[0] == "e":
                needed[t[1]].add(t[2])
        rank = {}
        for e in ENGS:
            rank[e] = {i: n + 1 for n, i in enumerate(sorted(needed[e]))}
        sems = {e: self.es.enter_context(nc.semaphore("s_" + e)) for e in ENGS}
        final_tokens = self.final_tokens

        def run(e, engine):
            known = {}
            def wait(tok):
                if tok[0] == "e":
                    if tok[1] == e and e not in SAME_ENGINE_SYNC:
                        return
                    key, sem, val = tok[1], sems[tok[1]], rank[tok[1]][tok[2]]
                else:
                    key, sem, val = id(tok[1]), tok[1].dsem, tok[2]
                if known.get(key, 0) >= val:
                    return
                known[key] = val
                engine.wait_ge(sem, val)
            for i, o in enumerate(self.ops[e]):
                for t in o["need"]:
                    wait(t)
                ins = o["fn"](engine)
                if o["dma"] is not None:
                    ins.then_inc(o["dma"].dsem, 16)
                elif i in rank[e]:
                    ins.then_inc(sems[e], 1)
            if e == "sp":
                for t in final_tokens:
                    wait(t)

        with nc.Block() as block:
            @block.tensor
            def _(eng):
                run("pe", eng)

            @block.scalar
            def _(eng):
                run("act", eng)

            @block.vector
            def _(eng):
                run("dve", eng)

            @block.gpsimd
            def _(eng):
                run("pool", eng)

            @block.sync
            def _(eng):
                run("sp", eng)


class Ctx:
    def __init__(self, P):
        self.P = P
        nc = P.nc
        self.ones = P.sb("c_ones", [128, 128], BF16)
        self.b_ones = Buf("ones")
        P.op("pool", lambda g: g.memset(self.ones[:], 1.0), writes=[self.b_ones])
        self.eps_col = P.sb("c_eps", [128, 1], F32)
        P.op("pool", lambda g: g.memset(self.eps_col[:], EPS), writes=[self.b_ones])
        self.psum = [P.ps(f"ps{i}", [128, 512], F32) for i in range(8)]
        self.b_ps = [Buf(f"ps{i}") for i in range(8)]
        self.ps_rr = 0

    def next_ps(self):
        i = self.ps_rr
        self.ps_rr = (self.ps_rr + 1) % 8
        return self.psum[i], self.b_ps[i]


def emit_rmsnorm_T(P, C, xT, b_x, gain, b_gain, gcol0, hT, b_h, tts, es, tag):
    sq = [P.sb(f"{tag}_sq{i}", [128, TT], BF16, es) for i in range(2)]
    b_sq = [Buf(f"{tag}_sq{i}") for i in range(2)]
    rstd = [P.sb(f"{tag}_rstd{i}", [128, TT], F32, es) for i in range(2)]
    b_rstd = [Buf(f"{tag}_rstd{i}") for i in range(2)]
    n = 0
    for j, tt in enumerate(tts):
        ts = slice(tt * TT, (tt + 1) * TT)
        ps, b_ps = C.next_ps()
        for k in range(KD):
            s, bs = sq[n % 2], b_sq[n % 2]
            n += 1
            P.op("act", lambda e, s=s, k=k, ts=ts: e.activation(out=s[:], in_=xT[:, k, ts], func=AF.Square),
                 reads=[b_x[k][tt]], writes=[bs])
            P.op("pe", lambda e, ps=ps, s=s, k=k: e.matmul(ps[:], lhsT=C.ones[:], rhs=s[:],
                                                             start=(k == 0), stop=(k == KD - 1)),
                 reads=[bs, C.b_ones], writes=[b_ps])
        r, br = rstd[j % 2], b_rstd[j % 2]
        P.op("act", lambda e, r=r, ps=ps: e.activation(out=r[:], in_=ps[:], func=AF.Sqrt, scale=1.0 / D, bias=C.eps_col[:]),
             reads=[b_ps, C.b_ones], writes=[br])
        P.op("dve", lambda e, r=r: e.reciprocal(out=r[:], in_=r[:]),
             reads=[br], writes=[br])
        js = slice(j * TT, (j + 1) * TT)
        for k in range(KD):
            P.op("dve", lambda e, k=k, ts=ts, js=js, r=r: e.scalar_tensor_tensor(
                out=hT[:, k, js], in0=xT[:, k, ts], scalar=gain[:, gcol0 + k:gcol0 + k + 1], in1=r[:],
                op0=ALU.mult, op1=ALU.mult),
                reads=[b_x[k][tt], br, b_gain], writes=[b_h[j]])


def emit_ffn(P, C, xT, b_x, wgu, wd, gain, b_gain, gcol0, tag="ffn"):
    nc = P.nc
    with ExitStack() as es:
        HT = 2 * TT
        hT = P.sb(f"{tag}_hT", [128, KD, HT], BF16, es)
        aT = P.sb(f"{tag}_aT", [128, KF, HT], BF16, es)
        wg_f = [P.sb(f"{tag}_wgf{i}", [128, KD, 256], F32, es) for i in range(2)]
        wg_b = [P.sb(f"{tag}_wgb{i}", [128, KD, 256], BF16, es) for i in range(2)]
        wd_f = [P.sb(f"{tag}_wdf{i}", [128, KF, 128], F32, es) for i in range(2)]
        wd_b = [P.sb(f"{tag}_wdb{i}", [128, KF, 128], BF16, es) for i in range(2)]
        sg = [P.sb(f"{tag}_sg{i}", [128, TT], F32, es) for i in range(2)]
        b_hT = [Buf(f"{tag}_hT{j}") for j in range(2)]
        b_aT = [[Buf(f"{tag}_aT{n}_{j}") for j in range(2)] for n in range(KF)]
        b_wgf = [Buf(f"{tag}_wgf{i}") for i in range(2)]
        b_wgb = [Buf(f"{tag}_wgb{i}") for i in range(2)]
        b_wdf = [Buf(f"{tag}_wdf{i}") for i in range(2)]
        b_wdb = [Buf(f"{tag}_wdb{i}") for i in range(2)]
        b_sg = [Buf(f"{tag}_sg{i}") for i in range(2)]
        wgu_v = wgu.rearrange("(k p) n -> p k n", p=128)
        wd_v = wd.rearrange("(k p) n -> p k n", p=128)
        nsg = 0
        nw = 0
        nwd = 0
        for half in range(NT // HT):
            tts = [half * 2, half * 2 + 1]
            emit_rmsnorm_T(P, C, xT, b_x, gain, b_gain, gcol0, hT, b_hT, tts, es, f"{tag}n{half}")
            for n in range(KF):
                s = nw % 2
                nw += 1
                P.dma("sp", lambda e, s=s, n=n: e.dma_start(out=wg_f[s][:, :, 0:128],
                                                            in_=wgu_v[:, :, n * 128:(n + 1) * 128]),
                      b_wgf[s], writes=[b_wgf[s]])
                P.dma("sp", lambda e, s=s, n=n: e.dma_start(out=wg_f[s][:, :, 128:256],
                                                            in_=wgu_v[:, :, DFF + n * 128:DFF + (n + 1) * 128]),
                      b_wgf[s], writes=[])
                b_wgf[s].w = ("d", b_wgf[s], b_wgf[s].dcount)
                P.op("pool", lambda e, s=s: e.tensor_copy(out=wg_b[s][:], in_=wg_f[s][:]),
                     reads=[b_wgf[s]], writes=[b_wgb[s]])
                for j in range(2):
                    js = slice(j * TT, (j + 1) * TT)
                    pg, b_pg = C.next_ps()
                    pu, b_pu = C.next_ps()
                    for k in range(KD):
                        P.op("pe", lambda e, pg=pg, s=s, k=k, js=js: e.matmul(
                            pg[:], lhsT=wg_b[s][:, k, 0:128], rhs=hT[:, k, js], start=(k == 0), stop=(k == KD - 1)),
                            reads=[b_wgb[s], b_hT[j]], writes=[b_pg])
                    for k in range(KD):
                        P.op("pe", lambda e, pu=pu, s=s, k=k, js=js: e.matmul(
                            pu[:], lhsT=wg_b[s][:, k, 128:256], rhs=hT[:, k, js], start=(k == 0), stop=(k == KD - 1)),
                            reads=[b_wgb[s], b_hT[j]], writes=[b_pu])
                    q = nsg % 2
                    nsg += 1
                    P.op("act", lambda e, q=q, pg=pg: e.activation(out=sg[q][:], in_=pg[:], func=AF.Silu),
                         reads=[b_pg], writes=[b_sg[q]])
                    P.op("dve", lambda e, q=q, pu=pu, n=n, js=js: e.tensor_tensor(
                        out=aT[:, n, js], in0=pu[:], in1=sg[q][:], op=ALU.mult),
                        reads=[b_pu, b_sg[q]], writes=[b_aT[n][j]])
            for m in range(KD):
                s = nwd % 2
                nwd += 1
                P.dma("sp", lambda e, s=s, m=m: e.dma_start(out=wd_f[s][:], in_=wd_v[:, :, m * 128:(m + 1) * 128]),
                      b_wdf[s], writes=[b_wdf[s]])
                P.op("pool", lambda e, s=s: e.tensor_copy(out=wd_b[s][:], in_=wd_f[s][:]),
                     reads=[b_wdf[s]], writes=[b_wdb[s]])
                for j in range(2):
                    tt = tts[j]
                    js = slice(j * TT, (j + 1) * TT)
                    ts = slice(tt * TT, (tt + 1) * TT)
                    po, b_po = C.next_ps()
                    for n in range(KF):
                        P.op("pe", lambda e, po=po, s=s, n=n, js=js: e.matmul(
                            po[:], lhsT=wd_b[s][:, n, :], rhs=aT[:, n, js], start=(n == 0), stop=(n == KF - 1)),
                            reads=[b_wdb[s], b_aT[n][j]], writes=[b_po])
                    P.op("dve", lambda e, po=po, m=m, ts=ts: e.tensor_tensor(
                        out=xT[:, m, ts], in0=po[:], in1=xT[:, m, ts], op=ALU.add),
                        reads=[b_po, b_x[m][tt]], writes=[b_x[m][tt]])
    P.barrier()


def load_xT(P, xT, b_x, x_dram, eng="sp"):
    xv = x_dram.rearrange("(k p) t -> p k t", p=128)
    for k in range(KD):
        for tt in range(NTT):
            ts = slice(tt * TT, (tt + 1) * TT)
            P.dma(eng, lambda e, k=k, ts=ts: e.dma_start(out=xT[:, k, ts], in_=xv[:, k, ts]),
                  b_x[k][tt], writes=[b_x[k][tt]])


def store_xT(P, xT, b_x, y_dram, eng="sp"):
    yv = y_dram.rearrange("(k p) t -> p k t", p=128)
    toks = []
    for k in range(KD):
        for tt in range(NTT):
            ts = slice(tt * TT, (tt + 1) * TT)
            toks.append(P.dma(eng, lambda e, k=k, ts=ts: e.dma_start(out=yv[:, k, ts], in_=xT[:, k, ts]),
                              b_x[k][tt], reads=[b_x[k][tt]]))
    P.finish(toks)


def build_ffn_prog():
    nc = bass.Bass("TRN2", target_bir_lowering=False)
    x = nc.dram_tensor("xT", [D, NT], F32, kind="ExternalInput").ap()
    wgu = nc.dram_tensor("wgu", [D, 2 * DFF], F32, kind="ExternalInput").ap()
    wd = nc.dram_tensor("wd", [DFF, D], F32, kind="ExternalInput").ap()
    gain = nc.dram_tensor("gain", [128, KD], F32, kind="ExternalInput").ap()
    y = nc.dram_tensor("yT", [D, NT], F32, kind="ExternalOutput").ap()
    with ExitStack() as es:
        P = Prog(nc, es)
        C = Ctx(P)
        xT = P.sb("xT_sb", [128, KD, NT], F32)
        b_x = [[Buf(f"x{k}_{t}") for t in range(NTT)] for k in range(KD)]
        g_sb = P.sb("gain_sb", [128, KD], F32)
        b_g = Buf("gain")
        P.dma("sp", lambda e: e.dma_start(out=g_sb[:], in_=gain[:]), b_g, writes=[b_g])
        load_xT(P, xT, b_x, x)
        emit_ffn(P, C, xT, b_x, wgu, wd, g_sb, b_g, 0)
        store_xT(P, xT, b_x, y)
        P.emit()
    return nc


def col_layout(v):
    return np.ascontiguousarray(np.asarray(v, np.float32).reshape(-1, 128).T)


def to_core_T(x2d, c):
    blocks = x2d.reshape(SEQ // 128, 128, -1)[c::NCORES]
    return np.ascontiguousarray(blocks.reshape(NT, -1).T)


def from_core_T(parts):
    dd = parts[0].shape[0]
    out = np.empty((SEQ // 128, 128, dd), np.float32)
    for c, p in enumerate(parts):
        out[c::NCORES] = p.T.reshape(NT // 128, 128, dd)
    return out.reshape(SEQ, dd)


def build_prenorm_prog():
    nc = bass.Bass("TRN2", target_bir_lowering=False)
    x = nc.dram_tensor("xT", [D, NT], F32, kind="ExternalInput").ap()
    gain = nc.dram_tensor("gain", [128, KD], F32, kind="ExternalInput").ap()
    y = nc.dram_tensor("hT", [D, NT], F32, kind="ExternalOutput").ap()
    with ExitStack() as es:
        P = Prog(nc, es)
        C = Ctx(P)
        xT = P.sb("xT_sb", [128, KD, NT], F32)
        b_x = [[Buf(f"x{k}_{t}") for t in range(NTT)] for k in range(KD)]
        g_sb = P.sb("gain_sb", [128, KD], F32)
        b_g = Buf("gain")
        P.dma("sp", lambda e: e.dma_start(out=g_sb[:], in_=gain[:]), b_g, writes=[b_g])
        load_xT(P, xT, b_x, x)
        hT = P.sb("hT_sb", [128, KD, NT], F32)
        b_h = [Buf(f"h{t}") for t in range(NTT)]
        emit_rmsnorm_T(P, C, xT, b_x, g_sb, b_g, 0, hT, b_h, list(range(NTT)), es, "pn")
        yv = y.rearrange("(k p) t -> p k t", p=128)
        toks = []
        for tt in range(NTT):
            ts = slice(tt * TT, (tt + 1) * TT)
            toks.append(P.dma("sp", lambda e, ts=ts: e.dma_start(out=yv[:, :, ts], in_=hT[:, :, ts]),
                              b_h[tt], reads=[b_h[tt]]))
        P.finish(toks)
        P.emit()
    return nc


S5T = 512
TWO_PI = 2.0 * math.pi

PI_LO = 3.1415925
CW1 = 6.28125
CW2 = TWO_PI - 6.28125


def emit_range_reduce(P, dst, src, ti, tf, reads, writes):
    rw = list(reads) + list(writes)
    P.op("dve", lambda e: e.tensor_scalar(out=tf, in0=src, scalar1=1.0 / TWO_PI, scalar2=None, op0=ALU.mult),
         reads=rw, writes=writes)
    P.op("dve", lambda e: e.tensor_copy(out=ti, in_=tf), reads=rw, writes=writes)
    P.op("dve", lambda e: e.tensor_copy(out=tf, in_=ti), reads=rw, writes=writes)
    P.op("dve", lambda e: e.scalar_tensor_tensor(out=dst, in0=tf, scalar=-CW1, in1=src, op0=ALU.mult, op1=ALU.add),
         reads=rw, writes=writes)
    P.op("dve", lambda e: e.scalar_tensor_tensor(out=dst, in0=tf, scalar=-CW2, in1=dst, op0=ALU.mult, op1=ALU.add),
         reads=rw, writes=writes)
    P.op("dve", lambda e: e.tensor_scalar(out=tf, in0=dst, scalar1=math.pi, scalar2=-TWO_PI, op0=ALU.is_gt, op1=ALU.mult),
         reads=rw, writes=writes)
    P.op("dve", lambda e: e.tensor_tensor(out=dst, in0=dst, in1=tf, op=ALU.add), reads=rw, writes=writes)
    P.op("dve", lambda e: e.tensor_scalar(out=tf, in0=dst, scalar1=-math.pi, scalar2=TWO_PI, op0=ALU.is_lt, op1=ALU.mult),
         reads=rw, writes=writes)
    P.op("dve", lambda e: e.tensor_tensor(out=dst, in0=dst, in1=tf, op=ALU.add), reads=rw, writes=writes)
    P.op("dve", lambda e: e.tensor_scalar(out=dst, in0=dst, scalar1=PI_LO, scalar2=-PI_LO, op0=ALU.min, op1=ALU.max),
         reads=rw, writes=writes)


def build_s5_prog(debug=0):
    nc = bass.Bass("TRN2", target_bir_lowering=False)
    u_d = nc.dram_tensor("uT", [128, SEQ], F32, kind="ExternalInput").ap()
    par_d = nc.dram_tensor("par", [128, 16], F32, kind="ExternalInput").ap()
    bt_d = nc.dram_tensor("bt", [128, 2, 512], F32, kind="ExternalInput").ap()
    ct_d = nc.dram_tensor("ct", [128, 2, 4, 128], F32, kind="ExternalInput").ap()
    tau_d = nc.dram_tensor("tau", [128, S5T], F32, kind="ExternalInput").ap()
    y_d = nc.dram_tensor("gyT", [128, SEQ], F32, kind="ExternalOutput").ap()
    NCH = SEQ // S5T
    with ExitStack() as es:
        P = Prog(nc, es)
        C = Ctx(P)
        par = P.sb("par_sb", [128, 16], F32)
        bt = P.sb("bt_sb", [128, 2, 512], F32)
        ct = P.sb("ct_sb", [128, 2, 4, 128], F32)
        tau = P.sb("tau_sb", [128, S5T], F32)
        b_par, b_bt, b_ct, b_tau = Buf("par"), Buf("bt"), Buf("ct"), Buf("tau")
        P.dma("sp", lambda e: e.dma_start(out=par[:], in_=par_d[:]), b_par, writes=[b_par])
        P.dma("sp", lambda e: e.dma_start(out=bt[:], in_=bt_d[:]), b_bt, writes=[b_bt])
        P.dma("sp", lambda e: e.dma_start(out=ct[:], in_=ct_d[:]), b_ct, writes=[b_ct])
        P.dma("sp", lambda e: e.dma_start(out=tau[:], in_=tau_d[:]), b_tau, writes=[b_tau])
        sm = P.sb("s5_small", [128, 24, 4], F32)
        b_sm = Buf("s5_small")
        halfpi = P.sb("halfpi", [128, 1], F32)
        P.op("pool", lambda e: e.memset(halfpi[:], math.pi / 2), writes=[b_sm])
        lam_re, lam_im, logdt, dcol = par[:, 0:4], par[:, 4:8], par[:, 8:12], par[:, 12:13]
        (DT, LR, TH, R, THR, ABS, ARE, AIM, NR, NUM_RE, NUM_IM, DEN, KRE, KIM, T1, T2, PHT, CT_, ST_, NST_) = range(20)
        col = lambda i: sm[:, i, :]

        def V(fn, reads=(b_par, b_sm)):
            P.op("dve", fn, reads=list(reads), writes=[b_sm])

        def A(fn):
            P.op("act", fn, reads=[b_par, b_sm], writes=[b_sm])

        def sincos(src, s_out, c_out, shape_ap_abs):
            A(lambda e: e.activation(out=s_out, in_=src, func=AF.Sin))
            A(lambda e: e.activation(out=shape_ap_abs, in_=src, func=AF.Abs))
            A(lambda e: e.activation(out=c_out, in_=shape_ap_abs, func=AF.Sin, scale=-1.0, bias=halfpi[:]))

        def reduce_phase(out, src_fn_desc):
            pass

        A(lambda e: e.activation(out=col(DT), in_=logdt, func=AF.Exp))
        V(lambda e: e.tensor_tensor(out=col(LR), in0=lam_re, in1=col(DT), op=ALU.mult))
        V(lambda e: e.tensor_tensor(out=col(TH), in0=lam_im, in1=col(DT), op=ALU.mult))
        A(lambda e: e.activation(out=col(R), in_=col(LR), func=AF.Exp))
        smi = P.sb("s5_smi", [128, 4], mybir.dt.int32)
        emit_range_reduce(P, col(THR), col(TH), smi[:], col(T1), [b_par], [b_sm])
        sincos(col(THR), col(AIM), col(ARE), col(ABS))
        V(lambda e: e.tensor_tensor(out=col(ARE), in0=col(ARE), in1=col(R), op=ALU.mult))
        V(lambda e: e.tensor_tensor(out=col(AIM), in0=col(AIM), in1=col(R), op=ALU.mult))
        V(lambda e: e.tensor_scalar(out=col(NR), in0=col(ARE), scalar1=-1.0, scalar2=None, op0=ALU.add))
        V(lambda e: e.tensor_tensor(out=col(T1), in0=col(NR), in1=lam_re, op=ALU.mult))
        V(lambda e: e.tensor_tensor(out=col(T2), in0=col(AIM), in1=lam_im, op=ALU.mult))
        V(lambda e: e.tensor_tensor(out=col(NUM_RE), in0=col(T1), in1=col(T2), op=ALU.add))
        V(lambda e: e.tensor_tensor(out=col(T1), in0=col(AIM), in1=lam_re, op=ALU.mult))
        V(lambda e: e.tensor_tensor(out=col(T2), in0=col(NR), in1=lam_im, op=ALU.mult))
        V(lambda e: e.tensor_tensor(out=col(NUM_IM), in0=col(T1), in1=col(T2), op=ALU.subtract))
        V(lambda e: e.tensor_tensor(out=col(T1), in0=lam_re, in1=lam_re, op=ALU.mult))
        V(lambda e: e.tensor_tensor(out=col(T2), in0=lam_im, in1=lam_im, op=ALU.mult))
        V(lambda e: e.tensor_tensor(out=col(DEN), in0=col(T1), in1=col(T2), op=ALU.add))
        V(lambda e: e.reciprocal(out=col(DEN), in_=col(DEN)))
        V(lambda e: e.tensor_tensor(out=col(KRE), in0=col(NUM_RE), in1=col(DEN), op=ALU.mult))
        V(lambda e: e.tensor_tensor(out=col(KIM), in0=col(NUM_IM), in1=col(DEN), op=ALU.mult))
        V(lambda e: e.tensor_scalar(out=col(T2), in0=col(THR), scalar1=float(S5T), scalar2=None, op0=ALU.mult))
        emit_range_reduce(P, col(PHT), col(T2), smi[:], col(T1), [b_par], [b_sm])
        sincos(col(PHT), col(ST_), col(CT_), col(ABS))
        V(lambda e: e.tensor_scalar(out=col(NST_), in0=col(ST_), scalar1=-1.0, scalar2=None, op0=ALU.mult))
        L = P.sb("s5_L", [128, 3, 4, 128], BF16)
        b_L = Buf("s5_L")
        ctmp = P.sb("s5_ctmp", [128, 2, 128], F32)
        b_ctmp = Buf("s5_ctmp")
        for k in range(4):
            kre, kim = sm[:, KRE, k:k + 1], sm[:, KIM, k:k + 1]
            P.op("dve", lambda e, k=k, kim=kim: e.tensor_scalar(out=ctmp[:, 0, :], in0=ct[:, 1, k, :], scalar1=kim,
                                                                 scalar2=-1.0, op0=ALU.mult, op1=ALU.mult),
                 reads=[b_ct, b_sm], writes=[b_ctmp])
            P.op("dve", lambda e, k=k, kre=kre: e.scalar_tensor_tensor(out=ctmp[:, 0, :], in0=ct[:, 0, k, :], scalar=kre,
                                                                        in1=ctmp[:, 0, :], op0=ALU.mult, op1=ALU.add),
                 reads=[b_ct, b_sm, b_ctmp], writes=[b_ctmp])
            P.op("dve", lambda e, k=k, kre=kre: e.tensor_scalar(out=ctmp[:, 1, :], in0=ct[:, 1, k, :], scalar1=kre,
                                                                 scalar2=None, op0=ALU.mult),
                 reads=[b_ct, b_sm], writes=[b_ctmp])
            P.op("dve", lambda e, k=k, kim=kim: e.scalar_tensor_tensor(out=ctmp[:, 1, :], in0=ct[:, 0, k, :], scalar=kim,
                                                                        in1=ctmp[:, 1, :], op0=ALU.mult, op1=ALU.add),
                 reads=[b_ct, b_sm, b_ctmp], writes=[b_ctmp])
            P.op("dve", lambda e, k=k: e.tensor_copy(out=L[:, 0, k, :], in_=ctmp[:, 0, :]), reads=[b_ctmp], writes=[b_L])
            P.op("dve", lambda e, k=k: e.tensor_scalar(out=L[:, 1, k, :], in0=ctmp[:, 0, :], scalar1=-1.0, scalar2=None,
                                                       op0=ALU.mult), reads=[b_ctmp], writes=[b_L])
            P.op("dve", lambda e, k=k: e.tensor_scalar(out=L[:, 2, k, :], in0=ctmp[:, 1, :], scalar1=-1.0, scalar2=None,
                                                       op0=ALU.mult), reads=[b_ctmp], writes=[b_L])
        btb = P.sb("s5_btb", [128, 2, 512], BF16)
        b_btb = Buf("s5_btb")
        P.op("dve", lambda e: e.tensor_copy(out=btb[:], in_=bt[:]), reads=[b_bt], writes=[b_btb])
        cosT = P.sb("s5_cos", [128, 4, S5T], F32)
        sinT = P.sb("s5_sin", [128, 4, S5T], F32)
        rT = P.sb("s5_rT", [128, 4, S5T], F32)
        b_tab = Buf("s5_tab")
        ph = P.sb("s5_ph", [128, S5T], F32)
        pha = P.sb("s5_pha", [128, S5T], F32)
        phx = P.sb("s5_phx", [128, S5T], F32)
        phi = P.sb("s5_phi", [128, S5T], mybir.dt.int32)
        b_ph = Buf("s5_ph")
        for k in range(4):
            P.op("dve", lambda e, k=k: e.tensor_scalar(out=phx[:], in0=tau[:], scalar1=sm[:, THR, k:k + 1],
                                                       scalar2=None, op0=ALU.mult),
                 reads=[b_tau, b_sm, b_tab], writes=[b_ph])
            emit_range_reduce(P, ph[:], phx[:], phi[:], pha[:], [b_sm], [b_ph])
            P.op("act", lambda e, k=k: e.activation(out=sinT[:, k, :], in_=ph[:], func=AF.Sin),
                 reads=[b_ph], writes=[b_tab])
            P.op("act", lambda e: e.activation(out=pha[:], in_=ph[:], func=AF.Abs),
                 reads=[b_ph], writes=[b_ph])
            P.op("act", lambda e, k=k: e.activation(out=cosT[:, k, :], in_=pha[:], func=AF.Sin, scale=-1.0, bias=halfpi[:]),
                 reads=[b_ph, b_sm], writes=[b_tab])
            P.op("dve", lambda e, k=k: e.tensor_scalar(out=rT[:, k, :], in0=tau[:], scalar1=0.0, scalar2=sm[:, R, k:k + 1],
                                                       op0=ALU.mult, op1=ALU.add),
                 reads=[b_tau, b_sm], writes=[b_tab])
        if debug == 2:
            tk = [P.dma("sp", lambda e: e.dma_start(out=y_d[:, 0:96], in_=sm[:].rearrange("p a b -> p (a b)")), b_sm, reads=[b_sm]),
                  P.dma("sp", lambda e: e.dma_start(out=y_d[:, 1024:3072], in_=cosT[:].rearrange("p a b -> p (a b)")), b_tab, reads=[b_tab]),
                  P.dma("sp", lambda e: e.dma_start(out=y_d[:, 3072:5120], in_=sinT[:].rearrange("p a b -> p (a b)")), b_tab, reads=[b_tab]),
                  P.dma("sp", lambda e: e.dma_start(out=y_d[:, 5120:7168], in_=rT[:].rearrange("p a b -> p (a b)")), b_tab, reads=[b_tab])]
            P.finish(tk)
            P.emit()
            return nc
        NB = 2
        uf = [P.sb(f"s5_uf{i}", [128, S5T], F32) for i in range(NB)]
        ub = [P.sb(f"s5_ub{i}", [128, S5T], BF16) for i in range(NB)]
        b_uf = [Buf(f"s5_uf{i}") for i in range(NB)]
        b_ub = [Buf(f"s5_ub{i}") for i in range(NB)]
        sA = [P.sb(f"s5_sA{i}", [128, S5T], F32) for i in range(2)]
        sB = [P.sb(f"s5_sB{i}", [128, S5T], F32) for i in range(2)]
        b_sA = [Buf(f"s5_sA{i}") for i in range(2)]
        b_sB = [Buf(f"s5_sB{i}") for i in range(2)]
        t_ = [[P.sb(f"s5_t{j}_{i}", [128, S5T], F32) for i in range(2)] for j in range(4)]
        b_t = [[Buf(f"s5_t{j}_{i}") for i in range(2)] for j in range(4)]
        bre = [P.sb(f"s5_bre{i}", [128, S5T], F32) for i in range(2)]
        bim = [P.sb(f"s5_bim{i}", [128, S5T], F32) for i in range(2)]
        b_bre = [Buf(f"s5_bre{i}") for i in range(2)]
        b_bim = [Buf(f"s5_bim{i}") for i in range(2)]
        zre = [P.sb(f"s5_zre{i}", [128, S5T], F32) for i in range(2)]
        zim = [P.sb(f"s5_zim{i}", [128, S5T], F32) for i in range(2)]
        b_zre = [Buf(f"s5_zre{i}") for i in range(2)]
        b_zim = [Buf(f"s5_zim{i}") for i in range(2)]
        pp = [[P.sb(f"s5_p{j}_{i}", [128, S5T], BF16) for i in range(2)] for j in range(4)]
        b_pp = [[Buf(f"s5_p{j}_{i}") for i in range(2)] for j in range(4)]
        init = P.sb("s5_init", [128, 2, 4], F32)
        itmp = P.sb("s5_itmp", [128, 2, 4], F32)
        b_init = [Buf(f"s5_init{k}") for k in range(4)]
        P.op("dve", lambda e: e.memset(init[:], 0.0), writes=b_init)
        ysb = [P.sb(f"s5_y{i}", [128, S5T], F32) for i in range(2)]
        g1 = [P.sb(f"s5_g1{i}", [128, S5T], F32) for i in range(2)]
        g2 = [P.sb(f"s5_g2{i}", [128, S5T], F32) for i in range(2)]
        b_y = [Buf(f"s5_y{i}") for i in range(2)]
        b_g1 = [Buf(f"s5_g1{i}") for i in range(2)]
        b_g2 = [Buf(f"s5_g2{i}") for i in range(2)]
        toks = []
        it = 0
        for c in range(NCH):
            cs = slice(c * S5T, (c + 1) * S5T)
            ui = c % NB
            P.dma("sp", lambda e, ui=ui, cs=cs: e.dma_start(out=uf[ui][:], in_=u_d[:, cs]), b_uf[ui], writes=[b_uf[ui]])
            P.op("act", lambda e, ui=ui: e.activation(out=ub[ui][:], in_=uf[ui][:], func=AF.Copy),
                 reads=[b_uf[ui]], writes=[b_ub[ui]])
            yps, b_yps = C.psum[6 + c % 2], C.b_ps[6 + c % 2]
            for k in range(4):
                s = it % 2
                pa, b_pa = C.psum[(2 * it) % 6], C.b_ps[(2 * it) % 6]
                pb, b_pb = C.psum[(2 * it + 1) % 6], C.b_ps[(2 * it + 1) % 6]
                it += 1
                ks = slice(k * 128, (k + 1) * 128)
                P.op("pe", lambda e, pa=pa, ks=ks, ui=ui: e.matmul(pa[:], lhsT=btb[:, 0, ks], rhs=ub[ui][:], start=True, stop=True),
                     reads=[b_btb, b_ub[ui]], writes=[b_pa])
                P.op("pe", lambda e, pb=pb, ks=ks, ui=ui: e.matmul(pb[:], lhsT=btb[:, 1, ks], rhs=ub[ui][:], start=True, stop=True),
                     reads=[b_btb, b_ub[ui]], writes=[b_pb])
                P.op("act", lambda e, s=s, pa=pa: e.activation(out=sA[s][:], in_=pa[:], func=AF.Copy), reads=[b_pa], writes=[b_sA[s]])
                P.op("act", lambda e, s=s, pb=pb: e.activation(out=sB[s][:], in_=pb[:], func=AF.Copy), reads=[b_pb], writes=[b_sB[s]])
                P.op("pool", lambda e, s=s, k=k: e.tensor_tensor(out=t_[0][s][:], in0=sA[s][:], in1=cosT[:, k, :], op=ALU.mult),
                     reads=[b_sA[s], b_tab], writes=[b_t[0][s]])
                P.op("dve", lambda e, s=s, k=k: e.tensor_tensor(out=t_[1][s][:], in0=sB[s][:], in1=sinT[:, k, :], op=ALU.mult),
                     reads=[b_sB[s], b_tab], writes=[b_t[1][s]])
                P.op("pool", lambda e, s=s, k=k: e.tensor_tensor(out=t_[2][s][:], in0=sB[s][:], in1=cosT[:, k, :], op=ALU.mult),
                     reads=[b_sB[s], b_tab], writes=[b_t[2][s]])
                P.op("dve", lambda e, s=s, k=k: e.tensor_tensor(out=t_[3][s][:], in0=sA[s][:], in1=sinT[:, k, :], op=ALU.mult),
                     reads=[b_sA[s], b_tab], writes=[b_t[3][s]])
                P.op("pool", lambda e, s=s: e.tensor_tensor(out=bre[s][:], in0=t_[0][s][:], in1=t_[1][s][:], op=ALU.add),
                     reads=[b_t[0][s], b_t[1][s]], writes=[b_bre[s]])
                P.op("pool", lambda e, s=s: e.tensor_tensor(out=bim[s][:], in0=t_[2][s][:], in1=t_[3][s][:], op=ALU.subtract),
                     reads=[b_t[2][s], b_t[3][s]], writes=[b_bim[s]])
                P.op("dve", lambda e, s=s, k=k: e.tensor_tensor_scan(out=zre[s][:], data0=rT[:, k, :], data1=bre[s][:],
                                                                      initial=init[:, 0, k:k + 1], op0=ALU.mult, op1=ALU.add),
                     reads=[b_tab, b_bre[s], b_init[k]], writes=[b_zre[s]])
                P.op("dve", lambda e, s=s, k=k: e.tensor_tensor_scan(out=zim[s][:], data0=rT[:, k, :], data1=bim[s][:],
                                                                      initial=init[:, 1, k:k + 1], op0=ALU.mult, op1=ALU.add),
                     reads=[b_tab, b_bim[s], b_init[k]], writes=[b_zim[s]])
                zlr, zli = zre[s][:, S5T - 1:S5T], zim[s][:, S5T - 1:S5T]
                cT_, sT_, nsT_ = sm[:, CT_, k:k + 1], sm[:, ST_, k:k + 1], sm[:, NST_, k:k + 1]
                P.op("dve", lambda e, k=k, zlr=zlr, cT_=cT_: e.tensor_tensor(out=itmp[:, 0, k:k + 1], in0=zlr, in1=cT_, op=ALU.mult),
                     reads=[b_zre[s], b_sm], writes=[b_init[k]])
                P.op("dve", lambda e, k=k, zlr=zlr, sT_=sT_: e.tensor_tensor(out=itmp[:, 1, k:k + 1], in0=zlr, in1=sT_, op=ALU.mult),
                     reads=[b_zre[s], b_sm], writes=[b_init[k]])
                P.op("dve", lambda e, k=k, zli=zli, nsT_=nsT_: e.scalar_tensor_tensor(
                    out=init[:, 0, k:k + 1], in0=zli, scalar=nsT_, in1=itmp[:, 0, k:k + 1], op0=ALU.mult, op1=ALU.add),
                    reads=[b_zim[s], b_sm], writes=[b_init[k]])
                P.op("dve", lambda e, k=k, zli=zli, cT_=cT_: e.scalar_tensor_tensor(
                    out=init[:, 1, k:k + 1], in0=zli, scalar=cT_, in1=itmp[:, 1, k:k + 1], op0=ALU.mult, op1=ALU.add),
                    reads=[b_zim[s], b_sm], writes=[b_init[k]])
                P.op("pool", lambda e, s=s, k=k: e.tensor_tensor(out=pp[0][s][:], in0=zre[s][:], in1=cosT[:, k, :], op=ALU.mult),
                     reads=[b_zre[s], b_tab], writes=[b_pp[0][s]])
                P.op("pool", lambda e, s=s, k=k: e.tensor_tensor(out=pp[1][s][:], in0=zim[s][:], in1=sinT[:, k, :], op=ALU.mult),
                     reads=[b_zim[s], b_tab], writes=[b_pp[1][s]])
                P.op("dve", lambda e, s=s, k=k: e.tensor_tensor(out=pp[2][s][:], in0=zre[s][:], in1=sinT[:, k, :], op=ALU.mult),
                     reads=[b_zre[s], b_tab], writes=[b_pp[2][s]])
                P.op("pool", lambda e, s=s, k=k: e.tensor_tensor(out=pp[3][s][:], in0=zim[s][:], in1=cosT[:, k, :], op=ALU.mult),
                     reads=[b_zim[s], b_tab], writes=[b_pp[3][s]])
                for j, li in enumerate((0, 1, 2, 2)):
                    P.op("pe", lambda e, yps=yps, li=li, k=k, j=j, s=s: e.matmul(
                        yps[:], lhsT=L[:, li, k, :], rhs=pp[j][s][:], start=(k == 0 and j == 0), stop=(k == 3 and j == 3)),
                        reads=[b_L, b_pp[j][s]], writes=[b_yps])
            q = c % 2
            P.op("dve", lambda e, q=q, ui=ui, yps=yps: e.scalar_tensor_tensor(
                out=ysb[q][:], in0=uf[ui][:], scalar=dcol, in1=yps[:], op0=ALU.mult, op1=ALU.add),
                reads=[b_uf[ui], b_par, b_yps], writes=[b_y[q]])
            P.op("pool", lambda e, q=q: e.tensor_tensor(out=g1[q][:], in0=ysb[q][:], in1=ysb[q][:], op=ALU.mult),
                 reads=[b_y[q]], writes=[b_g1[q]])
            P.op("pool", lambda e, q=q: e.tensor_scalar(out=g1[q][:], in0=g1[q][:], scalar1=0.044715, scalar2=1.0,
                                                        op0=ALU.mult, op1=ALU.add),
                 reads=[b_g1[q]], writes=[b_g1[q]])
            P.op("pool", lambda e, q=q: e.tensor_tensor(out=g1[q][:], in0=g1[q][:], in1=ysb[q][:], op=ALU.mult),
                 reads=[b_g1[q], b_y[q]], writes=[b_g1[q]])
            P.op("act", lambda e, q=q: e.activation(out=g2[q][:], in_=g1[q][:], func=AF.Sigmoid, scale=1.5957691216057308),
                 reads=[b_g1[q]], writes=[b_g2[q]])
            P.op("pool", lambda e, q=q: e.tensor_tensor(out=g2[q][:], in0=g2[q][:], in1=ysb[q][:], op=ALU.mult),
                 reads=[b_g2[q], b_y[q]], writes=[b_g2[q]])
            if debug == 1:
                toks.append(P.dma("sp", lambda e, q=q, cs=cs: e.dma_start(out=y_d[:, cs], in_=ysb[q][:]), b_g2[q], reads=[b_g2[q], b_y[q]]))
                continue
            toks.append(P.dma("sp", lambda e, q=q, cs=cs: e.dma_start(out=y_d[:, cs], in_=g2[q][:]), b_g2[q], reads=[b_g2[q]]))
        P.finish(toks[-2:])
        P.emit()
    return nc


def s5_host_layout(lam_re, lam_im, log_dt, b_re, b_im, c_re, c_im, d, core):
    g0 = core * 8
    par = np.zeros((128, 16), np.float32)
    bt = np.zeros((128, 2, 512), np.float32)
    ct = np.zeros((128, 2, 4, 128), np.float32)
    for gl in range(8):
        g = g0 + gl
        k, p0 = gl // 2, (gl % 2) * 64
        par[p0:p0 + 64, 0 + k] = lam_re[g]
        par[p0:p0 + 64, 4 + k] = lam_im[g]
        par[p0:p0 + 64, 8 + k] = log_dt[g]
        bt[16 * gl:16 * gl + 16, 0, 64 * gl:64 * gl + 64] = b_re[g].T
        bt[16 * gl:16 * gl + 16, 1, 64 * gl:64 * gl + 64] = b_im[g].T
        ct[p0:p0 + 64, 0, k, 16 * gl:16 * gl + 16] = c_re[g].T
        ct[p0:p0 + 64, 1, k, 16 * gl:16 * gl + 16] = c_im[g].T
    par[:, 12] = d[core * 128:(core + 1) * 128]
    return par, bt, ct


def emit_glu(P, C, xT, b_x, gy_dram, wglu):
    with ExitStack() as es:
        gyb = P.sb("glu_gyb", [128, KD, NT], BF16, es)
        b_gyb = [Buf(f"glu_gyb{t}") for t in range(NTT)]
        st = [P.sb(f"glu_st{i}", [128, TT], F32, es) for i in range(2)]
        b_st = [Buf(f"glu_st{i}") for i in range(2)]
        gv = gy_dram.rearrange("(k p) t -> p k t", p=128)
        n = 0
        for tt in range(NTT):
            ts = slice(tt * TT, (tt + 1) * TT)
            for k in range(KD):
                s = n % 2
                n += 1
                P.dma("sp", lambda e, s=s, k=k, ts=ts: e.dma_start(out=st[s][:], in_=gv[:, k, ts]), b_st[s], writes=[b_st[s]])
                P.op("pool", lambda e, s=s, k=k, ts=ts: e.tensor_copy(out=gyb[:, k, ts], in_=st[s][:]),
                     reads=[b_st[s]], writes=[b_gyb[tt]])
        w_f = [P.sb(f"glu_wf{i}", [128, KD, 256], F32, es) for i in range(2)]
        w_b = [P.sb(f"glu_wb{i}", [128, KD, 256], BF16, es) for i in range(2)]
        b_wf = [Buf(f"glu_wf{i}") for i in range(2)]
        b_wb = [Buf(f"glu_wb{i}") for i in range(2)]
        sg = [P.sb(f"glu_sg{i}", [128, TT], F32, es) for i in range(2)]
        b_sg = [Buf(f"glu_sg{i}") for i in range(2)]
        wv = wglu.rearrange("(k p) n -> p k n", p=128)
        q = 0
        for m in range(KD):
            s = m % 2
            P.dma("sp", lambda e, s=s, m=m: e.dma_start(out=w_f[s][:, :, 0:128], in_=wv[:, :, m * 128:(m + 1) * 128]),
                  b_wf[s], writes=[b_wf[s]])
            P.dma("sp", lambda e, s=s, m=m: e.dma_start(out=w_f[s][:, :, 128:256], in_=wv[:, :, D + m * 128:D + (m + 1) * 128]),
                  b_wf[s], writes=[])
            b_wf[s].w = ("d", b_wf[s], b_wf[s].dcount)
            P.op("pool", lambda e, s=s: e.tensor_copy(out=w_b[s][:], in_=w_f[s][:]), reads=[b_wf[s]], writes=[b_wb[s]])
            for tt in range(NTT):
                ts = slice(tt * TT, (tt + 1) * TT)
                pv, b_pv = C.next_ps()
                pg, b_pg = C.next_ps()
                for k in range(KD):
                    P.op("pe", lambda e, pv=pv, s=s, k=k, ts=ts: e.matmul(pv[:], lhsT=w_b[s][:, k, 0:128], rhs=gyb[:, k, ts],
                                                                         start=(k == 0), stop=(k == KD - 1)),
                         reads=[b_wb[s], b_gyb[tt]], writes=[b_pv])
                for k in range(KD):
                    P.op("pe", lambda e, pg=pg, s=s, k=k, ts=ts: e.matmul(pg[:], lhsT=w_b[s][:, k, 128:256], rhs=gyb[:, k, ts],
                                                                         start=(k == 0), stop=(k == KD - 1)),
                         reads=[b_wb[s], b_gyb[tt]], writes=[b_pg])
                qq = q % 2
                q += 1
                P.op("act", lambda e, qq=qq, pg=pg: e.activation(out=sg[qq][:], in_=pg[:], func=AF.Sigmoid),
                     reads=[b_pg], writes=[b_sg[qq]])
                P.op("dve", lambda e, qq=qq, pv=pv: e.tensor_tensor(out=sg[qq][:], in0=pv[:], in1=sg[qq][:], op=ALU.mult),
                     reads=[b_pv, b_sg[qq]], writes=[b_sg[qq]])
                P.op("pool", lambda e, qq=qq, m=m, ts=ts: e.tensor_tensor(out=xT[:, m, ts], in0=xT[:, m, ts], in1=sg[qq][:], op=ALU.add),
                     reads=[b_sg[qq], b_x[m][tt]], writes=[b_x[m][tt]])
    P.barrier()


def build_glu_ffn_prog():
    nc = bass.Bass("TRN2", target_bir_lowering=False)
    x = nc.dram_tensor("xT", [D, NT], F32, kind="ExternalInput").ap()
    gy = nc.dram_tensor("gyT", [D, NT], F32, kind="ExternalInput").ap()
    wglu = nc.dram_tensor("wglu", [D, 2 * D], F32, kind="ExternalInput").ap()
    wgu = nc.dram_tensor("wgu", [D, 2 * DFF], F32, kind="ExternalInput").ap()
    wd = nc.dram_tensor("wd", [DFF, D], F32, kind="ExternalInput").ap()
    gain = nc.dram_tensor("gain", [128, KD], F32, kind="ExternalInput").ap()
    y = nc.dram_tensor("yT", [D, NT], F32, kind="ExternalOutput").ap()
    with ExitStack() as es:
        P = Prog(nc, es)
        C = Ctx(P)
        xT = P.sb("xT_sb", [128, KD, NT], F32)
        b_x = [[Buf(f"x{k}_{t}") for t in range(NTT)] for k in range(KD)]
        g_sb = P.sb("gain_sb", [128, KD], F32)
        b_g = Buf("gain")
        P.dma("sp", lambda e: e.dma_start(out=g_sb[:], in_=gain[:]), b_g, writes=[b_g])
        load_xT(P, xT, b_x, x)
        emit_glu(P, C, xT, b_x, gy, wglu)
        emit_ffn(P, C, xT, b_x, wgu, wd, g_sb, b_g, 0)
        store_xT(P, xT, b_x, y)
        P.emit()
    return nc


_PROGS = {}


def _prog(name, builder):
    if name not in _PROGS:
        _PROGS[name] = builder()
    return _PROGS[name]


def _run(name, builder, in_maps):
    import time
    t0 = time.time()
    nc = _prog(name, builder)
    t1 = time.time()
    import os
    r = run_bass_kernel_spmd(nc, in_maps, core_ids=list(range(NCORES)), **({"trace": True} if os.environ.get("KPROF") else {}))
    res = r.results
    nb = sum(v.nbytes for m in in_maps for v in m.values())
    print(f"[launch {name}] build {t1 - t0:.1f}s run {time.time() - t1:.1f}s in_bytes {nb / 1e6:.0f}MB exec_ns {r.exec_time_ns}", flush=True)
    return res


def run_s5_layer(xT_parts, j, i, inp):
    gm = col_layout(inp["norm_mix"][i])
    res = _run("prenorm", build_prenorm_prog, [{"xT": xT_parts[c], "gain": gm} for c in range(NCORES)])
    h_full = from_core_T([r["hT"] for r in res])
    tau = np.tile(np.arange(S5T, dtype=np.float32)[None], (128, 1))
    maps = []
    for c in range(NCORES):
        par, bt, ct = s5_host_layout(inp["s5_lambda_re"][j], inp["s5_lambda_im"][j], inp["s5_log_dt"][j],
                                     inp["s5_b_re"][j], inp["s5_b_im"][j], inp["s5_c_re"][j], inp["s5_c_im"][j],
                                     inp["s5_d"][j], c)
        maps.append({"uT": np.ascontiguousarray(h_full[:, c * 128:(c + 1) * 128].T), "par": par, "bt": bt, "ct": ct, "tau": tau})
    res = _run("s5", build_s5_prog, maps)
    gy_full = np.concatenate([r["gyT"] for r in res], axis=0).T
    gf = col_layout(inp["norm_ffn"][i])
    maps = [{"xT": xT_parts[c], "gyT": to_core_T(gy_full, c), "wglu": inp["s5_w_glu"][j],
             "wgu": inp["ffn_w_gate_up"][i], "wd": inp["ffn_w_down"][i], "gain": gf} for c in range(NCORES)]
    res = _run("glu_ffn", build_glu_ffn_prog, maps)
    return [r["yT"] for r in res]


NH = 16
HD = 64
NIH = 8
PROJ = 3 * D + NIH * HD + HD + NIH


def build_dsa_proj_prog():
    nc = bass.Bass("TRN2", target_bir_lowering=False)
    x = nc.dram_tensor("xT", [D, NT], F32, kind="ExternalInput").ap()
    w_in = nc.dram_tensor("w_in", [D, PROJ], F32, kind="ExternalInput").ap()
    gain = nc.dram_tensor("gain", [128, KD], F32, kind="ExternalInput").ap()
    qk_g = nc.dram_tensor("qk_gain", [128, 2], F32, kind="ExternalInput").ap()
    cs_d = nc.dram_tensor("cossin", [128, 2, NT], F32, kind="ExternalInput").ap()
    cm_d = nc.dram_tensor("cmat", [128, 2, 128], F32, kind="ExternalInput").ap()
    qT_d = nc.dram_tensor("qT", [D, NT], BF16, kind="ExternalOutput").ap()
    kT_d = nc.dram_tensor("kT", [D, NT], BF16, kind="ExternalOutput").ap()
    v_d = nc.dram_tensor("v", [NT, NH * 65], BF16, kind="ExternalOutput").ap()
    qiT_d = nc.dram_tensor("qiT", [NIH * HD, NT], BF16, kind="ExternalOutput").ap()
    kiT_d = nc.dram_tensor("kiT", [HD, NT], BF16, kind="ExternalOutput").ap()
    w_d = nc.dram_tensor("w", [NT, NIH], F32, kind="ExternalOutput").ap()
    with ExitStack() as es:
        P = Prog(nc, es)
        C = Ctx(P)
        xT = P.sb("xT_sb", [128, KD, NT], F32)
        b_x = [[Buf(f"x{k}_{t}") for t in range(NTT)] for k in range(KD)]
        g_sb = P.sb("gain_sb", [128, KD], F32)
        qkg = P.sb("qkg_sb", [128, 2], F32)
        cs = P.sb("cs_sb", [128, 2, NT], F32)
        cm = P.sb("cm_sb", [128, 2, 128], F32)
        b_g, b_qkg, b_cs, b_cm = Buf("gain"), Buf("qkg"), Buf("cs"), Buf("cm")
        P.dma("sp", lambda e: e.dma_start(out=g_sb[:], in_=gain[:]), b_g, writes=[b_g])
        P.dma("sp", lambda e: e.dma_start(out=qkg[:], in_=qk_g[:]), b_qkg, writes=[b_qkg])
        P.dma("sp", lambda e: e.dma_start(out=cm[:], in_=cm_d[:]), b_cm, writes=[b_cm])
        P.dma("sp", lambda e: e.dma_start(out=cs[:, 0, :], in_=cs_d[:, 0, :]), b_cs, writes=[b_cs])
        P.dma("sp", lambda e: e.dma_start(out=cs[:, 1, :], in_=cs_d[:, 1, :]), b_cs, writes=[])
        b_cs.w = ("d", b_cs, b_cs.dcount)
        load_xT(P, xT, b_x, x)
        bones = P.sb("bones_bf", [128, 128], BF16)
        b_bones = Buf("bones")
        P.op("dve", lambda e: e.tensor_copy(out=bones[:], in_=cm[:, 0, :]), reads=[b_cm], writes=[b_bones])
        P.op("dve", lambda e: e.tensor_scalar(out=qkg[:, 0:1], in0=qkg[:, 0:1], scalar1=HD ** -0.5, scalar2=None, op0=ALU.mult),
             reads=[b_qkg], writes=[b_qkg])
        hT = P.sb("hT_sb", [128, KD, NT], BF16)
        b_h = [Buf(f"h{t}") for t in range(NTT)]
        emit_rmsnorm_T(P, C, xT, b_x, g_sb, b_g, 0, hT, b_h, list(range(NTT)), es, "pn")
        wv_ = w_in.rearrange("(k p) n -> p k n", p=128)
        w_f = [P.sb(f"pj_wf{i}", [128, KD, 128], F32) for i in range(2)]
        w_b = [P.sb(f"pj_wb{i}", [128, KD, 128], BF16) for i in range(2)]
        b_wf = [Buf(f"pj_wf{i}") for i in range(2)]
        b_wb = [Buf(f"pj_wb{i}") for i in range(2)]
        sq = [P.sb(f"pj_sq{i}", [128, TT], BF16) for i in range(2)]
        rs = [P.sb(f"pj_rs{i}", [128, TT], F32) for i in range(2)]
        tf = [P.sb(f"pj_t{i}", [128, TT], F32) for i in range(2)]
        o1 = [P.sb(f"pj_o1{i}", [128, TT], F32) for i in range(2)]
        o2 = [P.sb(f"pj_o2{i}", [128, TT], F32) for i in range(2)]
        ob = [P.sb(f"pj_ob{i}", [128, TT], BF16) for i in range(2)]
        b_sq = [Buf(f"pj_sq{i}") for i in range(2)]
        b_rs = [Buf(f"pj_rs{i}") for i in range(2)]
        b_tf = [Buf(f"pj_t{i}") for i in range(2)]
        b_o1 = [Buf(f"pj_o1{i}") for i in range(2)]
        b_o2 = [Buf(f"pj_o2{i}") for i in range(2)]
        b_ob = [Buf(f"pj_ob{i}") for i in range(2)]
        toks = []
        tiles = []
        for m in range(8):
            tiles.append((m * 128, 128, "norm", qT_d, m * 128, 0))
        for m in range(8):
            tiles.append((D + m * 128, 128, "norm", kT_d, m * 128, 1))
        for m in range(4):
            tiles.append((3 * D + m * 128, 128, "plain", qiT_d, m * 128, None))
        tiles.append((3 * D + NIH * HD, 64, "normnog", kiT_d, 0, None))
        it = 0
        for ti, (c0, M, kind, od, r0, gc) in enumerate(tiles):
            s = ti % 2
            P.dma("sp", lambda e, s=s, c0=c0, M=M: e.dma_start(out=w_f[s][:, :, 0:M], in_=wv_[:, :, c0:c0 + M]),
                  b_wf[s], writes=[b_wf[s]])
            P.op("pool", lambda e, s=s, M=M: e.tensor_copy(out=w_b[s][:, :, 0:M], in_=w_f[s][:, :, 0:M]),
                 reads=[b_wf[s]], writes=[b_wb[s]])
            for tt in range(NTT):
                ts = slice(tt * TT, (tt + 1) * TT)
                u = it % 2
                it += 1
                ps, b_ps = C.next_ps()
                for k in range(KD):
                    P.op("pe", lambda e, ps=ps, s=s, k=k, ts=ts, M=M: e.matmul(ps[0:M, :], lhsT=w_b[s][:, k, 0:M], rhs=hT[:, k, ts],
                                                                              start=(k == 0), stop=(k == KD - 1)),
                         reads=[b_wb[s], b_h[tt]], writes=[b_ps])
                if kind == "plain":
                    P.op("act", lambda e, u=u, ps=ps, M=M: e.activation(out=tf[u][0:M, :], in_=ps[0:M, :], func=AF.Copy),
                         reads=[b_ps], writes=[b_tf[u]])
                else:
                    P.op("act", lambda e, u=u, ps=ps, M=M: e.activation(out=sq[u][0:M, :], in_=ps[0:M, :], func=AF.Square),
                         reads=[b_ps], writes=[b_sq[u]])
                    p2, b_p2 = C.next_ps()
                    P.op("pe", lambda e, p2=p2, u=u, M=M: e.matmul(p2[0:M, :], lhsT=bones[0:M, 0:M], rhs=sq[u][0:M, :], start=True, stop=True),
                         reads=[b_bones, b_sq[u]], writes=[b_p2])
                    P.op("act", lambda e, u=u, p2=p2, M=M: e.activation(out=rs[u][0:M, :], in_=p2[0:M, :], func=AF.Sqrt, scale=1.0 / HD,
                                                                   bias=C.eps_col[0:M, :]),
                         reads=[b_p2, C.b_ones], writes=[b_rs[u]])
                    P.op("dve", lambda e, u=u, M=M: e.reciprocal(out=rs[u][0:M, :], in_=rs[u][0:M, :]), reads=[b_rs[u]], writes=[b_rs[u]])
                    if kind == "norm":
                        P.op("dve", lambda e, u=u, ps=ps, gc=gc, M=M: e.scalar_tensor_tensor(
                            out=tf[u][0:M, :], in0=ps[0:M, :], scalar=qkg[0:M, gc:gc + 1], in1=rs[u][0:M, :], op0=ALU.mult, op1=ALU.mult),
                            reads=[b_ps, b_qkg, b_rs[u]], writes=[b_tf[u]])
                    else:
                        P.op("dve", lambda e, u=u, ps=ps, M=M: e.tensor_tensor(out=tf[u][0:M, :], in0=ps[0:M, :], in1=rs[u][0:M, :], op=ALU.mult),
                             reads=[b_ps, b_rs[u]], writes=[b_tf[u]])
                p3, b_p3 = C.next_ps()
                P.op("pe", lambda e, p3=p3, u=u, M=M: e.matmul(p3[0:M, :], lhsT=cm[0:M, 1, 0:M], rhs=tf[u][0:M, :], start=True, stop=True),
                     reads=[b_cm, b_tf[u]], writes=[b_p3])
                P.op("pool", lambda e, u=u, ts=ts, M=M: e.tensor_tensor(out=o1[u][0:M, :], in0=tf[u][0:M, :], in1=cs[0:M, 0, ts], op=ALU.mult),
                     reads=[b_tf[u], b_cs], writes=[b_o1[u]])
                P.op("dve", lambda e, u=u, ts=ts, p3=p3, M=M: e.tensor_tensor(out=o2[u][0:M, :], in0=p3[0:M, :], in1=cs[0:M, 1, ts], op=ALU.mult),
                     reads=[b_p3, b_cs], writes=[b_o2[u]])
                P.op("pool", lambda e, u=u, M=M: e.tensor_tensor(out=ob[u][0:M, :], in0=o1[u][0:M, :], in1=o2[u][0:M, :], op=ALU.add),
                     reads=[b_o1[u], b_o2[u]], writes=[b_ob[u]])
                toks.append(P.dma("sp", lambda e, u=u, od=od, r0=r0, ts=ts, M=M: e.dma_start(out=od[r0:r0 + M, ts], in_=ob[u][0:M, :]),
                                  b_ob[u], reads=[b_ob[u]]))
        wvf = [P.sb(f"pj_vf{i}", [128, KD, 512], F32) for i in range(1)]
        wvb = [P.sb(f"pj_vb{i}", [128, KD, 512], BF16) for i in range(2)]
        b_wvf = [Buf("pj_vf0")]
        b_wvb = [Buf(f"pj_vb{i}") for i in range(2)]
        vo = [P.sb(f"pj_vo{i}", [128, 8, 65], BF16) for i in range(2)]
        b_vo = [Buf(f"pj_vo{i}") for i in range(2)]
        for i_ in range(2):
            P.op("pool", lambda e, i_=i_: e.memset(vo[i_][:], 1.0), writes=[b_vo[i_]])
        for hf in range(2):
            P.dma("sp", lambda e, hf=hf: e.dma_start(out=wvf[0][:], in_=wv_[:, :, 2 * D + hf * 512:2 * D + (hf + 1) * 512]),
                  b_wvf[0], writes=[b_wvf[0]])
            P.op("pool", lambda e, hf=hf: e.tensor_copy(out=wvb[hf][:], in_=wvf[0][:]), reads=[b_wvf[0]], writes=[b_wvb[hf]])
        ww_f = P.sb("pj_wwf", [128, KD, NIH], F32)
        ww_b = P.sb("pj_wwb", [128, KD, NIH], BF16)
        b_wwf, b_wwb = Buf("pj_wwf"), Buf("pj_wwb")
        P.dma("sp", lambda e: e.dma_start(out=ww_f[:], in_=wv_[:, :, PROJ - NIH:PROJ]), b_wwf, writes=[b_wwf])
        P.op("pool", lambda e: e.tensor_copy(out=ww_b[:], in_=ww_f[:]), reads=[b_wwf], writes=[b_wwb])
        wo_sb = P.sb("pj_wo", [128, NT // 128, NIH], F32)
        b_wo = Buf("pj_wo")
        n = 0
        for blk in range(NT // 128):
            tt = blk // 4
            bs = slice(blk * 128, (blk + 1) * 128)
            for hf in range(2):
                u = n % 2
                n += 1
                ps, b_ps = C.next_ps()
                for k in range(KD):
                    P.op("pe", lambda e, ps=ps, k=k, bs=bs, hf=hf: e.matmul(ps[:], lhsT=hT[:, k, bs], rhs=wvb[hf][:, k, :],
                                                                           start=(k == 0), stop=(k == KD - 1)),
                         reads=[b_wvb[hf], b_h[tt]], writes=[b_ps])
                P.op("act", lambda e, u=u, ps=ps: e.activation(out=vo[u][:, :, 0:64], in_=ps[:].rearrange("p (h d) -> p h d", h=8), func=AF.Copy),
                     reads=[b_ps], writes=[b_vo[u]])
                toks.append(P.dma("sp", lambda e, u=u, bs=bs, hf=hf: e.dma_start(out=v_d[bs, hf * 520:(hf + 1) * 520],
                                                                                  in_=vo[u][:].rearrange("p h d -> p (h d)")),
                                  b_vo[u], reads=[b_vo[u]]))
            ps, b_ps = C.next_ps()
            for k in range(KD):
                P.op("pe", lambda e, ps=ps, k=k, bs=bs: e.matmul(ps[:, 0:NIH], lhsT=hT[:, k, bs], rhs=ww_b[:, k, :],
                                                                 start=(k == 0), stop=(k == KD - 1)),
                     reads=[b_wwb, b_h[tt]], writes=[b_ps])
            P.op("act", lambda e, ps=ps, blk=blk: e.activation(out=wo_sb[:, blk, :], in_=ps[:, 0:NIH], func=AF.Copy,
                                                               scale=(NIH ** -0.5) * (HD ** -0.5)),
                 reads=[b_ps], writes=[b_wo])
        toks.append(P.dma("sp", lambda e: e.dma_start(out=w_d.rearrange("(b p) h -> p b h", p=128), in_=wo_sb[:]), b_wo, reads=[b_wo]))
        P.finish(toks)
        P.emit()
    return nc


def rope_consts(core):
    blocks = np.arange(SEQ // 128)[core::NCORES]
    pos = (blocks[:, None] * 128 + np.arange(128)[None, :]).reshape(-1).astype(np.float32)
    inv_freq = (10000.0 ** (-np.arange(0, HD, 2, dtype=np.float32) / HD)).astype(np.float32)
    ang = pos[None, :] * inv_freq[:, None]
    cos, sin = np.cos(ang).astype(np.float32), np.sin(ang).astype(np.float32)
    cs = np.empty((128, 2, NT), np.float32)
    for p in range(128):
        cs[p, 0] = cos[p % 32]
        cs[p, 1] = sin[p % 32]
    return cs


def const_mats():
    cm = np.zeros((128, 2, 128), np.float32)
    for p in range(128):
        for m in range(128):
            if p // 64 == m // 64:
                cm[p, 0, m] = 1.0
    for m in range(128):
        if (m % 64) < 32:
            cm[m + 32, 1, m] = -1.0
        else:
            cm[m - 32, 1, m] = 1.0
    return cm


TOPK = 256
NEG_SEL = -1.0e30
NEG_MASK = -2.0e30


def build_dsa_attn_prog(nblk=NT // 128):
    U8 = mybir.dt.uint8
    NBIS = 16
    nc = bass.Bass("TRN2", target_bir_lowering=False)
    x_d = nc.dram_tensor("xT", [D, NT], F32, kind="ExternalInput").ap()
    qT_d = nc.dram_tensor("qT", [D, NT], BF16, kind="ExternalInput").ap()
    qiT_d = nc.dram_tensor("qiT", [NIH * HD, NT], BF16, kind="ExternalInput").ap()
    w_d = nc.dram_tensor("wq", [128, NT // 128, NIH], F32, kind="ExternalInput").ap()
    kT_d = nc.dram_tensor("kTf", [D, SEQ], BF16, kind="ExternalInput").ap()
    v_d = nc.dram_tensor("vf", [SEQ, NH * 65], BF16, kind="ExternalInput").ap()
    kiT_d = nc.dram_tensor("kiTf", [HD, SEQ], BF16, kind="ExternalInput").ap()
    pen_d = nc.dram_tensor("pen", [128, 1024], F32, kind="ExternalInput").ap()
    id_d = nc.dram_tensor("ident", [128, 128], F32, kind="ExternalInput").ap()
    wo_d = nc.dram_tensor("w_o", [D, D], F32, kind="ExternalInput").ap()
    y_d = nc.dram_tensor("yT", [D, NT], F32, kind="ExternalOutput").ap()
    qT_v = qT_d.rearrange("(h p) t -> p h t", p=64)
    qiT_v = qiT_d.rearrange("(h p) t -> p h t", p=64)
    kT_v = kT_d.rearrange("(pr p) t -> p pr t", p=128)
    qT_pv = qT_d.rearrange("(pr two p) t -> two p pr t", two=2, p=64)
    x_v = x_d.rearrange("(k p) t -> p k t", p=128)
    y_v = y_d.rearrange("(k p) t -> p k t", p=128)
    wo_v = wo_d.rearrange("(h p) n -> p h n", p=64)
    with ExitStack() as es:
        P = Prog(nc, es)
        ones = P.sb("c_ones", [128, 64], BF16)
        half_c = P.sb("c_half", [128, 1], F32)
        b_c = Buf("consts")
        P.op("pool", lambda e: e.memset(ones[:], 1.0), writes=[b_c])
        P.op("pool", lambda e: e.memset(half_c[:], 0.5), writes=[b_c])
        pen = P.sb("pen_sb", [128, 1024], F32)
        idf = P.sb("id_f", [128, 128], F32)
        idb = P.sb("id_b", [128, 128], BF16)
        wq = P.sb("wq_sb", [128, NT // 128, NIH], F32)
        b_pen, b_id, b_wq = Buf("pen"), Buf("ident"), Buf("wq")
        P.dma("sp", lambda e: e.dma_start(out=pen[:], in_=pen_d[:]), b_pen, writes=[b_pen])
        P.dma("sp", lambda e: e.dma_start(out=idf[:], in_=id_d[:]), b_id, writes=[b_id])
        P.dma("sp", lambda e: e.dma_start(out=wq[:], in_=w_d[:]), b_wq, writes=[b_wq])
        P.op("pool", lambda e: e.tensor_copy(out=idb[:], in_=idf[:]), reads=[b_id], writes=[b_id])
        wob = P.sb("wo_b", [64, NH, D], BF16)
        wof = P.sb("wo_f", [64, NH, 64], F32)
        b_wob, b_wof = Buf("wo_b"), Buf("wo_f")
        for m in range(2 * KD):
            P.dma("sp", lambda e, m=m: e.dma_start(out=wof[:], in_=wo_v[:, :, m * 64:(m + 1) * 64]), b_wof, writes=[b_wof])
            P.op("pool", lambda e, m=m: e.tensor_copy(out=wob[:, :, m * 64:(m + 1) * 64], in_=wof[:]), reads=[b_wof], writes=[b_wob])
        score = P.sb("score", [128, SEQ], F32)
        b_score = Buf("score")
        junk = P.sb("junk", [128, SEQ], U8)
        b_junk = Buf("junk")
        maskT = P.sb("maskT", [128, SEQ // 128, 128], U8)
        b_maskT = Buf("maskT")
        qbd = P.sb("qbd", [128, NH // 2, 256], BF16)
        qih = P.sb("qih", [64, NIH, 128], BF16)
        b_qh, b_qih = Buf("qh"), Buf("qih")
        P.op("pool", lambda e: e.memset(qbd[:], 0.0), writes=[b_qh])
        kib = [P.sb(f"kib{i}", [64, 512], BF16) for i in range(2)]
        b_kib = [Buf(f"kib{i}") for i in range(2)]
        tmp = [P.sb(f"itmp{i}", [128, 512], F32) for i in range(2)]
        b_tmp = [Buf(f"itmp{i}") for i in range(2)]
        m8 = P.sb("m8", [128, 8], F32)
        bis = P.sb("bis", [128, 8], F32)
        b_bis = Buf("bis")
        LO, HI, MID, CNT, GE, D1, D2 = (bis[:, j:j + 1] for j in range(7))
        mk = [P.sb(f"mk{i}", [128, 512], BF16) for i in range(2)]
        b_mk = [Buf(f"mk{i}") for i in range(2)]
        kTc = [P.sb(f"kTc{i}", [128, 4, 256], BF16) for i in range(2)]
        vc = [P.sb(f"vc{i}", [128, 2, 520], BF16) for i in range(2)]
        b_kTc = [Buf(f"kTc{i}") for i in range(2)]
        sel = P.sb("sel", [65, 64], F32)
        P.op("pool", lambda e: e.memset(sel[:], 0.0), writes=[b_c])
        P.op("pool", lambda e: e.memset(sel[64:65, :], 1.0), writes=[b_c])
        accs = [P.sb(f"accs{i}", [65, 512], F32) for i in range(2)]
        b_accs = [Buf(f"accs{i}") for i in range(2)]
        b_vc = [Buf(f"vc{i}") for i in range(2)]
        pT = [P.sb(f"pT{i}", [128, 512], BF16) for i in range(3)]
        b_pT = [Buf(f"pT{i}") for i in range(3)]
        attn = P.sb("attn", [64, NH, 128], BF16)
        b_attn = Buf("attn")
        dsb = [P.sb(f"dsb{i}", [64, 512], F32) for i in range(2)]
        b_dsb = [Buf(f"dsb{i}") for i in range(2)]
        xq = P.sb("xq", [128, KD, 128], F32)
        b_xq = Buf("xq")
        psA = [P.ps(f"psA{i}", [128, 512], F32) for i in range(3)]
        b_psA = [Buf(f"psA{i}") for i in range(3)]
        psT = P.ps("psT", [128, 1024], BF16)
        b_psT = Buf("psT")
        acc = [P.ps(f"acc{i}", [128, 512], F32) for i in range(2)]
        b_acc = [Buf(f"acc{i}") for i in range(2)]
        den = [P.ps(f"den{i}", [128, 512], F32) for i in range(2)]
        b_den = [Buf(f"den{i}") for i in range(2)]
        rr = {"a": 0, "kib": 0, "tmp": 0, "mk": 0, "kv": 0, "pT": 0}

        def nxt(key, n):
            v = rr[key]
            rr[key] = (v + 1) % n
            return v

        def phase_A(i):
            qs = slice(i * 128, (i + 1) * 128)
            Lk = 1024 * (i + 1)
            P.dma("sp", lambda e: e.dma_start(out=qih[:], in_=qiT_v[:, :, qs]), b_qih, writes=[b_qih])
            for j in range(Lk // 512):
                cs_ = slice(j * 512, (j + 1) * 512)
                kb = nxt("kib", 2)
                P.dma("sp", lambda e, kb=kb, cs_=cs_: e.dma_start(out=kib[kb][:], in_=kiT_d[:, cs_]), b_kib[kb], writes=[b_kib[kb]])
                for h in range(NIH):
                    a = nxt("a", 3)
                    P.op("pe", lambda e, a=a, h=h, kb=kb: e.matmul(psA[a][:], lhsT=qih[:, h, :], rhs=kib[kb][:], start=True, stop=True),
                         reads=[b_qih, b_kib[kb]], writes=[b_psA[a]])
                    if h == 0:
                        P.op("dve", lambda e, a=a, cs_=cs_: e.tensor_scalar(
                            out=score[:, cs_], in0=psA[a][:], scalar1=0.0, scalar2=wq[:, i, 0:1], op0=ALU.max, op1=ALU.mult),
                            reads=[b_psA[a], b_wq], writes=[b_score])
                    else:
                        t = nxt("tmp", 2)
                        P.op("dve", lambda e, a=a, t=t, h=h: e.tensor_scalar(
                            out=tmp[t][:], in0=psA[a][:], scalar1=0.0, scalar2=wq[:, i, h:h + 1], op0=ALU.max, op1=ALU.mult),
                            reads=[b_psA[a], b_wq], writes=[b_tmp[t]])
                        P.op("pool", lambda e, t=t, cs_=cs_: e.tensor_tensor(out=score[:, cs_], in0=score[:, cs_], in1=tmp[t][:], op=ALU.add),
                             reads=[b_tmp[t], b_score], writes=[b_score])
            P.op("dve", lambda e: e.tensor_reduce(out=LO, in_=score[:, 0:Lk], axis=AX.X, op=ALU.min), reads=[b_score], writes=[b_bis])
            P.op("pool", lambda e: e.tensor_tensor(out=score[:, Lk - 1024:Lk], in0=score[:, Lk - 1024:Lk], in1=pen[:], op=ALU.add),
                 reads=[b_score, b_pen], writes=[b_score])
            P.op("dve", lambda e: e.max(out=m8[:], in_=score[:, 0:Lk]), reads=[b_score], writes=[b_bis])
            P.op("dve", lambda e: e.tensor_copy(out=HI, in_=m8[:, 0:1]), reads=[b_bis], writes=[b_bis])

        cntp = P.sb("cntp", [128, 8], F32)

        def bis_items(i):
            Lk = 1024 * (i + 1)
            npz = (Lk + 2047) // 2048
            V_ = lambda fn, rd=(): P.op("dve", fn, reads=[b_bis, b_c] + list(rd), writes=[b_bis])

            def piece(p):
                c0, c1 = p * 2048, min(Lk, (p + 1) * 2048)
                if p == 0:
                    V_(lambda e: e.scalar_tensor_tensor(out=MID, in0=LO, scalar=HI, in1=half_c[:], op0=ALU.add, op1=ALU.mult))
                P.op("dve", lambda e: e.tensor_scalar(out=junk[:, c0:c1], in0=score[:, c0:c1], scalar1=MID, scalar2=0.0,
                                                      op0=ALU.is_ge, op1=ALU.add, accum_out=cntp[:, p:p + 1]),
                     reads=[b_score, b_bis], writes=[b_junk, b_bis])

            def tail():
                V_(lambda e: e.tensor_reduce(out=CNT, in_=cntp[:, 0:npz], axis=AX.X, op=ALU.add))
                V_(lambda e: e.tensor_single_scalar(out=GE, in_=CNT, scalar=TOPK - 0.5, op=ALU.is_ge))
                V_(lambda e: e.tensor_tensor(out=D1, in0=MID, in1=LO, op=ALU.subtract))
                V_(lambda e: e.tensor_tensor(out=D2, in0=HI, in1=MID, op=ALU.subtract))
                V_(lambda e: e.scalar_tensor_tensor(out=LO, in0=D1, scalar=GE, in1=LO, op0=ALU.mult, op1=ALU.add))
                V_(lambda e: e.scalar_tensor_tensor(out=HI, in0=D2, scalar=GE, in1=MID, op0=ALU.mult, op1=ALU.add))

            items = []
            for p in range(npz):
                items.append(lambda p=p: piece(p))
            items.append(tail)
            return items

        def phase_C(i):
            Lk = 1024 * (i + 1)
            for j in range(Lk // 512):
                cs_ = slice(j * 512, (j + 1) * 512)
                u = nxt("mk", 2)
                P.op("pool", lambda e, u=u, cs_=cs_: e.tensor_scalar(out=mk[u][:], in0=score[:, cs_], scalar1=LO, scalar2=None, op0=ALU.is_ge),
                     reads=[b_score, b_bis], writes=[b_mk[u]])
                for jj in range(4):
                    P.op("pe", lambda e, u=u, jj=jj: e.transpose(out=psT[:, jj * 128:(jj + 1) * 128], in_=mk[u][:, jj * 128:(jj + 1) * 128],
                                                                 identity=idb[:]),
                         reads=[b_mk[u], b_id], writes=[b_psT])
                P.op("act", lambda e, j=j: e.activation(out=maskT[:, 4 * j:4 * j + 4, :].rearrange("p a b -> p (a b)"), in_=psT[:, 0:512],
                                                        func=AF.Copy),
                     reads=[b_psT], writes=[b_maskT])

        def phase_D(i, side):
            qs = slice(i * 128, (i + 1) * 128)
            nkc = 8 * (i + 1)
            P.dma("sp", lambda e: e.dma_start(out=qbd[0:64, :, 0:128], in_=qT_pv[0][:, :, qs]), b_qh, writes=[b_qh])
            P.dma("sp", lambda e: e.dma_start(out=qbd[64:128, :, 128:256], in_=qT_pv[1][:, :, qs]), b_qh, writes=[])
            b_qh.w = ("d", b_qh, b_qh.dcount)
            groups = [(half, kc, hg) for half in range(2) for kc in range(nkc) for hg in range(2)]
            st = {}

            def emit_S(gi):
                half, kc, hg = groups[gi]
                kk = kc % 2
                if kk == 0 and hg == 0:
                    s_ = nxt("kv", 2)
                    r0 = kc * 128
                    P.dma("sp", lambda e: e.dma_start(out=kTc[s_][:], in_=kT_v[:, half * 4:(half + 1) * 4, r0:r0 + 256]),
                          b_kTc[s_], writes=[b_kTc[s_]])
                    P.dma("sp", lambda e: e.dma_start(
                        out=vc[s_][:], in_=v_d[r0:r0 + 256, half * 520:(half + 1) * 520].rearrange("(c p) n -> p c n", p=128)),
                        b_vc[s_], writes=[b_vc[s_]])
                    st["slot", half, kc // 2] = s_
                s_ = st["slot", half, kc // 2]
                a = nxt("a", 3)
                for pl2 in range(2):
                    pl = hg * 2 + pl2
                    pair = half * 4 + pl
                    P.op("pe", lambda e, pl2=pl2, pl=pl, pair=pair: e.matmul(
                        psA[a][:, pl2 * 256:(pl2 + 1) * 256], lhsT=kTc[s_][:, pl, kk * 128:(kk + 1) * 128], rhs=qbd[:, pair, :],
                        start=True, stop=True),
                        reads=[b_kTc[s_], b_qh], writes=[b_psA[a]])
                st["a", gi] = a

            def emit_rest(gi):
                half, kc, hg = groups[gi]
                kk = kc % 2
                s_ = st["slot", half, kc // 2]
                a = st["a", gi]
                u = nxt("pT", 3)
                P.op("act", lambda e: e.activation(out=pT[u][:], in_=psA[a][:], func=AF.Exp),
                     reads=[b_psA[a]], writes=[b_pT[u]])
                P.op("dve", lambda e: e.tensor_tensor(
                    out=pT[u][:].rearrange("p (h q) -> p h q", h=4), in0=pT[u][:].rearrange("p (h q) -> p h q", h=4),
                    in1=maskT[:, kc, :].unsqueeze(1).to_broadcast([128, 4, 128]), op=ALU.mult),
                    reads=[b_pT[u], b_maskT], writes=[b_pT[u]])
                for hh in range(4):
                    h8 = hg * 4 + hh
                    P.op("pe", lambda e, hh=hh, h8=h8: e.matmul(
                        acc[hg][0:65, hh * 128:(hh + 1) * 128], lhsT=vc[s_][:, kk, h8 * 65:(h8 + 1) * 65],
                        rhs=pT[u][:, hh * 128:(hh + 1) * 128], start=(kc == 0 and hh == 0), stop=(kc == nkc - 1 and hh == 3),
                        skip_group_check=True),
                        reads=[b_vc[s_], b_pT[u]], writes=[b_acc[hg]])
                if kc == nkc - 1:
                    h0 = half * 8 + hg * 4
                    P.op("act", lambda e: e.activation(out=accs[hg][:], in_=acc[hg][0:65, :], func=AF.Copy),
                         reads=[b_acc[hg]], writes=[b_accs[hg]])
                    P.op("pe", lambda e: e.matmul(den[hg][0:64, :], lhsT=sel[:], rhs=accs[hg][:], start=True, stop=True),
                         reads=[b_c, b_accs[hg]], writes=[b_den[hg]])
                    P.op("act", lambda e: e.activation(out=dsb[hg][:], in_=den[hg][0:64, :], func=AF.Copy),
                         reads=[b_den[hg]], writes=[b_dsb[hg]])
                    P.op("dve", lambda e: e.reciprocal(out=dsb[hg][:], in_=dsb[hg][:]), reads=[b_dsb[hg]], writes=[b_dsb[hg]])
                    P.op("dve", lambda e: e.tensor_tensor(
                        out=attn[:, h0:h0 + 4, :].rearrange("p a b -> p (a b)"), in0=accs[hg][0:64, :], in1=dsb[hg][:], op=ALU.mult),
                        reads=[b_accs[hg], b_dsb[hg]], writes=[b_attn])

            G = len(groups)
            done = 0
            emit_S(0)
            for gi in range(G):
                if gi + 1 < G:
                    emit_S(gi + 1)
                emit_rest(gi)
                want = (len(side) * (gi + 1)) // G
                while done < want:
                    side[done]()
                    done += 1
            while done < len(side):
                side[done]()
                done += 1

        def phase_E(i):
            qs = slice(i * 128, (i + 1) * 128)
            P.dma("sp", lambda e: e.dma_start(out=xq[:], in_=x_v[:, :, qs]), b_xq, writes=[b_xq])
            for m in range(KD):
                a = nxt("a", 3)
                for h in range(NH):
                    P.op("pe", lambda e, a=a, h=h, m=m: e.matmul(psA[a][:, 0:128], lhsT=wob[:, h, m * 128:(m + 1) * 128], rhs=attn[:, h, :],
                                                                 start=(h == 0), stop=(h == NH - 1)),
                         reads=[b_wob, b_attn], writes=[b_psA[a]])
                P.op("dve", lambda e, a=a, m=m: e.tensor_tensor(out=xq[:, m, :], in0=psA[a][:, 0:128], in1=xq[:, m, :], op=ALU.add),
                     reads=[b_psA[a], b_xq], writes=[b_xq])
            return P.dma("sp", lambda e: e.dma_start(out=y_v[:, :, qs], in_=xq[:]), b_xq, reads=[b_xq])

        phase_A(0)
        for _ in range(NBIS):
            for f in bis_items(0):
                f()
        phase_C(0)
        tok = None
        for i in range(nblk):
            side = []
            if i + 1 < nblk:
                phase_A(i + 1)
                side = [f for _ in range(NBIS) for f in bis_items(i + 1)]
            phase_D(i, side)
            tok = phase_E(i)
            if i + 1 < nblk:
                phase_C(i + 1)
        P.finish([tok])
        P.emit()
    return nc


def causal_pen(core):
    j = np.arange(1024)[None, :]
    p = np.arange(128)[:, None]
    return np.where(j > 128 * core + p, np.float32(NEG_MASK), np.float32(0.0)).astype(np.float32)


def gather_tokens_T(parts):
    f = parts[0].shape[0]
    out = np.empty((f, SEQ // 128, 128), parts[0].dtype)
    for c, p in enumerate(parts):
        out[:, c::NCORES, :] = p.reshape(f, NT // 128, 128)
    return out.reshape(f, SEQ)


def gather_tokens(parts):
    f = parts[0].shape[1]
    out = np.empty((SEQ // 128, 128, f), parts[0].dtype)
    for c, p in enumerate(parts):
        out[c::NCORES] = p.reshape(NT // 128, 128, f)
    return out.reshape(SEQ, f)


def run_dsa_layer(xT_parts, j, i, inp):
    gm = col_layout(inp["norm_mix"][i])
    qkg = np.stack([np.tile(inp["dsa_q_norm"][j], 2), np.tile(inp["dsa_k_norm"][j], 2)], axis=1).astype(np.float32)
    cm = const_mats()
    maps = [{"xT": xT_parts[c], "w_in": inp["dsa_w_in"][j], "gain": gm, "qk_gain": qkg, "cossin": rope_consts(c), "cmat": cm}
            for c in range(NCORES)]
    pr = _run("dsa_proj", build_dsa_proj_prog, maps)
    kTf = gather_tokens_T([r["kT"] for r in pr])
    kiTf = gather_tokens_T([r["kiT"] for r in pr])
    vf = gather_tokens([r["v"] for r in pr])
    ident = np.eye(128, dtype=np.float32)
    maps = []
    for c in range(NCORES):
        wq = np.ascontiguousarray(pr[c]["w"].reshape(NT // 128, 128, NIH).transpose(1, 0, 2))
        maps.append({"xT": xT_parts[c], "qT": pr[c]["qT"], "qiT": pr[c]["qiT"], "wq": wq, "kTf": kTf, "vf": vf, "kiTf": kiTf,
                     "pen": causal_pen(c), "ident": ident, "w_o": inp["dsa_w_o"][j]})
    ar = _run("dsa_attn", build_dsa_attn_prog, maps)
    gf = col_layout(inp["norm_ffn"][i])
    maps = [{"xT": ar[c]["yT"], "wgu": inp["ffn_w_gate_up"][i], "wd": inp["ffn_w_down"][i], "gain": gf} for c in range(NCORES)]
    fr = _run("ffn", build_ffn_prog, maps)
    return [r["yT"] for r in fr]


def kernel(**inputs):
    inp = {k: np.asarray(v) for k, v in inputs.items()}
    x = np.ascontiguousarray(inp["x"][0], dtype=np.float32)
    parts = [to_core_T(x, c) for c in range(NCORES)]
    for i in range(4):
        if i % 2 == 0:
            parts = run_s5_layer(parts, i // 2, i, inp)
        else:
            parts = run_dsa_layer(parts, i // 2, i, inp)
    return from_core_T(parts)[None].astype(np.float32)
```

```python
import math
from contextlib import ExitStack

import numpy as np
import concourse.bass as bass
import concourse.mybir as mybir
from concourse.bass_utils import run_bass_kernel_spmd

F32 = mybir.dt.float32
BF16 = mybir.dt.bfloat16
ALU = mybir.AluOpType
AF = mybir.ActivationFunctionType
AX = mybir.AxisListType

NCORES = 8
D = 1024
KD = D // 128
SEQ = 16384
NT = SEQ // NCORES
TT = 512
NTT = NT // TT
DFF = 2816
KF = DFF // 128
EPS = 1e-6

ENGS = ("pe", "act", "dve", "pool", "sp")
SAME_ENGINE_SYNC = ("act", "dve", "pool")


class Buf:
    __slots__ = ("name", "w", "r", "dsem", "dcount")

    def __init__(self, name):
        self.name = name
        self.w = None
        self.r = {}
        self.dsem = None
        self.dcount = 0


class Prog:
    def __init__(self, nc, es):
        self.nc = nc
        self.es = es
        self.ops = {e: [] for e in ENGS}
        self.dma_bufs = []
        self.final_tokens = []
        self.bar = []
        self.bar_epoch = 0
        self.eng_epoch = {e: 0 for e in ENGS}

    def sb(self, name, shape, dt, es=None):
        return (es or self.es).enter_context(self.nc.sbuf_tensor(name, list(shape), dt))

    def ps(self, name, shape, dt=F32, es=None):
        return (es or self.es).enter_context(self.nc.psum_tensor(name, list(shape), dt))

    def _deps(self, reads, writes):
        need = []
        for b in reads:
            if b.w is not None:
                need.append(b.w)
        for b in writes:
            if b.w is not None:
                need.append(b.w)
            need.extend(b.r.values())
        return need

    def barrier(self):
        toks = []
        for e in ENGS:
            for idx in range(len(self.ops[e]) - 1, -1, -1):
                if self.ops[e][idx]["dma"] is None:
                    toks.append(("e", e, idx))
                    break
        for b in self.dma_bufs:
            toks.append(("d", b, b.dcount))
        self.bar = toks
        self.bar_epoch += 1

    def _bar_need(self, eng):
        if self.eng_epoch[eng] < self.bar_epoch:
            self.eng_epoch[eng] = self.bar_epoch
            return list(self.bar)
        return []

    def op(self, eng, fn, reads=(), writes=()):
        need = self._deps(reads, writes) + self._bar_need(eng)
        idx = len(self.ops[eng])
        tok = ("e", eng, idx)
        self.ops[eng].append({"need": need, "fn": fn, "dma": None})
        for b in reads:
            b.r[eng] = tok
        for b in writes:
            b.w = tok
            b.r = {}
        return tok

    def dma(self, eng, fn, sembuf, reads=(), writes=()):
        need = self._deps(reads, writes) + self._bar_need(eng)
        if sembuf.dsem is None:
            sembuf.dsem = self.es.enter_context(self.nc.semaphore("d_" + sembuf.name))
            self.dma_bufs.append(sembuf)
        sembuf.dcount += 16
        tok = ("d", sembuf, sembuf.dcount)
        self.ops[eng].append({"need": need, "fn": fn, "dma": sembuf})
        for b in reads:
            b.r[("d", id(sembuf))] = tok
        for b in writes:
            b.w = tok
            b.r = {}
        return tok

    def finish(self, tokens):
        self.final_tokens.extend(tokens)

    def emit(self):
        nc = self.nc
        needed = {e: set() for e in ENGS}
        for e in ENGS:
            for i, o in enumerate(self.ops[e]):
                for t in o["need"]:
                    if t[0] == "e":
                        if t[1] == e and e not in SAME_ENGINE_SYNC:
                            continue
                        needed[t[1]].add(t[2])
        for t in self.final_tokens:
            if t[0] == "e":
                needed[t[1]].add(t[2])
        rank = {}
        for e in ENGS:
            rank[e] = {i: n + 1 for n, i in enumerate(sorted(needed[e]))}
        sems = {e: self.es.enter_context(nc.semaphore("s_" + e)) for e in ENGS}
        final_tokens = self.final_tokens

        def run(e, engine):
            known = {}
            def wait(tok):
                if tok[0] == "e":
                    if tok[1] == e and e not in SAME_ENGINE_SYNC:
                        return
                    key, sem, val = tok[1], sems[tok[1]], rank[tok[1]][tok[2]]
                else:
                    key, sem, val = id(tok[1]), tok[1].dsem, tok[2]
                if known.get(key, 0) >= val:
                    return
                known[key] = val
                engine.wait_ge(sem, val)
            for i, o in enumerate(self.ops[e]):
                for t in o["need"]:
                    wait(t)
                ins = o["fn"](engine)
                if o["dma"] is not None:
                    ins.then_inc(o["dma"].dsem, 16)
                elif i in rank[e]:
                    ins.then_inc(sems[e], 1)
            if e == "sp":
                for t in final_tokens:
                    wait(t)

        with nc.Block() as block:
            @block.tensor
            def _(eng):
                run("pe", eng)

            @block.scalar
            def _(eng):
                run("act", eng)

            @block.vector
            def _(eng):
                run("dve", eng)

            @block.gpsimd
            def _(eng):
                run("pool", eng)

            @block.sync
            def _(eng):
                run("sp", eng)


class Ctx:
    def __init__(self, P):
        self.P = P
        nc = P.nc
        self.ones = P.sb("c_ones", [128, 128], BF16)
        self.b_ones = Buf("ones")
        P.op("pool", lambda g: g.memset(self.ones[:], 1.0), writes=[self.b_ones])
        self.eps_col = P.sb("c_eps", [128, 1], F32)
        P.op("pool", lambda g: g.memset(self.eps_col[:], EPS), writes=[self.b_ones])
        self.psum = [P.ps(f"ps{i}", [128, 512], F32) for i in range(8)]
        self.b_ps = [Buf(f"ps{i}") for i in range(8)]
        self.ps_rr = 0

    def next_ps(self):
        i = self.ps_rr
        self.ps_rr = (self.ps_rr + 1) % 8
        return self.psum[i], self.b_ps[i]


def emit_rmsnorm_T(P, C, xT, b_x, gain, b_gain, gcol0, hT, b_h, tts, es, tag):
    sq = [P.sb(f"{tag}_sq{i}", [128, TT], BF16, es) for i in range(2)]
    b_sq = [Buf(f"{tag}_sq{i}") for i in range(2)]
    rstd = [P.sb(f"{tag}_rstd{i}", [128, TT], F32, es) for i in range(2)]
    b_rstd = [Buf(f"{tag}_rstd{i}") for i in range(2)]
    n = 0
    for j, tt in enumerate(tts):
        ts = slice(tt * TT, (tt + 1) * TT)
        ps, b_ps = C.next_ps()
        for k in range(KD):
            s, bs = sq[n % 2], b_sq[n % 2]
            n += 1
            P.op("act", lambda e, s=s, k=k, ts=ts: e.activation(out=s[:], in_=xT[:, k, ts], func=AF.Square),
                 reads=[b_x[k][tt]], writes=[bs])
            P.op("pe", lambda e, ps=ps, s=s, k=k: e.matmul(ps[:], lhsT=C.ones[:], rhs=s[:],
                                                             start=(k == 0), stop=(k == KD - 1)),
                 reads=[bs, C.b_ones], writes=[b_ps])
        r, br = rstd[j % 2], b_rstd[j % 2]
        P.op("act", lambda e, r=r, ps=ps: e.activation(out=r[:], in_=ps[:], func=AF.Sqrt, scale=1.0 / D, bias=C.eps_col[:]),
             reads=[b_ps, C.b_ones], writes=[br])
        P.op("dve", lambda e, r=r: e.reciprocal(out=r[:], in_=r[:]),
             reads=[br], writes=[br])
        js = slice(j * TT, (j + 1) * TT)
        for k in range(KD):
            P.op("dve", lambda e, k=k, ts=ts, js=js, r=r: e.scalar_tensor_tensor(
                out=hT[:, k, js], in0=xT[:, k, ts], scalar=gain[:, gcol0 + k:gcol0 + k + 1], in1=r[:],
                op0=ALU.mult, op1=ALU.mult),
                reads=[b_x[k][tt], br, b_gain], writes=[b_h[j]])


def emit_ffn(P, C, xT, b_x, wgu, wd, gain, b_gain, gcol0, tag="ffn"):
    nc = P.nc
    with ExitStack() as es:
        HT = 2 * TT
        hT = P.sb(f"{tag}_hT", [128, KD, HT], BF16, es)
        aT = P.sb(f"{tag}_aT", [128, KF, HT], BF16, es)
        wg_f = [P.sb(f"{tag}_wgf{i}", [128, KD, 256], F32, es) for i in range(2)]
        wg_b = [P.sb(f"{tag}_wgb{i}", [128, KD, 256], BF16, es) for i in range(2)]
        wd_f = [P.sb(f"{tag}_wdf{i}", [128, KF, 128], F32, es) for i in range(2)]
        wd_b = [P.sb(f"{tag}_wdb{i}", [128, KF, 128], BF16, es) for i in range(2)]
        sg = [P.sb(f"{tag}_sg{i}", [128, TT], F32, es) for i in range(2)]
        b_hT = [Buf(f"{tag}_hT{j}") for j in range(2)]
        b_aT = [[Buf(f"{tag}_aT{n}_{j}") for j in range(2)] for n in range(KF)]
        b_wgf = [Buf(f"{tag}_wgf{i}") for i in range(2)]
        b_wgb = [Buf(f"{tag}_wgb{i}") for i in range(2)]
        b_wdf = [Buf(f"{tag}_wdf{i}") for i in range(2)]
        b_wdb = [Buf(f"{tag}_wdb{i}") for i in range(2)]
        b_sg = [Buf(f"{tag}_sg{i}") for i in range(2)]
        wgu_v = wgu.rearrange("(k p) n -> p k n", p=128)
        wd_v = wd.rearrange("(k p) n -> p k n", p=128)
        nsg = 0
        nw = 0
        nwd = 0
        for half in range(NT // HT):
            tts = [half * 2, half * 2 + 1]
            emit_rmsnorm_T(P, C, xT, b_x, gain, b_gain, gcol0, hT, b_hT, tts, es, f"{tag}n{half}")
            for n in range(KF):
                s = nw % 2
                nw += 1
                P.dma("sp", lambda e, s=s, n=n: e.dma_start(out=wg_f[s][:, :, 0:128],
                                                            in_=wgu_v[:, :, n * 128:(n + 1) * 128]),
                      b_wgf[s], writes=[b_wgf[s]])
                P.dma("sp", lambda e, s=s, n=n: e.dma_start(out=wg_f[s][:, :, 128:256],
                                                            in_=wgu_v[:, :, DFF + n * 128:DFF + (n + 1) * 128]),
                      b_wgf[s], writes=[])
                b_wgf[s].w = ("d", b_wgf[s], b_wgf[s].dcount)
                P.op("pool", lambda e, s=s: e.tensor_copy(out=wg_b[s][:], in_=wg_f[s][:]),
                     reads=[b_wgf[s]], writes=[b_wgb[s]])
                for j in range(2):
                    js = slice(j * TT, (j + 1) * TT)
                    pg, b_pg = C.next_ps()
                    pu, b_pu = C.next_ps()
                    for k in range(KD):
                        P.op("pe", lambda e, pg=pg, s=s, k=k, js=js: e.matmul(
                            pg[:], lhsT=wg_b[s][:, k, 0:128], rhs=hT[:, k, js], start=(k == 0), stop=(k == KD - 1)),
                            reads=[b_wgb[s], b_hT[j]], writes=[b_pg])
                    for k in range(KD):
                        P.op("pe", lambda e, pu=pu, s=s, k=k, js=js: e.matmul(
                            pu[:], lhsT=wg_b[s][:, k, 128:256], rhs=hT[:, k, js], start=(k == 0), stop=(k == KD - 1)),
                            reads=[b_wgb[s], b_hT[j]], writes=[b_pu])
                    q = nsg % 2
                    nsg += 1
                    P.op("act", lambda e, q=q, pg=pg: e.activation(out=sg[q][:], in_=pg[:], func=AF.Silu),
                         reads=[b_pg], writes=[b_sg[q]])
                    P.op("dve", lambda e, q=q, pu=pu, n=n, js=js: e.tensor_tensor(
                        out=aT[:, n, js], in0=pu[:], in1=sg[q][:], op=ALU.mult),
                        reads=[b_pu, b_sg[q]], writes=[b_aT[n][j]])
            for m in range(KD):
                s = nwd % 2
                nwd += 1
                P.dma("sp", lambda e, s=s, m=m: e.dma_start(out=wd_f[s][:], in_=wd_v[:, :, m * 128:(m + 1) * 128]),
                      b_wdf[s], writes=[b_wdf[s]])
                P.op("pool", lambda e, s=s: e.tensor_copy(out=wd_b[s][:], in_=wd_f[s][:]),
                     reads=[b_wdf[s]], writes=[b_wdb[s]])
                for j in range(2):
                    tt = tts[j]
                    js = slice(j * TT, (j + 1) * TT)
                    ts = slice(tt * TT, (tt + 1) * TT)
                    po, b_po = C.next_ps()
                    for n in range(KF):
                        P.op("pe", lambda e, po=po, s=s, n=n, js=js: e.matmul(
                            po[:], lhsT=wd_b[s][:, n, :], rhs=aT[:, n, js], start=(n == 0), stop=(n == KF - 1)),
                            reads=[b_wdb[s], b_aT[n][j]], writes=[b_po])
                    P.op("dve", lambda e, po=po, m=m, ts=ts: e.tensor_tensor(
                        out=xT[:, m, ts], in0=po[:], in1=xT[:, m, ts], op=ALU.add),
                        reads=[b_po, b_x[m][tt]], writes=[b_x[m][tt]])
    P.barrier()


def load_xT(P, xT, b_x, x_dram, eng="sp"):
    xv = x_dram.rearrange("(k p) t -> p k t", p=128)
    for k in range(KD):
        for tt in range(NTT):
            ts = slice(tt * TT, (tt + 1) * TT)
            P.dma(eng, lambda e, k=k, ts=ts: e.dma_start(out=xT[:, k, ts], in_=xv[:, k, ts]),
                  b_x[k][tt], writes=[b_x[k][tt]])


def store_xT(P, xT, b_x, y_dram, eng="sp"):
    yv = y_dram.rearrange("(k p) t -> p k t", p=128)
    toks = []
    for k in range(KD):
        for tt in range(NTT):
            ts = slice(tt * TT, (tt + 1) * TT)
            toks.append(P.dma(eng, lambda e, k=k, ts=ts: e.dma_start(out=yv[:, k, ts], in_=xT[:, k, ts]),
                              b_x[k][tt], reads=[b_x[k][tt]]))
    P.finish(toks)


def build_ffn_prog():
    nc = bass.Bass("TRN2", target_bir_lowering=False)
    x = nc.dram_tensor("xT", [D, NT], F32, kind="ExternalInput").ap()
    wgu = nc.dram_tensor("wgu", [D, 2 * DFF], F32, kind="ExternalInput").ap()
    wd = nc.dram_tensor("wd", [DFF, D], F32, kind="ExternalInput").ap()
    gain = nc.dram_tensor("gain", [128, KD], F32, kind="ExternalInput").ap()
    y = nc.dram_tensor("yT", [D, NT], F32, kind="ExternalOutput").ap()
    with ExitStack() as es:
        P = Prog(nc, es)
        C = Ctx(P)
        xT = P.sb("xT_sb", [128, KD, NT], F32)
        b_x = [[Buf(f"x{k}_{t}") for t in range(NTT)] for k in range(KD)]
        g_sb = P.sb("gain_sb", [128, KD], F32)
        b_g = Buf("gain")
        P.dma("sp", lambda e: e.dma_start(out=g_sb[:], in_=gain[:]), b_g, writes=[b_g])
        load_xT(P, xT, b_x, x)
        emit_ffn(P, C, xT, b_x, wgu, wd, g_sb, b_g, 0)
        store_xT(P, xT, b_x, y)
        P.emit()
    return nc


def build_ffn_prenorm_prog():
    nc = bass.Bass("TRN2", target_bir_lowering=False)
    x = nc.dram_tensor("xT", [D, NT], F32, kind="ExternalInput").ap()
    wgu = nc.dram_tensor("wgu", [D, 2 * DFF], F32, kind="ExternalInput").ap()
    wd = nc.dram_tensor("wd", [DFF, D], F32, kind="ExternalInput").ap()
    gain = nc.dram_tensor("gain", [128, KD], F32, kind="ExternalInput").ap()
    gain2 = nc.dram_tensor("gain2", [128, KD], F32, kind="ExternalInput").ap()
    y = nc.dram_tensor("yT", [D, NT], F32, kind="ExternalOutput").ap()
    h = nc.dram_tensor("hT", [D, NT], F32, kind="ExternalOutput").ap()
    with ExitStack() as es:
        P = Prog(nc, es)
        C = Ctx(P)
        xT = P.sb("xT_sb", [128, KD, NT], F32)
        b_x = [[Buf(f"x{k}_{t}") for t in range(NTT)] for k in range(KD)]
        g_sb = P.sb("gain_sb", [128, KD], F32)
        g2_sb = P.sb("gain2_sb", [128, KD], F32)
        b_g, b_g2 = Buf("gain"), Buf("gain2")
        P.dma("sp", lambda e: e.dma_start(out=g_sb[:], in_=gain[:]), b_g, writes=[b_g])
        P.dma("sp", lambda e: e.dma_start(out=g2_sb[:], in_=gain2[:]), b_g2, writes=[b_g2])
        load_xT(P, xT, b_x, x)
        emit_ffn(P, C, xT, b_x, wgu, wd, g_sb, b_g, 0)
        store_xT(P, xT, b_x, y)
        hT = P.sb("hT_sb", [128, KD, NT], F32)
        b_h = [Buf(f"h{t}") for t in range(NTT)]
        emit_rmsnorm_T(P, C, xT, b_x, g2_sb, b_g2, 0, hT, b_h, list(range(NTT)), es, "pn2")
        hv = h.rearrange("(k p) t -> p k t", p=128)
        toks = []
        for tt in range(NTT):
            ts = slice(tt * TT, (tt + 1) * TT)
            toks.append(P.dma("sp", lambda e, ts=ts: e.dma_start(out=hv[:, :, ts], in_=hT[:, :, ts]),
                              b_h[tt], reads=[b_h[tt]]))
        P.finish(toks)
        P.emit()
    return nc


def col_layout(v):
    return np.ascontiguousarray(np.asarray(v, np.float32).reshape(-1, 128).T)


def to_core_T(x2d, c):
    blocks = x2d.reshape(SEQ // 128, 128, -1)[c::NCORES]
    return np.ascontiguousarray(blocks.reshape(NT, -1).T)


def from_core_T(parts):
    dd = parts[0].shape[0]
    out = np.empty((SEQ // 128, 128, dd), np.float32)
    for c, p in enumerate(parts):
        out[c::NCORES] = p.T.reshape(NT // 128, 128, dd)
    return out.reshape(SEQ, dd)


def build_prenorm_prog():
    nc = bass.Bass("TRN2", target_bir_lowering=False)
    x = nc.dram_tensor("xT", [D, NT], F32, kind="ExternalInput").ap()
    gain = nc.dram_tensor("gain", [128, KD], F32, kind="ExternalInput").ap()
    y = nc.dram_tensor("hT", [D, NT], F32, kind="ExternalOutput").ap()
    with ExitStack() as es:
        P = Prog(nc, es)
        C = Ctx(P)
        xT = P.sb("xT_sb", [128, KD, NT], F32)
        b_x = [[Buf(f"x{k}_{t}") for t in range(NTT)] for k in range(KD)]
        g_sb = P.sb("gain_sb", [128, KD], F32)
        b_g = Buf("gain")
        P.dma("sp", lambda e: e.dma_start(out=g_sb[:], in_=gain[:]), b_g, writes=[b_g])
        load_xT(P, xT, b_x, x)
        hT = P.sb("hT_sb", [128, KD, NT], F32)
        b_h = [Buf(f"h{t}") for t in range(NTT)]
        emit_rmsnorm_T(P, C, xT, b_x, g_sb, b_g, 0, hT, b_h, list(range(NTT)), es, "pn")
        yv = y.rearrange("(k p) t -> p k t", p=128)
        toks = []
        for tt in range(NTT):
            ts = slice(tt * TT, (tt + 1) * TT)
            toks.append(P.dma("sp", lambda e, ts=ts: e.dma_start(out=yv[:, :, ts], in_=hT[:, :, ts]),
                              b_h[tt], reads=[b_h[tt]]))
        P.finish(toks)
        P.emit()
    return nc


S5T = 512
TWO_PI = 2.0 * math.pi

PI_LO = 3.1415925
CW1 = 6.28125
CW2 = TWO_PI - 6.28125


def emit_range_reduce(P, dst, src, ti, tf, reads, writes):
    rw = list(reads) + list(writes)
    P.op("dve", lambda e: e.tensor_scalar(out=tf, in0=src, scalar1=1.0 / TWO_PI, scalar2=None, op0=ALU.mult),
         reads=rw, writes=writes)
    P.op("dve", lambda e: e.tensor_copy(out=ti, in_=tf), reads=rw, writes=writes)
    P.op("dve", lambda e: e.tensor_copy(out=tf, in_=ti), reads=rw, writes=writes)
    P.op("dve", lambda e: e.scalar_tensor_tensor(out=dst, in0=tf, scalar=-CW1, in1=src, op0=ALU.mult, op1=ALU.add),
         reads=rw, writes=writes)
    P.op("dve", lambda e: e.scalar_tensor_tensor(out=dst, in0=tf, scalar=-CW2, in1=dst, op0=ALU.mult, op1=ALU.add),
         reads=rw, writes=writes)
    P.op("dve", lambda e: e.tensor_scalar(out=tf, in0=dst, scalar1=math.pi, scalar2=-TWO_PI, op0=ALU.is_gt, op1=ALU.mult),
         reads=rw, writes=writes)
    P.op("dve", lambda e: e.tensor_tensor(out=dst, in0=dst, in1=tf, op=ALU.add), reads=rw, writes=writes)
    P.op("dve", lambda e: e.tensor_scalar(out=tf, in0=dst, scalar1=-math.pi, scalar2=TWO_PI, op0=ALU.is_lt, op1=ALU.mult),
         reads=rw, writes=writes)
    P.op("dve", lambda e: e.tensor_tensor(out=dst, in0=dst, in1=tf, op=ALU.add), reads=rw, writes=writes)
    P.op("dve", lambda e: e.tensor_scalar(out=dst, in0=dst, scalar1=PI_LO, scalar2=-PI_LO, op0=ALU.min, op1=ALU.max),
         reads=rw, writes=writes)


def build_s5_prog(debug=0):
    nc = bass.Bass("TRN2", target_bir_lowering=False)
    u_d = nc.dram_tensor("uT", [128, SEQ], F32, kind="ExternalInput").ap()
    par_d = nc.dram_tensor("par", [128, 16], F32, kind="ExternalInput").ap()
    bt_d = nc.dram_tensor("bt", [128, 2, 512], F32, kind="ExternalInput").ap()
    ct_d = nc.dram_tensor("ct", [128, 2, 4, 128], F32, kind="ExternalInput").ap()
    tau_d = nc.dram_tensor("tau", [128, S5T], F32, kind="ExternalInput").ap()
    y_d = nc.dram_tensor("gyT", [128, SEQ], F32, kind="ExternalOutput").ap()
    NCH = SEQ // S5T
    with ExitStack() as es:
        P = Prog(nc, es)
        C = Ctx(P)
        par = P.sb("par_sb", [128, 16], F32)
        bt = P.sb("bt_sb", [128, 2, 512], F32)
        ct = P.sb("ct_sb", [128, 2, 4, 128], F32)
        tau = P.sb("tau_sb", [128, S5T], F32)
        b_par, b_bt, b_ct, b_tau = Buf("par"), Buf("bt"), Buf("ct"), Buf("tau")
        P.dma("sp", lambda e: e.dma_start(out=par[:], in_=par_d[:]), b_par, writes=[b_par])
        P.dma("sp", lambda e: e.dma_start(out=bt[:], in_=bt_d[:]), b_bt, writes=[b_bt])
        P.dma("sp", lambda e: e.dma_start(out=ct[:], in_=ct_d[:]), b_ct, writes=[b_ct])
        P.dma("sp", lambda e: e.dma_start(out=tau[:], in_=tau_d[:]), b_tau, writes=[b_tau])
        sm = P.sb("s5_small", [128, 24, 4], F32)
        b_sm = Buf("s5_small")
        halfpi = P.sb("halfpi", [128, 1], F32)
        P.op("pool", lambda e: e.memset(halfpi[:], math.pi / 2), writes=[b_sm])
        lam_re, lam_im, logdt, dcol = par[:, 0:4], par[:, 4:8], par[:, 8:12], par[:, 12:13]
        (DT, LR, TH, R, THR, ABS, ARE, AIM, NR, NUM_RE, NUM_IM, DEN, KRE, KIM, T1, T2, PHT, CT_, ST_, NST_) = range(20)
        col = lambda i: sm[:, i, :]

        def V(fn, reads=(b_par, b_sm)):
            P.op("dve", fn, reads=list(reads), writes=[b_sm])

        def A(fn):
            P.op("act", fn, reads=[b_par, b_sm], writes=[b_sm])

        def sincos(src, s_out, c_out, shape_ap_abs):
            A(lambda e: e.activation(out=s_out, in_=src, func=AF.Sin))
            A(lambda e: e.activation(out=shape_ap_abs, in_=src, func=AF.Abs))
            A(lambda e: e.activation(out=c_out, in_=shape_ap_abs, func=AF.Sin, scale=-1.0, bias=halfpi[:]))

        def reduce_phase(out, src_fn_desc):
            pass

        A(lambda e: e.activation(out=col(DT), in_=logdt, func=AF.Exp))
        V(lambda e: e.tensor_tensor(out=col(LR), in0=lam_re, in1=col(DT), op=ALU.mult))
        V(lambda e: e.tensor_tensor(out=col(TH), in0=lam_im, in1=col(DT), op=ALU.mult))
        A(lambda e: e.activation(out=col(R), in_=col(LR), func=AF.Exp))
        smi = P.sb("s5_smi", [128, 4], mybir.dt.int32)
        emit_range_reduce(P, col(THR), col(TH), smi[:], col(T1), [b_par], [b_sm])
        sincos(col(THR), col(AIM), col(ARE), col(ABS))
        V(lambda e: e.tensor_tensor(out=col(ARE), in0=col(ARE), in1=col(R), op=ALU.mult))
        V(lambda e: e.tensor_tensor(out=col(AIM), in0=col(AIM), in1=col(R), op=ALU.mult))
        V(lambda e: e.tensor_scalar(out=col(NR), in0=col(ARE), scalar1=-1.0, scalar2=None, op0=ALU.add))
        V(lambda e: e.tensor_tensor(out=col(T1), in0=col(NR), in1=lam_re, op=ALU.mult))
        V(lambda e: e.tensor_tensor(out=col(T2), in0=col(AIM), in1=lam_im, op=ALU.mult))
        V(lambda e: e.tensor_tensor(out=col(NUM_RE), in0=col(T1), in1=col(T2), op=ALU.add))
        V(lambda e: e.tensor_tensor(out=col(T1), in0=col(AIM), in1=lam_re, op=ALU.mult))
        V(lambda e: e.tensor_tensor(out=col(T2), in0=col(NR), in1=lam_im, op=ALU.mult))
        V(lambda e: e.tensor_tensor(out=col(NUM_IM), in0=col(T1), in1=col(T2), op=ALU.subtract))
        V(lambda e: e.tensor_tensor(out=col(T1), in0=lam_re, in1=lam_re, op=ALU.mult))
        V(lambda e: e.tensor_tensor(out=col(T2), in0=lam_im, in1=lam_im, op=ALU.mult))
        V(lambda e: e.tensor_tensor(out=col(DEN), in0=col(T1), in1=col(T2), op=ALU.add))
        V(lambda e: e.reciprocal(out=col(DEN), in_=col(DEN)))
        V(lambda e: e.tensor_tensor(out=col(KRE), in0=col(NUM_RE), in1=col(DEN), op=ALU.mult))
        V(lambda e: e.tensor_tensor(out=col(KIM), in0=col(NUM_IM), in1=col(DEN), op=ALU.mult))
        V(lambda e: e.tensor_scalar(out=col(T2), in0=col(THR), scalar1=float(S5T), scalar2=None, op0=ALU.mult))
        emit_range_reduce(P, col(PHT), col(T2), smi[:], col(T1), [b_par], [b_sm])
        sincos(col(PHT), col(ST_), col(CT_), col(ABS))
        V(lambda e: e.tensor_scalar(out=col(NST_), in0=col(ST_), scalar1=-1.0, scalar2=None, op0=ALU.mult))
        L = P.sb("s5_L", [128, 3, 4, 128], BF16)
        b_L = Buf("s5_L")
        ctmp = P.sb("s5_ctmp", [128, 2, 128], F32)
        b_ctmp = Buf("s5_ctmp")
        for k in range(4):
            kre, kim = sm[:, KRE, k:k + 1], sm[:, KIM, k:k + 1]
            P.op("dve", lambda e, k=k, kim=kim: e.tensor_scalar(out=ctmp[:, 0, :], in0=ct[:, 1, k, :], scalar1=kim,
                                                                 scalar2=-1.0, op0=ALU.mult, op1=ALU.mult),
                 reads=[b_ct, b_sm], writes=[b_ctmp])
            P.op("dve", lambda e, k=k, kre=kre: e.scalar_tensor_tensor(out=ctmp[:, 0, :], in0=ct[:, 0, k, :], scalar=kre,
                                                                        in1=ctmp[:, 0, :], op0=ALU.mult, op1=ALU.add),
                 reads=[b_ct, b_sm, b_ctmp], writes=[b_ctmp])
            P.op("dve", lambda e, k=k, kre=kre: e.tensor_scalar(out=ctmp[:, 1, :], in0=ct[:, 1, k, :], scalar1=kre,
                                                                 scalar2=None, op0=ALU.mult),
                 reads=[b_ct, b_sm], writes=[b_ctmp])
            P.op("dve", lambda e, k=k, kim=kim: e.scalar_tensor_tensor(out=ctmp[:, 1, :], in0=ct[:, 0, k, :], scalar=kim,
                                                                        in1=ctmp[:, 1, :], op0=ALU.mult, op1=ALU.add),
                 reads=[b_ct, b_sm, b_ctmp], writes=[b_ctmp])
            P.op("dve", lambda e, k=k: e.tensor_copy(out=L[:, 0, k, :], in_=ctmp[:, 0, :]), reads=[b_ctmp], writes=[b_L])
            P.op("dve", lambda e, k=k: e.tensor_scalar(out=L[:, 1, k, :], in0=ctmp[:, 0, :], scalar1=-1.0, scalar2=None,
                                                       op0=ALU.mult), reads=[b_ctmp], writes=[b_L])
            P.op("dve", lambda e, k=k: e.tensor_scalar(out=L[:, 2, k, :], in0=ctmp[:, 1, :], scalar1=-1.0, scalar2=None,
                                                       op0=ALU.mult), reads=[b_ctmp], writes=[b_L])
        btb = P.sb("s5_btb", [128, 2, 512], BF16)
        b_btb = Buf("s5_btb")
        P.op("dve", lambda e: e.tensor_copy(out=btb[:], in_=bt[:]), reads=[b_bt], writes=[b_btb])
        cosT = P.sb("s5_cos", [128, 4, S5T], F32)
        sinT = P.sb("s5_sin", [128, 4, S5T], F32)
        rT = P.sb("s5_rT", [128, 4, S5T], F32)
        b_tab = Buf("s5_tab")
        ph = P.sb("s5_ph", [128, S5T], F32)
        pha = P.sb("s5_pha", [128, S5T], F32)
        phx = P.sb("s5_phx", [128, S5T], F32)
        phi = P.sb("s5_phi", [128, S5T], mybir.dt.int32)
        b_ph = Buf("s5_ph")
        for k in range(4):
            P.op("dve", lambda e, k=k: e.tensor_scalar(out=phx[:], in0=tau[:], scalar1=sm[:, THR, k:k + 1],
                                                       scalar2=None, op0=ALU.mult),
                 reads=[b_tau, b_sm, b_tab], writes=[b_ph])
            emit_range_reduce(P, ph[:], phx[:], phi[:], pha[:], [b_sm], [b_ph])
            P.op("act", lambda e, k=k: e.activation(out=sinT[:, k, :], in_=ph[:], func=AF.Sin),
                 reads=[b_ph], writes=[b_tab])
            P.op("act", lambda e: e.activation(out=pha[:], in_=ph[:], func=AF.Abs),
                 reads=[b_ph], writes=[b_ph])
            P.op("act", lambda e, k=k: e.activation(out=cosT[:, k, :], in_=pha[:], func=AF.Sin, scale=-1.0, bias=halfpi[:]),
                 reads=[b_ph, b_sm], writes=[b_tab])
            P.op("dve", lambda e, k=k: e.tensor_scalar(out=rT[:, k, :], in0=tau[:], scalar1=0.0, scalar2=sm[:, R, k:k + 1],
                                                       op0=ALU.mult, op1=ALU.add),
                 reads=[b_tau, b_sm], writes=[b_tab])
        if debug == 2:
            tk = [P.dma("sp", lambda e: e.dma_start(out=y_d[:, 0:96], in_=sm[:].rearrange("p a b -> p (a b)")), b_sm, reads=[b_sm]),
                  P.dma("sp", lambda e: e.dma_start(out=y_d[:, 1024:3072], in_=cosT[:].rearrange("p a b -> p (a b)")), b_tab, reads=[b_tab]),
                  P.dma("sp", lambda e: e.dma_start(out=y_d[:, 3072:5120], in_=sinT[:].rearrange("p a b -> p (a b)")), b_tab, reads=[b_tab]),
                  P.dma("sp", lambda e: e.dma_start(out=y_d[:, 5120:7168], in_=rT[:].rearrange("p a b -> p (a b)")), b_tab, reads=[b_tab])]
            P.finish(tk)
            P.emit()
            return nc
        NB = 2
        uf = [P.sb(f"s5_uf{i}", [128, S5T], F32) for i in range(NB)]
        ub = [P.sb(f"s5_ub{i}", [128, S5T], BF16) for i in range(NB)]
        b_uf = [Buf(f"s5_uf{i}") for i in range(NB)]
        b_ub = [Buf(f"s5_ub{i}") for i in range(NB)]
        sA = [P.sb(f"s5_sA{i}", [128, S5T], F32) for i in range(2)]
        sB = [P.sb(f"s5_sB{i}", [128, S5T], F32) for i in range(2)]
        b_sA = [Buf(f"s5_sA{i}") for i in range(2)]
        b_sB = [Buf(f"s5_sB{i}") for i in range(2)]
        t_ = [[P.sb(f"s5_t{j}_{i}", [128, S5T], F32) for i in range(2)] for j in range(4)]
        b_t = [[Buf(f"s5_t{j}_{i}") for i in range(2)] for j in range(4)]
        bre = [P.sb(f"s5_bre{i}", [128, S5T], F32) for i in range(2)]
        bim = [P.sb(f"s5_bim{i}", [128, S5T], F32) for i in range(2)]
        b_bre = [Buf(f"s5_bre{i}") for i in range(2)]
        b_bim = [Buf(f"s5_bim{i}") for i in range(2)]
        zre = [P.sb(f"s5_zre{i}", [128, S5T], F32) for i in range(2)]
        zim = [P.sb(f"s5_zim{i}", [128, S5T], F32) for i in range(2)]
        b_zre = [Buf(f"s5_zre{i}") for i in range(2)]
        b_zim = [Buf(f"s5_zim{i}") for i in range(2)]
        pp = [[P.sb(f"s5_p{j}_{i}", [128, S5T], BF16) for i in range(2)] for j in range(4)]
        b_pp = [[Buf(f"s5_p{j}_{i}") for i in range(2)] for j in range(4)]
        init = P.sb("s5_init", [128, 2, 4], F32)
        itmp = P.sb("s5_itmp", [128, 2, 4], F32)
        b_init = [Buf(f"s5_init{k}") for k in range(4)]
        P.op("dve", lambda e: e.memset(init[:], 0.0), writes=b_init)
        ysb = [P.sb(f"s5_y{i}", [128, S5T], F32) for i in range(2)]
        g1 = [P.sb(f"s5_g1{i}", [128, S5T], F32) for i in range(2)]
        g2 = [P.sb(f"s5_g2{i}", [128, S5T], F32) for i in range(2)]
        b_y = [Buf(f"s5_y{i}") for i in range(2)]
        b_g1 = [Buf(f"s5_g1{i}") for i in range(2)]
        b_g2 = [Buf(f"s5_g2{i}") for i in range(2)]
        toks = []
        it = 0
        for c in range(NCH):
            cs = slice(c * S5T, (c + 1) * S5T)
            ui = c % NB
            P.dma("sp", lambda e, ui=ui, cs=cs: e.dma_start(out=uf[ui][:], in_=u_d[:, cs]), b_uf[ui], writes=[b_uf[ui]])
            P.op("act", lambda e, ui=ui: e.activation(out=ub[ui][:], in_=uf[ui][:], func=AF.Copy),
                 reads=[b_uf[ui]], writes=[b_ub[ui]])
            yps, b_yps = C.psum[6 + c % 2], C.b_ps[6 + c % 2]
            for k in range(4):
                s = it % 2
                pa, b_pa = C.psum[(2 * it) % 6], C.b_ps[(2 * it) % 6]
                pb, b_pb = C.psum[(2 * it + 1) % 6], C.b_ps[(2 * it + 1) % 6]
                it += 1
                ks = slice(k * 128, (k + 1) * 128)
                P.op("pe", lambda e, pa=pa, ks=ks, ui=ui: e.matmul(pa[:], lhsT=btb[:, 0, ks], rhs=ub[ui][:], start=True, stop=True),
                     reads=[b_btb, b_ub[ui]], writes=[b_pa])
                P.op("pe", lambda e, pb=pb, ks=ks, ui=ui: e.matmul(pb[:], lhsT=btb[:, 1, ks], rhs=ub[ui][:], start=True, stop=True),
                     reads=[b_btb, b_ub[ui]], writes=[b_pb])
                P.op("act", lambda e, s=s, pa=pa: e.activation(out=sA[s][:], in_=pa[:], func=AF.Copy), reads=[b_pa], writes=[b_sA[s]])
                P.op("act", lambda e, s=s, pb=pb: e.activation(out=sB[s][:], in_=pb[:], func=AF.Copy), reads=[b_pb], writes=[b_sB[s]])
                P.op("pool", lambda e, s=s, k=k: e.tensor_tensor(out=t_[0][s][:], in0=sA[s][:], in1=cosT[:, k, :], op=ALU.mult),
                     reads=[b_sA[s], b_tab], writes=[b_t[0][s]])
                P.op("dve", lambda e, s=s, k=k: e.tensor_tensor(out=t_[1][s][:], in0=sB[s][:], in1=sinT[:, k, :], op=ALU.mult),
                     reads=[b_sB[s], b_tab], writes=[b_t[1][s]])
                P.op("pool", lambda e, s=s, k=k: e.tensor_tensor(out=t_[2][s][:], in0=sB[s][:], in1=cosT[:, k, :], op=ALU.mult),
                     reads=[b_sB[s], b_tab], writes=[b_t[2][s]])
                P.op("dve", lambda e, s=s, k=k: e.tensor_tensor(out=t_[3][s][:], in0=sA[s][:], in1=sinT[:, k, :], op=ALU.mult),
                     reads=[b_sA[s], b_tab], writes=[b_t[3][s]])
                P.op("pool", lambda e, s=s: e.tensor_tensor(out=bre[s][:], in0=t_[0][s][:], in1=t_[1][s][:], op=ALU.add),
                     reads=[b_t[0][s], b_t[1][s]], writes=[b_bre[s]])
                P.op("pool", lambda e, s=s: e.tensor_tensor(out=bim[s][:], in0=t_[2][s][:], in1=t_[3][s][:], op=ALU.subtract),
                     reads=[b_t[2][s], b_t[3][s]], writes=[b_bim[s]])
                P.op("dve", lambda e, s=s, k=k: e.tensor_tensor_scan(out=zre[s][:], data0=rT[:, k, :], data1=bre[s][:],
                                                                      initial=init[:, 0, k:k + 1], op0=ALU.mult, op1=ALU.add),
                     reads=[b_tab, b_bre[s], b_init[k]], writes=[b_zre[s]])
                P.op("dve", lambda e, s=s, k=k: e.tensor_tensor_scan(out=zim[s][:], data0=rT[:, k, :], data1=bim[s][:],
                                                                      initial=init[:, 1, k:k + 1], op0=ALU.mult, op1=ALU.add),
                     reads=[b_tab, b_bim[s], b_init[k]], writes=[b_zim[s]])
                zlr, zli = zre[s][:, S5T - 1:S5T], zim[s][:, S5T - 1:S5T]
                cT_, sT_, nsT_ = sm[:, CT_, k:k + 1], sm[:, ST_, k:k + 1], sm[:, NST_, k:k + 1]
                P.op("dve", lambda e, k=k, zlr=zlr, cT_=cT_: e.tensor_tensor(out=itmp[:, 0, k:k + 1], in0=zlr, in1=cT_, op=ALU.mult),
                     reads=[b_zre[s], b_sm], writes=[b_init[k]])
                P.op("dve", lambda e, k=k, zlr=zlr, sT_=sT_: e.tensor_tensor(out=itmp[:, 1, k:k + 1], in0=zlr, in1=sT_, op=ALU.mult),
                     reads=[b_zre[s], b_sm], writes=[b_init[k]])
                P.op("dve", lambda e, k=k, zli=zli, nsT_=nsT_: e.scalar_tensor_tensor(
                    out=init[:, 0, k:k + 1], in0=zli, scalar=nsT_, in1=itmp[:, 0, k:k + 1], op0=ALU.mult, op1=ALU.add),
                    reads=[b_zim[s], b_sm], writes=[b_init[k]])
                P.op("dve", lambda e, k=k, zli=zli, cT_=cT_: e.scalar_tensor_tensor(
                    out=init[:, 1, k:k + 1], in0=zli, scalar=cT_, in1=itmp[:, 1, k:k + 1], op0=ALU.mult, op1=ALU.add),
                    reads=[b_zim[s], b_sm], writes=[b_init[k]])
                P.op("pool", lambda e, s=s, k=k: e.tensor_tensor(out=pp[0][s][:], in0=zre[s][:], in1=cosT[:, k, :], op=ALU.mult),
                     reads=[b_zre[s], b_tab], writes=[b_pp[0][s]])
                P.op("pool", lambda e, s=s, k=k: e.tensor_tensor(out=pp[1][s][:], in0=zim[s][:], in1=sinT[:, k, :], op=ALU.mult),
                     reads=[b_zim[s], b_tab], writes=[b_pp[1][s]])
                P.op("dve", lambda e, s=s, k=k: e.tensor_tensor(out=pp[2][s][:], in0=zre[s][:], in1=sinT[:, k, :], op=ALU.mult),
                     reads=[b_zre[s], b_tab], writes=[b_pp[2][s]])
                P.op("pool", lambda e, s=s, k=k: e.tensor_tensor(out=pp[3][s][:], in0=zim[s][:], in1=cosT[:, k, :], op=ALU.mult),
                     reads=[b_zim[s], b_tab], writes=[b_pp[3][s]])
                for j, li in enumerate((0, 1, 2, 2)):
                    P.op("pe", lambda e, yps=yps, li=li, k=k, j=j, s=s: e.matmul(
                        yps[:], lhsT=L[:, li, k, :], rhs=pp[j][s][:], start=(k == 0 and j == 0), stop=(k == 3 and j == 3)),
                        reads=[b_L, b_pp[j][s]], writes=[b_yps])
            q = c % 2
            P.op("dve", lambda e, q=q, ui=ui, yps=yps: e.scalar_tensor_tensor(
                out=ysb[q][:], in0=uf[ui][:], scalar=dcol, in1=yps[:], op0=ALU.mult, op1=ALU.add),
                reads=[b_uf[ui], b_par, b_yps], writes=[b_y[q]])
            P.op("pool", lambda e, q=q: e.tensor_tensor(out=g1[q][:], in0=ysb[q][:], in1=ysb[q][:], op=ALU.mult),
                 reads=[b_y[q]], writes=[b_g1[q]])
            P.op("pool", lambda e, q=q: e.tensor_scalar(out=g1[q][:], in0=g1[q][:], scalar1=0.044715, scalar2=1.0,
                                                        op0=ALU.mult, op1=ALU.add),
                 reads=[b_g1[q]], writes=[b_g1[q]])
            P.op("pool", lambda e, q=q: e.tensor_tensor(out=g1[q][:], in0=g1[q][:], in1=ysb[q][:], op=ALU.mult),
                 reads=[b_g1[q], b_y[q]], writes=[b_g1[q]])
            P.op("act", lambda e, q=q: e.activation(out=g2[q][:], in_=g1[q][:], func=AF.Sigmoid, scale=1.5957691216057308),
                 reads=[b_g1[q]], writes=[b_g2[q]])
            P.op("pool", lambda e, q=q: e.tensor_tensor(out=g2[q][:], in0=g2[q][:], in1=ysb[q][:], op=ALU.mult),
                 reads=[b_g2[q], b_y[q]], writes=[b_g2[q]])
            if debug == 1:
                toks.append(P.dma("sp", lambda e, q=q, cs=cs: e.dma_start(out=y_d[:, cs], in_=ysb[q][:]), b_g2[q], reads=[b_g2[q], b_y[q]]))
                continue
            toks.append(P.dma("sp", lambda e, q=q, cs=cs: e.dma_start(out=y_d[:, cs], in_=g2[q][:]), b_g2[q], reads=[b_g2[q]]))
        P.finish(toks[-2:])
        P.emit()
    return nc


def s5_host_layout(lam_re, lam_im, log_dt, b_re, b_im, c_re, c_im, d, core):
    g0 = core * 8
    par = np.zeros((128, 16), np.float32)
    bt = np.zeros((128, 2, 512), np.float32)
    ct = np.zeros((128, 2, 4, 128), np.float32)
    for gl in range(8):
        g = g0 + gl
        k, p0 = gl // 2, (gl % 2) * 64
        par[p0:p0 + 64, 0 + k] = lam_re[g]
        par[p0:p0 + 64, 4 + k] = lam_im[g]
        par[p0:p0 + 64, 8 + k] = log_dt[g]
        bt[16 * gl:16 * gl + 16, 0, 64 * gl:64 * gl + 64] = b_re[g].T
        bt[16 * gl:16 * gl + 16, 1, 64 * gl:64 * gl + 64] = b_im[g].T
        ct[p0:p0 + 64, 0, k, 16 * gl:16 * gl + 16] = c_re[g].T
        ct[p0:p0 + 64, 1, k, 16 * gl:16 * gl + 16] = c_im[g].T
    par[:, 12] = d[core * 128:(core + 1) * 128]
    return par, bt, ct


def emit_glu(P, C, xT, b_x, gy_dram, wglu):
    with ExitStack() as es:
        gyb = P.sb("glu_gyb", [128, KD, NT], BF16, es)
        b_gyb = [Buf(f"glu_gyb{t}") for t in range(NTT)]
        st = [P.sb(f"glu_st{i}", [128, TT], F32, es) for i in range(2)]
        b_st = [Buf(f"glu_st{i}") for i in range(2)]
        gv = gy_dram.rearrange("(k p) t -> p k t", p=128)
        n = 0
        for tt in range(NTT):
            ts = slice(tt * TT, (tt + 1) * TT)
            for k in range(KD):
                s = n % 2
                n += 1
                P.dma("sp", lambda e, s=s, k=k, ts=ts: e.dma_start(out=st[s][:], in_=gv[:, k, ts]), b_st[s], writes=[b_st[s]])
                P.op("pool", lambda e, s=s, k=k, ts=ts: e.tensor_copy(out=gyb[:, k, ts], in_=st[s][:]),
                     reads=[b_st[s]], writes=[b_gyb[tt]])
        w_f = [P.sb(f"glu_wf{i}", [128, KD, 256], F32, es) for i in range(2)]
        w_b = [P.sb(f"glu_wb{i}", [128, KD, 256], BF16, es) for i in range(2)]
        b_wf = [Buf(f"glu_wf{i}") for i in range(2)]
        b_wb = [Buf(f"glu_wb{i}") for i in range(2)]
        sg = [P.sb(f"glu_sg{i}", [128, TT], F32, es) for i in range(2)]
        b_sg = [Buf(f"glu_sg{i}") for i in range(2)]
        wv = wglu.rearrange("(k p) n -> p k n", p=128)
        q = 0
        for m in range(KD):
            s = m % 2
            P.dma("sp", lambda e, s=s, m=m: e.dma_start(out=w_f[s][:, :, 0:128], in_=wv[:, :, m * 128:(m + 1) * 128]),
                  b_wf[s], writes=[b_wf[s]])
            P.dma("sp", lambda e, s=s, m=m: e.dma_start(out=w_f[s][:, :, 128:256], in_=wv[:, :, D + m * 128:D + (m + 1) * 128]),
                  b_wf[s], writes=[])
            b_wf[s].w = ("d", b_wf[s], b_wf[s].dcount)
            P.op("pool", lambda e, s=s: e.tensor_copy(out=w_b[s][:], in_=w_f[s][:]), reads=[b_wf[s]], writes=[b_wb[s]])
            for tt in range(NTT):
                ts = slice(tt * TT, (tt + 1) * TT)
                pv, b_pv = C.next_ps()
                pg, b_pg = C.next_ps()
                for k in range(KD):
                    P.op("pe", lambda e, pv=pv, s=s, k=k, ts=ts: e.matmul(pv[:], lhsT=w_b[s][:, k, 0:128], rhs=gyb[:, k, ts],
                                                                         start=(k == 0), stop=(k == KD - 1)),
                         reads=[b_wb[s], b_gyb[tt]], writes=[b_pv])
                for k in range(KD):
                    P.op("pe", lambda e, pg=pg, s=s, k=k, ts=ts: e.matmul(pg[:], lhsT=w_b[s][:, k, 128:256], rhs=gyb[:, k, ts],
                                                                         start=(k == 0), stop=(k == KD - 1)),
                         reads=[b_wb[s], b_gyb[tt]], writes=[b_pg])
                qq = q % 2
                q += 1
                P.op("act", lambda e, qq=qq, pg=pg: e.activation(out=sg[qq][:], in_=pg[:], func=AF.Sigmoid),
                     reads=[b_pg], writes=[b_sg[qq]])
                P.op("dve", lambda e, qq=qq, pv=pv: e.tensor_tensor(out=sg[qq][:], in0=pv[:], in1=sg[qq][:], op=ALU.mult),
                     reads=[b_pv, b_sg[qq]], writes=[b_sg[qq]])
                P.op("pool", lambda e, qq=qq, m=m, ts=ts: e.tensor_tensor(out=xT[:, m, ts], in0=xT[:, m, ts], in1=sg[qq][:], op=ALU.add),
                     reads=[b_sg[qq], b_x[m][tt]], writes=[b_x[m][tt]])
    P.barrier()


def build_glu_ffn_prog():
    nc = bass.Bass("TRN2", target_bir_lowering=False)
    x = nc.dram_tensor("xT", [D, NT], F32, kind="ExternalInput").ap()
    gy = nc.dram_tensor("gyT", [D, NT], F32, kind="ExternalInput").ap()
    wglu = nc.dram_tensor("wglu", [D, 2 * D], F32, kind="ExternalInput").ap()
    wgu = nc.dram_tensor("wgu", [D, 2 * DFF], F32, kind="ExternalInput").ap()
    wd = nc.dram_tensor("wd", [DFF, D], F32, kind="ExternalInput").ap()
    gain = nc.dram_tensor("gain", [128, KD], F32, kind="ExternalInput").ap()
    y = nc.dram_tensor("yT", [D, NT], F32, kind="ExternalOutput").ap()
    with ExitStack() as es:
        P = Prog(nc, es)
        C = Ctx(P)
        xT = P.sb("xT_sb", [128, KD, NT], F32)
        b_x = [[Buf(f"x{k}_{t}") for t in range(NTT)] for k in range(KD)]
        g_sb = P.sb("gain_sb", [128, KD], F32)
        b_g = Buf("gain")
        P.dma("sp", lambda e: e.dma_start(out=g_sb[:], in_=gain[:]), b_g, writes=[b_g])
        load_xT(P, xT, b_x, x)
        emit_glu(P, C, xT, b_x, gy, wglu)
        emit_ffn(P, C, xT, b_x, wgu, wd, g_sb, b_g, 0)
        store_xT(P, xT, b_x, y)
        P.emit()
    return nc


_PROGS = {}


def _prog(name, builder):
    if name not in _PROGS:
        _PROGS[name] = builder()
    return _PROGS[name]


def _run(name, builder, in_maps):
    import time
    t0 = time.time()
    nc = _prog(name, builder)
    t1 = time.time()
    import os
    r = run_bass_kernel_spmd(nc, in_maps, core_ids=list(range(NCORES)), **({"trace": True} if os.environ.get("KPROF") else {}))
    res = r.results
    nb = sum(v.nbytes for m in in_maps for v in m.values())
    print(f"[launch {name}] build {t1 - t0:.1f}s run {time.time() - t1:.1f}s in_bytes {nb / 1e6:.0f}MB exec_ns {r.exec_time_ns}", flush=True)
    return res


def run_s5_layer(xT_parts, j, i, inp, h_parts=None):
    if h_parts is None:
        gm = col_layout(inp["norm_mix"][i])
        res = _run("prenorm", build_prenorm_prog, [{"xT": xT_parts[c], "gain": gm} for c in range(NCORES)])
        h_parts = [r["hT"] for r in res]
    h_full = from_core_T(h_parts)
    tau = np.tile(np.arange(S5T, dtype=np.float32)[None], (128, 1))
    maps = []
    for c in range(NCORES):
        par, bt, ct = s5_host_layout(inp["s5_lambda_re"][j], inp["s5_lambda_im"][j], inp["s5_log_dt"][j],
                                     inp["s5_b_re"][j], inp["s5_b_im"][j], inp["s5_c_re"][j], inp["s5_c_im"][j],
                                     inp["s5_d"][j], c)
        maps.append({"uT": np.ascontiguousarray(h_full[:, c * 128:(c + 1) * 128].T), "par": par, "bt": bt, "ct": ct, "tau": tau})
    res = _run("s5", build_s5_prog, maps)
    gy_full = np.concatenate([r["gyT"] for r in res], axis=0).T
    gf = col_layout(inp["norm_ffn"][i])
    maps = [{"xT": xT_parts[c], "gyT": to_core_T(gy_full, c), "wglu": inp["s5_w_glu"][j],
             "wgu": inp["ffn_w_gate_up"][i], "wd": inp["ffn_w_down"][i], "gain": gf} for c in range(NCORES)]
    res = _run("glu_ffn", build_glu_ffn_prog, maps)
    return [r["yT"] for r in res]


NH = 16
HD = 64
NIH = 8
PROJ = 3 * D + NIH * HD + HD + NIH


def build_dsa_proj_prog():
    nc = bass.Bass("TRN2", target_bir_lowering=False)
    x = nc.dram_tensor("xT", [D, NT], F32, kind="ExternalInput").ap()
    w_in = nc.dram_tensor("w_in", [D, PROJ], F32, kind="ExternalInput").ap()
    gain = nc.dram_tensor("gain", [128, KD], F32, kind="ExternalInput").ap()
    qk_g = nc.dram_tensor("qk_gain", [128, 2], F32, kind="ExternalInput").ap()
    cs_d = nc.dram_tensor("cossin", [128, 2, NT], F32, kind="ExternalInput").ap()
    cm_d = nc.dram_tensor("cmat", [128, 2, 128], F32, kind="ExternalInput").ap()
    qT_d = nc.dram_tensor("qT", [D, NT], BF16, kind="ExternalOutput").ap()
    kT_d = nc.dram_tensor("kT", [D, NT], BF16, kind="ExternalOutput").ap()
    v_d = nc.dram_tensor("v", [NT, NH * 65], BF16, kind="ExternalOutput").ap()
    qiT_d = nc.dram_tensor("qiT", [NIH * HD, NT], BF16, kind="ExternalOutput").ap()
    kiT_d = nc.dram_tensor("kiT", [HD, NT], BF16, kind="ExternalOutput").ap()
    w_d = nc.dram_tensor("w", [NT, NIH], F32, kind="ExternalOutput").ap()
    with ExitStack() as es:
        P = Prog(nc, es)
        C = Ctx(P)
        xT = P.sb("xT_sb", [128, KD, NT], F32)
        b_x = [[Buf(f"x{k}_{t}") for t in range(NTT)] for k in range(KD)]
        g_sb = P.sb("gain_sb", [128, KD], F32)
        qkg = P.sb("qkg_sb", [128, 2], F32)
        cs = P.sb("cs_sb", [128, 2, NT], F32)
        cm = P.sb("cm_sb", [128, 2, 128], F32)
        b_g, b_qkg, b_cs, b_cm = Buf("gain"), Buf("qkg"), Buf("cs"), Buf("cm")
        P.dma("sp", lambda e: e.dma_start(out=g_sb[:], in_=gain[:]), b_g, writes=[b_g])
        P.dma("sp", lambda e: e.dma_start(out=qkg[:], in_=qk_g[:]), b_qkg, writes=[b_qkg])
        P.dma("sp", lambda e: e.dma_start(out=cm[:], in_=cm_d[:]), b_cm, writes=[b_cm])
        P.dma("sp", lambda e: e.dma_start(out=cs[:, 0, :], in_=cs_d[:, 0, :]), b_cs, writes=[b_cs])
        P.dma("sp", lambda e: e.dma_start(out=cs[:, 1, :], in_=cs_d[:, 1, :]), b_cs, writes=[])
        b_cs.w = ("d", b_cs, b_cs.dcount)
        load_xT(P, xT, b_x, x)
        bones = P.sb("bones_bf", [128, 128], BF16)
        b_bones = Buf("bones")
        P.op("dve", lambda e: e.tensor_copy(out=bones[:], in_=cm[:, 0, :]), reads=[b_cm], writes=[b_bones])
        P.op("dve", lambda e: e.tensor_scalar(out=qkg[:, 0:1], in0=qkg[:, 0:1], scalar1=HD ** -0.5, scalar2=None, op0=ALU.mult),
             reads=[b_qkg], writes=[b_qkg])
        hT = P.sb("hT_sb", [128, KD, NT], BF16)
        b_h = [Buf(f"h{t}") for t in range(NTT)]
        emit_rmsnorm_T(P, C, xT, b_x, g_sb, b_g, 0, hT, b_h, list(range(NTT)), es, "pn")
        wv_ = w_in.rearrange("(k p) n -> p k n", p=128)
        w_f = [P.sb(f"pj_wf{i}", [128, KD, 128], F32) for i in range(2)]
        w_b = [P.sb(f"pj_wb{i}", [128, KD, 128], BF16) for i in range(2)]
        b_wf = [Buf(f"pj_wf{i}") for i in range(2)]
        b_wb = [Buf(f"pj_wb{i}") for i in range(2)]
        sq = [P.sb(f"pj_sq{i}", [128, TT], BF16) for i in range(2)]
        rs = [P.sb(f"pj_rs{i}", [128, TT], F32) for i in range(2)]
        tf = [P.sb(f"pj_t{i}", [128, TT], F32) for i in range(2)]
        o1 = [P.sb(f"pj_o1{i}", [128, TT], F32) for i in range(2)]
        o2 = [P.sb(f"pj_o2{i}", [128, TT], F32) for i in range(2)]
        ob = [P.sb(f"pj_ob{i}", [128, TT], BF16) for i in range(2)]
        b_sq = [Buf(f"pj_sq{i}") for i in range(2)]
        b_rs = [Buf(f"pj_rs{i}") for i in range(2)]
        b_tf = [Buf(f"pj_t{i}") for i in range(2)]
        b_o1 = [Buf(f"pj_o1{i}") for i in range(2)]
        b_o2 = [Buf(f"pj_o2{i}") for i in range(2)]
        b_ob = [Buf(f"pj_ob{i}") for i in range(2)]
        toks = []
        tiles = []
        for m in range(8):
            tiles.append((m * 128, 128, "norm", qT_d, m * 128, 0))
        for m in range(8):
            tiles.append((D + m * 128, 128, "norm", kT_d, m * 128, 1))
        for m in range(4):
            tiles.append((3 * D + m * 128, 128, "plain", qiT_d, m * 128, None))
        tiles.append((3 * D + NIH * HD, 64, "normnog", kiT_d, 0, None))
        it = 0
        for ti, (c0, M, kind, od, r0, gc) in enumerate(tiles):
            s = ti % 2
            P.dma("sp", lambda e, s=s, c0=c0, M=M: e.dma_start(out=w_f[s][:, :, 0:M], in_=wv_[:, :, c0:c0 + M]),
                  b_wf[s], writes=[b_wf[s]])
            P.op("pool", lambda e, s=s, M=M: e.tensor_copy(out=w_b[s][:, :, 0:M], in_=w_f[s][:, :, 0:M]),
                 reads=[b_wf[s]], writes=[b_wb[s]])
            for tt in range(NTT):
                ts = slice(tt * TT, (tt + 1) * TT)
                u = it % 2
                it += 1
                ps, b_ps = C.next_ps()
                for k in range(KD):
                    P.op("pe", lambda e, ps=ps, s=s, k=k, ts=ts, M=M: e.matmul(ps[0:M, :], lhsT=w_b[s][:, k, 0:M], rhs=hT[:, k, ts],
                                                                              start=(k == 0), stop=(k == KD - 1)),
                         reads=[b_wb[s], b_h[tt]], writes=[b_ps])
                if kind == "plain":
                    P.op("act", lambda e, u=u, ps=ps, M=M: e.activation(out=tf[u][0:M, :], in_=ps[0:M, :], func=AF.Copy),
                         reads=[b_ps], writes=[b_tf[u]])
                else:
                    P.op("act", lambda e, u=u, ps=ps, M=M: e.activation(out=sq[u][0:M, :], in_=ps[0:M, :], func=AF.Square),
                         reads=[b_ps], writes=[b_sq[u]])
                    p2, b_p2 = C.next_ps()
                    P.op("pe", lambda e, p2=p2, u=u, M=M: e.matmul(p2[0:M, :], lhsT=bones[0:M, 0:M], rhs=sq[u][0:M, :], start=True, stop=True),
                         reads=[b_bones, b_sq[u]], writes=[b_p2])
                    P.op("act", lambda e, u=u, p2=p2, M=M: e.activation(out=rs[u][0:M, :], in_=p2[0:M, :], func=AF.Sqrt, scale=1.0 / HD,
                                                                   bias=C.eps_col[0:M, :]),
                         reads=[b_p2, C.b_ones], writes=[b_rs[u]])
                    P.op("dve", lambda e, u=u, M=M: e.reciprocal(out=rs[u][0:M, :], in_=rs[u][0:M, :]), reads=[b_rs[u]], writes=[b_rs[u]])
                    if kind == "norm":
                        P.op("dve", lambda e, u=u, ps=ps, gc=gc, M=M: e.scalar_tensor_tensor(
                            out=tf[u][0:M, :], in0=ps[0:M, :], scalar=qkg[0:M, gc:gc + 1], in1=rs[u][0:M, :], op0=ALU.mult, op1=ALU.mult),
                            reads=[b_ps, b_qkg, b_rs[u]], writes=[b_tf[u]])
                    else:
                        P.op("dve", lambda e, u=u, ps=ps, M=M: e.tensor_tensor(out=tf[u][0:M, :], in0=ps[0:M, :], in1=rs[u][0:M, :], op=ALU.mult),
                             reads=[b_ps, b_rs[u]], writes=[b_tf[u]])
                p3, b_p3 = C.next_ps()
                P.op("pe", lambda e, p3=p3, u=u, M=M: e.matmul(p3[0:M, :], lhsT=cm[0:M, 1, 0:M], rhs=tf[u][0:M, :], start=True, stop=True),
                     reads=[b_cm, b_tf[u]], writes=[b_p3])
                P.op("pool", lambda e, u=u, ts=ts, M=M: e.tensor_tensor(out=o1[u][0:M, :], in0=tf[u][0:M, :], in1=cs[0:M, 0, ts], op=ALU.mult),
                     reads=[b_tf[u], b_cs], writes=[b_o1[u]])
                P.op("dve", lambda e, u=u, ts=ts, p3=p3, M=M: e.tensor_tensor(out=o2[u][0:M, :], in0=p3[0:M, :], in1=cs[0:M, 1, ts], op=ALU.mult),
                     reads=[b_p3, b_cs], writes=[b_o2[u]])
                P.op("pool", lambda e, u=u, M=M: e.tensor_tensor(out=ob[u][0:M, :], in0=o1[u][0:M, :], in1=o2[u][0:M, :], op=ALU.add),
                     reads=[b_o1[u], b_o2[u]], writes=[b_ob[u]])
                toks.append(P.dma("sp", lambda e, u=u, od=od, r0=r0, ts=ts, M=M: e.dma_start(out=od[r0:r0 + M, ts], in_=ob[u][0:M, :]),
                                  b_ob[u], reads=[b_ob[u]]))
        wvf = [P.sb(f"pj_vf{i}", [128, KD, 512], F32) for i in range(1)]
        wvb = [P.sb(f"pj_vb{i}", [128, KD, 512], BF16) for i in range(2)]
        b_wvf = [Buf("pj_vf0")]
        b_wvb = [Buf(f"pj_vb{i}") for i in range(2)]
        vo = [P.sb(f"pj_vo{i}", [128, 8, 65], BF16) for i in range(2)]
        b_vo = [Buf(f"pj_vo{i}") for i in range(2)]
        for i_ in range(2):
            P.op("pool", lambda e, i_=i_: e.memset(vo[i_][:], 1.0), writes=[b_vo[i_]])
        for hf in range(2):
            P.dma("sp", lambda e, hf=hf: e.dma_start(out=wvf[0][:], in_=wv_[:, :, 2 * D + hf * 512:2 * D + (hf + 1) * 512]),
                  b_wvf[0], writes=[b_wvf[0]])
            P.op("pool", lambda e, hf=hf: e.tensor_copy(out=wvb[hf][:], in_=wvf[0][:]), reads=[b_wvf[0]], writes=[b_wvb[hf]])
        ww_f = P.sb("pj_wwf", [128, KD, NIH], F32)
        ww_b = P.sb("pj_wwb", [128, KD, NIH], BF16)
        b_wwf, b_wwb = Buf("pj_wwf"), Buf("pj_wwb")
        P.dma("sp", lambda e: e.dma_start(out=ww_f[:], in_=wv_[:, :, PROJ - NIH:PROJ]), b_wwf, writes=[b_wwf])
        P.op("pool", lambda e: e.tensor_copy(out=ww_b[:], in_=ww_f[:]), reads=[b_wwf], writes=[b_wwb])
        wo_sb = P.sb("pj_wo", [128, NT // 128, NIH], F32)
        b_wo = Buf("pj_wo")
        n = 0
        for blk in range(NT // 128):
            tt = blk // 4
            bs = slice(blk * 128, (blk + 1) * 128)
            for hf in range(2):
                u = n % 2
                n += 1
                ps, b_ps = C.next_ps()
                for k in range(KD):
                    P.op("pe", lambda e, ps=ps, k=k, bs=bs, hf=hf: e.matmul(ps[:], lhsT=hT[:, k, bs], rhs=wvb[hf][:, k, :],
                                                                           start=(k == 0), stop=(k == KD - 1)),
                         reads=[b_wvb[hf], b_h[tt]], writes=[b_ps])
                P.op("act", lambda e, u=u, ps=ps: e.activation(out=vo[u][:, :, 0:64], in_=ps[:].rearrange("p (h d) -> p h d", h=8), func=AF.Copy),
                     reads=[b_ps], writes=[b_vo[u]])
                toks.append(P.dma("sp", lambda e, u=u, bs=bs, hf=hf: e.dma_start(out=v_d[bs, hf * 520:(hf + 1) * 520],
                                                                                  in_=vo[u][:].rearrange("p h d -> p (h d)")),
                                  b_vo[u], reads=[b_vo[u]]))
            ps, b_ps = C.next_ps()
            for k in range(KD):
                P.op("pe", lambda e, ps=ps, k=k, bs=bs: e.matmul(ps[:, 0:NIH], lhsT=hT[:, k, bs], rhs=ww_b[:, k, :],
                                                                 start=(k == 0), stop=(k == KD - 1)),
                     reads=[b_wwb, b_h[tt]], writes=[b_ps])
            P.op("act", lambda e, ps=ps, blk=blk: e.activation(out=wo_sb[:, blk, :], in_=ps[:, 0:NIH], func=AF.Copy,
                                                               scale=(NIH ** -0.5) * (HD ** -0.5)),
                 reads=[b_ps], writes=[b_wo])
        toks.append(P.dma("sp", lambda e: e.dma_start(out=w_d.rearrange("(b p) h -> p b h", p=128), in_=wo_sb[:]), b_wo, reads=[b_wo]))
        P.finish(toks)
        P.emit()
    return nc


def rope_consts(core):
    blocks = np.arange(SEQ // 128)[core::NCORES]
    pos = (blocks[:, None] * 128 + np.arange(128)[None, :]).reshape(-1).astype(np.float32)
    inv_freq = (10000.0 ** (-np.arange(0, HD, 2, dtype=np.float32) / HD)).astype(np.float32)
    ang = pos[None, :] * inv_freq[:, None]
    cos, sin = np.cos(ang).astype(np.float32), np.sin(ang).astype(np.float32)
    cs = np.empty((128, 2, NT), np.float32)
    for p in range(128):
        cs[p, 0] = cos[p % 32]
        cs[p, 1] = sin[p % 32]
    return cs


def const_mats():
    cm = np.zeros((128, 2, 128), np.float32)
    for p in range(128):
        for m in range(128):
            if p // 64 == m // 64:
                cm[p, 0, m] = 1.0
    for m in range(128):
        if (m % 64) < 32:
            cm[m + 32, 1, m] = -1.0
        else:
            cm[m - 32, 1, m] = 1.0
    return cm


TOPK = 256
NEG_SEL = -1.0e30
NEG_MASK = -2.0e30


def build_dsa_attn_prog(nblk=NT // 128):
    U8 = mybir.dt.uint8
    NBIS = 16
    nc = bass.Bass("TRN2", target_bir_lowering=False)
    x_d = nc.dram_tensor("xT", [D, NT], F32, kind="ExternalInput").ap()
    qT_d = nc.dram_tensor("qT", [D, NT], BF16, kind="ExternalInput").ap()
    qiT_d = nc.dram_tensor("qiT", [NIH * HD, NT], BF16, kind="ExternalInput").ap()
    w_d = nc.dram_tensor("wq", [128, NT // 128, NIH], F32, kind="ExternalInput").ap()
    kT_d = nc.dram_tensor("kTf", [D, SEQ], BF16, kind="ExternalInput").ap()
    v_d = nc.dram_tensor("vf", [SEQ, NH * 65], BF16, kind="ExternalInput").ap()
    kiT_d = nc.dram_tensor("kiTf", [HD, SEQ], BF16, kind="ExternalInput").ap()
    pen_d = nc.dram_tensor("pen", [128, 1024], F32, kind="ExternalInput").ap()
    id_d = nc.dram_tensor("ident", [128, 128], F32, kind="ExternalInput").ap()
    wo_d = nc.dram_tensor("w_o", [D, D], F32, kind="ExternalInput").ap()
    y_d = nc.dram_tensor("yT", [D, NT], F32, kind="ExternalOutput").ap()
    qT_v = qT_d.rearrange("(h p) t -> p h t", p=64)
    qiT_v = qiT_d.rearrange("(h p) t -> p h t", p=64)
    kT_v = kT_d.rearrange("(pr p) t -> p pr t", p=128)
    qT_pv = qT_d.rearrange("(pr two p) t -> two p pr t", two=2, p=64)
    x_v = x_d.rearrange("(k p) t -> p k t", p=128)
    y_v = y_d.rearrange("(k p) t -> p k t", p=128)
    wo_v = wo_d.rearrange("(h p) n -> p h n", p=64)
    with ExitStack() as es:
        P = Prog(nc, es)
        ones = P.sb("c_ones", [128, 64], BF16)
        half_c = P.sb("c_half", [128, 1], F32)
        b_c = Buf("consts")
        P.op("pool", lambda e: e.memset(ones[:], 1.0), writes=[b_c])
        P.op("pool", lambda e: e.memset(half_c[:], 0.5), writes=[b_c])
        pen = P.sb("pen_sb", [128, 1024], F32)
        idf = P.sb("id_f", [128, 128], F32)
        idb = P.sb("id_b", [128, 128], BF16)
        wq = P.sb("wq_sb", [128, NT // 128, NIH], F32)
        b_pen, b_id, b_wq = Buf("pen"), Buf("ident"), Buf("wq")
        P.dma("sp", lambda e: e.dma_start(out=pen[:], in_=pen_d[:]), b_pen, writes=[b_pen])
        P.dma("sp", lambda e: e.dma_start(out=idf[:], in_=id_d[:]), b_id, writes=[b_id])
        P.dma("sp", lambda e: e.dma_start(out=wq[:], in_=w_d[:]), b_wq, writes=[b_wq])
        P.op("pool", lambda e: e.tensor_copy(out=idb[:], in_=idf[:]), reads=[b_id], writes=[b_id])
        wob = P.sb("wo_b", [64, NH, D], BF16)
        wof = P.sb("wo_f", [64, NH, 64], F32)
        b_wob, b_wof = Buf("wo_b"), Buf("wo_f")
        for m in range(2 * KD):
            P.dma("sp", lambda e, m=m: e.dma_start(out=wof[:], in_=wo_v[:, :, m * 64:(m + 1) * 64]), b_wof, writes=[b_wof])
            P.op("pool", lambda e, m=m: e.tensor_copy(out=wob[:, :, m * 64:(m + 1) * 64], in_=wof[:]), reads=[b_wof], writes=[b_wob])
        score = P.sb("score", [128, SEQ], F32)
        b_score = Buf("score")
        junk = P.sb("junk", [128, SEQ], U8)
        b_junk = Buf("junk")
        maskT = P.sb("maskT", [128, SEQ // 128, 128], U8)
        b_maskT = Buf("maskT")
        qbd = P.sb("qbd", [128, NH // 2, 256], BF16)
        qih = P.sb("qih", [64, NIH, 128], BF16)
        b_qh, b_qih = Buf("qh"), Buf("qih")
        P.op("pool", lambda e: e.memset(qbd[:], 0.0), writes=[b_qh])
        kib = [P.sb(f"kib{i}", [64, 512], BF16) for i in range(2)]
        b_kib = [Buf(f"kib{i}") for i in range(2)]
        tmp = [P.sb(f"itmp{i}", [128, 512], F32) for i in range(2)]
        b_tmp = [Buf(f"itmp{i}") for i in range(2)]
        m8 = P.sb("m8", [128, 8], F32)
        bis = P.sb("bis", [128, 8], F32)
        b_bis = Buf("bis")
        LO, HI, MID, CNT, GE, D1, D2 = (bis[:, j:j + 1] for j in range(7))
        mk = [P.sb(f"mk{i}", [128, 512], BF16) for i in range(2)]
        b_mk = [Buf(f"mk{i}") for i in range(2)]
        kTc = [P.sb(f"kTc{i}", [128, 4, 256], BF16) for i in range(2)]
        vc = [P.sb(f"vc{i}", [128, 2, 520], BF16) for i in range(2)]
        b_kTc = [Buf(f"kTc{i}") for i in range(2)]
        sel = P.sb("sel", [65, 64], F32)
        P.op("pool", lambda e: e.memset(sel[:], 0.0), writes=[b_c])
        P.op("pool", lambda e: e.memset(sel[64:65, :], 1.0), writes=[b_c])
        accs = [P.sb(f"accs{i}", [65, 512], F32) for i in range(2)]
        b_accs = [Buf(f"accs{i}") for i in range(2)]
        b_vc = [Buf(f"vc{i}") for i in range(2)]
        pT = [P.sb(f"pT{i}", [128, 512], BF16) for i in range(3)]
        b_pT = [Buf(f"pT{i}") for i in range(3)]
        attn = P.sb("attn", [64, NH, 128], BF16)
        b_attn = Buf("attn")
        dsb = [P.sb(f"dsb{i}", [64, 512], F32) for i in range(2)]
        b_dsb = [Buf(f"dsb{i}") for i in range(2)]
        xq = P.sb("xq", [128, KD, 128], F32)
        b_xq = Buf("xq")
        psA = [P.ps(f"psA{i}", [128, 512], F32) for i in range(3)]
        b_psA = [Buf(f"psA{i}") for i in range(3)]
        psT = P.ps("psT", [128, 1024], BF16)
        b_psT = Buf("psT")
        acc = [P.ps(f"acc{i}", [128, 512], F32) for i in range(2)]
        b_acc = [Buf(f"acc{i}") for i in range(2)]
        den = [P.ps(f"den{i}", [128, 512], F32) for i in range(2)]
        b_den = [Buf(f"den{i}") for i in range(2)]
        rr = {"a": 0, "kib": 0, "tmp": 0, "mk": 0, "kv": 0, "pT": 0}

        def nxt(key, n):
            v = rr[key]
            rr[key] = (v + 1) % n
            return v

        def phase_A(i):
            qs = slice(i * 128, (i + 1) * 128)
            Lk = 1024 * (i + 1)
            P.dma("sp", lambda e: e.dma_start(out=qih[:], in_=qiT_v[:, :, qs]), b_qih, writes=[b_qih])
            for j in range(Lk // 512):
                cs_ = slice(j * 512, (j + 1) * 512)
                kb = nxt("kib", 2)
                P.dma("sp", lambda e, kb=kb, cs_=cs_: e.dma_start(out=kib[kb][:], in_=kiT_d[:, cs_]), b_kib[kb], writes=[b_kib[kb]])
                for h in range(NIH):
                    a = nxt("a", 3)
                    P.op("pe", lambda e, a=a, h=h, kb=kb: e.matmul(psA[a][:], lhsT=qih[:, h, :], rhs=kib[kb][:], start=True, stop=True),
                         reads=[b_qih, b_kib[kb]], writes=[b_psA[a]])
                    if h == 0:
                        P.op("dve", lambda e, a=a, cs_=cs_: e.tensor_scalar(
                            out=score[:, cs_], in0=psA[a][:], scalar1=0.0, scalar2=wq[:, i, 0:1], op0=ALU.max, op1=ALU.mult),
                            reads=[b_psA[a], b_wq], writes=[b_score])
                    else:
                        t = nxt("tmp", 2)
                        P.op("dve", lambda e, a=a, t=t, h=h: e.tensor_scalar(
                            out=tmp[t][:], in0=psA[a][:], scalar1=0.0, scalar2=wq[:, i, h:h + 1], op0=ALU.max, op1=ALU.mult),
                            reads=[b_psA[a], b_wq], writes=[b_tmp[t]])
                        P.op("pool", lambda e, t=t, cs_=cs_: e.tensor_tensor(out=score[:, cs_], in0=score[:, cs_], in1=tmp[t][:], op=ALU.add),
                             reads=[b_tmp[t], b_score], writes=[b_score])
            P.op("dve", lambda e: e.tensor_reduce(out=LO, in_=score[:, 0:Lk], axis=AX.X, op=ALU.min), reads=[b_score], writes=[b_bis])
            P.op("pool", lambda e: e.tensor_tensor(out=score[:, Lk - 1024:Lk], in0=score[:, Lk - 1024:Lk], in1=pen[:], op=ALU.add),
                 reads=[b_score, b_pen], writes=[b_score])
            P.op("dve", lambda e: e.max(out=m8[:], in_=score[:, 0:Lk]), reads=[b_score], writes=[b_bis])
            P.op("dve", lambda e: e.tensor_copy(out=HI, in_=m8[:, 0:1]), reads=[b_bis], writes=[b_bis])

        cntp = P.sb("cntp", [128, 8], F32)

        def bis_items(i):
            Lk = 1024 * (i + 1)
            npz = (Lk + 2047) // 2048
            V_ = lambda fn, rd=(): P.op("dve", fn, reads=[b_bis, b_c] + list(rd), writes=[b_bis])

            def piece(p):
                c0, c1 = p * 2048, min(Lk, (p + 1) * 2048)
                if p == 0:
                    V_(lambda e: e.scalar_tensor_tensor(out=MID, in0=LO, scalar=HI, in1=half_c[:], op0=ALU.add, op1=ALU.mult))
                P.op("dve", lambda e: e.tensor_scalar(out=junk[:, c0:c1], in0=score[:, c0:c1], scalar1=MID, scalar2=0.0,
                                                      op0=ALU.is_ge, op1=ALU.add, accum_out=cntp[:, p:p + 1]),
                     reads=[b_score, b_bis], writes=[b_junk, b_bis])

            def tail():
                V_(lambda e: e.tensor_reduce(out=CNT, in_=cntp[:, 0:npz], axis=AX.X, op=ALU.add))
                V_(lambda e: e.tensor_single_scalar(out=GE, in_=CNT, scalar=TOPK - 0.5, op=ALU.is_ge))
                V_(lambda e: e.tensor_tensor(out=D1, in0=MID, in1=LO, op=ALU.subtract))
                V_(lambda e: e.tensor_tensor(out=D2, in0=HI, in1=MID, op=ALU.subtract))
                V_(lambda e: e.scalar_tensor_tensor(out=LO, in0=D1, scalar=GE, in1=LO, op0=ALU.mult, op1=ALU.add))
                V_(lambda e: e.scalar_tensor_tensor(out=HI, in0=D2, scalar=GE, in1=MID, op0=ALU.mult, op1=ALU.add))

            items = []
            for p in range(npz):
                items.append(lambda p=p: piece(p))
            items.append(tail)
            return items

        def phase_C(i):
            Lk = 1024 * (i + 1)
            for j in range(Lk // 512):
                cs_ = slice(j * 512, (j + 1) * 512)
                u = nxt("mk", 2)
                P.op("pool", lambda e, u=u, cs_=cs_: e.tensor_scalar(out=mk[u][:], in0=score[:, cs_], scalar1=LO, scalar2=None, op0=ALU.is_ge),
                     reads=[b_score, b_bis], writes=[b_mk[u]])
                for jj in range(4):
                    P.op("pe", lambda e, u=u, jj=jj: e.transpose(out=psT[:, jj * 128:(jj + 1) * 128], in_=mk[u][:, jj * 128:(jj + 1) * 128],
                                                                 identity=idb[:]),
                         reads=[b_mk[u], b_id], writes=[b_psT])
                P.op("act", lambda e, j=j: e.activation(out=maskT[:, 4 * j:4 * j + 4, :].rearrange("p a b -> p (a b)"), in_=psT[:, 0:512],
                                                        func=AF.Copy),
                     reads=[b_psT], writes=[b_maskT])

        def phase_D(i, side):
            qs = slice(i * 128, (i + 1) * 128)
            nkc = 8 * (i + 1)
            P.dma("sp", lambda e: e.dma_start(out=qbd[0:64, :, 0:128], in_=qT_pv[0][:, :, qs]), b_qh, writes=[b_qh])
            P.dma("sp", lambda e: e.dma_start(out=qbd[64:128, :, 128:256], in_=qT_pv[1][:, :, qs]), b_qh, writes=[])
            b_qh.w = ("d", b_qh, b_qh.dcount)
            groups = [(half, kc, hg) for half in range(2) for kc in range(nkc) for hg in range(2)]
            st = {}

            def emit_S(gi):
                half, kc, hg = groups[gi]
                kk = kc % 2
                if kk == 0 and hg == 0:
                    s_ = nxt("kv", 2)
                    r0 = kc * 128
                    P.dma("sp", lambda e: e.dma_start(out=kTc[s_][:], in_=kT_v[:, half * 4:(half + 1) * 4, r0:r0 + 256]),
                          b_kTc[s_], writes=[b_kTc[s_]])
                    P.dma("sp", lambda e: e.dma_start(
                        out=vc[s_][:], in_=v_d[r0:r0 + 256, half * 520:(half + 1) * 520].rearrange("(c p) n -> p c n", p=128)),
                        b_vc[s_], writes=[b_vc[s_]])
                    st["slot", half, kc // 2] = s_
                s_ = st["slot", half, kc // 2]
                a = nxt("a", 3)
                for pl2 in range(2):
                    pl = hg * 2 + pl2
                    pair = half * 4 + pl
                    P.op("pe", lambda e, pl2=pl2, pl=pl, pair=pair: e.matmul(
                        psA[a][:, pl2 * 256:(pl2 + 1) * 256], lhsT=kTc[s_][:, pl, kk * 128:(kk + 1) * 128], rhs=qbd[:, pair, :],
                        start=True, stop=True),
                        reads=[b_kTc[s_], b_qh], writes=[b_psA[a]])
                st["a", gi] = a

            def emit_rest(gi):
                half, kc, hg = groups[gi]
                kk = kc % 2
                s_ = st["slot", half, kc // 2]
                a = st["a", gi]
                u = nxt("pT", 3)
                P.op("act", lambda e: e.activation(out=pT[u][:], in_=psA[a][:], func=AF.Exp),
                     reads=[b_psA[a]], writes=[b_pT[u]])
                P.op("dve", lambda e: e.tensor_tensor(
                    out=pT[u][:].rearrange("p (h q) -> p h q", h=4), in0=pT[u][:].rearrange("p (h q) -> p h q", h=4),
                    in1=maskT[:, kc, :].unsqueeze(1).to_broadcast([128, 4, 128]), op=ALU.mult),
                    reads=[b_pT[u], b_maskT], writes=[b_pT[u]])
                for hh in range(4):
                    h8 = hg * 4 + hh
                    P.op("pe", lambda e, hh=hh, h8=h8: e.matmul(
                        acc[hg][0:65, hh * 128:(hh + 1) * 128], lhsT=vc[s_][:, kk, h8 * 65:(h8 + 1) * 65],
                        rhs=pT[u][:, hh * 128:(hh + 1) * 128], start=(kc == 0 and hh == 0), stop=(kc == nkc - 1 and hh == 3),
                        skip_group_check=True),
                        reads=[b_vc[s_], b_pT[u]], writes=[b_acc[hg]])
                if kc == nkc - 1:
                    h0 = half * 8 + hg * 4
                    P.op("act", lambda e: e.activation(out=accs[hg][:], in_=acc[hg][0:65, :], func=AF.Copy),
                         reads=[b_acc[hg]], writes=[b_accs[hg]])
                    P.op("pe", lambda e: e.matmul(den[hg][0:64, :], lhsT=sel[:], rhs=accs[hg][:], start=True, stop=True),
                         reads=[b_c, b_accs[hg]], writes=[b_den[hg]])
                    P.op("act", lambda e: e.activation(out=dsb[hg][:], in_=den[hg][0:64, :], func=AF.Copy),
                         reads=[b_den[hg]], writes=[b_dsb[hg]])
                    P.op("dve", lambda e: e.reciprocal(out=dsb[hg][:], in_=dsb[hg][:]), reads=[b_dsb[hg]], writes=[b_dsb[hg]])
                    P.op("dve", lambda e: e.tensor_tensor(
                        out=attn[:, h0:h0 + 4, :].rearrange("p a b -> p (a b)"), in0=accs[hg][0:64, :], in1=dsb[hg][:], op=ALU.mult),
                        reads=[b_accs[hg], b_dsb[hg]], writes=[b_attn])

            G = len(groups)
            done = 0
            emit_S(0)
            for gi in range(G):
                if gi + 1 < G:
                    emit_S(gi + 1)
                emit_rest(gi)
                want = (len(side) * (gi + 1)) // G
                while done < want:
                    side[done]()
                    done += 1
            while done < len(side):
                side[done]()
                done += 1

        def phase_E(i):
            qs = slice(i * 128, (i + 1) * 128)
            P.dma("sp", lambda e: e.dma_start(out=xq[:], in_=x_v[:, :, qs]), b_xq, writes=[b_xq])
            for m in range(KD):
                a = nxt("a", 3)
                for h in range(NH):
                    P.op("pe", lambda e, a=a, h=h, m=m: e.matmul(psA[a][:, 0:128], lhsT=wob[:, h, m * 128:(m + 1) * 128], rhs=attn[:, h, :],
                                                                 start=(h == 0), stop=(h == NH - 1)),
                         reads=[b_wob, b_attn], writes=[b_psA[a]])
                P.op("dve", lambda e, a=a, m=m: e.tensor_tensor(out=xq[:, m, :], in0=psA[a][:, 0:128], in1=xq[:, m, :], op=ALU.add),
                     reads=[b_psA[a], b_xq], writes=[b_xq])
            return P.dma("sp", lambda e: e.dma_start(out=y_v[:, :, qs], in_=xq[:]), b_xq, reads=[b_xq])

        phase_A(0)
        for _ in range(NBIS):
            for f in bis_items(0):
                f()
        phase_C(0)
        tok = None
        for i in range(nblk):
            side = []
            if i + 1 < nblk:
                phase_A(i + 1)
                side = [f for _ in range(NBIS) for f in bis_items(i + 1)]
            phase_D(i, side)
            tok = phase_E(i)
            if i + 1 < nblk:
                phase_C(i + 1)
        P.finish([tok])
        P.emit()
    return nc


def causal_pen(core):
    j = np.arange(1024)[None, :]
    p = np.arange(128)[:, None]
    return np.where(j > 128 * core + p, np.float32(NEG_MASK), np.float32(0.0)).astype(np.float32)


def gather_tokens_T(parts):
    f = parts[0].shape[0]
    out = np.empty((f, SEQ // 128, 128), parts[0].dtype)
    for c, p in enumerate(parts):
        out[:, c::NCORES, :] = p.reshape(f, NT // 128, 128)
    return out.reshape(f, SEQ)


def gather_tokens(parts):
    f = parts[0].shape[1]
    out = np.empty((SEQ // 128, 128, f), parts[0].dtype)
    for c, p in enumerate(parts):
        out[c::NCORES] = p.reshape(NT // 128, 128, f)
    return out.reshape(SEQ, f)


def run_dsa_layer(xT_parts, j, i, inp):
    gm = col_layout(inp["norm_mix"][i])
    qkg = np.stack([np.tile(inp["dsa_q_norm"][j], 2), np.tile(inp["dsa_k_norm"][j], 2)], axis=1).astype(np.float32)
    cm = const_mats()
    maps = [{"xT": xT_parts[c], "w_in": inp["dsa_w_in"][j], "gain": gm, "qk_gain": qkg, "cossin": rope_consts(c), "cmat": cm}
            for c in range(NCORES)]
    pr = _run("dsa_proj", build_dsa_proj_prog, maps)
    kTf = gather_tokens_T([r["kT"] for r in pr])
    kiTf = gather_tokens_T([r["kiT"] for r in pr])
    vf = gather_tokens([r["v"] for r in pr])
    ident = np.eye(128, dtype=np.float32)
    maps = []
    for c in range(NCORES):
        wq = np.ascontiguousarray(pr[c]["w"].reshape(NT // 128, 128, NIH).transpose(1, 0, 2))
        maps.append({"xT": xT_parts[c], "qT": pr[c]["qT"], "qiT": pr[c]["qiT"], "wq": wq, "kTf": kTf, "vf": vf, "kiTf": kiTf,
                     "pen": causal_pen(c), "ident": ident, "w_o": inp["dsa_w_o"][j]})
    ar = _run("dsa_attn", build_dsa_attn_prog, maps)
    gf = col_layout(inp["norm_ffn"][i])
    maps = [{"xT": ar[c]["yT"], "wgu": inp["ffn_w_gate_up"][i], "wd": inp["ffn_w_down"][i], "gain": gf} for c in range(NCORES)]
    if i + 1 < 4:
        g2 = col_layout(inp["norm_mix"][i + 1])
        for m in maps:
            m["gain2"] = g2
        fr = _run("ffn_pn", build_ffn_prenorm_prog, maps)
        return [r["yT"] for r in fr], [r["hT"] for r in fr]
    fr = _run("ffn", build_ffn_prog, maps)
    return [r["yT"] for r in fr], None


def kernel(**inputs):
    inp = {k: np.asarray(v) for k, v in inputs.items()}
    x = np.ascontiguousarray(inp["x"][0], dtype=np.float32)
    parts = [to_core_T(x, c) for c in range(NCORES)]
    h_parts = None
    for i in range(4):
        if i % 2 == 0:
            parts = run_s5_layer(parts, i // 2, i, inp, h_parts)
            h_parts = None
        else:
            parts, h_parts = run_dsa_layer(parts, i // 2, i, inp)
    return from_core_T(parts)[None].astype(np.float32)
```
